# Optimizing a Trainium2 kernel written in Bass

```python
import math
import jax, jax.numpy as jnp
from jax import lax
import numpy as np

D_MODEL = 1024
BATCH = 16
SEQ = 2048
DEPTH = 1

CHUNK = 64
Q_BLOCK = 128
MLA_HEADS = 8
MLA_Q_RANK = 256
MLA_KV_RANK = 128
MLA_NOPE = 64
MLA_ROPE = 32
MLA_V = 64
ROPE_THETA = 10000.0
DSA_HEADS = 8
DSA_HEAD_DIM = 64
IDX_HEADS = 8
IDX_DIM = 32
IDX_TOPK_MAX = 256
REL_BUCKETS = 32
REL_MAX_DIST = 128
N_GROUPS = 4
EXPERTS_PER_GROUP = 8
N_EXPERTS = N_GROUPS * EXPERTS_PER_GROUP
TOP_K_IN_GROUP = 2
EXPERT_HIDDEN = 256
LN_EPS = 1e-5
RMS_EPS = 1e-6
DEEPNORM_ALPHA = (2.0 * DEPTH) ** 0.25
DEEPNORM_BETA = (8.0 * DEPTH) ** -0.25
NEG = -1e30
IN_SIZES = (MLA_Q_RANK, MLA_KV_RANK, MLA_ROPE, DSA_HEADS * DSA_HEAD_DIM, DSA_HEAD_DIM, DSA_HEAD_DIM, IDX_HEADS * IDX_DIM, IDX_DIM, IDX_HEADS)
IN_WIDTH = MLA_Q_RANK + MLA_KV_RANK + MLA_ROPE + DSA_HEADS * DSA_HEAD_DIM + 2 * DSA_HEAD_DIM + IDX_HEADS * IDX_DIM + IDX_DIM + IDX_HEADS

kernel_name = 'hybrid_mla_dsa_hmoe_block'


def layer_norm(x, g, b):
    xf = x.astype(jnp.float32)
    mu = jnp.mean(xf, axis=-1, keepdims=True)
    var = jnp.mean(jnp.square(xf - mu), axis=-1, keepdims=True)
    return ((xf - mu) * lax.rsqrt(var + LN_EPS) * g + b).astype(x.dtype)


def rms_norm(x, g):
    xf = x.astype(jnp.float32)
    return (xf * lax.rsqrt(jnp.mean(jnp.square(xf), axis=-1, keepdims=True) + RMS_EPS) * g).astype(x.dtype)


def apply_rope(x, pos):
    half = x.shape[-1] // 2
    inv = ROPE_THETA ** (-jnp.arange(half, dtype=jnp.float32) / half)
    ang = pos.astype(jnp.float32)[..., None] * inv
    cos = jnp.cos(ang)[:, :, None, :]
    sin = jnp.sin(ang)[:, :, None, :]
    x1 = x[..., :half].astype(jnp.float32)
    x2 = x[..., half:].astype(jnp.float32)
    return jnp.concatenate([x1 * cos - x2 * sin, x1 * sin + x2 * cos], axis=-1).astype(x.dtype)


def t5_bucket(rel):
    nb = REL_BUCKETS // 2
    max_exact = nb // 2
    ret = jnp.where(rel > 0, nb, 0)
    n = jnp.abs(rel)
    large = max_exact + (jnp.log(jnp.maximum(n, 1).astype(jnp.float32) / max_exact)
                         / math.log(REL_MAX_DIST / max_exact) * (nb - max_exact)).astype(jnp.int32)
    large = jnp.minimum(large, nb - 1)
    return ret + jnp.where(n < max_exact, n, large)


def to_blocks(a):
    B, T = a.shape[:2]
    a = a.reshape((B, T // Q_BLOCK, Q_BLOCK) + a.shape[2:])
    return jnp.swapaxes(a, 0, 1)


def from_blocks(a):
    a = jnp.swapaxes(a, 0, 1)
    return a.reshape((a.shape[0], a.shape[1] * a.shape[2]) + a.shape[3:])


def mla_branch(c_q, c_kv, k_rope_raw, pos, q_norm_g, w_uq, kv_norm_g, w_uk, w_uv):
    B, T = pos.shape
    q = (rms_norm(c_q, q_norm_g) @ w_uq).reshape(B, T, MLA_HEADS, MLA_NOPE + MLA_ROPE)
    q = jnp.concatenate([q[..., :MLA_NOPE], apply_rope(q[..., MLA_NOPE:], pos)], axis=-1)
    ckv = rms_norm(c_kv, kv_norm_g)
    k_nope = (ckv @ w_uk).reshape(B, T, MLA_HEADS, MLA_NOPE)
    v = (ckv @ w_uv).reshape(B, T, MLA_HEADS, MLA_V)
    k_rope = apply_rope(k_rope_raw[:, :, None, :], pos)
    k = jnp.concatenate([k_nope, jnp.broadcast_to(k_rope, (B, T, MLA_HEADS, MLA_ROPE))], axis=-1)
    scale = (MLA_NOPE + MLA_ROPE) ** -0.5
    k_chunk = pos // CHUNK

    def one_block(args):
        qb, pb = args
        s = jnp.einsum('bqhd,bshd->bhqs', qb, k).astype(jnp.float32) * scale
        allowed = k_chunk[:, None, :] <= (pb // CHUNK)[:, :, None]
        s = jnp.where(allowed[:, None], s, NEG)
        p = jax.nn.softmax(s, axis=-1).astype(v.dtype)
        return jnp.einsum('bhqs,bshd->bqhd', p, v)

    o = from_blocks(lax.map(one_block, (to_blocks(q), to_blocks(pos))))
    return o.reshape(B, T, MLA_HEADS * MLA_V)


def dsa_branch(q_b, k_b, v_b, q_idx, k_idx, w_idx, pos, rel_bias):
    B, T = pos.shape
    L = k_b.shape[1]
    topk = min(IDX_TOPK_MAX, L // 4)
    q = q_b.reshape(B, T, DSA_HEADS, DSA_HEAD_DIM)
    qi = q_idx.reshape(B, T, IDX_HEADS, IDX_DIM)
    w = w_idx * IDX_HEADS ** -0.5
    k_chunk = pos // CHUNK

    def one_block(args):
        qb, qib, wb, pb = args
        rel = jax.nn.relu(jnp.einsum('bqhd,bsd->bqhs', qib, k_idx).astype(jnp.float32) * IDX_DIM ** -0.5)
        score = jnp.einsum('bqh,bqhs->bqs', wb.astype(jnp.float32), rel)
        q_chunk = pb // CHUNK
        score = jnp.where(k_chunk[:, None, :] <= q_chunk[:, :, None], score, NEG)
        _, sel = lax.top_k(score, topk)
        k_sel = jax.vmap(lambda a, i: a[i])(k_b, sel)
        v_sel = jax.vmap(lambda a, i: a[i])(v_b, sel)
        pos_sel = jax.vmap(lambda a, i: a[i])(pos, sel)
        valid = (pos_sel // CHUNK) <= q_chunk[:, :, None]
        bias = jnp.transpose(rel_bias[t5_bucket(pos_sel - pb[:, :, None])], (0, 3, 1, 2))
        s = jnp.einsum('bqhd,bqkd->bhqk', qb, k_sel).astype(jnp.float32) * DSA_HEAD_DIM ** -0.5 + bias
        s = jnp.where(valid[:, None], s, NEG)
        p = jax.nn.softmax(s, axis=-1).astype(v_sel.dtype)
        return jnp.einsum('bhqk,bqkd->bqhd', p, v_sel)

    o = from_blocks(lax.map(one_block, (to_blocks(q), to_blocks(qi), to_blocks(w), to_blocks(pos))))
    return o.reshape(B, T, DSA_HEADS * DSA_HEAD_DIM)


def hierarchical_moe(h, w_grp, b_grp, w_rt, b_rt, w_eg, w_eu, w_ed):
    B, T, _ = h.shape
    g_logits = (h @ w_grp).astype(jnp.float32) + b_grp
    p_grp = jax.nn.softmax(g_logits, axis=-1)
    g_sel = jnp.argmax(g_logits, axis=-1)
    g_w = jnp.take_along_axis(p_grp, g_sel[..., None], axis=-1)
    e_logits = ((h @ w_rt).astype(jnp.float32) + b_rt).reshape(B, T, N_GROUPS, EXPERTS_PER_GROUP)
    e_logits = jnp.take_along_axis(e_logits, g_sel[..., None, None], axis=2)[:, :, 0, :]
    p_e = jax.nn.softmax(e_logits, axis=-1)
    top_v, top_i = lax.top_k(p_e, TOP_K_IN_GROUP)
    top_v = top_v / jnp.sum(top_v, axis=-1, keepdims=True)
    expert_id = g_sel[..., None] * EXPERTS_PER_GROUP + top_i
    comb = jnp.sum(jax.nn.one_hot(expert_id, N_EXPERTS, dtype=jnp.float32) * (g_w * top_v)[..., None], axis=-2)
    comb = comb.astype(h.dtype)

    def per_row(args):
        hr, cr = args
        a = jnp.einsum('td,edf->tef', hr, w_eg)
        u = jnp.einsum('td,edf->tef', hr, w_eu)
        hid = jax.nn.silu(a) * u * cr[..., None]
        return jnp.einsum('tef,efd->td', hid, w_ed)

    return lax.map(per_row, (h, comb))


def setup_inputs(seed: int = 0) -> dict:
    key = jax.random.key(seed)
    ks = jax.random.split(key, 32)
    f32 = jnp.float32
    D = D_MODEL
    nrm = lambda k, shape, s: jax.random.normal(k, shape, f32) * s
    gain = lambda k, shape: 1.0 + 0.02 * jax.random.normal(k, shape, f32)
    x = jax.random.normal(ks[0], (BATCH, SEQ, D), f32)
    start = jax.random.randint(ks[1], (BATCH, 1), 0, 64, dtype=jnp.int32) * CHUNK
    positions = (start + jnp.arange(SEQ, dtype=jnp.int32)[None, :]).astype(jnp.int32)
    offs = np.cumsum((0,) + IN_SIZES)
    v_off = int(offs[5])
    col_scale = jnp.concatenate([jnp.ones((v_off,), f32), jnp.full((DSA_HEAD_DIM,), DEEPNORM_BETA, f32),
                                 jnp.ones((IN_WIDTH - v_off - DSA_HEAD_DIM,), f32)])
    L = DEPTH
    beta = DEEPNORM_BETA
    return {
        'x': x,
        'positions': positions,
        'ln0_g': gain(ks[2], (D,)),
        'ln0_b': nrm(ks[3], (D,), 0.02),
        'w_in': nrm(ks[4], (L, D, IN_WIDTH), D ** -0.5) * col_scale,
        'q_norm_g': gain(ks[5], (L, MLA_Q_RANK)),
        'w_uq': nrm(ks[6], (L, MLA_Q_RANK, MLA_HEADS * (MLA_NOPE + MLA_ROPE)), MLA_Q_RANK ** -0.5),
        'kv_norm_g': gain(ks[7], (L, MLA_KV_RANK)),
        'w_uk': nrm(ks[8], (L, MLA_KV_RANK, MLA_HEADS * MLA_NOPE), MLA_KV_RANK ** -0.5),
        'w_uv': nrm(ks[9], (L, MLA_KV_RANK, MLA_HEADS * MLA_V), beta * MLA_KV_RANK ** -0.5),
        'rel_bias': nrm(ks[10], (REL_BUCKETS, DSA_HEADS), 0.2),
        'w_up_a': nrm(ks[11], (L, MLA_HEADS * MLA_V, D), beta * (MLA_HEADS * MLA_V) ** -0.5),
        'w_up_b': nrm(ks[12], (L, DSA_HEADS * DSA_HEAD_DIM, D), beta * (DSA_HEADS * DSA_HEAD_DIM) ** -0.5),
        'w_gate': nrm(ks[13], (L, D, 2 * D), D ** -0.5),
        'b_gate': nrm(ks[14], (L, 2 * D), 0.02),
        'w_o': nrm(ks[15], (L, D, D), beta * D ** -0.5),
        'ln1_g': gain(ks[16], (L, D)),
        'ln1_b': nrm(ks[17], (L, D), 0.02),
        'w_grp': nrm(ks[18], (L, D, N_GROUPS), D ** -0.5),
        'b_grp': nrm(ks[19], (L, N_GROUPS), 0.01),
        'w_rt': nrm(ks[20], (L, D, N_EXPERTS), D ** -0.5),
        'b_rt': nrm(ks[21], (L, N_EXPERTS), 0.01),
        'w_exp_gate': nrm(ks[22], (L, N_EXPERTS, D, EXPERT_HIDDEN), D ** -0.5),
        'w_exp_up': nrm(ks[23], (L, N_EXPERTS, D, EXPERT_HIDDEN), D ** -0.5),
        'w_exp_down': nrm(ks[24], (L, N_EXPERTS, EXPERT_HIDDEN, D), beta * EXPERT_HIDDEN ** -0.5),
        'ln2_g': gain(ks[25], (L, D)),
        'ln2_b': nrm(ks[26], (L, D), 0.02),
    }


def reference(x, positions, ln0_g, ln0_b, w_in, q_norm_g, w_uq, kv_norm_g, w_uk, w_uv, rel_bias,
              w_up_a, w_up_b, w_gate, b_gate, w_o, ln1_g, ln1_b, w_grp, b_grp, w_rt, b_rt,
              w_exp_gate, w_exp_up, w_exp_down, ln2_g, ln2_b):
    split_at = [int(o) for o in np.cumsum(IN_SIZES)[:-1]]
    x = layer_norm(x, ln0_g, ln0_b)
    for l in range(DEPTH):
        proj = x @ w_in[l]
        c_q, c_kv, k_rope, q_b, k_b, v_b, q_idx, k_idx, w_idx = jnp.split(proj, split_at, axis=-1)
        o_a = mla_branch(c_q, c_kv, k_rope, positions, q_norm_g[l], w_uq[l], kv_norm_g[l], w_uk[l], w_uv[l])
        o_b = dsa_branch(q_b, k_b, v_b, q_idx, k_idx, w_idx, positions, rel_bias)
        gates = jax.nn.sigmoid(x @ w_gate[l] + b_gate[l])
        g_a, g_b = jnp.split(gates, 2, axis=-1)
        mixed = (g_a * (o_a @ w_up_a[l]) + g_b * (o_b @ w_up_b[l])) @ w_o[l]
        h = layer_norm(DEEPNORM_ALPHA * x + mixed, ln1_g[l], ln1_b[l])
        ffn = hierarchical_moe(h, w_grp[l], b_grp[l], w_rt[l], b_rt[l], w_exp_gate[l], w_exp_up[l], w_exp_down[l])
        x = layer_norm(DEEPNORM_ALPHA * h + ffn, ln2_g[l], ln2_b[l])
    return x
```

```python
import math
from contextlib import ExitStack
import numpy as np
import concourse.bass as bass
import concourse.mybir as mybir
from concourse.bass_utils import run_bass_kernel_spmd

F32 = mybir.dt.float32
BF16 = mybir.dt.bfloat16
I32 = mybir.dt.int32
ALU = mybir.AluOpType
AF = mybir.ActivationFunctionType
AX = mybir.AxisListType

T = 2048
DM = 1024
NT = 16
NB = 2
ALPHA = 2.0 ** 0.25
LN_EPS = 1e-5
RMS_EPS = 1e-6
NEGBIG = -1.0e30
BIS_ITERS = 14
TOPK = 256


class Buf:
    __slots__ = ("ap", "name", "ws", "reads", "dsem", "dcount")

    def __init__(self, ap, name=""):
        self.ap = ap
        self.name = name
        self.ws = {}
        self.reads = {}
        self.dsem = None
        self.dcount = 0

    def __getitem__(self, idx):
        return self.ap[idx]


class Sched:
    def __init__(self, nc, stack):
        self.nc = nc
        self.stack = stack
        self.engs = {}
        for name, eng in (("pe", nc.tensor), ("act", nc.scalar), ("dve", nc.vector),
                          ("pool", nc.gpsimd), ("sp", nc.sync)):
            sem = stack.enter_context(nc.semaphore("s_" + name))
            self.engs[name] = dict(eng=eng, sem=sem, count=0, waited={})
        self.ndsem = 0
        self.dpool = {}
        self.nobar = set()
        self.n_ins = 0
        self.uid = 0

    def sbuf(self, st, name, shape, dtype):
        self.uid += 1
        name = "%s_%d" % (name, self.uid)
        return Buf(st.enter_context(self.nc.sbuf_tensor(name, shape, dtype)), name)

    def psum(self, st, name, shape, dtype):
        self.uid += 1
        name = "%s_%d" % (name, self.uid)
        return Buf(st.enter_context(self.nc.psum_tensor(name, shape, dtype)), name)

    def _wait(self, engname, ev):
        sem, val, src = ev
        if src == "pe" and engname == "pe":
            return
        E = self.engs[engname]
        key = id(sem)
        if E["waited"].get(key, 0) < val:
            E["eng"].wait_ge(sem, val)
            E["waited"][key] = val
            self.n_ins += 1

    def _deps(self, engname, reads, writes):
        for b in reads:
            for ev in b.ws.values():
                self._wait(engname, ev)
        for b in writes:
            for ev in b.ws.values():
                self._wait(engname, ev)
            for ev in b.reads.values():
                self._wait(engname, ev)

    def _commit(self, ev, reads, writes):
        k = id(ev[0])
        for b in writes:
            b.ws[k] = ev
            b.reads = {}
        for b in reads:
            if b in writes:
                continue
            b.reads[k] = ev

    def op(self, engname, fn, reads=(), writes=(), inc=True):
        E = self.engs[engname]
        self._deps(engname, reads, writes)
        ins = fn(E["eng"])
        self.n_ins += 1
        if inc:
            E["count"] += 1
            ins.then_inc(E["sem"], 1)
            self._commit((E["sem"], E["count"], engname), reads, writes)
        else:
            self._commit((E["sem"], E["count"] + 1, engname), reads, writes)

    def dma(self, qname, out_ap, in_ap, reads=(), writes=(), sembuf=None):
        E = self.engs[qname]
        self._deps(qname, reads, writes)
        sb = sembuf or (writes[0] if writes else reads[0])
        key = sb.name.rsplit("_", 1)[0] if "_" in sb.name else sb.name
        ent = self.dpool.get(key)
        if ent is None:
            ent = [self.stack.enter_context(self.nc.semaphore("d%d" % self.ndsem)), 0]
            self.ndsem += 1
            self.dpool[key] = ent
        ins = E["eng"].dma_start(out=out_ap, in_=in_ap)
        ent[1] += 16
        ins.then_inc(ent[0], 16)
        self.n_ins += 1
        ev = (ent[0], ent[1], "dma")
        self._commit(ev, reads, writes)
        return ev

    def dmaf(self, qname, fn, reads=(), writes=(), sembuf=None):
        E = self.engs[qname]
        self._deps(qname, reads, writes)
        sb = sembuf or (writes[0] if writes else reads[0])
        key = sb.name.rsplit("_", 1)[0] if "_" in sb.name else sb.name
        ent = self.dpool.get(key)
        if ent is None:
            ent = [self.stack.enter_context(self.nc.semaphore("d%d" % self.ndsem)), 0]
            self.ndsem += 1
            self.dpool[key] = ent
        ins = fn(E["eng"])
        ent[1] += 16
        ins.then_inc(ent[0], 16)
        self.n_ins += 1
        ev = (ent[0], ent[1], "dma")
        self._commit(ev, reads, writes)
        return ev

    def barrier(self):
        evs = []
        for n, E in self.engs.items():
            if E["count"] > 0:
                evs.append((E["sem"], E["count"], "bar_" + n))
        for key, ent in self.dpool.items():
            if ent[1] > 0 and key not in self.nobar:
                evs.append((ent[0], ent[1], "dma"))
        for n in self.engs:
            for ev in evs:
                if ev[2] == "bar_" + n:
                    continue
                self._wait(n, ev)


def build():
    nc = bass.Bass("TRN2", target_bir_lowering=False)

    def din(name, shape, dt=F32):
        return nc.dram_tensor(name, shape, dt, kind="ExternalInput").ap()

    x = din("x", [NB, T, DM])
    pos = din("pos", [NB, T], I32)
    w_in = din("w_in", [DM, 1352])
    w_uq = din("w_uq", [256, 768])
    w_uk = din("w_uk", [128, 512])
    w_uv = din("w_uv", [128, 512])
    w_up_a = din("w_up_a", [512, DM])
    w_up_b = din("w_up_b", [512, DM])
    w_gate = din("w_gate", [DM, 2048])
    w_o = din("w_o", [DM, DM])
    w_r = din("w_r", [DM, 36])
    b_r = din("b_r", [36])
    w_eg = din("w_eg", [32 * 128, 2048])
    w_eu = din("w_eu", [32 * 128, 2048])
    w_ed = din("w_ed", [32 * 128, 2048])
    ustr = din("ustr", [128, 128])
    jv = din("jv", [128, 64])
    pcol = din("pcol", [128, 1])
    lnv = din("lnv", [6, DM])
    qng = din("qng", [128, 2])
    kvg = din("kvg", [128, 1])
    bgate = din("bgate", [128, 16])
    tzr = din("tzr", [128, 8, 2, 128])
    cfar = din("cfar", [8])
    identf = din("identf", [128, 128])
    invf = din("invf", [32, 1])
    out = nc.dram_tensor("out", [NB, T, DM], F32, kind="ExternalOutput").ap()
    hs = nc.dram_tensor("hs", [T, DM], F32, kind="Internal").ap()
    hbd = nc.dram_tensor("hbd", [T, DM], BF16, kind="Internal").ap()
    Hs = nc.dram_tensor("Hs", [64 * 128, DM], BF16, kind="Internal").ap()
    Ys = nc.dram_tensor("Ys", [64 * 128, DM], F32, kind="Internal").ap()
    weg_b = nc.dram_tensor("weg_b", [32 * 128, 2048], BF16, kind="Internal").ap()
    weu_b = nc.dram_tensor("weu_b", [32 * 128, 2048], BF16, kind="Internal").ap()
    wed_b = nc.dram_tensor("wed_b", [32 * 128, 2048], BF16, kind="Internal").ap()

    with ExitStack() as st0:
        S = Sched(nc, st0)
        hsB = Buf(hs, "hs")
        outB = Buf(out, "out")
        bcreg = st0.enter_context(nc.gpsimd.register("bcreg"))
        nc.gpsimd.reg_mov(bcreg, 32 * 128 - 1)
        hbdB = Buf(hbd, "hbd")
        HsB = Buf(Hs, "Hs")
        YsB = Buf(Ys, "Ys")

        identb = S.sbuf(st0, "identb", [128, 128], BF16)
        identF = S.sbuf(st0, "identF", [128, 128], F32)
        onesb = S.sbuf(st0, "onesb", [128, 128], BF16)
        lnbc = S.sbuf(st0, "lnbc", [128, 6, DM], F32)
        S.dma("pool", identb[:], identf, writes=[identb])
        ustrb = S.sbuf(st0, "ustrb", [128, 128], BF16)
        jvS = S.sbuf(st0, "jvS", [128, 64], F32)
        pcolS = S.sbuf(st0, "pcolS", [128, 1], F32)
        S.dma("pool", ustrb[:], ustr, writes=[ustrb])
        S.dma("sp", jvS[:], jv, writes=[jvS])
        S.dma("sp", pcolS[:], pcol, writes=[pcolS])
        WcB = Buf(None, "wcast")
        S.nobar.add("wcast")
        conv_list = [(srcw, dstw, e_) for e_ in range(32) for srcw, dstw in ((w_eg, weg_b), (w_eu, weu_b), (w_ed, wed_b))]

        def conv_some(n):
            for _ in range(n):
                if not conv_list:
                    return
                srcw, dstw, e_ = conv_list.pop(0)
                S.dma("pool", dstw[e_ * 128:(e_ + 1) * 128, :], srcw[e_ * 128:(e_ + 1) * 128, :], writes=[WcB])
        with ExitStack() as stz:
            zt = S.sbuf(stz, "zt", [128, 8, DM], BF16)
            S.op("pool", lambda e: e.memset(zt[:], 0.0), writes=[zt])
            for q in range(8):
                S.dma("sp", Hs[q * 1024:(q + 1) * 1024, :].rearrange("(p r) n -> p r n", p=128), zt[:], reads=[zt], writes=[HsB], sembuf=zt)
            S.barrier()
        S.dma("sp", identF[:], identf, writes=[identF])
        S.op("dve", lambda e: e.memset(onesb[:], 1.0), writes=[onesb])
        for k in range(6):
            S.dma("sp", lnbc[:, k, :], lnv[k].partition_broadcast(128), writes=[lnbc])

        def layer_norm_tile(st_bufs, src, dst_ap_fn, gi, out_bufs, scale_after=None):
            stats, mv, rstd, nmr, z = (st_bufs[k] for k in ("stats", "mv", "rstd", "nmr", "z"))
            S.op("dve", lambda e: e.bn_stats(stats[:, 0, :], src[:, 0:512]), reads=[src], writes=[stats])
            S.op("dve", lambda e: e.bn_stats(stats[:, 1, :], src[:, 512:1024]), reads=[src], writes=[stats])
            S.op("dve", lambda e: e.bn_aggr(mv[:], stats[:].rearrange("p a b -> p (a b)")), reads=[stats], writes=[mv])
            S.op("dve", lambda e: e.tensor_scalar(rstd[:], mv[:, 1:2], LN_EPS, None, ALU.add), reads=[mv], writes=[rstd])
            S.op("act", lambda e: e.activation(rstd[:], rstd[:], AF.Sqrt), reads=[rstd], writes=[rstd])
            S.op("dve", lambda e: e.reciprocal(rstd[:], rstd[:]), reads=[rstd], writes=[rstd])
            S.op("dve", lambda e: e.tensor_scalar(nmr[:], mv[:, 0:1], rstd[:, 0:1], -1.0, ALU.mult, ALU.mult),
                 reads=[mv, rstd], writes=[nmr])
            S.op("act", lambda e: e.activation(z[:], src[:], AF.Identity, bias=nmr[:, 0:1], scale=rstd[:, 0:1]),
                 reads=[src, nmr, rstd], writes=[z])
            S.op("dve", lambda e: e.tensor_tensor(z[:], z[:], lnbc[:, gi, :], ALU.mult), reads=[z, lnbc], writes=[z])

        for b in range(NB):
            stB = ExitStack()
            stB.__enter__()
            xnT = S.sbuf(stB, "xnT", [128, 8, T], BF16)
            ca = S.sbuf(stB, "ca", [128, NT], F32)
            cb = S.sbuf(stB, "cb", [128, NT], F32)
            M1 = S.sbuf(stB, "M1", [128, NT, 32], F32)
            M2 = S.sbuf(stB, "M2", [128, NT, 32], F32)
            Mb = S.sbuf(stB, "Mb", [128, NT, 32], BF16)
            with ExitStack() as stA:
                oaT = S.sbuf(stA, "oaT", [128, 4, T], BF16)
                obT = S.sbuf(stA, "obT", [128, 4, T], BF16)
                lns = dict(stats=S.sbuf(stA, "stats", [128, 2, 6], F32), mv=S.sbuf(stA, "mv", [128, 2], F32),
                           rstd=S.sbuf(stA, "rstd", [128, 1], F32), nmr=S.sbuf(stA, "nmr", [128, 1], F32),
                           z=S.sbuf(stA, "z", [128, DM], F32))
                xt = [S.sbuf(stA, "xt%d" % i, [128, DM], F32) for i in range(2)]

                with ExitStack() as st1:
                    xnb = [S.sbuf(st1, "xnb%d" % i, [128, DM], BF16) for i in range(2)]
                    ptr = [S.psum(st1, "ptr%d" % i, [128, 1024], BF16) for i in range(2)]
                    for i in range(NT):
                        xs = xt[i % 2]
                        S.dma("sp", xs[:], x[b, i * 128:(i + 1) * 128, :], writes=[xs])
                        layer_norm_tile(lns, xs, None, 0, None)
                        z = lns["z"]
                        xb = xnb[i % 2]
                        S.op("pool", lambda e: e.tensor_tensor(xb[:], z[:], lnbc[:, 1, :], ALU.add),
                             reads=[z, lnbc], writes=[xb])
                        pt = ptr[i % 2]
                        for c in range(8):
                            S.op("pe", lambda e: e.transpose(pt[:, c * 128:(c + 1) * 128], xb[:, c * 128:(c + 1) * 128], identb[:]),
                                 reads=[xb, identb], writes=[pt], inc=(c == 7))
                        S.op("act", lambda e: e.activation(xnT[:, :, i * 128:(i + 1) * 128],
                                                           pt[:].rearrange("p (c t) -> p c t", c=8), AF.Copy),
                             reads=[pt], writes=[xnT])
                    S.barrier()

                with ExitStack() as st2:
                    pp = [S.psum(st2, "pp%d" % i, [128, 512], F32) for i in range(3)]
                    ps = [S.psum(st2, "ps%d" % i, [128, 512], F32) for i in range(3)]
                    po = [S.psum(st2, "po%d" % i, [128, 512], F32) for i in range(2)]
                    Wa = S.sbuf(st2, "Wa", [128, 8, 416], BF16)
                    Wkrr = S.sbuf(st2, "Wkrr", [128, 8, 32], BF16)
                    Wq = S.sbuf(st2, "Wq", [128, 2, 8, 96], BF16)
                    Wqr = S.sbuf(st2, "Wqr", [128, 2, 8, 32], BF16)
                    Wk = S.sbuf(st2, "Wk", [128, 8, 64], BF16)
                    Wv = S.sbuf(st2, "Wv", [128, 512], BF16)
                    gq = S.sbuf(st2, "gq", [128, 2], F32)
                    gkv = S.sbuf(st2, "gkv", [128, 1], F32)
                    invfS = S.sbuf(st2, "invfS", [96, 1], F32)
                    cos32 = S.sbuf(st2, "cos32", [96, T], F32)
                    sin32 = S.sbuf(st2, "sin32", [96, T], F32)
                    cqn = S.sbuf(st2, "cqn", [128, 2, T], BF16)
                    ckvn = S.sbuf(st2, "ckvn", [128, T], BF16)
                    krT = S.sbuf(st2, "krT", [96, T], BF16)
                    st2a = ExitStack()
                    st2a.__enter__()
                    wq32 = S.sbuf(st2a, "wq32", [128, 2, 768], F32)
                    wk32 = S.sbuf(st2a, "wk32", [128, 512], F32)
                    wv32 = S.sbuf(st2a, "wv32", [128, 512], F32)
                    posi = S.sbuf(st2a, "posi", [96, T], I32)
                    rr = S.sbuf(st2a, "rr", [96, T], F32)
                    rf = S.sbuf(st2a, "rf", [96, T], F32)
                    tq = S.sbuf(st2a, "tq", [96, T], F32)

                    wsrc = w_in.rearrange("(c p) n -> p c n", p=128)
                    S.dma("pool", Wa[:], wsrc[:, :, 0:416], writes=[Wa])
                    S.dma("sp", wq32[:], w_uq.rearrange("(c p) n -> p c n", p=128), writes=[wq32])
                    S.dma("sp", wk32[:], w_uk, writes=[wk32])
                    S.dma("sp", wv32[:], w_uv, writes=[wv32])
                    S.dma("sp", gq[:], qng, writes=[gq])
                    S.dma("sp", gkv[:], kvg, writes=[gkv])
                    S.dma("sp", invfS[64:96, :], invf, writes=[invfS])
                    S.dma("sp", posi[64:96, :], pos[b].partition_broadcast(32), writes=[posi])
                    S.op("dve", lambda e: e.tensor_scalar(Wkrr[:, :, 0:16], Wa[:, :, 400:416], -1.0, None, ALU.mult),
                         reads=[Wa], writes=[Wkrr])
                    S.op("dve", lambda e: e.tensor_copy(Wkrr[:, :, 16:32], Wa[:, :, 384:400]), reads=[Wa], writes=[Wkrr])
                    for c in range(2):
                        src = wq32[:, c, :].rearrange("p (h d) -> p h d", h=8)
                        g = gq[:, c:c + 1]
                        S.op("dve", lambda e: e.tensor_scalar(Wq[:, c, :, :], src[:, :, :], g, None, ALU.mult),
                             reads=[wq32, gq], writes=[Wq])
                        S.op("dve", lambda e: e.tensor_scalar(Wqr[:, c, :, 0:16], src[:, :, 80:96], g, -1.0, ALU.mult, ALU.mult),
                             reads=[wq32, gq], writes=[Wqr])
                        S.op("dve", lambda e: e.tensor_scalar(Wqr[:, c, :, 16:32], src[:, :, 64:80], g, None, ALU.mult),
                             reads=[wq32, gq], writes=[Wqr])
                    S.op("dve", lambda e: e.tensor_scalar(Wk[:, :, :], wk32[:].rearrange("p (h d) -> p h d", h=8),
                                                          gkv[:, 0:1], None, ALU.mult), reads=[wk32, gkv], writes=[Wk])
                    S.op("dve", lambda e: e.tensor_scalar(Wv[:], wv32[:], gkv[:, 0:1], None, ALU.mult),
                         reads=[wv32, gkv], writes=[Wv])

                    S.op("dve", lambda e: e.tensor_copy(rr[64:96, :], posi[64:96, :]), reads=[posi], writes=[rr])
                    S.op("dve", lambda e: e.tensor_scalar(rr[64:96, :], rr[64:96, :], invfS[64:96, 0:1], None, ALU.mult), reads=[rr, invfS], writes=[rr])
                    S.op("dve", lambda e: e.tensor_copy(posi[64:96, :], rr[64:96, :]), reads=[rr], writes=[posi])
                    S.op("dve", lambda e: e.tensor_copy(rf[64:96, :], posi[64:96, :]), reads=[posi], writes=[rf])
                    S.op("dve", lambda e: e.tensor_tensor(rr[64:96, :], rr[64:96, :], rf[64:96, :], ALU.subtract), reads=[rr, rf], writes=[rr])

                    def wrap_sin(dst, shift):
                        S.op("dve", lambda e: e.tensor_scalar(rf[64:96, :], rr[64:96, :], shift, None, ALU.add), reads=[rr], writes=[rf])
                        for _ in range(2):
                            S.op("dve", lambda e: e.tensor_scalar(tq[64:96, :], rf[64:96, :], 0.5, None, ALU.is_gt), reads=[rf], writes=[tq])
                            S.op("dve", lambda e: e.tensor_tensor(rf[64:96, :], rf[64:96, :], tq[64:96, :], ALU.subtract), reads=[rf, tq], writes=[rf])
                        S.op("dve", lambda e: e.tensor_scalar(tq[64:96, :], rf[64:96, :], -0.5, None, ALU.is_lt), reads=[rf], writes=[tq])
                        S.op("dve", lambda e: e.tensor_tensor(rf[64:96, :], rf[64:96, :], tq[64:96, :], ALU.add), reads=[rf, tq], writes=[rf])
                        S.op("act", lambda e: e.activation(dst[64:96, :], rf[64:96, :], AF.Sin, scale=2.0 * math.pi * (1.0 - 2e-6)),
                             reads=[rf], writes=[dst])

                    wrap_sin(sin32, 0.0)
                    wrap_sin(cos32, 0.25)
                    S.barrier()
                    st2a.close()
                    Vh = [S.sbuf(st2, "Vh%d" % i, [128, NT, 128], BF16) for i in range(2)]
                    qTs = [S.sbuf(st2, "qT%d" % i, [96, T], BF16) for i in range(2)]
                    kTs = [S.sbuf(st2, "kT%d" % i, [96, T], BF16) for i in range(2)]
                    c32 = S.sbuf(st2, "c32", [128, 2, 512], F32)
                    sqb = S.sbuf(st2, "sqb", [128, 2, 512], BF16)
                    rq = S.sbuf(st2, "rq", [128, 512], F32)
                    t1 = S.sbuf(st2, "t1", [96, 512], F32)
                    t2 = S.sbuf(st2, "t2", [96, 512], F32)
                    PTs = [S.sbuf(st2, "PT%d" % i, [128, 512], BF16) for i in range(3)]
                    rd = S.sbuf(st2, "rd", [128, 512], F32)
                    for i in range(2):
                        S.op("pool", lambda e: e.memset(Vh[i][:], 1.0), writes=[Vh[i]])

                    def rms_block(psrc_list, dstT_fn, nfeat, tb):
                        n = len(psrc_list)
                        for m, pb in enumerate(psrc_list):
                            S.op("act", lambda e: e.activation(c32[:, m, :], pb[:], AF.Copy), reads=[pb], writes=[c32])
                            S.op("act", lambda e: e.activation(sqb[:, m, :], pb[:], AF.Square), reads=[pb], writes=[sqb])
                        pss = pp[2]
                        for m in range(n):
                            S.op("pe", lambda e: e.matmul(pss[:], onesb[:], sqb[:, m, :], start=(m == 0), stop=(m == n - 1)),
                                 reads=[onesb, sqb], writes=[pss], inc=(m == n - 1))
                        S.op("dve", lambda e: e.tensor_scalar(rq[:], pss[:], 1.0 / nfeat, RMS_EPS, ALU.mult, ALU.add),
                             reads=[pss], writes=[rq])
                        S.op("act", lambda e: e.activation(rq[:], rq[:], AF.Sqrt), reads=[rq], writes=[rq])
                        S.op("dve", lambda e: e.reciprocal(rq[:], rq[:]), reads=[rq], writes=[rq])
                        for m in range(n):
                            S.op("dve", lambda e: e.tensor_tensor(dstT_fn(m), c32[:, m, :], rq[:], ALU.mult),
                                 reads=[c32, rq], writes=[dstT_fn.buf])

                    def proj_fm(pb, lhs_fn, tb, M=128, p0=0):
                        for c in range(8):
                            S.op("pe", lambda e: e.matmul(pb[p0:p0 + M, :], lhs_fn(c), xnT[:, c, tb * 512:(tb + 1) * 512],
                                                          start=(c == 0), stop=(c == 7)),
                                 reads=[xnT, Wa, Wkrr], writes=[pb], inc=(c == 7))

                    for tb in range(4):
                        cols = slice(tb * 512, (tb + 1) * 512)
                        proj_fm(pp[0], lambda c: Wa[:, c, 0:128], tb)
                        proj_fm(pp[1], lambda c: Wa[:, c, 128:256], tb)
                        f = lambda m: cqn[:, m, cols]
                        f.buf = cqn
                        rms_block([pp[0], pp[1]], f, 256.0, tb)
                        proj_fm(pp[0], lambda c: Wa[:, c, 256:384], tb)
                        f2 = lambda m: ckvn[:, cols]
                        f2.buf = ckvn
                        rms_block([pp[0]], f2, 128.0, tb)
                        proj_fm(pp[0], lambda c: Wa[:, c, 384:416], tb, M=32, p0=64)
                        proj_fm(pp[1], lambda c: Wkrr[:, c, :], tb, M=32, p0=64)
                        S.op("dve", lambda e: e.tensor_tensor(t1[64:96, :], pp[0][64:96, :], cos32[64:96, cols], ALU.mult),
                             reads=[pp[0], cos32], writes=[t1])
                        S.op("dve", lambda e: e.tensor_tensor(t2[64:96, :], pp[1][64:96, :], sin32[64:96, cols], ALU.mult),
                             reads=[pp[1], sin32], writes=[t2])
                        S.op("dve", lambda e: e.tensor_tensor(krT[64:96, cols], t1[64:96, :], t2[64:96, :], ALU.add), reads=[t1, t2], writes=[krT])
                    sc_mla = 96.0 ** -0.5
                    LA = 2

                    def mla_proj(h):
                        qT = qTs[h % 2]
                        kT = kTs[h % 2]
                        for tb in range(4):
                            cols = slice(tb * 512, (tb + 1) * 512)
                            pa, pbb = pp[0], pp[1]
                            for m in range(2):
                                S.op("pe", lambda e: e.matmul(pa[0:96, :], Wq[:, m, h, :], cqn[:, m, cols], start=(m == 0), stop=(m == 1)),
                                     reads=[Wq, cqn], writes=[pa], inc=(m == 1))
                            for m in range(2):
                                S.op("pe", lambda e: e.matmul(pbb[64:96, :], Wqr[:, m, h, :], cqn[:, m, cols], start=(m == 0), stop=(m == 1)),
                                     reads=[Wqr, cqn], writes=[pbb], inc=(m == 1))
                            pk = pp[2]
                            S.op("pe", lambda e: e.matmul(pk[0:64, :], Wk[:, h, :], ckvn[:, cols], start=True, stop=True),
                                 reads=[Wk, ckvn], writes=[pk])
                            S.op("dve", lambda e: e.tensor_tensor(t1[64:96, :], pa[64:96, :], cos32[64:96, cols], ALU.mult), reads=[pa, cos32], writes=[t1])
                            S.op("dve", lambda e: e.tensor_tensor(t2[64:96, :], pbb[64:96, :], sin32[64:96, cols], ALU.mult), reads=[pbb, sin32], writes=[t2])
                            S.op("dve", lambda e: e.tensor_tensor(qT[64:96, cols], t1[64:96, :], t2[64:96, :], ALU.add), reads=[t1, t2], writes=[qT])
                            S.op("act", lambda e: e.activation(qT[0:64, cols], pa[0:64, :], AF.Copy), reads=[pa], writes=[qT])
                            S.op("act", lambda e: e.activation(kT[0:64, cols], pk[0:64, :], AF.Copy), reads=[pk], writes=[kT])
                        S.op("pool", lambda e: e.tensor_copy(kT[64:96, :], krT[64:96, :]), reads=[krT], writes=[kT])
                        Vc = Vh[h % 2]
                        voff = 64 * (h % 2)
                        for k4 in range(4):
                            pv = pp[k4 % 2]
                            for j in range(4):
                                kt = k4 * 4 + j
                                S.op("pe", lambda e: e.matmul(pv[:, j * 64:(j + 1) * 64], ckvn[:, kt * 128:(kt + 1) * 128],
                                                              Wv[:, h * 64:(h + 1) * 64], start=True, stop=True),
                                     reads=[ckvn, Wv], writes=[pv], inc=(j == 3))
                            S.op("act", lambda e: e.activation(Vc[:, k4 * 4:(k4 + 1) * 4, voff:voff + 64],
                                                               pv[:, 0:256].rearrange("p (j d) -> p j d", j=4), AF.Copy),
                                 reads=[pv], writes=[Vc])

                    st_ = dict(si=0, oi=0)

                    def mla_attn(h):
                        conv_some(6)
                        qT = qTs[h % 2]
                        kT = kTs[h % 2]
                        Vc = Vh[h % 2]
                        for qb in range(4):
                            pO = po[st_["oi"] % 2]
                            st_["oi"] += 1
                            nk = 4 * qb + 4
                            slots = {}

                            def emit_S(kt):
                                c0 = max(0, kt - 4 * qb) * 128
                                sl = st_["si"] % 3
                                st_["si"] += 1
                                slots[kt] = sl
                                pS, PT = ps[sl], PTs[sl]
                                S.op("pe", lambda e: e.matmul(pS[:, c0:512], kT[:, kt * 128:(kt + 1) * 128],
                                                              qT[:, qb * 512 + c0:(qb + 1) * 512], start=True, stop=True),
                                     reads=[kT, qT], writes=[pS])
                                S.op("act", lambda e: e.activation(PT[:, c0:512], pS[:, c0:512], AF.Exp, scale=sc_mla),
                                     reads=[pS], writes=[PT])
                                if kt >= 4 * qb:
                                    S.op("pool", lambda e: e.memset(PT[64:128, c0:c0 + 64], 0.0), writes=[PT])

                            def emit_PV(kt):
                                c0 = max(0, kt - 4 * qb) * 128
                                PT = PTs[slots[kt]]
                                S.op("pe", lambda e: e.matmul(pO[:, c0:512], Vc[:, kt, :], PT[:, c0:512],
                                                              start=(kt == 0), stop=(kt == nk - 1), skip_group_check=True),
                                     reads=[Vc, PT], writes=[pO], inc=(kt == nk - 1))

                            for s_ in range(nk + LA):
                                if s_ < nk:
                                    emit_S(s_)
                                if s_ >= LA:
                                    emit_PV(s_ - LA)
                            ocols = slice(qb * 512, (qb + 1) * 512)
                            if h % 2 == 0:
                                S.op("dve", lambda e: e.reciprocal(rd[0:64, :], pO[64:128, :]), reads=[pO], writes=[rd])
                                S.op("dve", lambda e: e.tensor_tensor(oaT[0:64, h // 2, ocols], pO[0:64, :], rd[0:64, :], ALU.mult),
                                     reads=[pO, rd], writes=[oaT])
                            else:
                                S.op("dve", lambda e: e.reciprocal(rd[64:128, :], pO[0:64, :]), reads=[pO], writes=[rd])
                                S.op("dve", lambda e: e.tensor_tensor(oaT[64:128, h // 2, ocols], pO[64:128, :], rd[64:128, :], ALU.mult),
                                     reads=[pO, rd], writes=[oaT])

                    mla_proj(0)
                    for h in range(8):
                        if h + 1 < 8:
                            mla_proj(h + 1)
                        mla_attn(h)
                    S.barrier()

                with ExitStack() as st3:
                    pp = [S.psum(st3, "qp%d" % i, [128, 512], F32) for i in range(2)]
                    ps = [S.psum(st3, "qs%d" % i, [128, 512], F32) for i in range(3)]
                    po = [S.psum(st3, "qo%d" % i, [128, 512], F32) for i in range(2)]
                    pmt = S.psum(st3, "pmt", [128, 1024], BF16)
                    qbT = S.sbuf(st3, "qbT", [128, 4, T], BF16)
                    kbT2 = S.sbuf(st3, "kbT2", [128, T], BF16)
                    qiT = S.sbuf(st3, "qiT", [128, 2, T], BF16)
                    kiT4 = S.sbuf(st3, "kiT4", [128, T], BF16)
                    Vb = S.sbuf(st3, "Vb", [128, NT, 2, 128], BF16)
                    widx = S.sbuf(st3, "widx", [128, NT, 8], F32)
                    tz = S.sbuf(st3, "tz", [128, 8, 2, 128], F32)
                    cfb = S.sbuf(st3, "cfb", [128, 8], F32)
                    st3a = ExitStack()
                    st3a.__enter__()
                    Wb = S.sbuf(st3a, "Wb", [128, 8, 936], BF16)
                    Wkb2 = S.sbuf(st3a, "Wkb2", [128, 8, 128], BF16)
                    Wki4 = S.sbuf(st3a, "Wki4", [128, 8, 128], BF16)

                    wsrc = w_in.rearrange("(c p) n -> p c n", p=128)
                    S.dma("pool", Wb[:], wsrc[:, :, 416:1352], writes=[Wb])
                    for r in range(2):
                        S.dma("pool", Wkb2[:, :, r * 64:(r + 1) * 64], wsrc[:, :, 928:992], writes=[Wkb2])
                    for r in range(4):
                        S.dma("pool", Wki4[:, :, r * 32:(r + 1) * 32], wsrc[:, :, 1312:1344], writes=[Wki4])
                    S.dma("sp", tz[:], tzr, writes=[tz])
                    S.dma("sp", cfb[:], cfar.partition_broadcast(128), writes=[cfb])
                    for h in range(8):
                        S.op("dve", lambda e: e.tensor_scalar(tz[:, h], tz[:, h], cfb[:, h:h + 1], 8.0, ALU.subtract, ALU.mult),
                             reads=[tz, cfb], writes=[tz])
                    S.op("pool", lambda e: e.memset(Vb[:], 1.0), writes=[Vb])

                    def proj3(dst_ap, dstbuf, lhs_fn, wbuf, tb, k):
                        pb = pp[k % 2]
                        for c in range(8):
                            S.op("pe", lambda e: e.matmul(pb[:], lhs_fn(c), xnT[:, c, tb * 512:(tb + 1) * 512],
                                                          start=(c == 0), stop=(c == 7)), reads=[xnT, wbuf], writes=[pb], inc=(c == 7))
                        S.op("act", lambda e: e.activation(dst_ap, pb[:], AF.Copy), reads=[pb], writes=[dstbuf])

                    k = 0
                    for tb in range(4):
                        cols = slice(tb * 512, (tb + 1) * 512)
                        for p in range(4):
                            proj3(qbT[:, p, cols], qbT, lambda c: Wb[:, c, p * 128:(p + 1) * 128], Wb, tb, k); k += 1
                        proj3(kbT2[:, cols], kbT2, lambda c: Wkb2[:, c, :], Wkb2, tb, k); k += 1
                        for g in range(2):
                            proj3(qiT[:, g, cols], qiT, lambda c: Wb[:, c, 640 + g * 128:640 + (g + 1) * 128], Wb, tb, k); k += 1
                        proj3(kiT4[:, cols], kiT4, lambda c: Wki4[:, c, :], Wki4, tb, k); k += 1
                    for kt in range(NT):
                        pb = pp[kt % 2]
                        tsl = slice(kt * 128, (kt + 1) * 128)
                        for c in range(8):
                            S.op("pe", lambda e: e.matmul(pb[:, 0:64], xnT[:, c, tsl], Wb[:, c, 576:640], start=(c == 0), stop=(c == 7)),
                                 reads=[xnT, Wb], writes=[pb], inc=(c == 7))
                        for c in range(8):
                            S.op("pe", lambda e: e.matmul(pb[:, 64:72], xnT[:, c, tsl], Wb[:, c, 928:936], start=(c == 0), stop=(c == 7)),
                                 reads=[xnT, Wb], writes=[pb], inc=(c == 7))
                        S.op("act", lambda e: e.activation(Vb[:, kt, 0, 0:64], pb[:, 0:64], AF.Copy), reads=[pb], writes=[Vb])
                        S.op("act", lambda e: e.activation(Vb[:, kt, 1, 64:128], pb[:, 0:64], AF.Copy), reads=[pb], writes=[Vb])
                        S.op("act", lambda e: e.activation(widx[:, kt, :], pb[:, 64:72], AF.Copy, scale=0.0625), reads=[pb], writes=[widx])

                    S.barrier()
                    st3a.close()
                    Sc = [S.sbuf(st3, "Sc%d" % i, [128, T], F32) for i in range(1)]
                    msk = [S.sbuf(st3, "msk%d" % i, [128, T], BF16) for i in range(4)]
                    mskT = S.sbuf(st3, "mskT", [128, NT, 512], BF16)
                    rl = [S.sbuf(st3, "rl%d" % i, [128, 512], F32) for i in range(2)]
                    bmx = S.sbuf(st3, "bmx", [128, 1], F32)
                    bmn = S.sbuf(st3, "bmn", [128, 1], F32)
                    brg = S.sbuf(st3, "brg", [128, 1], F32)
                    bmid = S.sbuf(st3, "bmid", [128, 1], F32)
                    bcnt = S.sbuf(st3, "bcnt", [128, 1], F32)
                    bt = S.sbuf(st3, "bt", [128, 1], F32)
                    Es = [S.sbuf(st3, "E%d" % i, [128, 512], BF16) for i in range(3)]
                    PTs = [S.sbuf(st3, "PTb%d" % i, [128, 512], BF16) for i in range(3)]
                    rd = S.sbuf(st3, "rdb", [128, 512], F32)
                    sidx = [0]
                    oi = 0
                    ri = 0
                    for qb in range(4):
                        for j in range(4):
                            qt = 4 * qb + j
                            n = (qt + 1) * 128
                            sc = Sc[0]
                            mk = msk[j]
                            tsl = slice(qt * 128, (qt + 1) * 128)
                            for hh in range(8):
                                g, jj = hh // 4, hh % 4
                                for sb in range((n + 511) // 512):
                                    w = min(512, n - sb * 512)
                                    pr = pp[ri % 2]
                                    rlb = rl[ri % 2]
                                    ri += 1
                                    S.op("pe", lambda e: e.matmul(pr[:, 0:w], qiT[32 * jj:32 * jj + 32, g, tsl],
                                                                  kiT4[32 * jj:32 * jj + 32, sb * 512:sb * 512 + w],
                                                                  start=True, stop=True, tile_position=(32 * jj, 0)),
                                         reads=[qiT, kiT4], writes=[pr])
                                    S.op("act", lambda e: e.activation(rlb[:, 0:w], pr[:, 0:w], AF.Relu), reads=[pr], writes=[rlb])
                                    dst = sc[:, sb * 512:sb * 512 + w]
                                    if hh == 0:
                                        S.op("dve", lambda e: e.tensor_scalar(dst, rlb[:, 0:w], widx[:, qt, 0:1], None, ALU.mult),
                                             reads=[rlb, widx], writes=[sc])
                                    else:
                                        S.op("dve", lambda e: e.scalar_tensor_tensor(dst, rlb[:, 0:w], widx[:, qt, hh:hh + 1], dst,
                                                                                     ALU.mult, ALU.add),
                                             reads=[rlb, widx, sc], writes=[sc])
                            S.op("pool", lambda e: e.memset(sc[0:64, n - 64:n], NEGBIG), writes=[sc])
                            if qt >= 2:
                                S.op("dve", lambda e: e.reduce_max(bmx[:], sc[:, 0:n], AX.X), reads=[sc], writes=[bmx])
                                S.op("dve", lambda e: e.tensor_reduce(bmn[:], sc[:, 0:n - 64], AX.X, ALU.min), reads=[sc], writes=[bmn])
                                S.op("dve", lambda e: e.tensor_tensor(brg[:], bmx[:], bmn[:], ALU.subtract), reads=[bmx, bmn], writes=[brg])
                                S.op("dve", lambda e: e.tensor_scalar(brg[:], brg[:], 1e-20, None, ALU.add), reads=[brg], writes=[brg])
                                S.op("dve", lambda e: e.reciprocal(brg[:], brg[:]), reads=[brg], writes=[brg])
                                S.op("dve", lambda e: e.tensor_scalar(sc[:, 0:n], sc[:, 0:n], bmn[:, 0:1], brg[:, 0:1], ALU.subtract, ALU.mult),
                                     reads=[sc, bmn, brg], writes=[sc])
                                S.op("dve", lambda e: e.memset(bmid[:], 0.5), writes=[bmid])
                                for it in range(BIS_ITERS):
                                    S.op("dve", lambda e: e.tensor_scalar(mk[:, 0:n], sc[:, 0:n], bmid[:, 0:1], None, ALU.is_ge, ALU.add,
                                                                          accum_out=bcnt[:]),
                                         reads=[sc, bmid], writes=[mk, bcnt])
                                    s_next = 2.0 ** -(it + 2)
                                    S.op("dve", lambda e: e.tensor_scalar(bt[:], bcnt[:], float(TOPK), 2.0 * s_next, ALU.is_ge, ALU.mult),
                                         reads=[bcnt], writes=[bt])
                                    S.op("dve", lambda e: e.scalar_tensor_tensor(bmid[:], bt[:], -s_next, bmid[:], ALU.add, ALU.add),
                                         reads=[bt, bmid], writes=[bmid])
                                S.op("dve", lambda e: e.tensor_scalar(bmid[:], bmid[:], -(2.0 ** -(BIS_ITERS + 1)), None, ALU.add),
                                     reads=[bmid], writes=[bmid])
                                S.op("dve", lambda e: e.tensor_scalar(mk[:, 0:n], sc[:, 0:n], bmid[:, 0:1], None, ALU.is_ge),
                                     reads=[sc, bmid], writes=[mk])
                            else:
                                S.op("dve", lambda e: e.tensor_scalar(mk[:, 0:n], sc[:, 0:n], -1.0e29, None, ALU.is_ge),
                                     reads=[sc], writes=[mk])
                        for kt in range(4 * qb + 4):
                            j0 = max(0, kt - 4 * qb)
                            half = (kt % 2) * 512
                            for j in range(j0, 4):
                                S.op("pe", lambda e: e.transpose(pmt[:, half + j * 128:half + (j + 1) * 128],
                                                                 msk[j][:, kt * 128:(kt + 1) * 128], identb[:]),
                                     reads=[msk[j], identb], writes=[pmt], inc=(j == 3))
                            S.op("act", lambda e: e.activation(mskT[:, kt, j0 * 128:512], pmt[:, half + j0 * 128:half + 512], AF.Copy),
                                 reads=[pmt], writes=[mskT])
                        for h in range(8):
                            conv_some(2)
                            p, hf = h // 2, h % 2
                            base = 64 * hf
                            pO = po[oi % 2]
                            oi += 1
                            nk = 4 * qb + 4
                            slots = {}

                            def emit_S(kt):
                                global_si = sidx[0]
                                sidx[0] += 1
                                sl = global_si % 3
                                slots[kt] = sl
                                j0 = max(0, kt - 4 * qb)
                                c0 = j0 * 128
                                pS, E, PT = ps[sl], Es[sl], PTs[sl]
                                S.op("pe", lambda e: e.matmul(pS[:, c0:512], kbT2[base:base + 64, kt * 128:(kt + 1) * 128],
                                                              qbT[base:base + 64, p, qb * 512 + c0:(qb + 1) * 512], start=True, stop=True),
                                     reads=[kbT2, qbT], writes=[pS])
                                for j in range(j0, 4):
                                    d = 4 * qb + j - kt
                                    if d in (0, 1):
                                        S.op("dve", lambda e: e.tensor_tensor(pS[:, j * 128:(j + 1) * 128], pS[:, j * 128:(j + 1) * 128],
                                                                              tz[:, h, d, :], ALU.add), reads=[pS, tz], writes=[pS])
                                S.op("act", lambda e: e.activation(E[:, c0:512], pS[:, c0:512], AF.Exp, bias=cfb[:, h:h + 1], scale=0.125),
                                     reads=[pS, cfb], writes=[E])
                                S.op("dve", lambda e: e.tensor_tensor(PT[:, c0:512], E[:, c0:512], mskT[:, kt, c0:512], ALU.mult),
                                     reads=[E, mskT], writes=[PT])

                            def emit_PV(kt):
                                c0 = max(0, kt - 4 * qb) * 128
                                PT = PTs[slots[kt]]
                                S.op("pe", lambda e: e.matmul(pO[:, c0:512], Vb[:, kt, hf, :], PT[:, c0:512],
                                                              start=(kt == 0), stop=(kt == nk - 1), skip_group_check=True),
                                     reads=[Vb, PT], writes=[pO], inc=(kt == nk - 1))

                            for s_ in range(nk + 2):
                                if s_ < nk:
                                    emit_S(s_)
                                if s_ >= 2:
                                    emit_PV(s_ - 2)
                            ocols = slice(qb * 512, (qb + 1) * 512)
                            if hf == 0:
                                S.op("dve", lambda e: e.reciprocal(rd[0:64, :], pO[64:128, :]), reads=[pO], writes=[rd])
                                S.op("dve", lambda e: e.tensor_tensor(obT[0:64, p, ocols], pO[0:64, :], rd[0:64, :], ALU.mult),
                                     reads=[pO, rd], writes=[obT])
                            else:
                                S.op("dve", lambda e: e.reciprocal(rd[64:128, :], pO[0:64, :]), reads=[pO], writes=[rd])
                                S.op("dve", lambda e: e.tensor_tensor(obT[64:128, p, ocols], pO[64:128, :], rd[64:128, :], ALU.mult),
                                     reads=[pO, rd], writes=[obT])
                    S.barrier()

                with ExitStack() as st4:
                    Wo = S.sbuf(st4, "Wo", [128, 8, DM], BF16)
                    mixT = S.sbuf(st4, "mixT", [128, 8, T], BF16)
                    bg = S.sbuf(st4, "bg", [128, 16], F32)
                    Wr = S.sbuf(st4, "Wr", [128, 8, 36], F32)
                    brb = S.sbuf(st4, "brb", [128, 36], F32)
                    st4a = ExitStack()
                    st4a.__enter__()
                    Wua = S.sbuf(st4a, "Wua", [128, 4, DM], BF16)
                    Wub = S.sbuf(st4a, "Wub", [128, 4, DM], BF16)
                    Wg = [S.sbuf(st4a, "Wg%d" % i, [128, 8, 2, 128], BF16) for i in range(2)]
                    sga = S.sbuf(st4a, "sga", [128, 512], F32)
                    sgb = S.sbuf(st4a, "sgb", [128, 512], F32)
                    m1 = S.sbuf(st4a, "m1", [128, 512], F32)
                    m2 = S.sbuf(st4a, "m2", [128, 512], F32)
                    pgs = [[S.psum(st4a, "pg%d_%d" % (q, i), [128, 512], F32) for i in range(4)] for q in range(2)]
                    sgas = [sga, S.sbuf(st4a, "sga2", [128, 512], F32)]
                    sgbs = [sgb, S.sbuf(st4a, "sgb2", [128, 512], F32)]
                    m1s = [m1, S.sbuf(st4a, "m1b", [128, 512], F32)]
                    m2s = [m2, S.sbuf(st4a, "m2b", [128, 512], F32)]
                    git = 0

                    S.dma("pool", Wua[:], w_up_a.rearrange("(c p) n -> p c n", p=128), writes=[Wua])
                    S.dma("pool", Wub[:], w_up_b.rearrange("(c p) n -> p c n", p=128), writes=[Wub])
                    S.dma("pool", Wo[:], w_o.rearrange("(c p) n -> p c n", p=128), writes=[Wo])
                    S.dma("sp", bg[:], bgate, writes=[bg])
                    S.dma("sp", Wr[:], w_r.rearrange("(c p) n -> p c n", p=128), writes=[Wr])
                    S.dma("sp", brb[:], b_r.partition_broadcast(128), writes=[brb])
                    gsrc = w_gate.rearrange("(c p) n -> p c n", p=128)
                    for m in range(8):
                        wg = Wg[m % 2]
                        S.dma("pool", wg[:, :, 0, :], gsrc[:, :, m * 128:(m + 1) * 128], writes=[wg])
                        S.dma("pool", wg[:, :, 1, :], gsrc[:, :, 1024 + m * 128:1024 + (m + 1) * 128], writes=[wg])
                        for tb in range(4):
                            cols = slice(tb * 512, (tb + 1) * 512)
                            pg = pgs[git % 2]
                            sga, sgb, m1, m2 = sgas[git % 2], sgbs[git % 2], m1s[git % 2], m2s[git % 2]
                            git += 1
                            for c in range(8):
                                S.op("pe", lambda e: e.matmul(pg[0][:], wg[:, c, 0, :], xnT[:, c, cols], start=(c == 0), stop=(c == 7)),
                                     reads=[wg, xnT], writes=[pg[0]], inc=(c == 7))
                            for c in range(8):
                                S.op("pe", lambda e: e.matmul(pg[1][:], wg[:, c, 1, :], xnT[:, c, cols], start=(c == 0), stop=(c == 7)),
                                     reads=[wg, xnT], writes=[pg[1]], inc=(c == 7))
                            for c in range(4):
                                S.op("pe", lambda e: e.matmul(pg[2][:], Wua[:, c, m * 128:(m + 1) * 128], oaT[:, c, cols], start=(c == 0), stop=(c == 3)),
                                     reads=[Wua, oaT], writes=[pg[2]], inc=(c == 3))
                            for c in range(4):
                                S.op("pe", lambda e: e.matmul(pg[3][:], Wub[:, c, m * 128:(m + 1) * 128], obT[:, c, cols], start=(c == 0), stop=(c == 3)),
                                     reads=[Wub, obT], writes=[pg[3]], inc=(c == 3))
                            S.op("act", lambda e: e.activation(sga[:], pg[0][:], AF.Sigmoid, bias=bg[:, m:m + 1]), reads=[pg[0], bg], writes=[sga])
                            S.op("act", lambda e: e.activation(sgb[:], pg[1][:], AF.Sigmoid, bias=bg[:, 8 + m:9 + m]), reads=[pg[1], bg], writes=[sgb])
                            S.op("dve", lambda e: e.tensor_tensor(m1[:], pg[2][:], sga[:], ALU.mult), reads=[pg[2], sga], writes=[m1])
                            S.op("dve", lambda e: e.tensor_tensor(m2[:], pg[3][:], sgb[:], ALU.mult), reads=[pg[3], sgb], writes=[m2])
                            S.op("dve", lambda e: e.tensor_tensor(mixT[:, m, cols], m1[:], m2[:], ALU.add), reads=[m1, m2], writes=[mixT])
                    S.barrier()
                    st4a.close()
                    pg = [S.psum(st4, "pgt%d" % i, [128, 512], F32) for i in range(2)]
                    pm = [S.psum(st4, "pm%d" % i, [128, 512], F32) for i in range(2)]
                    pl = S.psum(st4, "pl", [128, 512], F32)
                    pre = S.sbuf(st4, "pre", [128, DM], F32)
                    hbs = [S.sbuf(st4, "hb%d" % i, [128, DM], BF16) for i in range(2)]
                    hst = [S.sbuf(st4, "hst%d" % i, [128, DM], F32) for i in range(2)]
                    hT32 = S.sbuf(st4, "hT32", [128, 8, 128], F32)
                    lgA = S.sbuf(st4, "lgA", [128, NT, 36], F32)
                    gmxA = S.sbuf(st4, "gmxA", [128, NT], F32)
                    gselA = S.sbuf(st4, "gselA", [128, NT, 4], F32)
                    r4A = S.sbuf(st4, "r4A", [128, NT, 4], F32)
                    gwA = S.sbuf(st4, "gwA", [128, NT], F32)
                    le4 = S.sbuf(st4, "le4", [128, NT, 4, 8], F32)
                    seA = S.sbuf(st4, "seA", [128, NT, 8], F32)
                    se2A = S.sbuf(st4, "se2A", [128, NT, 8], F32)
                    oh1A = S.sbuf(st4, "oh1A", [128, NT, 8], F32)
                    oh2A = S.sbuf(st4, "oh2A", [128, NT, 8], F32)
                    mx1A = S.sbuf(st4, "mx1A", [128, NT], F32)
                    mx2A = S.sbuf(st4, "mx2A", [128, NT], F32)
                    w1A = S.sbuf(st4, "w1A", [128, NT], F32)
                    w2A = S.sbuf(st4, "w2A", [128, NT], F32)
                    lnsB = [dict(stats=S.sbuf(st4, "statsB%d" % k, [128, 2, 6], F32), mv=S.sbuf(st4, "mvB%d" % k, [128, 2], F32),
                                 rstd=S.sbuf(st4, "rstdB%d" % k, [128, 1], F32), nmr=S.sbuf(st4, "nmrB%d" % k, [128, 1], F32),
                                 z=S.sbuf(st4, "zB%d" % k, [128, DM], F32)) for k in range(2)]

                    def tile_A(i):
                        tsl = slice(i * 128, (i + 1) * 128)
                        xs = xt[i % 2]
                        S.dma("sp", xs[:], x[b, tsl, :], writes=[xs])
                        layer_norm_tile(lns, xs, None, 0, None)
                        z = lns["z"]
                        S.op("pool", lambda e: e.tensor_tensor(z[:], z[:], lnbc[:, 1, :], ALU.add), reads=[z, lnbc], writes=[z])
                        for hf in range(2):
                            for m in range(8):
                                S.op("pe", lambda e: e.matmul(pm[hf][:], mixT[:, m, tsl], Wo[:, m, hf * 512:(hf + 1) * 512],
                                                              start=(m == 0), stop=(m == 7)), reads=[mixT, Wo], writes=[pm[hf]], inc=(m == 7))
                            S.op("dve", lambda e: e.scalar_tensor_tensor(pre[:, hf * 512:(hf + 1) * 512], z[:, hf * 512:(hf + 1) * 512],
                                                                         ALPHA, pm[hf][:], ALU.mult, ALU.add),
                                 reads=[z, pm[hf]], writes=[pre])
                        lb = lnsB[i % 2]
                        layer_norm_tile(lb, pre, None, 2, None)
                        z1 = lb["z"]
                        S.op("pool", lambda e: e.tensor_tensor(z1[:], z1[:], lnbc[:, 3, :], ALU.add), reads=[z1, lnbc], writes=[z1])

                    def tile_B(i):
                        tsl = slice(i * 128, (i + 1) * 128)
                        z = lnsB[i % 2]["z"]
                        hh = hst[i % 2]
                        hb = hbs[i % 2]
                        S.op("act", lambda e: e.activation(hb[:], z[:], AF.Copy), reads=[z], writes=[hb])
                        S.op("act", lambda e: e.activation(hh[:], z[:], AF.Copy, scale=ALPHA), reads=[z], writes=[hh])
                        S.dma("sp", hs[tsl, :], hh[:], reads=[hh], writes=[hsB], sembuf=hh)
                        S.dma("sp", hbd[tsl, :], hb[:], reads=[hb], writes=[hbdB], sembuf=hb)
                        for hf in range(2):
                            for c in range(4):
                                cc = hf * 4 + c
                                S.op("pe", lambda e: e.transpose(pg[hf][:, c * 128:(c + 1) * 128], z[:, cc * 128:(cc + 1) * 128], identF[:]),
                                     reads=[z, identF], writes=[pg[hf]], inc=(c == 3))
                            S.op("act", lambda e: e.activation(hT32[:, hf * 4:(hf + 1) * 4, :], pg[hf][:].rearrange("p (c t) -> p c t", c=4), AF.Copy),
                                 reads=[pg[hf]], writes=[hT32])
                        for c in range(8):
                            S.op("pe", lambda e: e.matmul(pl[:, 0:36], hT32[:, c, :], Wr[:, c, :], start=(c == 0), stop=(c == 7)),
                                 reads=[hT32, Wr], writes=[pl], inc=(c == 7))
                        S.op("dve", lambda e: e.tensor_tensor(lgA[:, i, :], pl[:, 0:36], brb[:], ALU.add), reads=[pl, brb], writes=[lgA])

                    tile_A(0)
                    for i in range(NT):
                        if i + 1 < NT:
                            tile_A(i + 1)
                        tile_B(i)
                    NTl = NT
                    G3 = lgA[:, :, 0:4]
                    E4 = lgA[:, :, 4:36].rearrange("p t (g e) -> p t g e", g=4)
                    bc_t = lambda ap2, n: ap2.rearrange("p (t o) -> p t o", o=1).to_broadcast([128, NTl, n])
                    S.op("dve", lambda e: e.tensor_reduce(gmxA[:], G3, AX.X, ALU.max), reads=[lgA], writes=[gmxA])
                    S.op("dve", lambda e: e.tensor_tensor(gselA[:], G3, bc_t(gmxA[:], 4), ALU.is_ge), reads=[lgA, gmxA], writes=[gselA])
                    S.op("dve", lambda e: e.tensor_tensor(r4A[:], G3, bc_t(gmxA[:], 4), ALU.subtract), reads=[lgA, gmxA], writes=[r4A])
                    S.op("act", lambda e: e.activation(r4A[:], r4A[:], AF.Exp), reads=[r4A], writes=[r4A])
                    S.op("dve", lambda e: e.tensor_reduce(gwA[:], r4A[:], AX.X, ALU.add), reads=[r4A], writes=[gwA])
                    S.op("dve", lambda e: e.reciprocal(gwA[:], gwA[:]), reads=[gwA], writes=[gwA])
                    gsel4 = gselA[:].rearrange("p t (g o) -> p t g o", o=1).to_broadcast([128, NTl, 4, 8])
                    S.op("dve", lambda e: e.tensor_tensor(le4[:], E4, gsel4, ALU.mult), reads=[lgA, gselA], writes=[le4])
                    S.op("dve", lambda e: e.tensor_reduce(seA[:], le4[:].rearrange("p t g e -> p t e g"), AX.X, ALU.add), reads=[le4], writes=[seA])
                    S.op("dve", lambda e: e.tensor_reduce(mx1A[:], seA[:], AX.X, ALU.max), reads=[seA], writes=[mx1A])
                    S.op("dve", lambda e: e.tensor_tensor(oh1A[:], seA[:], bc_t(mx1A[:], 8), ALU.is_ge), reads=[seA, mx1A], writes=[oh1A])
                    S.op("dve", lambda e: e.scalar_tensor_tensor(se2A[:], oh1A[:], NEGBIG, seA[:], ALU.mult, ALU.add), reads=[oh1A, seA], writes=[se2A])
                    S.op("dve", lambda e: e.tensor_reduce(mx2A[:], se2A[:], AX.X, ALU.max), reads=[se2A], writes=[mx2A])
                    S.op("dve", lambda e: e.tensor_tensor(oh2A[:], se2A[:], bc_t(mx2A[:], 8), ALU.is_ge), reads=[se2A, mx2A], writes=[oh2A])
                    S.op("dve", lambda e: e.tensor_tensor(w2A[:], mx2A[:], mx1A[:], ALU.subtract), reads=[mx1A, mx2A], writes=[w2A])
                    S.op("act", lambda e: e.activation(w2A[:], w2A[:], AF.Exp), reads=[w2A], writes=[w2A])
                    S.op("dve", lambda e: e.tensor_scalar(w1A[:], w2A[:], 1.0, None, ALU.add), reads=[w2A], writes=[w1A])
                    S.op("dve", lambda e: e.reciprocal(w1A[:], w1A[:]), reads=[w1A], writes=[w1A])
                    S.op("dve", lambda e: e.tensor_tensor(w2A[:], w2A[:], w1A[:], ALU.mult), reads=[w1A, w2A], writes=[w2A])
                    S.op("dve", lambda e: e.tensor_tensor(ca[:], w1A[:], gwA[:], ALU.mult), reads=[w1A, gwA], writes=[ca])
                    S.op("dve", lambda e: e.tensor_tensor(cb[:], w2A[:], gwA[:], ALU.mult), reads=[w2A, gwA], writes=[cb])
                    for Mx, ohx in ((M1, oh1A), (M2, oh2A)):
                        S.op("dve", lambda e: e.tensor_tensor(Mx[:].rearrange("p t (g e) -> p t g e", g=4),
                                                              ohx[:].rearrange("p t (o e) -> p t o e", o=1).to_broadcast([128, NTl, 4, 8]),
                                                              gsel4, ALU.mult), reads=[ohx, gselA], writes=[Mx])
                    S.op("dve", lambda e: e.tensor_tensor(Mb[:], M1[:], M2[:], ALU.add), reads=[M1, M2], writes=[Mb])
                    S.barrier()

            conv_some(1000)
            with ExitStack() as st5:
                NSL = 64
                pr = [S.psum(st5, "pr%d" % i, [128, 512], F32) for i in range(2)]
                Rall = S.sbuf(st5, "Rall", [128, NT, 32], F32)
                cntf = S.sbuf(st5, "cntf", [128, 32], F32)
                cntI = S.sbuf(st5, "cntI", [128, 32], I32)
                pcf = S.sbuf(st5, "pcf", [128, 32], F32)
                scA = S.sbuf(st5, "scA", [128, 32], F32)
                scB = S.sbuf(st5, "scB", [128, 32], F32)
                off = S.sbuf(st5, "off", [128, 32], F32)
                Pm = S.sbuf(st5, "Pm", [128, NT, 32], F32)
                prod = S.sbuf(st5, "prod", [128, NT, 32], F32)
                posaF = S.sbuf(st5, "posaF", [128, NT], F32)
                posbF = S.sbuf(st5, "posbF", [128, NT], F32)
                posaI = S.sbuf(st5, "posaI", [128, NT], I32)
                posbI = S.sbuf(st5, "posbI", [128, NT], I32)
                cmpb = S.sbuf(st5, "cmpb", [128, NSL, 32], F32)
                eidf = S.sbuf(st5, "eidf", [128, NSL], F32)
                actf = S.sbuf(st5, "actf", [128, NSL], F32)
                widF = S.sbuf(st5, "widF", [128, NSL], F32)
                widI = S.sbuf(st5, "widI", [128, NSL], I32)
                for i in range(NT):
                    S.op("pe", lambda e: e.matmul(pr[0][:, 0:32], onesb[:], Mb[:, i, :], start=(i == 0), stop=(i == NT - 1)),
                         reads=[onesb, Mb], writes=[pr[0]], inc=(i == NT - 1))
                S.op("dve", lambda e: e.tensor_copy(cntf[:], pr[0][:, 0:32]), reads=[pr[0]], writes=[cntf])
                for i in range(NT):
                    pb = pr[1]
                    for i2 in range(i):
                        S.op("pe", lambda e: e.matmul(pb[:, 0:32], onesb[:], Mb[:, i2, :], start=(i2 == 0), stop=False),
                             reads=[onesb, Mb], writes=[pb], inc=False)
                    S.op("pe", lambda e: e.matmul(pb[:, 0:32], ustrb[:], Mb[:, i, :], start=(i == 0), stop=True),
                         reads=[ustrb, Mb], writes=[pb])
                    S.op("act", lambda e: e.activation(Rall[:, i, :], pb[:, 0:32], AF.Copy), reads=[pb], writes=[Rall])
                S.op("dve", lambda e: e.tensor_scalar(pcf[:], cntf[:], 127.0, None, ALU.add), reads=[cntf], writes=[pcf])
                S.op("dve", lambda e: e.tensor_copy(cntI[:], pcf[:]), reads=[pcf], writes=[cntI])
                S.op("dve", lambda e: e.tensor_scalar(cntI[:], cntI[:], 7, None, ALU.arith_shift_right), reads=[cntI], writes=[cntI])
                S.op("dve", lambda e: e.tensor_scalar(cntI[:], cntI[:], 7, None, ALU.logical_shift_left), reads=[cntI], writes=[cntI])
                S.op("dve", lambda e: e.tensor_copy(pcf[:], cntI[:]), reads=[cntI], writes=[pcf])
                S.op("dve", lambda e: e.tensor_copy(scA[:], pcf[:]), reads=[pcf], writes=[scA])
                cur, nxt = scA, scB
                for sh in (1, 2, 4, 8, 16):
                    S.op("dve", lambda e: e.tensor_copy(nxt[:, 0:sh], cur[:, 0:sh]), reads=[cur], writes=[nxt])
                    S.op("dve", lambda e: e.tensor_tensor(nxt[:, sh:32], cur[:, sh:32], cur[:, 0:32 - sh], ALU.add), reads=[cur], writes=[nxt])
                    cur, nxt = nxt, cur
                incl = cur
                S.op("dve", lambda e: e.tensor_tensor(off[:], incl[:], pcf[:], ALU.subtract), reads=[incl, pcf], writes=[off])
                S.op("dve", lambda e: e.tensor_tensor(Pm[:], Rall[:], off[:].rearrange("p (o e) -> p o e", o=1).to_broadcast([128, NT, 32]), ALU.add),
                     reads=[Rall, off], writes=[Pm])
                for Mx, pF, pI in ((M1, posaF, posaI), (M2, posbF, posbI)):
                    S.op("dve", lambda e: e.tensor_tensor(prod[:], Mx[:], Pm[:], ALU.mult), reads=[Mx, Pm], writes=[prod])
                    S.op("dve", lambda e: e.tensor_reduce(pF[:], prod[:], AX.X, ALU.add), reads=[prod], writes=[pF])
                    S.op("dve", lambda e: e.tensor_copy(pI[:], pF[:]), reads=[pF], writes=[pI])
                S.op("dve", lambda e: e.tensor_tensor(cmpb[:], off[:].rearrange("p (o e) -> p o e", o=1).to_broadcast([128, NSL, 32]),
                                                      jvS[:].rearrange("p (j o) -> p j o", o=1).to_broadcast([128, NSL, 32]), ALU.is_le),
                     reads=[off, jvS], writes=[cmpb])
                S.op("dve", lambda e: e.tensor_reduce(eidf[:], cmpb[:], AX.X, ALU.add), reads=[cmpb], writes=[eidf])
                S.op("dve", lambda e: e.tensor_scalar(actf[:], jvS[:], incl[:, 31:32], 1.0e6, ALU.is_ge, ALU.mult), reads=[jvS, incl], writes=[actf])
                S.op("dve", lambda e: e.tensor_scalar(widF[:], eidf[:], -1.0, 128.0, ALU.add, ALU.mult), reads=[eidf], writes=[widF])
                S.op("dve", lambda e: e.tensor_scalar(widF[:], widF[:], pcolS[:, 0:1], None, ALU.add), reads=[widF, pcolS], writes=[widF])
                S.op("dve", lambda e: e.tensor_tensor(widF[:], widF[:], actf[:], ALU.add), reads=[widF, actf], writes=[widF])
                S.op("dve", lambda e: e.tensor_copy(widI[:], widF[:]), reads=[widF], writes=[widI])

                hld = [S.sbuf(st5, "hld%d" % i, [128, DM], BF16) for i in range(2)]
                for i in range(NT):
                    hl = hld[i % 2]
                    S.dma("sp", hl[:], hbd[i * 128:(i + 1) * 128, :], reads=[hbdB], writes=[hl])
                    for pI in (posaI, posbI):
                        S.dmaf("pool", lambda e: e.indirect_dma_start(out=Hs, out_offset=bass.IndirectOffsetOnAxis(ap=pI[:, i:i + 1], axis=0),
                                                                      in_=hl[:, :], in_offset=None),
                               reads=[hl, pI], writes=[HsB], sembuf=hl)

                Wg_s = [S.sbuf(st5, "Wgs%d" % i, [128, 8, 256], BF16) for i in range(2)]
                Wu_s = [S.sbuf(st5, "Wus%d" % i, [128, 8, 256], BF16) for i in range(2)]
                Wd_s = [S.sbuf(st5, "Wds%d" % i, [128, 2, DM], BF16) for i in range(2)]
                hsl = [S.sbuf(st5, "hsl%d" % i, [128, DM], BF16) for i in range(2)]
                hslT = [S.sbuf(st5, "hslT%d" % i, [128, 8, 128], BF16) for i in range(2)]
                sa = [S.sbuf(st5, "sa%d" % i, [128, 256], F32) for i in range(2)]
                hid = [S.sbuf(st5, "hid%d" % i, [128, 256], BF16) for i in range(2)]
                hidT = [S.sbuf(st5, "hidT%d" % i, [128, 2, 128], BF16) for i in range(2)]
                ysb = [S.sbuf(st5, "ysb%d" % i, [128, DM], F32) for i in range(2)]
                pht = S.psum(st5, "pht", [128, 1024], BF16)
                ptx = S.psum(st5, "ptx", [128, 1024], BF16)
                pau = [S.psum(st5, "pau%d" % i, [128, 512], F32) for i in range(2)]
                py = [S.psum(st5, "py%d" % i, [128, 512], F32) for i in range(2)]

                def st_load_a(j):
                    k = j % 2
                    S.dma("sp", hsl[k][:], Hs[j * 128:(j + 1) * 128, :], reads=[HsB], writes=[hsl[k]])
                    for wt, src in ((Wg_s[k], weg_b), (Wu_s[k], weu_b)):
                        S.dmaf("pool", lambda e: e.indirect_dma_start(out=wt[:].rearrange("p a b -> p (a b)"), out_offset=None, in_=src,
                                                                      in_offset=bass.IndirectOffsetOnAxis(ap=widI[:, j:j + 1], axis=0),
                                                                      bounds_check=bcreg, oob_is_err=False),
                               reads=[widI, WcB], writes=[wt])

                def st_load_d(j):
                    k = j % 2
                    wt = Wd_s[k]
                    S.dmaf("pool", lambda e: e.indirect_dma_start(out=wt[:].rearrange("p a b -> p (a b)"), out_offset=None, in_=wed_b,
                                                                  in_offset=bass.IndirectOffsetOnAxis(ap=widI[:, j:j + 1], axis=0),
                                                                  bounds_check=bcreg, oob_is_err=False),
                           reads=[widI, WcB], writes=[wt])

                def st_au(j):
                    k = j % 2
                    for c in range(8):
                        S.op("pe", lambda e: e.transpose(ptx[:, c * 128:(c + 1) * 128], hsl[k][:, c * 128:(c + 1) * 128], identb[:]),
                             reads=[hsl[k], identb], writes=[ptx], inc=(c == 7))
                    S.op("act", lambda e: e.activation(hslT[k][:], ptx[:].rearrange("p (c t) -> p c t", c=8), AF.Copy),
                         reads=[ptx], writes=[hslT[k]])
                    pa = pau[k]
                    for c in range(8):
                        S.op("pe", lambda e: e.matmul(pa[:, 0:256], hslT[k][:, c, :], Wg_s[k][:, c, :], start=(c == 0), stop=(c == 7)),
                             reads=[hslT[k], Wg_s[k]], writes=[pa], inc=False)
                    for c in range(8):
                        S.op("pe", lambda e: e.matmul(pa[:, 256:512], hslT[k][:, c, :], Wu_s[k][:, c, :], start=(c == 0), stop=(c == 7)),
                             reads=[hslT[k], Wu_s[k]], writes=[pa], inc=(c == 7))
                    S.op("act", lambda e: e.activation(sa[k][:], pa[:, 0:256], AF.Silu), reads=[pa], writes=[sa[k]])
                    S.op("dve", lambda e: e.tensor_tensor(hid[k][:], pa[:, 256:512], sa[k][:], ALU.mult), reads=[pa, sa[k]], writes=[hid[k]])

                def st_tr(j):
                    k = j % 2
                    o = k * 512
                    for f in range(2):
                        S.op("pe", lambda e: e.transpose(pht[:, o + f * 128:o + (f + 1) * 128], hid[k][:, f * 128:(f + 1) * 128], identb[:]),
                             reads=[hid[k], identb], writes=[pht], inc=(f == 1))
                    S.op("act", lambda e: e.activation(hidT[k][:], pht[:, o:o + 256].rearrange("p (f t) -> p f t", f=2), AF.Copy),
                         reads=[pht], writes=[hidT[k]])

                def st_y(j):
                    k = j % 2
                    for hf in range(2):
                        for f in range(2):
                            S.op("pe", lambda e: e.matmul(py[hf][:], hidT[k][:, f, :], Wd_s[k][:, f, hf * 512:(hf + 1) * 512],
                                                          start=(f == 0), stop=(f == 1)),
                                 reads=[hidT[k], Wd_s[k]], writes=[py[hf]], inc=(f == 1))
                        if hf == 0:
                            S.op("act", lambda e: e.activation(ysb[k][:, 0:512], py[0][:], AF.Copy), reads=[py[0]], writes=[ysb[k]])
                        else:
                            S.op("dve", lambda e: e.tensor_copy(ysb[k][:, 512:1024], py[1][:]), reads=[py[1]], writes=[ysb[k]])
                    S.dma("sp", Ys[j * 128:(j + 1) * 128, :], ysb[k][:], reads=[ysb[k]], writes=[YsB], sembuf=ysb[k])

                st_load_a(0)
                st_load_d(0)
                for j in range(NSL + 2):
                    if j < NSL:
                        if j + 1 < NSL:
                            st_load_a(j + 1)
                        st_au(j)
                    if 1 <= j <= NSL:
                        st_tr(j - 1)
                    if j >= 2:
                        st_y(j - 2)
                    if 1 <= j < NSL:
                        st_load_d(j)

                lns5 = dict(stats=S.sbuf(st5, "stats5", [128, 2, 6], F32), mv=S.sbuf(st5, "mv5", [128, 2], F32),
                            rstd=S.sbuf(st5, "rstd5", [128, 1], F32), nmr=S.sbuf(st5, "nmr5", [128, 1], F32),
                            z=S.sbuf(st5, "z5", [128, DM], F32))
                accs = [S.sbuf(st5, "accs%d" % i, [128, DM], F32) for i in range(2)]
                yas = [S.sbuf(st5, "yas%d" % i, [128, DM], F32) for i in range(2)]
                ybs = [S.sbuf(st5, "ybs%d" % i, [128, DM], F32) for i in range(2)]
                ost = [S.sbuf(st5, "ost%d" % i, [128, DM], F32) for i in range(2)]
                for i in range(NT):
                    k = i % 2
                    S.dma("sp", accs[k][:], hs[i * 128:(i + 1) * 128, :], reads=[hsB], writes=[accs[k]])
                    for yt, pI in ((yas[k], posaI), (ybs[k], posbI)):
                        S.dmaf("pool", lambda e: e.indirect_dma_start(out=yt[:, :], out_offset=None, in_=Ys,
                                                                      in_offset=bass.IndirectOffsetOnAxis(ap=pI[:, i:i + 1], axis=0)),
                               reads=[YsB, pI], writes=[yt])
                    S.op("dve", lambda e: e.scalar_tensor_tensor(accs[k][:], yas[k][:], ca[:, i:i + 1], accs[k][:], ALU.mult, ALU.add),
                         reads=[yas[k], ca], writes=[accs[k]])
                    S.op("dve", lambda e: e.scalar_tensor_tensor(accs[k][:], ybs[k][:], cb[:, i:i + 1], accs[k][:], ALU.mult, ALU.add),
                         reads=[ybs[k], cb], writes=[accs[k]])
                    layer_norm_tile(lns5, accs[k], None, 4, None)
                    z = lns5["z"]
                    o = ost[k]
                    S.op("pool", lambda e: e.tensor_tensor(o[:], z[:], lnbc[:, 5, :], ALU.add), reads=[z, lnbc], writes=[o])
                    S.dma("sp", out[b, i * 128:(i + 1) * 128, :], o[:], reads=[o], writes=[outB], sembuf=o)
                S.barrier()
            stB.close()
        S.nobar.clear()
        S.barrier()
    return nc


_NC = None


def _bucket_tables():
    import jax
    import jax.numpy as jnp
    with jax.default_device(jax.devices("cpu")[0]):
        kk = jnp.arange(128, dtype=jnp.int32)[:, None]
        qq = jnp.arange(128, dtype=jnp.int32)[None, :]
        tabs = []
        for d in range(2):
            rel = kk - qq - 128 * d
            nb = 16
            max_exact = 8
            ret = jnp.where(rel > 0, nb, 0)
            n = jnp.abs(rel)
            large = max_exact + (jnp.log(jnp.maximum(n, 1).astype(jnp.float32) / max_exact)
                                 / math.log(128 / max_exact) * (nb - max_exact)).astype(jnp.int32)
            large = jnp.minimum(large, nb - 1)
            tabs.append(np.asarray(ret + jnp.where(n < max_exact, n, large)))
    return np.stack(tabs, 0)


def kernel(**inputs):
    global _NC
    f32 = np.float32
    g = lambda k: np.ascontiguousarray(np.asarray(inputs[k]))
    x = g("x").astype(f32, copy=False)
    pos = g("positions").astype(np.int32, copy=False)
    rel_bias = g("rel_bias")
    bk = _bucket_tables()
    tzr = rel_bias[bk]
    tzr = np.ascontiguousarray(np.transpose(tzr, (1, 3, 0, 2))).astype(f32)
    shared = {
        "w_in": g("w_in")[0], "w_uq": g("w_uq")[0], "w_uk": g("w_uk")[0], "w_uv": g("w_uv")[0],
        "w_up_a": g("w_up_a")[0], "w_up_b": g("w_up_b")[0], "w_gate": g("w_gate")[0], "w_o": g("w_o")[0],
        "w_r": np.ascontiguousarray(np.concatenate([g("w_grp")[0], g("w_rt")[0]], axis=1)),
        "b_r": np.ascontiguousarray(np.concatenate([g("b_grp")[0], g("b_rt")[0]], axis=0)),
        "w_eg": g("w_exp_gate")[0].reshape(32, 8, 128, 256).transpose(0, 2, 1, 3).reshape(32 * 128, 2048),
        "w_eu": g("w_exp_up")[0].reshape(32, 8, 128, 256).transpose(0, 2, 1, 3).reshape(32 * 128, 2048),
        "w_ed": g("w_exp_down")[0].reshape(32, 2, 128, 1024).transpose(0, 2, 1, 3).reshape(32 * 128, 2048),
        "ustr": np.triu(np.ones((128, 128), dtype=f32), 1),
        "jv": np.tile((np.arange(64, dtype=f32) * 128.0)[None, :], (128, 1)),
        "pcol": np.arange(128, dtype=f32).reshape(128, 1),
        "lnv": np.ascontiguousarray(np.stack([g("ln0_g"), g("ln0_b"), g("ln1_g")[0], g("ln1_b")[0], g("ln2_g")[0], g("ln2_b")[0]], 0)),
        "qng": np.ascontiguousarray(g("q_norm_g")[0].reshape(2, 128).T),
        "kvg": np.ascontiguousarray(g("kv_norm_g")[0].reshape(128, 1)),
        "bgate": np.ascontiguousarray(g("b_gate")[0].reshape(16, 128).T),
        "tzr": tzr,
        "cfar": np.ascontiguousarray(rel_bias[15, :]),
        "identf": np.eye(128, dtype=f32),
        "invf": np.tile((10000.0 ** (-np.arange(16, dtype=np.float64) / 16.0) / (2.0 * math.pi)).astype(f32), 2).reshape(32, 1),
    }
    shared = {k: np.ascontiguousarray(v.astype(f32, copy=False)) for k, v in shared.items()}
    if _NC is None:
        _NC = build()
    in_maps = []
    for c in range(8):
        m = dict(shared)
        m["x"] = np.ascontiguousarray(x[NB * c:NB * (c + 1)])
        m["pos"] = np.ascontiguousarray(pos[NB * c:NB * (c + 1)])
        in_maps.append(m)
    res = run_bass_kernel_spmd(_NC, in_maps, core_ids=list(range(8)))
    return np.concatenate([np.asarray(r["out"]) for r in res.results], axis=0).astype(f32, copy=False)
```

```python
import math
from contextlib import ExitStack
import numpy as np
import concourse.bass as bass
import concourse.mybir as mybir
from concourse.bass_utils import run_bass_kernel_spmd

F32 = mybir.dt.float32
BF16 = mybir.dt.bfloat16
I32 = mybir.dt.int32
ALU = mybir.AluOpType
AF = mybir.ActivationFunctionType
AX = mybir.AxisListType

T = 2048
DM = 1024
NT = 16
NB = 2
ALPHA = 2.0 ** 0.25
LN_EPS = 1e-5
RMS_EPS = 1e-6
NEGBIG = -1.0e30
BIS_ITERS = 14
TOPK = 256


class Buf:
    __slots__ = ("ap", "name", "ws", "reads", "dsem", "dcount")

    def __init__(self, ap, name=""):
        self.ap = ap
        self.name = name
        self.ws = {}
        self.reads = {}
        self.dsem = None
        self.dcount = 0

    def __getitem__(self, idx):
        return self.ap[idx]


class Sched:
    def __init__(self, nc, stack):
        self.nc = nc
        self.stack = stack
        self.engs = {}
        for name, eng in (("pe", nc.tensor), ("act", nc.scalar), ("dve", nc.vector),
                          ("pool", nc.gpsimd), ("sp", nc.sync)):
            sem = stack.enter_context(nc.semaphore("s_" + name))
            self.engs[name] = dict(eng=eng, sem=sem, count=0, waited={})
        self.ndsem = 0
        self.dpool = {}
        self.nobar = set()
        self.n_ins = 0
        self.uid = 0

    def sbuf(self, st, name, shape, dtype):
        self.uid += 1
        name = "%s_%d" % (name, self.uid)
        return Buf(st.enter_context(self.nc.sbuf_tensor(name, shape, dtype)), name)

    def psum(self, st, name, shape, dtype):
        self.uid += 1
        name = "%s_%d" % (name, self.uid)
        return Buf(st.enter_context(self.nc.psum_tensor(name, shape, dtype)), name)

    def _wait(self, engname, ev):
        sem, val, src = ev
        if src == "pe" and engname == "pe":
            return
        E = self.engs[engname]
        key = id(sem)
        if E["waited"].get(key, 0) < val:
            E["eng"].wait_ge(sem, val)
            E["waited"][key] = val
            self.n_ins += 1

    def _deps(self, engname, reads, writes):
        for b in reads:
            for ev in b.ws.values():
                self._wait(engname, ev)
        for b in writes:
            for ev in b.ws.values():
                self._wait(engname, ev)
            for ev in b.reads.values():
                self._wait(engname, ev)

    def _commit(self, ev, reads, writes):
        k = id(ev[0])
        for b in writes:
            b.ws[k] = ev
            b.reads = {}
        for b in reads:
            if b in writes:
                continue
            b.reads[k] = ev

    def op(self, engname, fn, reads=(), writes=(), inc=True):
        E = self.engs[engname]
        self._deps(engname, reads, writes)
        ins = fn(E["eng"])
        self.n_ins += 1
        if inc:
            E["count"] += 1
            ins.then_inc(E["sem"], 1)
            self._commit((E["sem"], E["count"], engname), reads, writes)
        else:
            self._commit((E["sem"], E["count"] + 1, engname), reads, writes)

    def dma(self, qname, out_ap, in_ap, reads=(), writes=(), sembuf=None):
        E = self.engs[qname]
        self._deps(qname, reads, writes)
        sb = sembuf or (writes[0] if writes else reads[0])
        key = sb.name.rsplit("_", 1)[0] if "_" in sb.name else sb.name
        ent = self.dpool.get(key)
        if ent is None:
            ent = [self.stack.enter_context(self.nc.semaphore("d%d" % self.ndsem)), 0]
            self.ndsem += 1
            self.dpool[key] = ent
        ins = E["eng"].dma_start(out=out_ap, in_=in_ap)
        ent[1] += 16
        ins.then_inc(ent[0], 16)
        self.n_ins += 1
        ev = (ent[0], ent[1], "dma")
        self._commit(ev, reads, writes)
        return ev

    def dmaf(self, qname, fn, reads=(), writes=(), sembuf=None):
        E = self.engs[qname]
        self._deps(qname, reads, writes)
        sb = sembuf or (writes[0] if writes else reads[0])
        key = sb.name.rsplit("_", 1)[0] if "_" in sb.name else sb.name
        ent = self.dpool.get(key)
        if ent is None:
            ent = [self.stack.enter_context(self.nc.semaphore("d%d" % self.ndsem)), 0]
            self.ndsem += 1
            self.dpool[key] = ent
        ins = fn(E["eng"])
        ent[1] += 16
        ins.then_inc(ent[0], 16)
        self.n_ins += 1
        ev = (ent[0], ent[1], "dma")
        self._commit(ev, reads, writes)
        return ev

    def barrier(self):
        evs = []
        for n, E in self.engs.items():
            if E["count"] > 0:
                evs.append((E["sem"], E["count"], "bar_" + n))
        for key, ent in self.dpool.items():
            if ent[1] > 0 and key not in self.nobar:
                evs.append((ent[0], ent[1], "dma"))
        for n in self.engs:
            for ev in evs:
                if ev[2] == "bar_" + n:
                    continue
                self._wait(n, ev)


def build():
    nc = bass.Bass("TRN2", target_bir_lowering=False)

    def din(name, shape, dt=F32):
        return nc.dram_tensor(name, shape, dt, kind="ExternalInput").ap()

    x = din("x", [NB, T, DM])
    pos = din("pos", [NB, T], I32)
    w_in = din("w_in", [DM, 1352])
    w_uq = din("w_uq", [256, 768])
    w_uk = din("w_uk", [128, 512])
    w_uv = din("w_uv", [128, 512])
    w_up_a = din("w_up_a", [512, DM])
    w_up_b = din("w_up_b", [512, DM])
    w_gate = din("w_gate", [DM, 2048])
    w_o = din("w_o", [DM, DM])
    w_r = din("w_r", [DM, 36])
    b_r = din("b_r", [36])
    w_eg = din("w_eg", [32 * 128, 2048])
    w_eu = din("w_eu", [32 * 128, 2048])
    w_ed = din("w_ed", [32 * 128, 2048])
    ustr = din("ustr", [128, 128])
    jv = din("jv", [128, 64])
    pcol = din("pcol", [128, 1])
    lnv = din("lnv", [6, DM])
    qng = din("qng", [128, 2])
    kvg = din("kvg", [128, 1])
    bgate = din("bgate", [128, 16])
    tzr = din("tzr", [128, 8, 2, 128])
    cfar = din("cfar", [8])
    identf = din("identf", [128, 128])
    invf = din("invf", [32, 1])
    out = nc.dram_tensor("out", [NB, T, DM], F32, kind="ExternalOutput").ap()
    hs = nc.dram_tensor("hs", [T, DM], F32, kind="Internal").ap()
    hbd = nc.dram_tensor("hbd", [T, DM], BF16, kind="Internal").ap()
    Hs = nc.dram_tensor("Hs", [64 * 128, DM], BF16, kind="Internal").ap()
    Ys = nc.dram_tensor("Ys", [64 * 128, DM], F32, kind="Internal").ap()
    weg_b = nc.dram_tensor("weg_b", [32 * 128, 2048], BF16, kind="Internal").ap()
    weu_b = nc.dram_tensor("weu_b", [32 * 128, 2048], BF16, kind="Internal").ap()
    wed_b = nc.dram_tensor("wed_b", [32 * 128, 2048], BF16, kind="Internal").ap()

    with ExitStack() as st0:
        S = Sched(nc, st0)
        hsB = Buf(hs, "hs")
        outB = Buf(out, "out")
        bcreg = st0.enter_context(nc.gpsimd.register("bcreg"))
        nc.gpsimd.reg_mov(bcreg, 32 * 128 - 1)
        hbdB = Buf(hbd, "hbd")
        HsB = Buf(Hs, "Hs")
        YsB = Buf(Ys, "Ys")

        identb = S.sbuf(st0, "identb", [128, 128], BF16)
        identF = S.sbuf(st0, "identF", [128, 128], F32)
        onesb = S.sbuf(st0, "onesb", [128, 128], BF16)
        lnbc = S.sbuf(st0, "lnbc", [128, 6, DM], F32)
        S.dma("pool", identb[:], identf, writes=[identb])
        ustrb = S.sbuf(st0, "ustrb", [128, 128], BF16)
        jvS = S.sbuf(st0, "jvS", [128, 64], F32)
        pcolS = S.sbuf(st0, "pcolS", [128, 1], F32)
        S.dma("pool", ustrb[:], ustr, writes=[ustrb])
        S.dma("sp", jvS[:], jv, writes=[jvS])
        S.dma("sp", pcolS[:], pcol, writes=[pcolS])
        WcB = Buf(None, "wcast")
        S.nobar.add("wcast")
        conv_list = [(srcw, dstw, e_) for e_ in range(32) for srcw, dstw in ((w_eg, weg_b), (w_eu, weu_b), (w_ed, wed_b))]

        def conv_some(n):
            for _ in range(n):
                if not conv_list:
                    return
                srcw, dstw, e_ = conv_list.pop(0)
                S.dma("pool", dstw[e_ * 128:(e_ + 1) * 128, :], srcw[e_ * 128:(e_ + 1) * 128, :], writes=[WcB])
        with ExitStack() as stz:
            zt = S.sbuf(stz, "zt", [128, 8, DM], BF16)
            S.op("pool", lambda e: e.memset(zt[:], 0.0), writes=[zt])
            for q in range(8):
                S.dma("sp", Hs[q * 1024:(q + 1) * 1024, :].rearrange("(p r) n -> p r n", p=128), zt[:], reads=[zt], writes=[HsB], sembuf=zt)
            S.barrier()
        S.dma("sp", identF[:], identf, writes=[identF])
        S.op("dve", lambda e: e.memset(onesb[:], 1.0), writes=[onesb])
        for k in range(6):
            S.dma("sp", lnbc[:, k, :], lnv[k].partition_broadcast(128), writes=[lnbc])

        def layer_norm_tile(st_bufs, src, dst_ap_fn, gi, out_bufs, scale_after=None):
            stats, mv, rstd, nmr, z = (st_bufs[k] for k in ("stats", "mv", "rstd", "nmr", "z"))
            S.op("dve", lambda e: e.bn_stats(stats[:, 0, :], src[:, 0:512]), reads=[src], writes=[stats])
            S.op("dve", lambda e: e.bn_stats(stats[:, 1, :], src[:, 512:1024]), reads=[src], writes=[stats])
            S.op("dve", lambda e: e.bn_aggr(mv[:], stats[:].rearrange("p a b -> p (a b)")), reads=[stats], writes=[mv])
            S.op("dve", lambda e: e.tensor_scalar(rstd[:], mv[:, 1:2], LN_EPS, None, ALU.add), reads=[mv], writes=[rstd])
            S.op("act", lambda e: e.activation(rstd[:], rstd[:], AF.Sqrt), reads=[rstd], writes=[rstd])
            S.op("dve", lambda e: e.reciprocal(rstd[:], rstd[:]), reads=[rstd], writes=[rstd])
            S.op("dve", lambda e: e.tensor_scalar(nmr[:], mv[:, 0:1], rstd[:, 0:1], -1.0, ALU.mult, ALU.mult),
                 reads=[mv, rstd], writes=[nmr])
            S.op("act", lambda e: e.activation(z[:], src[:], AF.Identity, bias=nmr[:, 0:1], scale=rstd[:, 0:1]),
                 reads=[src, nmr, rstd], writes=[z])
            S.op("dve", lambda e: e.tensor_tensor(z[:], z[:], lnbc[:, gi, :], ALU.mult), reads=[z, lnbc], writes=[z])

        for b in range(NB):
            stB = ExitStack()
            stB.__enter__()
            xnT = S.sbuf(stB, "xnT", [128, 8, T], BF16)
            ca = S.sbuf(stB, "ca", [128, NT], F32)
            cb = S.sbuf(stB, "cb", [128, NT], F32)
            M1 = S.sbuf(stB, "M1", [128, NT, 32], F32)
            M2 = S.sbuf(stB, "M2", [128, NT, 32], F32)
            Mb = S.sbuf(stB, "Mb", [128, NT, 32], BF16)
            with ExitStack() as stA:
                oaT = S.sbuf(stA, "oaT", [128, 4, T], BF16)
                obT = S.sbuf(stA, "obT", [128, 4, T], BF16)
                lns = dict(stats=S.sbuf(stA, "stats", [128, 2, 6], F32), mv=S.sbuf(stA, "mv", [128, 2], F32),
                           rstd=S.sbuf(stA, "rstd", [128, 1], F32), nmr=S.sbuf(stA, "nmr", [128, 1], F32),
                           z=S.sbuf(stA, "z", [128, DM], F32))
                xt = [S.sbuf(stA, "xt%d" % i, [128, DM], F32) for i in range(2)]

                with ExitStack() as st1:
                    xnb = [S.sbuf(st1, "xnb%d" % i, [128, DM], BF16) for i in range(2)]
                    ptr = [S.psum(st1, "ptr%d" % i, [128, 1024], BF16) for i in range(2)]
                    for i in range(NT):
                        xs = xt[i % 2]
                        S.dma("sp", xs[:], x[b, i * 128:(i + 1) * 128, :], writes=[xs])
                        layer_norm_tile(lns, xs, None, 0, None)
                        z = lns["z"]
                        xb = xnb[i % 2]
                        S.op("dve", lambda e: e.tensor_tensor(xb[:], z[:], lnbc[:, 1, :], ALU.add),
                             reads=[z, lnbc], writes=[xb])
                        pt = ptr[i % 2]
                        for c in range(8):
                            S.op("pe", lambda e: e.transpose(pt[:, c * 128:(c + 1) * 128], xb[:, c * 128:(c + 1) * 128], identb[:]),
                                 reads=[xb, identb], writes=[pt], inc=(c == 7))
                        S.op("act", lambda e: e.activation(xnT[:, :, i * 128:(i + 1) * 128],
                                                           pt[:].rearrange("p (c t) -> p c t", c=8), AF.Copy),
                             reads=[pt], writes=[xnT])
                    S.barrier()

                with ExitStack() as st2:
                    pp = [S.psum(st2, "pp%d" % i, [128, 512], F32) for i in range(3)]
                    ps = [S.psum(st2, "ps%d" % i, [128, 512], F32) for i in range(3)]
                    po = [S.psum(st2, "po%d" % i, [128, 512], F32) for i in range(2)]
                    Wa = S.sbuf(st2, "Wa", [128, 8, 416], BF16)
                    Wkrr = S.sbuf(st2, "Wkrr", [128, 8, 32], BF16)
                    Wq = S.sbuf(st2, "Wq", [128, 2, 8, 96], BF16)
                    Wqr = S.sbuf(st2, "Wqr", [128, 2, 8, 32], BF16)
                    Wk = S.sbuf(st2, "Wk", [128, 8, 64], BF16)
                    Wv = S.sbuf(st2, "Wv", [128, 512], BF16)
                    gq = S.sbuf(st2, "gq", [128, 2], F32)
                    gkv = S.sbuf(st2, "gkv", [128, 1], F32)
                    invfS = S.sbuf(st2, "invfS", [96, 1], F32)
                    cos32 = S.sbuf(st2, "cos32", [96, T], F32)
                    sin32 = S.sbuf(st2, "sin32", [96, T], F32)
                    cqn = S.sbuf(st2, "cqn", [128, 2, T], BF16)
                    ckvn = S.sbuf(st2, "ckvn", [128, T], BF16)
                    krT = S.sbuf(st2, "krT", [96, T], BF16)
                    st2a = ExitStack()
                    st2a.__enter__()
                    wq32 = S.sbuf(st2a, "wq32", [128, 2, 768], F32)
                    wk32 = S.sbuf(st2a, "wk32", [128, 512], F32)
                    wv32 = S.sbuf(st2a, "wv32", [128, 512], F32)
                    posi = S.sbuf(st2a, "posi", [96, T], I32)
                    rr = S.sbuf(st2a, "rr", [96, T], F32)
                    rf = S.sbuf(st2a, "rf", [96, T], F32)
                    tq = S.sbuf(st2a, "tq", [96, T], F32)

                    wsrc = w_in.rearrange("(c p) n -> p c n", p=128)
                    S.dma("pool", Wa[:], wsrc[:, :, 0:416], writes=[Wa])
                    S.dma("sp", wq32[:], w_uq.rearrange("(c p) n -> p c n", p=128), writes=[wq32])
                    S.dma("sp", wk32[:], w_uk, writes=[wk32])
                    S.dma("sp", wv32[:], w_uv, writes=[wv32])
                    S.dma("sp", gq[:], qng, writes=[gq])
                    S.dma("sp", gkv[:], kvg, writes=[gkv])
                    S.dma("sp", invfS[64:96, :], invf, writes=[invfS])
                    S.dma("sp", posi[64:96, :], pos[b].partition_broadcast(32), writes=[posi])
                    S.op("dve", lambda e: e.tensor_scalar(Wkrr[:, :, 0:16], Wa[:, :, 400:416], -1.0, None, ALU.mult),
                         reads=[Wa], writes=[Wkrr])
                    S.op("dve", lambda e: e.tensor_copy(Wkrr[:, :, 16:32], Wa[:, :, 384:400]), reads=[Wa], writes=[Wkrr])
                    for c in range(2):
                        src = wq32[:, c, :].rearrange("p (h d) -> p h d", h=8)
                        g = gq[:, c:c + 1]
                        S.op("dve", lambda e: e.tensor_scalar(Wq[:, c, :, :], src[:, :, :], g, None, ALU.mult),
                             reads=[wq32, gq], writes=[Wq])
                        S.op("dve", lambda e: e.tensor_scalar(Wqr[:, c, :, 0:16], src[:, :, 80:96], g, -1.0, ALU.mult, ALU.mult),
                             reads=[wq32, gq], writes=[Wqr])
                        S.op("dve", lambda e: e.tensor_scalar(Wqr[:, c, :, 16:32], src[:, :, 64:80], g, None, ALU.mult),
                             reads=[wq32, gq], writes=[Wqr])
                    S.op("dve", lambda e: e.tensor_scalar(Wk[:, :, :], wk32[:].rearrange("p (h d) -> p h d", h=8),
                                                          gkv[:, 0:1], None, ALU.mult), reads=[wk32, gkv], writes=[Wk])
                    S.op("dve", lambda e: e.tensor_scalar(Wv[:], wv32[:], gkv[:, 0:1], None, ALU.mult),
                         reads=[wv32, gkv], writes=[Wv])

                    S.op("dve", lambda e: e.tensor_copy(rr[64:96, :], posi[64:96, :]), reads=[posi], writes=[rr])
                    S.op("dve", lambda e: e.tensor_scalar(rr[64:96, :], rr[64:96, :], invfS[64:96, 0:1], None, ALU.mult), reads=[rr, invfS], writes=[rr])
                    S.op("dve", lambda e: e.tensor_copy(posi[64:96, :], rr[64:96, :]), reads=[rr], writes=[posi])
                    S.op("dve", lambda e: e.tensor_copy(rf[64:96, :], posi[64:96, :]), reads=[posi], writes=[rf])
                    S.op("dve", lambda e: e.tensor_tensor(rr[64:96, :], rr[64:96, :], rf[64:96, :], ALU.subtract), reads=[rr, rf], writes=[rr])

                    def wrap_sin(dst, shift):
                        S.op("dve", lambda e: e.tensor_scalar(rf[64:96, :], rr[64:96, :], shift, None, ALU.add), reads=[rr], writes=[rf])
                        for _ in range(2):
                            S.op("dve", lambda e: e.tensor_scalar(tq[64:96, :], rf[64:96, :], 0.5, None, ALU.is_gt), reads=[rf], writes=[tq])
                            S.op("dve", lambda e: e.tensor_tensor(rf[64:96, :], rf[64:96, :], tq[64:96, :], ALU.subtract), reads=[rf, tq], writes=[rf])
                        S.op("dve", lambda e: e.tensor_scalar(tq[64:96, :], rf[64:96, :], -0.5, None, ALU.is_lt), reads=[rf], writes=[tq])
                        S.op("dve", lambda e: e.tensor_tensor(rf[64:96, :], rf[64:96, :], tq[64:96, :], ALU.add), reads=[rf, tq], writes=[rf])
                        S.op("act", lambda e: e.activation(dst[64:96, :], rf[64:96, :], AF.Sin, scale=2.0 * math.pi * (1.0 - 2e-6)),
                             reads=[rf], writes=[dst])

                    wrap_sin(sin32, 0.0)
                    wrap_sin(cos32, 0.25)
                    S.barrier()
                    st2a.close()
                    Vh = [S.sbuf(st2, "Vh%d" % i, [128, NT, 128], BF16) for i in range(2)]
                    qTs = [S.sbuf(st2, "qT%d" % i, [96, T], BF16) for i in range(2)]
                    kTs = [S.sbuf(st2, "kT%d" % i, [96, T], BF16) for i in range(2)]
                    c32 = S.sbuf(st2, "c32", [128, 2, 512], F32)
                    sqb = S.sbuf(st2, "sqb", [128, 2, 512], BF16)
                    rq = S.sbuf(st2, "rq", [128, 512], F32)
                    t1 = S.sbuf(st2, "t1", [96, 512], F32)
                    t2 = S.sbuf(st2, "t2", [96, 512], F32)
                    PTs = [S.sbuf(st2, "PT%d" % i, [128, 512], BF16) for i in range(3)]
                    rd = S.sbuf(st2, "rd", [128, 512], F32)
                    for i in range(2):
                        S.op("pool", lambda e: e.memset(Vh[i][:], 1.0), writes=[Vh[i]])

                    def rms_block(psrc_list, dstT_fn, nfeat, tb):
                        n = len(psrc_list)
                        for m, pb in enumerate(psrc_list):
                            S.op("act", lambda e: e.activation(c32[:, m, :], pb[:], AF.Copy), reads=[pb], writes=[c32])
                            S.op("act", lambda e: e.activation(sqb[:, m, :], pb[:], AF.Square), reads=[pb], writes=[sqb])
                        pss = pp[2]
                        for m in range(n):
                            S.op("pe", lambda e: e.matmul(pss[:], onesb[:], sqb[:, m, :], start=(m == 0), stop=(m == n - 1)),
                                 reads=[onesb, sqb], writes=[pss], inc=(m == n - 1))
                        S.op("dve", lambda e: e.tensor_scalar(rq[:], pss[:], 1.0 / nfeat, RMS_EPS, ALU.mult, ALU.add),
                             reads=[pss], writes=[rq])
                        S.op("act", lambda e: e.activation(rq[:], rq[:], AF.Sqrt), reads=[rq], writes=[rq])
                        S.op("dve", lambda e: e.reciprocal(rq[:], rq[:]), reads=[rq], writes=[rq])
                        for m in range(n):
                            S.op("dve", lambda e: e.tensor_tensor(dstT_fn(m), c32[:, m, :], rq[:], ALU.mult),
                                 reads=[c32, rq], writes=[dstT_fn.buf])

                    def proj_fm(pb, lhs_fn, tb, M=128, p0=0):
                        for c in range(8):
                            S.op("pe", lambda e: e.matmul(pb[p0:p0 + M, :], lhs_fn(c), xnT[:, c, tb * 512:(tb + 1) * 512],
                                                          start=(c == 0), stop=(c == 7)),
                                 reads=[xnT, Wa, Wkrr], writes=[pb], inc=(c == 7))

                    for tb in range(4):
                        cols = slice(tb * 512, (tb + 1) * 512)
                        proj_fm(pp[0], lambda c: Wa[:, c, 0:128], tb)
                        proj_fm(pp[1], lambda c: Wa[:, c, 128:256], tb)
                        f = lambda m: cqn[:, m, cols]
                        f.buf = cqn
                        rms_block([pp[0], pp[1]], f, 256.0, tb)
                        proj_fm(pp[0], lambda c: Wa[:, c, 256:384], tb)
                        f2 = lambda m: ckvn[:, cols]
                        f2.buf = ckvn
                        rms_block([pp[0]], f2, 128.0, tb)
                        proj_fm(pp[0], lambda c: Wa[:, c, 384:416], tb, M=32, p0=64)
                        proj_fm(pp[1], lambda c: Wkrr[:, c, :], tb, M=32, p0=64)
                        S.op("dve", lambda e: e.tensor_tensor(t1[64:96, :], pp[0][64:96, :], cos32[64:96, cols], ALU.mult),
                             reads=[pp[0], cos32], writes=[t1])
                        S.op("dve", lambda e: e.tensor_tensor(t2[64:96, :], pp[1][64:96, :], sin32[64:96, cols], ALU.mult),
                             reads=[pp[1], sin32], writes=[t2])
                        S.op("dve", lambda e: e.tensor_tensor(krT[64:96, cols], t1[64:96, :], t2[64:96, :], ALU.add), reads=[t1, t2], writes=[krT])
                    sc_mla = 96.0 ** -0.5
                    LA = 2

                    def mla_proj(h):
                        qT = qTs[h % 2]
                        kT = kTs[h % 2]
                        for tb in range(4):
                            cols = slice(tb * 512, (tb + 1) * 512)
                            pa, pbb = pp[0], pp[1]
                            for m in range(2):
                                S.op("pe", lambda e: e.matmul(pa[0:96, :], Wq[:, m, h, :], cqn[:, m, cols], start=(m == 0), stop=(m == 1)),
                                     reads=[Wq, cqn], writes=[pa], inc=(m == 1))
                            for m in range(2):
                                S.op("pe", lambda e: e.matmul(pbb[64:96, :], Wqr[:, m, h, :], cqn[:, m, cols], start=(m == 0), stop=(m == 1)),
                                     reads=[Wqr, cqn], writes=[pbb], inc=(m == 1))
                            pk = pp[2]
                            S.op("pe", lambda e: e.matmul(pk[0:64, :], Wk[:, h, :], ckvn[:, cols], start=True, stop=True),
                                 reads=[Wk, ckvn], writes=[pk])
                            S.op("dve", lambda e: e.tensor_tensor(t1[64:96, :], pa[64:96, :], cos32[64:96, cols], ALU.mult), reads=[pa, cos32], writes=[t1])
                            S.op("dve", lambda e: e.tensor_tensor(t2[64:96, :], pbb[64:96, :], sin32[64:96, cols], ALU.mult), reads=[pbb, sin32], writes=[t2])
                            S.op("dve", lambda e: e.tensor_tensor(qT[64:96, cols], t1[64:96, :], t2[64:96, :], ALU.add), reads=[t1, t2], writes=[qT])
                            S.op("act", lambda e: e.activation(qT[0:64, cols], pa[0:64, :], AF.Copy), reads=[pa], writes=[qT])
                            S.op("act", lambda e: e.activation(kT[0:64, cols], pk[0:64, :], AF.Copy), reads=[pk], writes=[kT])
                        S.op("pool", lambda e: e.tensor_copy(kT[64:96, :], krT[64:96, :]), reads=[krT], writes=[kT])
                        Vc = Vh[h % 2]
                        voff = 64 * (h % 2)
                        for k4 in range(4):
                            pv = pp[k4 % 2]
                            for j in range(4):
                                kt = k4 * 4 + j
                                S.op("pe", lambda e: e.matmul(pv[:, j * 64:(j + 1) * 64], ckvn[:, kt * 128:(kt + 1) * 128],
                                                              Wv[:, h * 64:(h + 1) * 64], start=True, stop=True),
                                     reads=[ckvn, Wv], writes=[pv], inc=(j == 3))
                            S.op("act", lambda e: e.activation(Vc[:, k4 * 4:(k4 + 1) * 4, voff:voff + 64],
                                                               pv[:, 0:256].rearrange("p (j d) -> p j d", j=4), AF.Copy),
                                 reads=[pv], writes=[Vc])

                    st_ = dict(si=0, oi=0)

                    def mla_attn(h):
                        conv_some(6)
                        qT = qTs[h % 2]
                        kT = kTs[h % 2]
                        Vc = Vh[h % 2]
                        for qb in range(4):
                            pO = po[st_["oi"] % 2]
                            st_["oi"] += 1
                            nk = 4 * qb + 4
                            slots = {}

                            def emit_S(kt):
                                c0 = max(0, kt - 4 * qb) * 128
                                sl = st_["si"] % 3
                                st_["si"] += 1
                                slots[kt] = sl
                                pS, PT = ps[sl], PTs[sl]
                                S.op("pe", lambda e: e.matmul(pS[:, c0:512], kT[:, kt * 128:(kt + 1) * 128],
                                                              qT[:, qb * 512 + c0:(qb + 1) * 512], start=True, stop=True),
                                     reads=[kT, qT], writes=[pS])
                                S.op("act", lambda e: e.activation(PT[:, c0:512], pS[:, c0:512], AF.Exp, scale=sc_mla),
                                     reads=[pS], writes=[PT])
                                if kt >= 4 * qb:
                                    S.op("pool", lambda e: e.memset(PT[64:128, c0:c0 + 64], 0.0), writes=[PT])

                            def emit_PV(kt):
                                c0 = max(0, kt - 4 * qb) * 128
                                PT = PTs[slots[kt]]
                                S.op("pe", lambda e: e.matmul(pO[:, c0:512], Vc[:, kt, :], PT[:, c0:512],
                                                              start=(kt == 0), stop=(kt == nk - 1), skip_group_check=True),
                                     reads=[Vc, PT], writes=[pO], inc=(kt == nk - 1))

                            for s_ in range(nk + LA):
                                if s_ < nk:
                                    emit_S(s_)
                                if s_ >= LA:
                                    emit_PV(s_ - LA)
                            ocols = slice(qb * 512, (qb + 1) * 512)
                            if h % 2 == 0:
                                S.op("dve", lambda e: e.reciprocal(rd[0:64, :], pO[64:128, :]), reads=[pO], writes=[rd])
                                S.op("dve", lambda e: e.tensor_tensor(oaT[0:64, h // 2, ocols], pO[0:64, :], rd[0:64, :], ALU.mult),
                                     reads=[pO, rd], writes=[oaT])
                            else:
                                S.op("dve", lambda e: e.reciprocal(rd[64:128, :], pO[0:64, :]), reads=[pO], writes=[rd])
                                S.op("dve", lambda e: e.tensor_tensor(oaT[64:128, h // 2, ocols], pO[64:128, :], rd[64:128, :], ALU.mult),
                                     reads=[pO, rd], writes=[oaT])

                    mla_proj(0)
                    for h in range(8):
                        if h + 1 < 8:
                            mla_proj(h + 1)
                        mla_attn(h)
                    S.barrier()

                with ExitStack() as st3:
                    pp = [S.psum(st3, "qp%d" % i, [128, 512], F32) for i in range(2)]
                    ps = [S.psum(st3, "qs%d" % i, [128, 512], F32) for i in range(3)]
                    po = [S.psum(st3, "qo%d" % i, [128, 512], F32) for i in range(2)]
                    pmt = S.psum(st3, "pmt", [128, 1024], BF16)
                    qbT = S.sbuf(st3, "qbT", [128, 4, T], BF16)
                    kbT2 = S.sbuf(st3, "kbT2", [128, T], BF16)
                    qiT = S.sbuf(st3, "qiT", [128, 2, T], BF16)
                    kiT4 = S.sbuf(st3, "kiT4", [128, T], BF16)
                    Vb = S.sbuf(st3, "Vb", [128, NT, 2, 128], BF16)
                    widx = S.sbuf(st3, "widx", [128, NT, 8], F32)
                    tz = S.sbuf(st3, "tz", [128, 8, 2, 128], F32)
                    cfb = S.sbuf(st3, "cfb", [128, 8], F32)
                    st3a = ExitStack()
                    st3a.__enter__()
                    Wb = S.sbuf(st3a, "Wb", [128, 8, 936], BF16)
                    Wkb2 = S.sbuf(st3a, "Wkb2", [128, 8, 128], BF16)
                    Wki4 = S.sbuf(st3a, "Wki4", [128, 8, 128], BF16)

                    wsrc = w_in.rearrange("(c p) n -> p c n", p=128)
                    S.dma("pool", Wb[:], wsrc[:, :, 416:1352], writes=[Wb])
                    for r in range(2):
                        S.dma("pool", Wkb2[:, :, r * 64:(r + 1) * 64], wsrc[:, :, 928:992], writes=[Wkb2])
                    for r in range(4):
                        S.dma("pool", Wki4[:, :, r * 32:(r + 1) * 32], wsrc[:, :, 1312:1344], writes=[Wki4])
                    S.dma("sp", tz[:], tzr, writes=[tz])
                    S.dma("sp", cfb[:], cfar.partition_broadcast(128), writes=[cfb])
                    for h in range(8):
                        S.op("dve", lambda e: e.tensor_scalar(tz[:, h], tz[:, h], cfb[:, h:h + 1], 8.0, ALU.subtract, ALU.mult),
                             reads=[tz, cfb], writes=[tz])
                    S.op("pool", lambda e: e.memset(Vb[:], 1.0), writes=[Vb])

                    def proj3(dst_ap, dstbuf, lhs_fn, wbuf, tb, k):
                        pb = pp[k % 2]
                        for c in range(8):
                            S.op("pe", lambda e: e.matmul(pb[:], lhs_fn(c), xnT[:, c, tb * 512:(tb + 1) * 512],
                                                          start=(c == 0), stop=(c == 7)), reads=[xnT, wbuf], writes=[pb], inc=(c == 7))
                        S.op("act", lambda e: e.activation(dst_ap, pb[:], AF.Copy), reads=[pb], writes=[dstbuf])

                    k = 0
                    for tb in range(4):
                        cols = slice(tb * 512, (tb + 1) * 512)
                        for p in range(4):
                            proj3(qbT[:, p, cols], qbT, lambda c: Wb[:, c, p * 128:(p + 1) * 128], Wb, tb, k); k += 1
                        proj3(kbT2[:, cols], kbT2, lambda c: Wkb2[:, c, :], Wkb2, tb, k); k += 1
                        for g in range(2):
                            proj3(qiT[:, g, cols], qiT, lambda c: Wb[:, c, 640 + g * 128:640 + (g + 1) * 128], Wb, tb, k); k += 1
                        proj3(kiT4[:, cols], kiT4, lambda c: Wki4[:, c, :], Wki4, tb, k); k += 1
                    for kt in range(NT):
                        pb = pp[kt % 2]
                        tsl = slice(kt * 128, (kt + 1) * 128)
                        for c in range(8):
                            S.op("pe", lambda e: e.matmul(pb[:, 0:64], xnT[:, c, tsl], Wb[:, c, 576:640], start=(c == 0), stop=(c == 7)),
                                 reads=[xnT, Wb], writes=[pb], inc=(c == 7))
                        for c in range(8):
                            S.op("pe", lambda e: e.matmul(pb[:, 64:72], xnT[:, c, tsl], Wb[:, c, 928:936], start=(c == 0), stop=(c == 7)),
                                 reads=[xnT, Wb], writes=[pb], inc=(c == 7))
                        S.op("act", lambda e: e.activation(Vb[:, kt, 0, 0:64], pb[:, 0:64], AF.Copy), reads=[pb], writes=[Vb])
                        S.op("act", lambda e: e.activation(Vb[:, kt, 1, 64:128], pb[:, 0:64], AF.Copy), reads=[pb], writes=[Vb])
                        S.op("act", lambda e: e.activation(widx[:, kt, :], pb[:, 64:72], AF.Copy, scale=0.0625), reads=[pb], writes=[widx])

                    S.barrier()
                    st3a.close()
                    Sc = [S.sbuf(st3, "Sc%d" % i, [128, T], F32) for i in range(1)]
                    msk = [S.sbuf(st3, "msk%d" % i, [128, T], BF16) for i in range(4)]
                    mskT = S.sbuf(st3, "mskT", [128, NT, 512], BF16)
                    rl = [S.sbuf(st3, "rl%d" % i, [128, 512], F32) for i in range(2)]
                    bmx = S.sbuf(st3, "bmx", [128, 1], F32)
                    bmn = S.sbuf(st3, "bmn", [128, 1], F32)
                    brg = S.sbuf(st3, "brg", [128, 1], F32)
                    bmid = S.sbuf(st3, "bmid", [128, 1], F32)
                    bcnt = S.sbuf(st3, "bcnt", [128, 1], F32)
                    bt = S.sbuf(st3, "bt", [128, 1], F32)
                    Es = [S.sbuf(st3, "E%d" % i, [128, 512], BF16) for i in range(3)]
                    PTs = [S.sbuf(st3, "PTb%d" % i, [128, 512], BF16) for i in range(3)]
                    rd = S.sbuf(st3, "rdb", [128, 512], F32)
                    sidx = [0]
                    oi = 0
                    ri = 0
                    for qb in range(4):
                        for j in range(4):
                            qt = 4 * qb + j
                            n = (qt + 1) * 128
                            sc = Sc[0]
                            mk = msk[j]
                            tsl = slice(qt * 128, (qt + 1) * 128)
                            for hh in range(8):
                                g, jj = hh // 4, hh % 4
                                for sb in range((n + 511) // 512):
                                    w = min(512, n - sb * 512)
                                    pr = pp[ri % 2]
                                    rlb = rl[ri % 2]
                                    ri += 1
                                    S.op("pe", lambda e: e.matmul(pr[:, 0:w], qiT[32 * jj:32 * jj + 32, g, tsl],
                                                                  kiT4[32 * jj:32 * jj + 32, sb * 512:sb * 512 + w],
                                                                  start=True, stop=True, tile_position=(32 * jj, 0)),
                                         reads=[qiT, kiT4], writes=[pr])
                                    S.op("act", lambda e: e.activation(rlb[:, 0:w], pr[:, 0:w], AF.Relu), reads=[pr], writes=[rlb])
                                    dst = sc[:, sb * 512:sb * 512 + w]
                                    if hh == 0:
                                        S.op("dve", lambda e: e.tensor_scalar(dst, rlb[:, 0:w], widx[:, qt, 0:1], None, ALU.mult),
                                             reads=[rlb, widx], writes=[sc])
                                    else:
                                        S.op("dve", lambda e: e.scalar_tensor_tensor(dst, rlb[:, 0:w], widx[:, qt, hh:hh + 1], dst,
                                                                                     ALU.mult, ALU.add),
                                             reads=[rlb, widx, sc], writes=[sc])
                            S.op("pool", lambda e: e.memset(sc[0:64, n - 64:n], NEGBIG), writes=[sc])
                            if qt >= 2:
                                S.op("dve", lambda e: e.reduce_max(bmx[:], sc[:, 0:n], AX.X), reads=[sc], writes=[bmx])
                                S.op("dve", lambda e: e.tensor_reduce(bmn[:], sc[:, 0:n - 64], AX.X, ALU.min), reads=[sc], writes=[bmn])
                                S.op("dve", lambda e: e.tensor_tensor(brg[:], bmx[:], bmn[:], ALU.subtract), reads=[bmx, bmn], writes=[brg])
                                S.op("dve", lambda e: e.tensor_scalar(brg[:], brg[:], 1e-20, None, ALU.add), reads=[brg], writes=[brg])
                                S.op("dve", lambda e: e.reciprocal(brg[:], brg[:]), reads=[brg], writes=[brg])
                                S.op("dve", lambda e: e.tensor_scalar(sc[:, 0:n], sc[:, 0:n], bmn[:, 0:1], brg[:, 0:1], ALU.subtract, ALU.mult),
                                     reads=[sc, bmn, brg], writes=[sc])
                                S.op("dve", lambda e: e.memset(bmid[:], 0.5), writes=[bmid])
                                for it in range(BIS_ITERS):
                                    S.op("dve", lambda e: e.tensor_scalar(mk[:, 0:n], sc[:, 0:n], bmid[:, 0:1], None, ALU.is_ge, ALU.add,
                                                                          accum_out=bcnt[:]),
                                         reads=[sc, bmid], writes=[mk, bcnt])
                                    s_next = 2.0 ** -(it + 2)
                                    S.op("dve", lambda e: e.tensor_scalar(bt[:], bcnt[:], float(TOPK), 2.0 * s_next, ALU.is_ge, ALU.mult),
                                         reads=[bcnt], writes=[bt])
                                    S.op("dve", lambda e: e.scalar_tensor_tensor(bmid[:], bt[:], -s_next, bmid[:], ALU.add, ALU.add),
                                         reads=[bt, bmid], writes=[bmid])
                                S.op("dve", lambda e: e.tensor_scalar(bmid[:], bmid[:], -(2.0 ** -(BIS_ITERS + 1)), None, ALU.add),
                                     reads=[bmid], writes=[bmid])
                                S.op("dve", lambda e: e.tensor_scalar(mk[:, 0:n], sc[:, 0:n], bmid[:, 0:1], None, ALU.is_ge),
                                     reads=[sc, bmid], writes=[mk])
                            else:
                                S.op("dve", lambda e: e.tensor_scalar(mk[:, 0:n], sc[:, 0:n], -1.0e29, None, ALU.is_ge),
                                     reads=[sc], writes=[mk])
                        for kt in range(4 * qb + 4):
                            j0 = max(0, kt - 4 * qb)
                            half = (kt % 2) * 512
                            for j in range(j0, 4):
                                S.op("pe", lambda e: e.transpose(pmt[:, half + j * 128:half + (j + 1) * 128],
                                                                 msk[j][:, kt * 128:(kt + 1) * 128], identb[:]),
                                     reads=[msk[j], identb], writes=[pmt], inc=(j == 3))
                            S.op("act", lambda e: e.activation(mskT[:, kt, j0 * 128:512], pmt[:, half + j0 * 128:half + 512], AF.Copy),
                                 reads=[pmt], writes=[mskT])
                        for h in range(8):
                            conv_some(2)
                            p, hf = h // 2, h % 2
                            base = 64 * hf
                            pO = po[oi % 2]
                            oi += 1
                            nk = 4 * qb + 4
                            slots = {}

                            def emit_S(kt):
                                global_si = sidx[0]
                                sidx[0] += 1
                                sl = global_si % 3
                                slots[kt] = sl
                                j0 = max(0, kt - 4 * qb)
                                c0 = j0 * 128
                                pS, E, PT = ps[sl], Es[sl], PTs[sl]
                                S.op("pe", lambda e: e.matmul(pS[:, c0:512], kbT2[base:base + 64, kt * 128:(kt + 1) * 128],
                                                              qbT[base:base + 64, p, qb * 512 + c0:(qb + 1) * 512], start=True, stop=True),
                                     reads=[kbT2, qbT], writes=[pS])
                                for j in range(j0, 4):
                                    d = 4 * qb + j - kt
                                    if d in (0, 1):
                                        S.op("dve", lambda e: e.tensor_tensor(pS[:, j * 128:(j + 1) * 128], pS[:, j * 128:(j + 1) * 128],
                                                                              tz[:, h, d, :], ALU.add), reads=[pS, tz], writes=[pS])
                                S.op("act", lambda e: e.activation(E[:, c0:512], pS[:, c0:512], AF.Exp, bias=cfb[:, h:h + 1], scale=0.125),
                                     reads=[pS, cfb], writes=[E])
                                S.op("dve", lambda e: e.tensor_tensor(PT[:, c0:512], E[:, c0:512], mskT[:, kt, c0:512], ALU.mult),
                                     reads=[E, mskT], writes=[PT])

                            def emit_PV(kt):
                                c0 = max(0, kt - 4 * qb) * 128
                                PT = PTs[slots[kt]]
                                S.op("pe", lambda e: e.matmul(pO[:, c0:512], Vb[:, kt, hf, :], PT[:, c0:512],
                                                              start=(kt == 0), stop=(kt == nk - 1), skip_group_check=True),
                                     reads=[Vb, PT], writes=[pO], inc=(kt == nk - 1))

                            for s_ in range(nk + 2):
                                if s_ < nk:
                                    emit_S(s_)
                                if s_ >= 2:
                                    emit_PV(s_ - 2)
                            ocols = slice(qb * 512, (qb + 1) * 512)
                            if hf == 0:
                                S.op("dve", lambda e: e.reciprocal(rd[0:64, :], pO[64:128, :]), reads=[pO], writes=[rd])
                                S.op("dve", lambda e: e.tensor_tensor(obT[0:64, p, ocols], pO[0:64, :], rd[0:64, :], ALU.mult),
                                     reads=[pO, rd], writes=[obT])
                            else:
                                S.op("dve", lambda e: e.reciprocal(rd[64:128, :], pO[0:64, :]), reads=[pO], writes=[rd])
                                S.op("dve", lambda e: e.tensor_tensor(obT[64:128, p, ocols], pO[64:128, :], rd[64:128, :], ALU.mult),
                                     reads=[pO, rd], writes=[obT])
                    S.barrier()

                with ExitStack() as st4:
                    Wo = S.sbuf(st4, "Wo", [128, 8, DM], BF16)
                    mixT = S.sbuf(st4, "mixT", [128, 8, T], BF16)
                    bg = S.sbuf(st4, "bg", [128, 16], F32)
                    Wr = S.sbuf(st4, "Wr", [128, 8, 36], F32)
                    brb = S.sbuf(st4, "brb", [128, 36], F32)
                    st4a = ExitStack()
                    st4a.__enter__()
                    Wua = S.sbuf(st4a, "Wua", [128, 4, DM], BF16)
                    Wub = S.sbuf(st4a, "Wub", [128, 4, DM], BF16)
                    Wg = [S.sbuf(st4a, "Wg%d" % i, [128, 8, 2, 128], BF16) for i in range(2)]
                    sga = S.sbuf(st4a, "sga", [128, 512], F32)
                    sgb = S.sbuf(st4a, "sgb", [128, 512], F32)
                    m1 = S.sbuf(st4a, "m1", [128, 512], F32)
                    m2 = S.sbuf(st4a, "m2", [128, 512], F32)
                    pgs = [[S.psum(st4a, "pg%d_%d" % (q, i), [128, 512], F32) for i in range(4)] for q in range(2)]
                    sgas = [sga, S.sbuf(st4a, "sga2", [128, 512], F32)]
                    sgbs = [sgb, S.sbuf(st4a, "sgb2", [128, 512], F32)]
                    m1s = [m1, S.sbuf(st4a, "m1b", [128, 512], F32)]
                    m2s = [m2, S.sbuf(st4a, "m2b", [128, 512], F32)]
                    git = 0

                    S.dma("pool", Wua[:], w_up_a.rearrange("(c p) n -> p c n", p=128), writes=[Wua])
                    S.dma("pool", Wub[:], w_up_b.rearrange("(c p) n -> p c n", p=128), writes=[Wub])
                    S.dma("pool", Wo[:], w_o.rearrange("(c p) n -> p c n", p=128), writes=[Wo])
                    S.dma("sp", bg[:], bgate, writes=[bg])
                    S.dma("sp", Wr[:], w_r.rearrange("(c p) n -> p c n", p=128), writes=[Wr])
                    S.dma("sp", brb[:], b_r.partition_broadcast(128), writes=[brb])
                    gsrc = w_gate.rearrange("(c p) n -> p c n", p=128)
                    for m in range(8):
                        wg = Wg[m % 2]
                        S.dma("pool", wg[:, :, 0, :], gsrc[:, :, m * 128:(m + 1) * 128], writes=[wg])
                        S.dma("pool", wg[:, :, 1, :], gsrc[:, :, 1024 + m * 128:1024 + (m + 1) * 128], writes=[wg])
                        for tb in range(4):
                            cols = slice(tb * 512, (tb + 1) * 512)
                            pg = pgs[git % 2]
                            sga, sgb, m1, m2 = sgas[git % 2], sgbs[git % 2], m1s[git % 2], m2s[git % 2]
                            git += 1
                            for c in range(8):
                                S.op("pe", lambda e: e.matmul(pg[0][:], wg[:, c, 0, :], xnT[:, c, cols], start=(c == 0), stop=(c == 7)),
                                     reads=[wg, xnT], writes=[pg[0]], inc=(c == 7))
                            for c in range(8):
                                S.op("pe", lambda e: e.matmul(pg[1][:], wg[:, c, 1, :], xnT[:, c, cols], start=(c == 0), stop=(c == 7)),
                                     reads=[wg, xnT], writes=[pg[1]], inc=(c == 7))
                            for c in range(4):
                                S.op("pe", lambda e: e.matmul(pg[2][:], Wua[:, c, m * 128:(m + 1) * 128], oaT[:, c, cols], start=(c == 0), stop=(c == 3)),
                                     reads=[Wua, oaT], writes=[pg[2]], inc=(c == 3))
                            for c in range(4):
                                S.op("pe", lambda e: e.matmul(pg[3][:], Wub[:, c, m * 128:(m + 1) * 128], obT[:, c, cols], start=(c == 0), stop=(c == 3)),
                                     reads=[Wub, obT], writes=[pg[3]], inc=(c == 3))
                            S.op("act", lambda e: e.activation(sga[:], pg[0][:], AF.Sigmoid, bias=bg[:, m:m + 1]), reads=[pg[0], bg], writes=[sga])
                            S.op("act", lambda e: e.activation(sgb[:], pg[1][:], AF.Sigmoid, bias=bg[:, 8 + m:9 + m]), reads=[pg[1], bg], writes=[sgb])
                            S.op("dve", lambda e: e.tensor_tensor(m1[:], pg[2][:], sga[:], ALU.mult), reads=[pg[2], sga], writes=[m1])
                            S.op("dve", lambda e: e.tensor_tensor(m2[:], pg[3][:], sgb[:], ALU.mult), reads=[pg[3], sgb], writes=[m2])
                            S.op("dve", lambda e: e.tensor_tensor(mixT[:, m, cols], m1[:], m2[:], ALU.add), reads=[m1, m2], writes=[mixT])
                    S.barrier()
                    st4a.close()
                    pg = [S.psum(st4, "pgt%d" % i, [128, 512], F32) for i in range(2)]
                    pm = [S.psum(st4, "pm%d" % i, [128, 512], F32) for i in range(2)]
                    pl = S.psum(st4, "pl", [128, 512], F32)
                    pre = S.sbuf(st4, "pre", [128, DM], F32)
                    hbs = [S.sbuf(st4, "hb%d" % i, [128, DM], BF16) for i in range(2)]
                    hst = [S.sbuf(st4, "hst%d" % i, [128, DM], F32) for i in range(2)]
                    hT32 = S.sbuf(st4, "hT32", [128, 8, 128], F32)
                    lgA = S.sbuf(st4, "lgA", [128, NT, 36], F32)
                    gmxA = S.sbuf(st4, "gmxA", [128, NT], F32)
                    gselA = S.sbuf(st4, "gselA", [128, NT, 4], F32)
                    r4A = S.sbuf(st4, "r4A", [128, NT, 4], F32)
                    gwA = S.sbuf(st4, "gwA", [128, NT], F32)
                    le4 = S.sbuf(st4, "le4", [128, NT, 4, 8], F32)
                    seA = S.sbuf(st4, "seA", [128, NT, 8], F32)
                    se2A = S.sbuf(st4, "se2A", [128, NT, 8], F32)
                    oh1A = S.sbuf(st4, "oh1A", [128, NT, 8], F32)
                    oh2A = S.sbuf(st4, "oh2A", [128, NT, 8], F32)
                    mx1A = S.sbuf(st4, "mx1A", [128, NT], F32)
                    mx2A = S.sbuf(st4, "mx2A", [128, NT], F32)
                    w1A = S.sbuf(st4, "w1A", [128, NT], F32)
                    w2A = S.sbuf(st4, "w2A", [128, NT], F32)
                    lnsB = [dict(stats=S.sbuf(st4, "statsB%d" % k, [128, 2, 6], F32), mv=S.sbuf(st4, "mvB%d" % k, [128, 2], F32),
                                 rstd=S.sbuf(st4, "rstdB%d" % k, [128, 1], F32), nmr=S.sbuf(st4, "nmrB%d" % k, [128, 1], F32),
                                 z=S.sbuf(st4, "zB%d" % k, [128, DM], F32)) for k in range(2)]

                    def tile_A(i):
                        tsl = slice(i * 128, (i + 1) * 128)
                        xs = xt[i % 2]
                        S.dma("sp", xs[:], x[b, tsl, :], writes=[xs])
                        layer_norm_tile(lns, xs, None, 0, None)
                        z = lns["z"]
                        S.op("dve", lambda e: e.tensor_tensor(z[:], z[:], lnbc[:, 1, :], ALU.add), reads=[z, lnbc], writes=[z])
                        for hf in range(2):
                            for m in range(8):
                                S.op("pe", lambda e: e.matmul(pm[hf][:], mixT[:, m, tsl], Wo[:, m, hf * 512:(hf + 1) * 512],
                                                              start=(m == 0), stop=(m == 7)), reads=[mixT, Wo], writes=[pm[hf]], inc=(m == 7))
                            S.op("dve", lambda e: e.scalar_tensor_tensor(pre[:, hf * 512:(hf + 1) * 512], z[:, hf * 512:(hf + 1) * 512],
                                                                         ALPHA, pm[hf][:], ALU.mult, ALU.add),
                                 reads=[z, pm[hf]], writes=[pre])
                        lb = lnsB[i % 2]
                        layer_norm_tile(lb, pre, None, 2, None)
                        z1 = lb["z"]
                        S.op("dve", lambda e: e.tensor_tensor(z1[:], z1[:], lnbc[:, 3, :], ALU.add), reads=[z1, lnbc], writes=[z1])

                    def tile_B(i):
                        tsl = slice(i * 128, (i + 1) * 128)
                        z = lnsB[i % 2]["z"]
                        hh = hst[i % 2]
                        hb = hbs[i % 2]
                        S.op("act", lambda e: e.activation(hb[:], z[:], AF.Copy), reads=[z], writes=[hb])
                        S.op("act", lambda e: e.activation(hh[:], z[:], AF.Copy, scale=ALPHA), reads=[z], writes=[hh])
                        S.dma("sp", hs[tsl, :], hh[:], reads=[hh], writes=[hsB], sembuf=hh)
                        S.dma("sp", hbd[tsl, :], hb[:], reads=[hb], writes=[hbdB], sembuf=hb)
                        for hf in range(2):
                            for c in range(4):
                                cc = hf * 4 + c
                                S.op("pe", lambda e: e.transpose(pg[hf][:, c * 128:(c + 1) * 128], z[:, cc * 128:(cc + 1) * 128], identF[:]),
                                     reads=[z, identF], writes=[pg[hf]], inc=(c == 3))
                            S.op("act", lambda e: e.activation(hT32[:, hf * 4:(hf + 1) * 4, :], pg[hf][:].rearrange("p (c t) -> p c t", c=4), AF.Copy),
                                 reads=[pg[hf]], writes=[hT32])
                        for c in range(8):
                            S.op("pe", lambda e: e.matmul(pl[:, 0:36], hT32[:, c, :], Wr[:, c, :], start=(c == 0), stop=(c == 7)),
                                 reads=[hT32, Wr], writes=[pl], inc=(c == 7))
                        S.op("dve", lambda e: e.tensor_tensor(lgA[:, i, :], pl[:, 0:36], brb[:], ALU.add), reads=[pl, brb], writes=[lgA])

                    tile_A(0)
                    for i in range(NT):
                        if i + 1 < NT:
                            tile_A(i + 1)
                        tile_B(i)
                    NTl = NT
                    G3 = lgA[:, :, 0:4]
                    E4 = lgA[:, :, 4:36].rearrange("p t (g e) -> p t g e", g=4)
                    bc_t = lambda ap2, n: ap2.rearrange("p (t o) -> p t o", o=1).to_broadcast([128, NTl, n])
                    S.op("dve", lambda e: e.tensor_reduce(gmxA[:], G3, AX.X, ALU.max), reads=[lgA], writes=[gmxA])
                    S.op("dve", lambda e: e.tensor_tensor(gselA[:], G3, bc_t(gmxA[:], 4), ALU.is_ge), reads=[lgA, gmxA], writes=[gselA])
                    S.op("dve", lambda e: e.tensor_tensor(r4A[:], G3, bc_t(gmxA[:], 4), ALU.subtract), reads=[lgA, gmxA], writes=[r4A])
                    S.op("act", lambda e: e.activation(r4A[:], r4A[:], AF.Exp), reads=[r4A], writes=[r4A])
                    S.op("dve", lambda e: e.tensor_reduce(gwA[:], r4A[:], AX.X, ALU.add), reads=[r4A], writes=[gwA])
                    S.op("dve", lambda e: e.reciprocal(gwA[:], gwA[:]), reads=[gwA], writes=[gwA])
                    gsel4 = gselA[:].rearrange("p t (g o) -> p t g o", o=1).to_broadcast([128, NTl, 4, 8])
                    S.op("dve", lambda e: e.tensor_tensor(le4[:], E4, gsel4, ALU.mult), reads=[lgA, gselA], writes=[le4])
                    S.op("dve", lambda e: e.tensor_reduce(seA[:], le4[:].rearrange("p t g e -> p t e g"), AX.X, ALU.add), reads=[le4], writes=[seA])
                    S.op("dve", lambda e: e.tensor_reduce(mx1A[:], seA[:], AX.X, ALU.max), reads=[seA], writes=[mx1A])
                    S.op("dve", lambda e: e.tensor_tensor(oh1A[:], seA[:], bc_t(mx1A[:], 8), ALU.is_ge), reads=[seA, mx1A], writes=[oh1A])
                    S.op("dve", lambda e: e.scalar_tensor_tensor(se2A[:], oh1A[:], NEGBIG, seA[:], ALU.mult, ALU.add), reads=[oh1A, seA], writes=[se2A])
                    S.op("dve", lambda e: e.tensor_reduce(mx2A[:], se2A[:], AX.X, ALU.max), reads=[se2A], writes=[mx2A])
                    S.op("dve", lambda e: e.tensor_tensor(oh2A[:], se2A[:], bc_t(mx2A[:], 8), ALU.is_ge), reads=[se2A, mx2A], writes=[oh2A])
                    S.op("dve", lambda e: e.tensor_tensor(w2A[:], mx2A[:], mx1A[:], ALU.subtract), reads=[mx1A, mx2A], writes=[w2A])
                    S.op("act", lambda e: e.activation(w2A[:], w2A[:], AF.Exp), reads=[w2A], writes=[w2A])
                    S.op("dve", lambda e: e.tensor_scalar(w1A[:], w2A[:], 1.0, None, ALU.add), reads=[w2A], writes=[w1A])
                    S.op("dve", lambda e: e.reciprocal(w1A[:], w1A[:]), reads=[w1A], writes=[w1A])
                    S.op("dve", lambda e: e.tensor_tensor(w2A[:], w2A[:], w1A[:], ALU.mult), reads=[w1A, w2A], writes=[w2A])
                    S.op("dve", lambda e: e.tensor_tensor(ca[:], w1A[:], gwA[:], ALU.mult), reads=[w1A, gwA], writes=[ca])
                    S.op("dve", lambda e: e.tensor_tensor(cb[:], w2A[:], gwA[:], ALU.mult), reads=[w2A, gwA], writes=[cb])
                    for Mx, ohx in ((M1, oh1A), (M2, oh2A)):
                        S.op("dve", lambda e: e.tensor_tensor(Mx[:].rearrange("p t (g e) -> p t g e", g=4),
                                                              ohx[:].rearrange("p t (o e) -> p t o e", o=1).to_broadcast([128, NTl, 4, 8]),
                                                              gsel4, ALU.mult), reads=[ohx, gselA], writes=[Mx])
                    S.op("dve", lambda e: e.tensor_tensor(Mb[:], M1[:], M2[:], ALU.add), reads=[M1, M2], writes=[Mb])
                    S.barrier()

            conv_some(1000)
            with ExitStack() as st5:
                NSL = 64
                pr = [S.psum(st5, "pr%d" % i, [128, 512], F32) for i in range(2)]
                Rall = S.sbuf(st5, "Rall", [128, NT, 32], F32)
                cntf = S.sbuf(st5, "cntf", [128, 32], F32)
                cntI = S.sbuf(st5, "cntI", [128, 32], I32)
                pcf = S.sbuf(st5, "pcf", [128, 32], F32)
                scA = S.sbuf(st5, "scA", [128, 32], F32)
                scB = S.sbuf(st5, "scB", [128, 32], F32)
                off = S.sbuf(st5, "off", [128, 32], F32)
                Pm = S.sbuf(st5, "Pm", [128, NT, 32], F32)
                prod = S.sbuf(st5, "prod", [128, NT, 32], F32)
                posaF = S.sbuf(st5, "posaF", [128, NT], F32)
                posbF = S.sbuf(st5, "posbF", [128, NT], F32)
                posaI = S.sbuf(st5, "posaI", [128, NT], I32)
                posbI = S.sbuf(st5, "posbI", [128, NT], I32)
                cmpb = S.sbuf(st5, "cmpb", [128, NSL, 32], F32)
                eidf = S.sbuf(st5, "eidf", [128, NSL], F32)
                actf = S.sbuf(st5, "actf", [128, NSL], F32)
                widF = S.sbuf(st5, "widF", [128, NSL], F32)
                widI = S.sbuf(st5, "widI", [128, NSL], I32)
                for i in range(NT):
                    S.op("pe", lambda e: e.matmul(pr[0][:, 0:32], onesb[:], Mb[:, i, :], start=(i == 0), stop=(i == NT - 1)),
                         reads=[onesb, Mb], writes=[pr[0]], inc=(i == NT - 1))
                S.op("dve", lambda e: e.tensor_copy(cntf[:], pr[0][:, 0:32]), reads=[pr[0]], writes=[cntf])
                for i in range(NT):
                    pb = pr[1]
                    for i2 in range(i):
                        S.op("pe", lambda e: e.matmul(pb[:, 0:32], onesb[:], Mb[:, i2, :], start=(i2 == 0), stop=False),
                             reads=[onesb, Mb], writes=[pb], inc=False)
                    S.op("pe", lambda e: e.matmul(pb[:, 0:32], ustrb[:], Mb[:, i, :], start=(i == 0), stop=True),
                         reads=[ustrb, Mb], writes=[pb])
                    S.op("act", lambda e: e.activation(Rall[:, i, :], pb[:, 0:32], AF.Copy), reads=[pb], writes=[Rall])
                S.op("dve", lambda e: e.tensor_scalar(pcf[:], cntf[:], 127.0, None, ALU.add), reads=[cntf], writes=[pcf])
                S.op("dve", lambda e: e.tensor_copy(cntI[:], pcf[:]), reads=[pcf], writes=[cntI])
                S.op("dve", lambda e: e.tensor_scalar(cntI[:], cntI[:], 7, None, ALU.arith_shift_right), reads=[cntI], writes=[cntI])
                S.op("dve", lambda e: e.tensor_scalar(cntI[:], cntI[:], 7, None, ALU.logical_shift_left), reads=[cntI], writes=[cntI])
                S.op("dve", lambda e: e.tensor_copy(pcf[:], cntI[:]), reads=[cntI], writes=[pcf])
                S.op("dve", lambda e: e.tensor_copy(scA[:], pcf[:]), reads=[pcf], writes=[scA])
                cur, nxt = scA, scB
                for sh in (1, 2, 4, 8, 16):
                    S.op("dve", lambda e: e.tensor_copy(nxt[:, 0:sh], cur[:, 0:sh]), reads=[cur], writes=[nxt])
                    S.op("dve", lambda e: e.tensor_tensor(nxt[:, sh:32], cur[:, sh:32], cur[:, 0:32 - sh], ALU.add), reads=[cur], writes=[nxt])
                    cur, nxt = nxt, cur
                incl = cur
                S.op("dve", lambda e: e.tensor_tensor(off[:], incl[:], pcf[:], ALU.subtract), reads=[incl, pcf], writes=[off])
                S.op("dve", lambda e: e.tensor_tensor(Pm[:], Rall[:], off[:].rearrange("p (o e) -> p o e", o=1).to_broadcast([128, NT, 32]), ALU.add),
                     reads=[Rall, off], writes=[Pm])
                for Mx, pF, pI in ((M1, posaF, posaI), (M2, posbF, posbI)):
                    S.op("dve", lambda e: e.tensor_tensor(prod[:], Mx[:], Pm[:], ALU.mult), reads=[Mx, Pm], writes=[prod])
                    S.op("dve", lambda e: e.tensor_reduce(pF[:], prod[:], AX.X, ALU.add), reads=[prod], writes=[pF])
                    S.op("dve", lambda e: e.tensor_copy(pI[:], pF[:]), reads=[pF], writes=[pI])
                S.op("dve", lambda e: e.tensor_tensor(cmpb[:], off[:].rearrange("p (o e) -> p o e", o=1).to_broadcast([128, NSL, 32]),
                                                      jvS[:].rearrange("p (j o) -> p j o", o=1).to_broadcast([128, NSL, 32]), ALU.is_le),
                     reads=[off, jvS], writes=[cmpb])
                S.op("dve", lambda e: e.tensor_reduce(eidf[:], cmpb[:], AX.X, ALU.add), reads=[cmpb], writes=[eidf])
                S.op("dve", lambda e: e.tensor_scalar(actf[:], jvS[:], incl[:, 31:32], 1.0e6, ALU.is_ge, ALU.mult), reads=[jvS, incl], writes=[actf])
                S.op("dve", lambda e: e.tensor_scalar(widF[:], eidf[:], -1.0, 128.0, ALU.add, ALU.mult), reads=[eidf], writes=[widF])
                S.op("dve", lambda e: e.tensor_scalar(widF[:], widF[:], pcolS[:, 0:1], None, ALU.add), reads=[widF, pcolS], writes=[widF])
                S.op("dve", lambda e: e.tensor_tensor(widF[:], widF[:], actf[:], ALU.add), reads=[widF, actf], writes=[widF])
                S.op("dve", lambda e: e.tensor_copy(widI[:], widF[:]), reads=[widF], writes=[widI])

                hld = [S.sbuf(st5, "hld%d" % i, [128, DM], BF16) for i in range(2)]
                for i in range(NT):
                    hl = hld[i % 2]
                    S.dma("sp", hl[:], hbd[i * 128:(i + 1) * 128, :], reads=[hbdB], writes=[hl])
                    for pI in (posaI, posbI):
                        S.dmaf("pool", lambda e: e.indirect_dma_start(out=Hs, out_offset=bass.IndirectOffsetOnAxis(ap=pI[:, i:i + 1], axis=0),
                                                                      in_=hl[:, :], in_offset=None),
                               reads=[hl, pI], writes=[HsB], sembuf=hl)

                Wg_s = [S.sbuf(st5, "Wgs%d" % i, [128, 8, 256], BF16) for i in range(2)]
                Wu_s = [S.sbuf(st5, "Wus%d" % i, [128, 8, 256], BF16) for i in range(2)]
                Wd_s = [S.sbuf(st5, "Wds%d" % i, [128, 2, DM], BF16) for i in range(2)]
                hsl = [S.sbuf(st5, "hsl%d" % i, [128, DM], BF16) for i in range(2)]
                hslT = [S.sbuf(st5, "hslT%d" % i, [128, 8, 128], BF16) for i in range(2)]
                sa = [S.sbuf(st5, "sa%d" % i, [128, 256], F32) for i in range(2)]
                hid = [S.sbuf(st5, "hid%d" % i, [128, 256], BF16) for i in range(2)]
                hidT = [S.sbuf(st5, "hidT%d" % i, [128, 2, 128], BF16) for i in range(2)]
                ysb = [S.sbuf(st5, "ysb%d" % i, [128, DM], F32) for i in range(2)]
                pht = S.psum(st5, "pht", [128, 1024], BF16)
                ptx = S.psum(st5, "ptx", [128, 1024], BF16)
                pau = [S.psum(st5, "pau%d" % i, [128, 512], F32) for i in range(2)]
                py = [S.psum(st5, "py%d" % i, [128, 512], F32) for i in range(2)]

                def st_load_a(j):
                    k = j % 2
                    S.dma("sp", hsl[k][:], Hs[j * 128:(j + 1) * 128, :], reads=[HsB], writes=[hsl[k]])
                    for wt, src in ((Wg_s[k], weg_b), (Wu_s[k], weu_b)):
                        S.dmaf("pool", lambda e: e.indirect_dma_start(out=wt[:].rearrange("p a b -> p (a b)"), out_offset=None, in_=src,
                                                                      in_offset=bass.IndirectOffsetOnAxis(ap=widI[:, j:j + 1], axis=0),
                                                                      bounds_check=bcreg, oob_is_err=False),
                               reads=[widI, WcB], writes=[wt])

                def st_load_d(j):
                    k = j % 2
                    wt = Wd_s[k]
                    S.dmaf("pool", lambda e: e.indirect_dma_start(out=wt[:].rearrange("p a b -> p (a b)"), out_offset=None, in_=wed_b,
                                                                  in_offset=bass.IndirectOffsetOnAxis(ap=widI[:, j:j + 1], axis=0),
                                                                  bounds_check=bcreg, oob_is_err=False),
                           reads=[widI, WcB], writes=[wt])

                def st_au(j):
                    k = j % 2
                    for c in range(8):
                        S.op("pe", lambda e: e.transpose(ptx[:, c * 128:(c + 1) * 128], hsl[k][:, c * 128:(c + 1) * 128], identb[:]),
                             reads=[hsl[k], identb], writes=[ptx], inc=(c == 7))
                    S.op("act", lambda e: e.activation(hslT[k][:], ptx[:].rearrange("p (c t) -> p c t", c=8), AF.Copy),
                         reads=[ptx], writes=[hslT[k]])
                    pa = pau[k]
                    for c in range(8):
                        S.op("pe", lambda e: e.matmul(pa[:, 0:256], hslT[k][:, c, :], Wg_s[k][:, c, :], start=(c == 0), stop=(c == 7)),
                             reads=[hslT[k], Wg_s[k]], writes=[pa], inc=False)
                    for c in range(8):
                        S.op("pe", lambda e: e.matmul(pa[:, 256:512], hslT[k][:, c, :], Wu_s[k][:, c, :], start=(c == 0), stop=(c == 7)),
                             reads=[hslT[k], Wu_s[k]], writes=[pa], inc=(c == 7))
                    S.op("act", lambda e: e.activation(sa[k][:], pa[:, 0:256], AF.Silu), reads=[pa], writes=[sa[k]])
                    S.op("dve", lambda e: e.tensor_tensor(hid[k][:], pa[:, 256:512], sa[k][:], ALU.mult), reads=[pa, sa[k]], writes=[hid[k]])

                def st_tr(j):
                    k = j % 2
                    o = k * 512
                    for f in range(2):
                        S.op("pe", lambda e: e.transpose(pht[:, o + f * 128:o + (f + 1) * 128], hid[k][:, f * 128:(f + 1) * 128], identb[:]),
                             reads=[hid[k], identb], writes=[pht], inc=(f == 1))
                    S.op("act", lambda e: e.activation(hidT[k][:], pht[:, o:o + 256].rearrange("p (f t) -> p f t", f=2), AF.Copy),
                         reads=[pht], writes=[hidT[k]])

                def st_y(j):
                    k = j % 2
                    for hf in range(2):
                        for f in range(2):
                            S.op("pe", lambda e: e.matmul(py[hf][:], hidT[k][:, f, :], Wd_s[k][:, f, hf * 512:(hf + 1) * 512],
                                                          start=(f == 0), stop=(f == 1)),
                                 reads=[hidT[k], Wd_s[k]], writes=[py[hf]], inc=(f == 1))
                        if hf == 0:
                            S.op("act", lambda e: e.activation(ysb[k][:, 0:512], py[0][:], AF.Copy), reads=[py[0]], writes=[ysb[k]])
                        else:
                            S.op("dve", lambda e: e.tensor_copy(ysb[k][:, 512:1024], py[1][:]), reads=[py[1]], writes=[ysb[k]])
                    S.dma("sp", Ys[j * 128:(j + 1) * 128, :], ysb[k][:], reads=[ysb[k]], writes=[YsB], sembuf=ysb[k])

                st_load_a(0)
                st_load_d(0)
                for j in range(NSL + 2):
                    if j < NSL:
                        if j + 1 < NSL:
                            st_load_a(j + 1)
                        st_au(j)
                    if 1 <= j <= NSL:
                        st_tr(j - 1)
                    if j >= 2:
                        st_y(j - 2)
                    if 1 <= j < NSL:
                        st_load_d(j)

                lns5 = dict(stats=S.sbuf(st5, "stats5", [128, 2, 6], F32), mv=S.sbuf(st5, "mv5", [128, 2], F32),
                            rstd=S.sbuf(st5, "rstd5", [128, 1], F32), nmr=S.sbuf(st5, "nmr5", [128, 1], F32),
                            z=S.sbuf(st5, "z5", [128, DM], F32))
                accs = [S.sbuf(st5, "accs%d" % i, [128, DM], F32) for i in range(2)]
                yas = [S.sbuf(st5, "yas%d" % i, [128, DM], F32) for i in range(2)]
                ybs = [S.sbuf(st5, "ybs%d" % i, [128, DM], F32) for i in range(2)]
                ost = [S.sbuf(st5, "ost%d" % i, [128, DM], F32) for i in range(2)]
                for i in range(NT):
                    k = i % 2
                    S.dma("sp", accs[k][:], hs[i * 128:(i + 1) * 128, :], reads=[hsB], writes=[accs[k]])
                    for yt, pI in ((yas[k], posaI), (ybs[k], posbI)):
                        S.dmaf("pool", lambda e: e.indirect_dma_start(out=yt[:, :], out_offset=None, in_=Ys,
                                                                      in_offset=bass.IndirectOffsetOnAxis(ap=pI[:, i:i + 1], axis=0)),
                               reads=[YsB, pI], writes=[yt])
                    S.op("dve", lambda e: e.scalar_tensor_tensor(accs[k][:], yas[k][:], ca[:, i:i + 1], accs[k][:], ALU.mult, ALU.add),
                         reads=[yas[k], ca], writes=[accs[k]])
                    S.op("dve", lambda e: e.scalar_tensor_tensor(accs[k][:], ybs[k][:], cb[:, i:i + 1], accs[k][:], ALU.mult, ALU.add),
                         reads=[ybs[k], cb], writes=[accs[k]])
                    layer_norm_tile(lns5, accs[k], None, 4, None)
                    z = lns5["z"]
                    o = ost[k]
                    S.op("dve", lambda e: e.tensor_tensor(o[:], z[:], lnbc[:, 5, :], ALU.add), reads=[z, lnbc], writes=[o])
                    S.dma("sp", out[b, i * 128:(i + 1) * 128, :], o[:], reads=[o], writes=[outB], sembuf=o)
                S.barrier()
            stB.close()
        S.nobar.clear()
        S.barrier()
    return nc


_NC = None


def _bucket_tables():
    import jax
    import jax.numpy as jnp
    with jax.default_device(jax.devices("cpu")[0]):
        kk = jnp.arange(128, dtype=jnp.int32)[:, None]
        qq = jnp.arange(128, dtype=jnp.int32)[None, :]
        tabs = []
        for d in range(2):
            rel = kk - qq - 128 * d
            nb = 16
            max_exact = 8
            ret = jnp.where(rel > 0, nb, 0)
            n = jnp.abs(rel)
            large = max_exact + (jnp.log(jnp.maximum(n, 1).astype(jnp.float32) / max_exact)
                                 / math.log(128 / max_exact) * (nb - max_exact)).astype(jnp.int32)
            large = jnp.minimum(large, nb - 1)
            tabs.append(np.asarray(ret + jnp.where(n < max_exact, n, large)))
    return np.stack(tabs, 0)


def kernel(**inputs):
    global _NC
    f32 = np.float32
    g = lambda k: np.ascontiguousarray(np.asarray(inputs[k]))
    x = g("x").astype(f32, copy=False)
    pos = g("positions").astype(np.int32, copy=False)
    rel_bias = g("rel_bias")
    bk = _bucket_tables()
    tzr = rel_bias[bk]
    tzr = np.ascontiguousarray(np.transpose(tzr, (1, 3, 0, 2))).astype(f32)
    shared = {
        "w_in": g("w_in")[0], "w_uq": g("w_uq")[0], "w_uk": g("w_uk")[0], "w_uv": g("w_uv")[0],
        "w_up_a": g("w_up_a")[0], "w_up_b": g("w_up_b")[0], "w_gate": g("w_gate")[0], "w_o": g("w_o")[0],
        "w_r": np.ascontiguousarray(np.concatenate([g("w_grp")[0], g("w_rt")[0]], axis=1)),
        "b_r": np.ascontiguousarray(np.concatenate([g("b_grp")[0], g("b_rt")[0]], axis=0)),
        "w_eg": g("w_exp_gate")[0].reshape(32, 8, 128, 256).transpose(0, 2, 1, 3).reshape(32 * 128, 2048),
        "w_eu": g("w_exp_up")[0].reshape(32, 8, 128, 256).transpose(0, 2, 1, 3).reshape(32 * 128, 2048),
        "w_ed": g("w_exp_down")[0].reshape(32, 2, 128, 1024).transpose(0, 2, 1, 3).reshape(32 * 128, 2048),
        "ustr": np.triu(np.ones((128, 128), dtype=f32), 1),
        "jv": np.tile((np.arange(64, dtype=f32) * 128.0)[None, :], (128, 1)),
        "pcol": np.arange(128, dtype=f32).reshape(128, 1),
        "lnv": np.ascontiguousarray(np.stack([g("ln0_g"), g("ln0_b"), g("ln1_g")[0], g("ln1_b")[0], g("ln2_g")[0], g("ln2_b")[0]], 0)),
        "qng": np.ascontiguousarray(g("q_norm_g")[0].reshape(2, 128).T),
        "kvg": np.ascontiguousarray(g("kv_norm_g")[0].reshape(128, 1)),
        "bgate": np.ascontiguousarray(g("b_gate")[0].reshape(16, 128).T),
        "tzr": tzr,
        "cfar": np.ascontiguousarray(rel_bias[15, :]),
        "identf": np.eye(128, dtype=f32),
        "invf": np.tile((10000.0 ** (-np.arange(16, dtype=np.float64) / 16.0) / (2.0 * math.pi)).astype(f32), 2).reshape(32, 1),
    }
    shared = {k: np.ascontiguousarray(v.astype(f32, copy=False)) for k, v in shared.items()}
    if _NC is None:
        _NC = build()
    in_maps = []
    for c in range(8):
        m = dict(shared)
        m["x"] = np.ascontiguousarray(x[NB * c:NB * (c + 1)])
        m["pos"] = np.ascontiguousarray(pos[NB * c:NB * (c + 1)])
        in_maps.append(m)
    res = run_bass_kernel_spmd(_NC, in_maps, core_ids=list(range(8)))
    return np.concatenate([np.asarray(r["out"]) for r in res.results], axis=0).astype(f32, copy=False)
```

```python
import math
from contextlib import ExitStack
import numpy as np
import concourse.bass as bass
import concourse.mybir as mybir
from concourse.bass_utils import run_bass_kernel_spmd

F32 = mybir.dt.float32
BF16 = mybir.dt.bfloat16
I32 = mybir.dt.int32
ALU = mybir.AluOpType
AF = mybir.ActivationFunctionType
AX = mybir.AxisListType

T = 2048
DM = 1024
NT = 16
NB = 2
ALPHA = 2.0 ** 0.25
LN_EPS = 1e-5
RMS_EPS = 1e-6
NEGBIG = -1.0e30
BIS_ITERS = 14
TOPK = 256


class Buf:
    __slots__ = ("ap", "name", "ws", "reads", "dsem", "dcount")

    def __init__(self, ap, name=""):
        self.ap = ap
        self.name = name
        self.ws = {}
        self.reads = {}
        self.dsem = None
        self.dcount = 0

    def __getitem__(self, idx):
        return self.ap[idx]


class Sched:
    def __init__(self, nc, stack):
        self.nc = nc
        self.stack = stack
        self.engs = {}
        for name, eng in (("pe", nc.tensor), ("act", nc.scalar), ("dve", nc.vector),
                          ("pool", nc.gpsimd), ("sp", nc.sync)):
            sem = stack.enter_context(nc.semaphore("s_" + name))
            self.engs[name] = dict(eng=eng, sem=sem, count=0, waited={})
        self.ndsem = 0
        self.dpool = {}
        self.nobar = set()
        self.n_ins = 0
        self.uid = 0

    def sbuf(self, st, name, shape, dtype):
        self.uid += 1
        name = "%s_%d" % (name, self.uid)
        return Buf(st.enter_context(self.nc.sbuf_tensor(name, shape, dtype)), name)

    def psum(self, st, name, shape, dtype):
        self.uid += 1
        name = "%s_%d" % (name, self.uid)
        return Buf(st.enter_context(self.nc.psum_tensor(name, shape, dtype)), name)

    def _wait(self, engname, ev):
        sem, val, src = ev
        if src == "pe" and engname == "pe":
            return
        E = self.engs[engname]
        key = id(sem)
        if E["waited"].get(key, 0) < val:
            E["eng"].wait_ge(sem, val)
            E["waited"][key] = val
            self.n_ins += 1

    def _deps(self, engname, reads, writes):
        for b in reads:
            for ev in b.ws.values():
                self._wait(engname, ev)
        for b in writes:
            for ev in b.ws.values():
                self._wait(engname, ev)
            for ev in b.reads.values():
                self._wait(engname, ev)

    def _commit(self, ev, reads, writes):
        k = id(ev[0])
        for b in writes:
            b.ws[k] = ev
            b.reads = {}
        for b in reads:
            if b in writes:
                continue
            b.reads[k] = ev

    def op(self, engname, fn, reads=(), writes=(), inc=True):
        E = self.engs[engname]
        self._deps(engname, reads, writes)
        ins = fn(E["eng"])
        self.n_ins += 1
        if inc:
            E["count"] += 1
            ins.then_inc(E["sem"], 1)
            self._commit((E["sem"], E["count"], engname), reads, writes)
        else:
            self._commit((E["sem"], E["count"] + 1, engname), reads, writes)

    def dma(self, qname, out_ap, in_ap, reads=(), writes=(), sembuf=None):
        E = self.engs[qname]
        self._deps(qname, reads, writes)
        sb = sembuf or (writes[0] if writes else reads[0])
        key = sb.name.rsplit("_", 1)[0] if "_" in sb.name else sb.name
        ent = self.dpool.get(key)
        if ent is None:
            ent = [self.stack.enter_context(self.nc.semaphore("d%d" % self.ndsem)), 0]
            self.ndsem += 1
            self.dpool[key] = ent
        ins = E["eng"].dma_start(out=out_ap, in_=in_ap)
        ent[1] += 16
        ins.then_inc(ent[0], 16)
        self.n_ins += 1
        ev = (ent[0], ent[1], "dma")
        self._commit(ev, reads, writes)
        return ev

    def dmaf(self, qname, fn, reads=(), writes=(), sembuf=None):
        E = self.engs[qname]
        self._deps(qname, reads, writes)
        sb = sembuf or (writes[0] if writes else reads[0])
        key = sb.name.rsplit("_", 1)[0] if "_" in sb.name else sb.name
        ent = self.dpool.get(key)
        if ent is None:
            ent = [self.stack.enter_context(self.nc.semaphore("d%d" % self.ndsem)), 0]
            self.ndsem += 1
            self.dpool[key] = ent
        ins = fn(E["eng"])
        ent[1] += 16
        ins.then_inc(ent[0], 16)
        self.n_ins += 1
        ev = (ent[0], ent[1], "dma")
        self._commit(ev, reads, writes)
        return ev

    def barrier(self):
        evs = []
        for n, E in self.engs.items():
            if E["count"] > 0:
                evs.append((E["sem"], E["count"], "bar_" + n))
        for key, ent in self.dpool.items():
            if ent[1] > 0 and key not in self.nobar:
                evs.append((ent[0], ent[1], "dma"))
        for n in self.engs:
            for ev in evs:
                if ev[2] == "bar_" + n:
                    continue
                self._wait(n, ev)


def build():
    nc = bass.Bass("TRN2", target_bir_lowering=False)

    def din(name, shape, dt=F32):
        return nc.dram_tensor(name, shape, dt, kind="ExternalInput").ap()

    x = din("x", [NB, T, DM])
    pos = din("pos", [NB, T], I32)
    w_in = din("w_in", [DM, 1352])
    w_uq = din("w_uq", [256, 768])
    w_uk = din("w_uk", [128, 512])
    w_uv = din("w_uv", [128, 512])
    w_up_a = din("w_up_a", [512, DM])
    w_up_b = din("w_up_b", [512, DM])
    w_gate = din("w_gate", [DM, 2048])
    w_o = din("w_o", [DM, DM])
    w_r = din("w_r", [DM, 36])
    b_r = din("b_r", [36])
    w_eg = din("w_eg", [32 * 128, 2048])
    w_eu = din("w_eu", [32 * 128, 2048])
    w_ed = din("w_ed", [32 * 128, 2048])
    ustr = din("ustr", [128, 128])
    jv = din("jv", [128, 64])
    pcol = din("pcol", [128, 1])
    lnv = din("lnv", [6, DM])
    qng = din("qng", [128, 2])
    kvg = din("kvg", [128, 1])
    bgate = din("bgate", [128, 16])
    tzr = din("tzr", [128, 8, 2, 128])
    cfar = din("cfar", [8])
    identf = din("identf", [128, 128])
    invf = din("invf", [32, 1])
    out = nc.dram_tensor("out", [NB, T, DM], F32, kind="ExternalOutput").ap()
    hs = nc.dram_tensor("hs", [T, DM], F32, kind="Internal").ap()
    hbd = nc.dram_tensor("hbd", [T, DM], BF16, kind="Internal").ap()
    Hs = nc.dram_tensor("Hs", [64 * 128, DM], BF16, kind="Internal").ap()
    Ys = nc.dram_tensor("Ys", [64 * 128, DM], F32, kind="Internal").ap()
    weg_b = nc.dram_tensor("weg_b", [32 * 128, 2048], BF16, kind="Internal").ap()
    weu_b = nc.dram_tensor("weu_b", [32 * 128, 2048], BF16, kind="Internal").ap()
    wed_b = nc.dram_tensor("wed_b", [32 * 128, 2048], BF16, kind="Internal").ap()

    with ExitStack() as st0:
        S = Sched(nc, st0)
        hsB = Buf(hs, "hs")
        outB = Buf(out, "out")
        bcreg = st0.enter_context(nc.gpsimd.register("bcreg"))
        nc.gpsimd.reg_mov(bcreg, 32 * 128 - 1)
        hbdB = Buf(hbd, "hbd")
        HsB = Buf(Hs, "Hs")
        YsB = Buf(Ys, "Ys")

        identb = S.sbuf(st0, "identb", [128, 128], BF16)
        identF = S.sbuf(st0, "identF", [128, 128], F32)
        onesb = S.sbuf(st0, "onesb", [128, 128], BF16)
        lnbc = S.sbuf(st0, "lnbc", [128, 6, DM], F32)
        S.dma("pool", identb[:], identf, writes=[identb])
        ustrb = S.sbuf(st0, "ustrb", [128, 128], BF16)
        jvS = S.sbuf(st0, "jvS", [128, 64], F32)
        pcolS = S.sbuf(st0, "pcolS", [128, 1], F32)
        S.dma("pool", ustrb[:], ustr, writes=[ustrb])
        S.dma("sp", jvS[:], jv, writes=[jvS])
        S.dma("sp", pcolS[:], pcol, writes=[pcolS])
        WcB = Buf(None, "wcast")
        S.nobar.add("wcast")
        conv_list = [(srcw, dstw, e_) for e_ in range(32) for srcw, dstw in ((w_eg, weg_b), (w_eu, weu_b), (w_ed, wed_b))]

        def conv_some(n):
            for _ in range(n):
                if not conv_list:
                    return
                srcw, dstw, e_ = conv_list.pop(0)
                S.dma("pool", dstw[e_ * 128:(e_ + 1) * 128, :], srcw[e_ * 128:(e_ + 1) * 128, :], writes=[WcB])
        with ExitStack() as stz:
            zt = S.sbuf(stz, "zt", [128, 8, DM], BF16)
            S.op("pool", lambda e: e.memset(zt[:], 0.0), writes=[zt])
            for q in range(8):
                S.dma("sp", Hs[q * 1024:(q + 1) * 1024, :].rearrange("(p r) n -> p r n", p=128), zt[:], reads=[zt], writes=[HsB], sembuf=zt)
            S.barrier()
        S.dma("sp", identF[:], identf, writes=[identF])
        S.op("dve", lambda e: e.memset(onesb[:], 1.0), writes=[onesb])
        for k in range(6):
            S.dma("sp", lnbc[:, k, :], lnv[k].partition_broadcast(128), writes=[lnbc])

        def layer_norm_tile(st_bufs, src, dst_ap_fn, gi, out_bufs, scale_after=None):
            stats, mv, rstd, nmr, z = (st_bufs[k] for k in ("stats", "mv", "rstd", "nmr", "z"))
            S.op("dve", lambda e: e.bn_stats(stats[:, 0, :], src[:, 0:512]), reads=[src], writes=[stats])
            S.op("dve", lambda e: e.bn_stats(stats[:, 1, :], src[:, 512:1024]), reads=[src], writes=[stats])
            S.op("dve", lambda e: e.bn_aggr(mv[:], stats[:].rearrange("p a b -> p (a b)")), reads=[stats], writes=[mv])
            S.op("dve", lambda e: e.tensor_scalar(rstd[:], mv[:, 1:2], LN_EPS, None, ALU.add), reads=[mv], writes=[rstd])
            S.op("act", lambda e: e.activation(rstd[:], rstd[:], AF.Sqrt), reads=[rstd], writes=[rstd])
            S.op("dve", lambda e: e.reciprocal(rstd[:], rstd[:]), reads=[rstd], writes=[rstd])
            S.op("dve", lambda e: e.tensor_scalar(nmr[:], mv[:, 0:1], rstd[:, 0:1], -1.0, ALU.mult, ALU.mult),
                 reads=[mv, rstd], writes=[nmr])
            S.op("act", lambda e: e.activation(z[:], src[:], AF.Identity, bias=nmr[:, 0:1], scale=rstd[:, 0:1]),
                 reads=[src, nmr, rstd], writes=[z])
            S.op("dve", lambda e: e.tensor_tensor(z[:], z[:], lnbc[:, gi, :], ALU.mult), reads=[z, lnbc], writes=[z])

        for b in range(NB):
            stB = ExitStack()
            stB.__enter__()
            xnT = S.sbuf(stB, "xnT", [128, 8, T], BF16)
            ca = S.sbuf(stB, "ca", [128, NT], F32)
            cb = S.sbuf(stB, "cb", [128, NT], F32)
            M1 = S.sbuf(stB, "M1", [128, NT, 32], F32)
            M2 = S.sbuf(stB, "M2", [128, NT, 32], F32)
            Mb = S.sbuf(stB, "Mb", [128, NT, 32], BF16)
            with ExitStack() as stA:
                oaT = S.sbuf(stA, "oaT", [128, 4, T], BF16)
                obT = S.sbuf(stA, "obT", [128, 4, T], BF16)
                lns = dict(stats=S.sbuf(stA, "stats", [128, 2, 6], F32), mv=S.sbuf(stA, "mv", [128, 2], F32),
                           rstd=S.sbuf(stA, "rstd", [128, 1], F32), nmr=S.sbuf(stA, "nmr", [128, 1], F32),
                           z=S.sbuf(stA, "z", [128, DM], F32))
                xt = [S.sbuf(stA, "xt%d" % i, [128, DM], F32) for i in range(2)]

                with ExitStack() as st1:
                    xnb = [S.sbuf(st1, "xnb%d" % i, [128, DM], BF16) for i in range(2)]
                    ptr = [S.psum(st1, "ptr%d" % i, [128, 1024], BF16) for i in range(2)]
                    for i in range(NT):
                        xs = xt[i % 2]
                        S.dma("sp", xs[:], x[b, i * 128:(i + 1) * 128, :], writes=[xs])
                        layer_norm_tile(lns, xs, None, 0, None)
                        z = lns["z"]
                        xb = xnb[i % 2]
                        S.op("dve", lambda e: e.tensor_tensor(xb[:], z[:], lnbc[:, 1, :], ALU.add),
                             reads=[z, lnbc], writes=[xb])
                        pt = ptr[i % 2]
                        for c in range(8):
                            S.op("pe", lambda e: e.transpose(pt[:, c * 128:(c + 1) * 128], xb[:, c * 128:(c + 1) * 128], identb[:]),
                                 reads=[xb, identb], writes=[pt], inc=(c == 7))
                        S.op("act", lambda e: e.activation(xnT[:, :, i * 128:(i + 1) * 128],
                                                           pt[:].rearrange("p (c t) -> p c t", c=8), AF.Copy),
                             reads=[pt], writes=[xnT])
                    S.barrier()

                with ExitStack() as st2:
                    pp = [S.psum(st2, "pp%d" % i, [128, 512], F32) for i in range(3)]
                    ps = [S.psum(st2, "ps%d" % i, [128, 512], F32) for i in range(3)]
                    po = [S.psum(st2, "po%d" % i, [128, 512], F32) for i in range(2)]
                    Wa = S.sbuf(st2, "Wa", [128, 8, 416], BF16)
                    Wkrr = S.sbuf(st2, "Wkrr", [128, 8, 32], BF16)
                    Wq = S.sbuf(st2, "Wq", [128, 2, 8, 96], BF16)
                    Wqr = S.sbuf(st2, "Wqr", [128, 2, 8, 32], BF16)
                    Wk = S.sbuf(st2, "Wk", [128, 8, 64], BF16)
                    Wv = S.sbuf(st2, "Wv", [128, 512], BF16)
                    gq = S.sbuf(st2, "gq", [128, 2], F32)
                    gkv = S.sbuf(st2, "gkv", [128, 1], F32)
                    invfS = S.sbuf(st2, "invfS", [96, 1], F32)
                    cos32 = S.sbuf(st2, "cos32", [96, T], F32)
                    sin32 = S.sbuf(st2, "sin32", [96, T], F32)
                    cqn = S.sbuf(st2, "cqn", [128, 2, T], BF16)
                    ckvn = S.sbuf(st2, "ckvn", [128, T], BF16)
                    krT = S.sbuf(st2, "krT", [96, T], BF16)
                    st2a = ExitStack()
                    st2a.__enter__()
                    wq32 = S.sbuf(st2a, "wq32", [128, 2, 768], F32)
                    wk32 = S.sbuf(st2a, "wk32", [128, 512], F32)
                    wv32 = S.sbuf(st2a, "wv32", [128, 512], F32)
                    posi = S.sbuf(st2a, "posi", [96, T], I32)
                    rr = S.sbuf(st2a, "rr", [96, T], F32)
                    rf = S.sbuf(st2a, "rf", [96, T], F32)
                    tq = S.sbuf(st2a, "tq", [96, T], F32)

                    wsrc = w_in.rearrange("(c p) n -> p c n", p=128)
                    S.dma("pool", Wa[:], wsrc[:, :, 0:416], writes=[Wa])
                    S.dma("sp", wq32[:], w_uq.rearrange("(c p) n -> p c n", p=128), writes=[wq32])
                    S.dma("sp", wk32[:], w_uk, writes=[wk32])
                    S.dma("sp", wv32[:], w_uv, writes=[wv32])
                    S.dma("sp", gq[:], qng, writes=[gq])
                    S.dma("sp", gkv[:], kvg, writes=[gkv])
                    S.dma("sp", invfS[64:96, :], invf, writes=[invfS])
                    S.dma("sp", posi[64:96, :], pos[b].partition_broadcast(32), writes=[posi])
                    S.op("dve", lambda e: e.tensor_scalar(Wkrr[:, :, 0:16], Wa[:, :, 400:416], -1.0, None, ALU.mult),
                         reads=[Wa], writes=[Wkrr])
                    S.op("dve", lambda e: e.tensor_copy(Wkrr[:, :, 16:32], Wa[:, :, 384:400]), reads=[Wa], writes=[Wkrr])
                    for c in range(2):
                        src = wq32[:, c, :].rearrange("p (h d) -> p h d", h=8)
                        g = gq[:, c:c + 1]
                        S.op("dve", lambda e: e.tensor_scalar(Wq[:, c, :, :], src[:, :, :], g, None, ALU.mult),
                             reads=[wq32, gq], writes=[Wq])
                        S.op("dve", lambda e: e.tensor_scalar(Wqr[:, c, :, 0:16], src[:, :, 80:96], g, -1.0, ALU.mult, ALU.mult),
                             reads=[wq32, gq], writes=[Wqr])
                        S.op("dve", lambda e: e.tensor_scalar(Wqr[:, c, :, 16:32], src[:, :, 64:80], g, None, ALU.mult),
                             reads=[wq32, gq], writes=[Wqr])
                    S.op("dve", lambda e: e.tensor_scalar(Wk[:, :, :], wk32[:].rearrange("p (h d) -> p h d", h=8),
                                                          gkv[:, 0:1], None, ALU.mult), reads=[wk32, gkv], writes=[Wk])
                    S.op("dve", lambda e: e.tensor_scalar(Wv[:], wv32[:], gkv[:, 0:1], None, ALU.mult),
                         reads=[wv32, gkv], writes=[Wv])

                    S.op("dve", lambda e: e.tensor_copy(rr[64:96, :], posi[64:96, :]), reads=[posi], writes=[rr])
                    S.op("dve", lambda e: e.tensor_scalar(rr[64:96, :], rr[64:96, :], invfS[64:96, 0:1], None, ALU.mult), reads=[rr, invfS], writes=[rr])
                    S.op("dve", lambda e: e.tensor_copy(posi[64:96, :], rr[64:96, :]), reads=[rr], writes=[posi])
                    S.op("dve", lambda e: e.tensor_copy(rf[64:96, :], posi[64:96, :]), reads=[posi], writes=[rf])
                    S.op("dve", lambda e: e.tensor_tensor(rr[64:96, :], rr[64:96, :], rf[64:96, :], ALU.subtract), reads=[rr, rf], writes=[rr])

                    def wrap_sin(dst, shift):
                        S.op("dve", lambda e: e.tensor_scalar(rf[64:96, :], rr[64:96, :], shift, None, ALU.add), reads=[rr], writes=[rf])
                        for _ in range(2):
                            S.op("dve", lambda e: e.tensor_scalar(tq[64:96, :], rf[64:96, :], 0.5, None, ALU.is_gt), reads=[rf], writes=[tq])
                            S.op("dve", lambda e: e.tensor_tensor(rf[64:96, :], rf[64:96, :], tq[64:96, :], ALU.subtract), reads=[rf, tq], writes=[rf])
                        S.op("dve", lambda e: e.tensor_scalar(tq[64:96, :], rf[64:96, :], -0.5, None, ALU.is_lt), reads=[rf], writes=[tq])
                        S.op("dve", lambda e: e.tensor_tensor(rf[64:96, :], rf[64:96, :], tq[64:96, :], ALU.add), reads=[rf, tq], writes=[rf])
                        S.op("act", lambda e: e.activation(dst[64:96, :], rf[64:96, :], AF.Sin, scale=2.0 * math.pi * (1.0 - 2e-6)),
                             reads=[rf], writes=[dst])

                    wrap_sin(sin32, 0.0)
                    wrap_sin(cos32, 0.25)
                    S.barrier()
                    st2a.close()
                    Vh = [S.sbuf(st2, "Vh%d" % i, [128, NT, 128], BF16) for i in range(2)]
                    qTs = [S.sbuf(st2, "qT%d" % i, [96, T], BF16) for i in range(2)]
                    kTs = [S.sbuf(st2, "kT%d" % i, [96, T], BF16) for i in range(2)]
                    c32 = S.sbuf(st2, "c32", [128, 2, 512], F32)
                    sqb = S.sbuf(st2, "sqb", [128, 2, 512], BF16)
                    rq = S.sbuf(st2, "rq", [128, 512], F32)
                    t1 = S.sbuf(st2, "t1", [96, 512], F32)
                    t2 = S.sbuf(st2, "t2", [96, 512], F32)
                    PTs = [S.sbuf(st2, "PT%d" % i, [128, 512], BF16) for i in range(3)]
                    rd = S.sbuf(st2, "rd", [128, 512], F32)
                    for i in range(2):
                        S.op("pool", lambda e: e.memset(Vh[i][:], 1.0), writes=[Vh[i]])

                    def rms_block(psrc_list, dstT_fn, nfeat, tb):
                        n = len(psrc_list)
                        for m, pb in enumerate(psrc_list):
                            S.op("act", lambda e: e.activation(c32[:, m, :], pb[:], AF.Copy), reads=[pb], writes=[c32])
                            S.op("act", lambda e: e.activation(sqb[:, m, :], pb[:], AF.Square), reads=[pb], writes=[sqb])
                        pss = pp[2]
                        for m in range(n):
                            S.op("pe", lambda e: e.matmul(pss[:], onesb[:], sqb[:, m, :], start=(m == 0), stop=(m == n - 1)),
                                 reads=[onesb, sqb], writes=[pss], inc=(m == n - 1))
                        S.op("dve", lambda e: e.tensor_scalar(rq[:], pss[:], 1.0 / nfeat, RMS_EPS, ALU.mult, ALU.add),
                             reads=[pss], writes=[rq])
                        S.op("act", lambda e: e.activation(rq[:], rq[:], AF.Sqrt), reads=[rq], writes=[rq])
                        S.op("dve", lambda e: e.reciprocal(rq[:], rq[:]), reads=[rq], writes=[rq])
                        for m in range(n):
                            S.op("dve", lambda e: e.tensor_tensor(dstT_fn(m), c32[:, m, :], rq[:], ALU.mult),
                                 reads=[c32, rq], writes=[dstT_fn.buf])

                    def proj_fm(pb, lhs_fn, tb, M=128, p0=0):
                        for c in range(8):
                            S.op("pe", lambda e: e.matmul(pb[p0:p0 + M, :], lhs_fn(c), xnT[:, c, tb * 512:(tb + 1) * 512],
                                                          start=(c == 0), stop=(c == 7)),
                                 reads=[xnT, Wa, Wkrr], writes=[pb], inc=(c == 7))

                    for tb in range(4):
                        cols = slice(tb * 512, (tb + 1) * 512)
                        proj_fm(pp[0], lambda c: Wa[:, c, 0:128], tb)
                        proj_fm(pp[1], lambda c: Wa[:, c, 128:256], tb)
                        f = lambda m: cqn[:, m, cols]
                        f.buf = cqn
                        rms_block([pp[0], pp[1]], f, 256.0, tb)
                        proj_fm(pp[0], lambda c: Wa[:, c, 256:384], tb)
                        f2 = lambda m: ckvn[:, cols]
                        f2.buf = ckvn
                        rms_block([pp[0]], f2, 128.0, tb)
                        proj_fm(pp[0], lambda c: Wa[:, c, 384:416], tb, M=32, p0=64)
                        proj_fm(pp[1], lambda c: Wkrr[:, c, :], tb, M=32, p0=64)
                        S.op("dve", lambda e: e.tensor_tensor(t1[64:96, :], pp[0][64:96, :], cos32[64:96, cols], ALU.mult),
                             reads=[pp[0], cos32], writes=[t1])
                        S.op("dve", lambda e: e.tensor_tensor(t2[64:96, :], pp[1][64:96, :], sin32[64:96, cols], ALU.mult),
                             reads=[pp[1], sin32], writes=[t2])
                        S.op("dve", lambda e: e.tensor_tensor(krT[64:96, cols], t1[64:96, :], t2[64:96, :], ALU.add), reads=[t1, t2], writes=[krT])
                    sc_mla = 96.0 ** -0.5
                    LA = 2

                    def mla_proj(h):
                        qT = qTs[h % 2]
                        kT = kTs[h % 2]
                        for tb in range(4):
                            cols = slice(tb * 512, (tb + 1) * 512)
                            pa, pbb = pp[0], pp[1]
                            for m in range(2):
                                S.op("pe", lambda e: e.matmul(pa[0:96, :], Wq[:, m, h, :], cqn[:, m, cols], start=(m == 0), stop=(m == 1)),
                                     reads=[Wq, cqn], writes=[pa], inc=(m == 1))
                            for m in range(2):
                                S.op("pe", lambda e: e.matmul(pbb[64:96, :], Wqr[:, m, h, :], cqn[:, m, cols], start=(m == 0), stop=(m == 1)),
                                     reads=[Wqr, cqn], writes=[pbb], inc=(m == 1))
                            pk = pp[2]
                            S.op("pe", lambda e: e.matmul(pk[0:64, :], Wk[:, h, :], ckvn[:, cols], start=True, stop=True),
                                 reads=[Wk, ckvn], writes=[pk])
                            S.op("dve", lambda e: e.tensor_tensor(t1[64:96, :], pa[64:96, :], cos32[64:96, cols], ALU.mult), reads=[pa, cos32], writes=[t1])
                            S.op("dve", lambda e: e.tensor_tensor(t2[64:96, :], pbb[64:96, :], sin32[64:96, cols], ALU.mult), reads=[pbb, sin32], writes=[t2])
                            S.op("dve", lambda e: e.tensor_tensor(qT[64:96, cols], t1[64:96, :], t2[64:96, :], ALU.add), reads=[t1, t2], writes=[qT])
                            S.op("act", lambda e: e.activation(qT[0:64, cols], pa[0:64, :], AF.Copy), reads=[pa], writes=[qT])
                            S.op("act", lambda e: e.activation(kT[0:64, cols], pk[0:64, :], AF.Copy), reads=[pk], writes=[kT])
                        S.op("pool", lambda e: e.tensor_copy(kT[64:96, :], krT[64:96, :]), reads=[krT], writes=[kT])
                        Vc = Vh[h % 2]
                        voff = 64 * (h % 2)
                        for k4 in range(4):
                            pv = pp[k4 % 2]
                            for j in range(4):
                                kt = k4 * 4 + j
                                S.op("pe", lambda e: e.matmul(pv[:, j * 64:(j + 1) * 64], ckvn[:, kt * 128:(kt + 1) * 128],
                                                              Wv[:, h * 64:(h + 1) * 64], start=True, stop=True),
                                     reads=[ckvn, Wv], writes=[pv], inc=(j == 3))
                            S.op("act", lambda e: e.activation(Vc[:, k4 * 4:(k4 + 1) * 4, voff:voff + 64],
                                                               pv[:, 0:256].rearrange("p (j d) -> p j d", j=4), AF.Copy),
                                 reads=[pv], writes=[Vc])

                    st_ = dict(si=0, oi=0)

                    def mla_attn(h):
                        conv_some(6)
                        qT = qTs[h % 2]
                        kT = kTs[h % 2]
                        Vc = Vh[h % 2]
                        for qb in range(4):
                            pO = po[st_["oi"] % 2]
                            st_["oi"] += 1
                            nk = 4 * qb + 4
                            slots = {}

                            def emit_S(kt):
                                c0 = max(0, kt - 4 * qb) * 128
                                sl = st_["si"] % 3
                                st_["si"] += 1
                                slots[kt] = sl
                                pS, PT = ps[sl], PTs[sl]
                                S.op("pe", lambda e: e.matmul(pS[:, c0:512], kT[:, kt * 128:(kt + 1) * 128],
                                                              qT[:, qb * 512 + c0:(qb + 1) * 512], start=True, stop=True),
                                     reads=[kT, qT], writes=[pS])
                                S.op("act", lambda e: e.activation(PT[:, c0:512], pS[:, c0:512], AF.Exp, scale=sc_mla),
                                     reads=[pS], writes=[PT])
                                if kt >= 4 * qb:
                                    S.op("pool", lambda e: e.memset(PT[64:128, c0:c0 + 64], 0.0), writes=[PT])

                            def emit_PV(kt):
                                c0 = max(0, kt - 4 * qb) * 128
                                PT = PTs[slots[kt]]
                                S.op("pe", lambda e: e.matmul(pO[:, c0:512], Vc[:, kt, :], PT[:, c0:512],
                                                              start=(kt == 0), stop=(kt == nk - 1), skip_group_check=True),
                                     reads=[Vc, PT], writes=[pO], inc=(kt == nk - 1))

                            for s_ in range(nk + LA):
                                if s_ < nk:
                                    emit_S(s_)
                                if s_ >= LA:
                                    emit_PV(s_ - LA)
                            ocols = slice(qb * 512, (qb + 1) * 512)
                            if h % 2 == 0:
                                S.op("dve", lambda e: e.reciprocal(rd[0:64, :], pO[64:128, :]), reads=[pO], writes=[rd])
                                S.op("dve", lambda e: e.tensor_tensor(oaT[0:64, h // 2, ocols], pO[0:64, :], rd[0:64, :], ALU.mult),
                                     reads=[pO, rd], writes=[oaT])
                            else:
                                S.op("dve", lambda e: e.reciprocal(rd[64:128, :], pO[0:64, :]), reads=[pO], writes=[rd])
                                S.op("dve", lambda e: e.tensor_tensor(oaT[64:128, h // 2, ocols], pO[64:128, :], rd[64:128, :], ALU.mult),
                                     reads=[pO, rd], writes=[oaT])

                    mla_proj(0)
                    for h in range(8):
                        if h + 1 < 8:
                            mla_proj(h + 1)
                        mla_attn(h)
                    S.barrier()

                with ExitStack() as st3:
                    pp = [S.psum(st3, "qp%d" % i, [128, 512], F32) for i in range(2)]
                    ps = [S.psum(st3, "qs%d" % i, [128, 512], F32) for i in range(3)]
                    po = [S.psum(st3, "qo%d" % i, [128, 512], F32) for i in range(2)]
                    pmt = S.psum(st3, "pmt", [128, 1024], BF16)
                    qbT = S.sbuf(st3, "qbT", [128, 4, T], BF16)
                    kbT2 = S.sbuf(st3, "kbT2", [128, T], BF16)
                    qiT = S.sbuf(st3, "qiT", [128, 2, T], BF16)
                    kiT4 = S.sbuf(st3, "kiT4", [128, T], BF16)
                    Vb = S.sbuf(st3, "Vb", [128, NT, 2, 128], BF16)
                    widx = S.sbuf(st3, "widx", [128, NT, 8], F32)
                    tz = S.sbuf(st3, "tz", [128, 8, 2, 128], F32)
                    cfb = S.sbuf(st3, "cfb", [128, 8], F32)
                    st3a = ExitStack()
                    st3a.__enter__()
                    Wb = S.sbuf(st3a, "Wb", [128, 8, 936], BF16)
                    Wkb2 = S.sbuf(st3a, "Wkb2", [128, 8, 128], BF16)
                    Wki4 = S.sbuf(st3a, "Wki4", [128, 8, 128], BF16)

                    wsrc = w_in.rearrange("(c p) n -> p c n", p=128)
                    S.dma("pool", Wb[:], wsrc[:, :, 416:1352], writes=[Wb])
                    for r in range(2):
                        S.dma("pool", Wkb2[:, :, r * 64:(r + 1) * 64], wsrc[:, :, 928:992], writes=[Wkb2])
                    for r in range(4):
                        S.dma("pool", Wki4[:, :, r * 32:(r + 1) * 32], wsrc[:, :, 1312:1344], writes=[Wki4])
                    S.dma("sp", tz[:], tzr, writes=[tz])
                    S.dma("sp", cfb[:], cfar.partition_broadcast(128), writes=[cfb])
                    for h in range(8):
                        S.op("dve", lambda e: e.tensor_scalar(tz[:, h], tz[:, h], cfb[:, h:h + 1], 8.0, ALU.subtract, ALU.mult),
                             reads=[tz, cfb], writes=[tz])
                    S.op("pool", lambda e: e.memset(Vb[:], 1.0), writes=[Vb])

                    def proj3(dst_ap, dstbuf, lhs_fn, wbuf, tb, k):
                        pb = pp[k % 2]
                        for c in range(8):
                            S.op("pe", lambda e: e.matmul(pb[:], lhs_fn(c), xnT[:, c, tb * 512:(tb + 1) * 512],
                                                          start=(c == 0), stop=(c == 7)), reads=[xnT, wbuf], writes=[pb], inc=(c == 7))
                        S.op("act", lambda e: e.activation(dst_ap, pb[:], AF.Copy), reads=[pb], writes=[dstbuf])

                    k = 0
                    for tb in range(4):
                        cols = slice(tb * 512, (tb + 1) * 512)
                        for p in range(4):
                            proj3(qbT[:, p, cols], qbT, lambda c: Wb[:, c, p * 128:(p + 1) * 128], Wb, tb, k); k += 1
                        proj3(kbT2[:, cols], kbT2, lambda c: Wkb2[:, c, :], Wkb2, tb, k); k += 1
                        for g in range(2):
                            proj3(qiT[:, g, cols], qiT, lambda c: Wb[:, c, 640 + g * 128:640 + (g + 1) * 128], Wb, tb, k); k += 1
                        proj3(kiT4[:, cols], kiT4, lambda c: Wki4[:, c, :], Wki4, tb, k); k += 1
                    for kt in range(NT):
                        pb = pp[kt % 2]
                        tsl = slice(kt * 128, (kt + 1) * 128)
                        for c in range(8):
                            S.op("pe", lambda e: e.matmul(pb[:, 0:64], xnT[:, c, tsl], Wb[:, c, 576:640], start=(c == 0), stop=(c == 7)),
                                 reads=[xnT, Wb], writes=[pb], inc=(c == 7))
                        for c in range(8):
                            S.op("pe", lambda e: e.matmul(pb[:, 64:72], xnT[:, c, tsl], Wb[:, c, 928:936], start=(c == 0), stop=(c == 7)),
                                 reads=[xnT, Wb], writes=[pb], inc=(c == 7))
                        S.op("act", lambda e: e.activation(Vb[:, kt, 0, 0:64], pb[:, 0:64], AF.Copy), reads=[pb], writes=[Vb])
                        S.op("act", lambda e: e.activation(Vb[:, kt, 1, 64:128], pb[:, 0:64], AF.Copy), reads=[pb], writes=[Vb])
                        S.op("act", lambda e: e.activation(widx[:, kt, :], pb[:, 64:72], AF.Copy, scale=0.0625), reads=[pb], writes=[widx])

                    S.barrier()
                    st3a.close()
                    Sc = [S.sbuf(st3, "Sc%d" % i, [128, T], F32) for i in range(2)]
                    msk = [S.sbuf(st3, "msk%d" % i, [128, T], BF16) for i in range(2)]
                    mskT = S.sbuf(st3, "mskT", [128, NT, 512], BF16)
                    rl = [S.sbuf(st3, "rl%d" % i, [128, 512], F32) for i in range(2)]
                    bmx = S.sbuf(st3, "bmx", [128, 1], F32)
                    bmn = S.sbuf(st3, "bmn", [128, 1], F32)
                    brg = S.sbuf(st3, "brg", [128, 1], F32)
                    bmid = S.sbuf(st3, "bmid", [128, 1], F32)
                    bcnt = S.sbuf(st3, "bcnt", [128, 1], F32)
                    bt = S.sbuf(st3, "bt", [128, 1], F32)
                    Es = [S.sbuf(st3, "E%d" % i, [128, 512], BF16) for i in range(3)]
                    PTs = [S.sbuf(st3, "PTb%d" % i, [128, 512], BF16) for i in range(3)]
                    rd = S.sbuf(st3, "rdb", [128, 512], F32)
                    sidx = [0]
                    oi = 0

                    def idx_steps(qt):
                        n = (qt + 1) * 128
                        sc = Sc[qt % 2]
                        tsl = slice(qt * 128, (qt + 1) * 128)
                        steps = []
                        for hh in range(8):
                            for sb in range((n + 511) // 512):
                                def step(hh=hh, sb=sb):
                                    g, jj = hh // 4, hh % 4
                                    w = min(512, n - sb * 512)
                                    k = ridx[0]
                                    ridx[0] += 1
                                    pr = pp[k % 2]
                                    rlb = rl[k % 2]
                                    S.op("pe", lambda e: e.matmul(pr[:, 0:w], qiT[32 * jj:32 * jj + 32, g, tsl],
                                                                  kiT4[32 * jj:32 * jj + 32, sb * 512:sb * 512 + w],
                                                                  start=True, stop=True, tile_position=(32 * jj, 0)),
                                         reads=[qiT, kiT4], writes=[pr])
                                    S.op("act", lambda e: e.activation(rlb[:, 0:w], pr[:, 0:w], AF.Relu), reads=[pr], writes=[rlb])
                                    dst = sc[:, sb * 512:sb * 512 + w]
                                    if hh == 0:
                                        S.op("dve", lambda e: e.tensor_scalar(dst, rlb[:, 0:w], widx[:, qt, 0:1], None, ALU.mult),
                                             reads=[rlb, widx], writes=[sc])
                                    else:
                                        S.op("dve", lambda e: e.scalar_tensor_tensor(dst, rlb[:, 0:w], widx[:, qt, hh:hh + 1], dst,
                                                                                     ALU.mult, ALU.add),
                                             reads=[rlb, widx, sc], writes=[sc])
                                steps.append(step)
                        return steps

                    def bisect(qt, filler):
                        n = (qt + 1) * 128
                        sc = Sc[qt % 2]
                        mk = msk[qt % 2]
                        S.op("pool", lambda e: e.memset(sc[0:64, n - 64:n], NEGBIG), writes=[sc])
                        per = (len(filler) + BIS_ITERS - 1) // BIS_ITERS if qt >= 2 else len(filler)
                        if qt >= 2:
                            S.op("dve", lambda e: e.reduce_max(bmx[:], sc[:, 0:n], AX.X), reads=[sc], writes=[bmx])
                            S.op("dve", lambda e: e.tensor_reduce(bmn[:], sc[:, 0:n - 64], AX.X, ALU.min), reads=[sc], writes=[bmn])
                            S.op("dve", lambda e: e.tensor_tensor(brg[:], bmx[:], bmn[:], ALU.subtract), reads=[bmx, bmn], writes=[brg])
                            S.op("dve", lambda e: e.tensor_scalar(brg[:], brg[:], 1e-20, None, ALU.add), reads=[brg], writes=[brg])
                            S.op("dve", lambda e: e.reciprocal(brg[:], brg[:]), reads=[brg], writes=[brg])
                            S.op("dve", lambda e: e.tensor_scalar(sc[:, 0:n], sc[:, 0:n], bmn[:, 0:1], brg[:, 0:1], ALU.subtract, ALU.mult),
                                 reads=[sc, bmn, brg], writes=[sc])
                            S.op("dve", lambda e: e.memset(bmid[:], 0.5), writes=[bmid])
                            for it in range(BIS_ITERS):
                                S.op("dve", lambda e: e.tensor_scalar(mk[:, 0:n], sc[:, 0:n], bmid[:, 0:1], None, ALU.is_ge, ALU.add,
                                                                      accum_out=bcnt[:]),
                                     reads=[sc, bmid], writes=[mk, bcnt])
                                for _ in range(per):
                                    if filler:
                                        filler.pop(0)()
                                s_next = 2.0 ** -(it + 2)
                                S.op("dve", lambda e: e.tensor_scalar(bt[:], bcnt[:], float(TOPK), 2.0 * s_next, ALU.is_ge, ALU.mult),
                                     reads=[bcnt], writes=[bt])
                                S.op("dve", lambda e: e.scalar_tensor_tensor(bmid[:], bt[:], -s_next, bmid[:], ALU.add, ALU.add),
                                     reads=[bt, bmid], writes=[bmid])
                            S.op("dve", lambda e: e.tensor_scalar(bmid[:], bmid[:], -(2.0 ** -(BIS_ITERS + 1)), None, ALU.add),
                                 reads=[bmid], writes=[bmid])
                            S.op("dve", lambda e: e.tensor_scalar(mk[:, 0:n], sc[:, 0:n], bmid[:, 0:1], None, ALU.is_ge),
                                 reads=[sc, bmid], writes=[mk])
                        else:
                            S.op("dve", lambda e: e.tensor_scalar(mk[:, 0:n], sc[:, 0:n], -1.0e29, None, ALU.is_ge),
                                 reads=[sc], writes=[mk])
                        while filler:
                            filler.pop(0)()

                    def mask_transpose(qt):
                        mk = msk[qt % 2]
                        j = qt % 4
                        for k0 in range(0, qt + 1, 8):
                            nk_ = min(8, qt + 1 - k0)
                            for q in range(nk_):
                                kt = k0 + q
                                S.op("pe", lambda e: e.transpose(pmt[:, q * 128:(q + 1) * 128], mk[:, kt * 128:(kt + 1) * 128], identb[:]),
                                     reads=[mk, identb], writes=[pmt], inc=(q == nk_ - 1))
                            S.op("act", lambda e: e.activation(mskT[:, k0:k0 + nk_, j * 128:(j + 1) * 128],
                                                               pmt[:, 0:nk_ * 128].rearrange("p (q t) -> p q t", q=nk_), AF.Copy),
                                 reads=[pmt], writes=[mskT])

                    ridx = [0]
                    for st_ in idx_steps(0):
                        st_()
                    for qt_ in range(NT):
                        filler = idx_steps(qt_ + 1) if qt_ + 1 < NT else []
                        bisect(qt_, filler)
                        mask_transpose(qt_)
                        if qt_ % 4 != 3:
                            continue
                        qb = qt_ // 4
                        for h in range(8):
                            conv_some(2)
                            p, hf = h // 2, h % 2
                            base = 64 * hf
                            pO = po[oi % 2]
                            oi += 1
                            nk = 4 * qb + 4
                            slots = {}

                            def emit_S(kt):
                                global_si = sidx[0]
                                sidx[0] += 1
                                sl = global_si % 3
                                slots[kt] = sl
                                j0 = max(0, kt - 4 * qb)
                                c0 = j0 * 128
                                pS, E, PT = ps[sl], Es[sl], PTs[sl]
                                S.op("pe", lambda e: e.matmul(pS[:, c0:512], kbT2[base:base + 64, kt * 128:(kt + 1) * 128],
                                                              qbT[base:base + 64, p, qb * 512 + c0:(qb + 1) * 512], start=True, stop=True),
                                     reads=[kbT2, qbT], writes=[pS])
                                for j in range(j0, 4):
                                    d = 4 * qb + j - kt
                                    if d in (0, 1):
                                        S.op("dve", lambda e: e.tensor_tensor(pS[:, j * 128:(j + 1) * 128], pS[:, j * 128:(j + 1) * 128],
                                                                              tz[:, h, d, :], ALU.add), reads=[pS, tz], writes=[pS])
                                S.op("act", lambda e: e.activation(E[:, c0:512], pS[:, c0:512], AF.Exp, bias=cfb[:, h:h + 1], scale=0.125),
                                     reads=[pS, cfb], writes=[E])
                                S.op("dve", lambda e: e.tensor_tensor(PT[:, c0:512], E[:, c0:512], mskT[:, kt, c0:512], ALU.mult),
                                     reads=[E, mskT], writes=[PT])

                            def emit_PV(kt):
                                c0 = max(0, kt - 4 * qb) * 128
                                PT = PTs[slots[kt]]
                                S.op("pe", lambda e: e.matmul(pO[:, c0:512], Vb[:, kt, hf, :], PT[:, c0:512],
                                                              start=(kt == 0), stop=(kt == nk - 1), skip_group_check=True),
                                     reads=[Vb, PT], writes=[pO], inc=(kt == nk - 1))

                            for s_ in range(nk + 2):
                                if s_ < nk:
                                    emit_S(s_)
                                if s_ >= 2:
                                    emit_PV(s_ - 2)
                            ocols = slice(qb * 512, (qb + 1) * 512)
                            if hf == 0:
                                S.op("dve", lambda e: e.reciprocal(rd[0:64, :], pO[64:128, :]), reads=[pO], writes=[rd])
                                S.op("dve", lambda e: e.tensor_tensor(obT[0:64, p, ocols], pO[0:64, :], rd[0:64, :], ALU.mult),
                                     reads=[pO, rd], writes=[obT])
                            else:
                                S.op("dve", lambda e: e.reciprocal(rd[64:128, :], pO[0:64, :]), reads=[pO], writes=[rd])
                                S.op("dve", lambda e: e.tensor_tensor(obT[64:128, p, ocols], pO[64:128, :], rd[64:128, :], ALU.mult),
                                     reads=[pO, rd], writes=[obT])
                    S.barrier()

                with ExitStack() as st4:
                    Wo = S.sbuf(st4, "Wo", [128, 8, DM], BF16)
                    mixT = S.sbuf(st4, "mixT", [128, 8, T], BF16)
                    bg = S.sbuf(st4, "bg", [128, 16], F32)
                    Wr = S.sbuf(st4, "Wr", [128, 8, 36], F32)
                    brb = S.sbuf(st4, "brb", [128, 36], F32)
                    st4a = ExitStack()
                    st4a.__enter__()
                    Wua = S.sbuf(st4a, "Wua", [128, 4, DM], BF16)
                    Wub = S.sbuf(st4a, "Wub", [128, 4, DM], BF16)
                    Wg = [S.sbuf(st4a, "Wg%d" % i, [128, 8, 2, 128], BF16) for i in range(2)]
                    sga = S.sbuf(st4a, "sga", [128, 512], F32)
                    sgb = S.sbuf(st4a, "sgb", [128, 512], F32)
                    m1 = S.sbuf(st4a, "m1", [128, 512], F32)
                    m2 = S.sbuf(st4a, "m2", [128, 512], F32)
                    pgs = [[S.psum(st4a, "pg%d_%d" % (q, i), [128, 512], F32) for i in range(4)] for q in range(2)]
                    sgas = [sga, S.sbuf(st4a, "sga2", [128, 512], F32)]
                    sgbs = [sgb, S.sbuf(st4a, "sgb2", [128, 512], F32)]
                    m1s = [m1, S.sbuf(st4a, "m1b", [128, 512], F32)]
                    m2s = [m2, S.sbuf(st4a, "m2b", [128, 512], F32)]
                    git = 0

                    S.dma("pool", Wua[:], w_up_a.rearrange("(c p) n -> p c n", p=128), writes=[Wua])
                    S.dma("pool", Wub[:], w_up_b.rearrange("(c p) n -> p c n", p=128), writes=[Wub])
                    S.dma("pool", Wo[:], w_o.rearrange("(c p) n -> p c n", p=128), writes=[Wo])
                    S.dma("sp", bg[:], bgate, writes=[bg])
                    S.dma("sp", Wr[:], w_r.rearrange("(c p) n -> p c n", p=128), writes=[Wr])
                    S.dma("sp", brb[:], b_r.partition_broadcast(128), writes=[brb])
                    gsrc = w_gate.rearrange("(c p) n -> p c n", p=128)
                    for m in range(8):
                        wg = Wg[m % 2]
                        S.dma("pool", wg[:, :, 0, :], gsrc[:, :, m * 128:(m + 1) * 128], writes=[wg])
                        S.dma("pool", wg[:, :, 1, :], gsrc[:, :, 1024 + m * 128:1024 + (m + 1) * 128], writes=[wg])
                        for tb in range(4):
                            cols = slice(tb * 512, (tb + 1) * 512)
                            pg = pgs[git % 2]
                            sga, sgb, m1, m2 = sgas[git % 2], sgbs[git % 2], m1s[git % 2], m2s[git % 2]
                            git += 1
                            for c in range(8):
                                S.op("pe", lambda e: e.matmul(pg[0][:], wg[:, c, 0, :], xnT[:, c, cols], start=(c == 0), stop=(c == 7)),
                                     reads=[wg, xnT], writes=[pg[0]], inc=(c == 7))
                            for c in range(8):
                                S.op("pe", lambda e: e.matmul(pg[1][:], wg[:, c, 1, :], xnT[:, c, cols], start=(c == 0), stop=(c == 7)),
                                     reads=[wg, xnT], writes=[pg[1]], inc=(c == 7))
                            for c in range(4):
                                S.op("pe", lambda e: e.matmul(pg[2][:], Wua[:, c, m * 128:(m + 1) * 128], oaT[:, c, cols], start=(c == 0), stop=(c == 3)),
                                     reads=[Wua, oaT], writes=[pg[2]], inc=(c == 3))
                            for c in range(4):
                                S.op("pe", lambda e: e.matmul(pg[3][:], Wub[:, c, m * 128:(m + 1) * 128], obT[:, c, cols], start=(c == 0), stop=(c == 3)),
                                     reads=[Wub, obT], writes=[pg[3]], inc=(c == 3))
                            S.op("act", lambda e: e.activation(sga[:], pg[0][:], AF.Sigmoid, bias=bg[:, m:m + 1]), reads=[pg[0], bg], writes=[sga])
                            S.op("act", lambda e: e.activation(sgb[:], pg[1][:], AF.Sigmoid, bias=bg[:, 8 + m:9 + m]), reads=[pg[1], bg], writes=[sgb])
                            S.op("dve", lambda e: e.tensor_tensor(m1[:], pg[2][:], sga[:], ALU.mult), reads=[pg[2], sga], writes=[m1])
                            S.op("dve", lambda e: e.tensor_tensor(m2[:], pg[3][:], sgb[:], ALU.mult), reads=[pg[3], sgb], writes=[m2])
                            S.op("dve", lambda e: e.tensor_tensor(mixT[:, m, cols], m1[:], m2[:], ALU.add), reads=[m1, m2], writes=[mixT])
                    S.barrier()
                    st4a.close()
                    pg = [S.psum(st4, "pgt%d" % i, [128, 512], F32) for i in range(2)]
                    pm = [S.psum(st4, "pm%d" % i, [128, 512], F32) for i in range(2)]
                    pl = S.psum(st4, "pl", [128, 512], F32)
                    pre = S.sbuf(st4, "pre", [128, DM], F32)
                    hbs = [S.sbuf(st4, "hb%d" % i, [128, DM], BF16) for i in range(2)]
                    hst = [S.sbuf(st4, "hst%d" % i, [128, DM], F32) for i in range(2)]
                    hT32 = S.sbuf(st4, "hT32", [128, 8, 128], F32)
                    lgA = S.sbuf(st4, "lgA", [128, NT, 36], F32)
                    gmxA = S.sbuf(st4, "gmxA", [128, NT], F32)
                    gselA = S.sbuf(st4, "gselA", [128, NT, 4], F32)
                    r4A = S.sbuf(st4, "r4A", [128, NT, 4], F32)
                    gwA = S.sbuf(st4, "gwA", [128, NT], F32)
                    le4 = S.sbuf(st4, "le4", [128, NT, 4, 8], F32)
                    seA = S.sbuf(st4, "seA", [128, NT, 8], F32)
                    se2A = S.sbuf(st4, "se2A", [128, NT, 8], F32)
                    oh1A = S.sbuf(st4, "oh1A", [128, NT, 8], F32)
                    oh2A = S.sbuf(st4, "oh2A", [128, NT, 8], F32)
                    mx1A = S.sbuf(st4, "mx1A", [128, NT], F32)
                    mx2A = S.sbuf(st4, "mx2A", [128, NT], F32)
                    w1A = S.sbuf(st4, "w1A", [128, NT], F32)
                    w2A = S.sbuf(st4, "w2A", [128, NT], F32)
                    lnsB = [dict(stats=S.sbuf(st4, "statsB%d" % k, [128, 2, 6], F32), mv=S.sbuf(st4, "mvB%d" % k, [128, 2], F32),
                                 rstd=S.sbuf(st4, "rstdB%d" % k, [128, 1], F32), nmr=S.sbuf(st4, "nmrB%d" % k, [128, 1], F32),
                                 z=S.sbuf(st4, "zB%d" % k, [128, DM], F32)) for k in range(2)]

                    def tile_A(i):
                        tsl = slice(i * 128, (i + 1) * 128)
                        xs = xt[i % 2]
                        S.dma("sp", xs[:], x[b, tsl, :], writes=[xs])
                        layer_norm_tile(lns, xs, None, 0, None)
                        z = lns["z"]
                        S.op("dve", lambda e: e.tensor_tensor(z[:], z[:], lnbc[:, 1, :], ALU.add), reads=[z, lnbc], writes=[z])
                        for hf in range(2):
                            for m in range(8):
                                S.op("pe", lambda e: e.matmul(pm[hf][:], mixT[:, m, tsl], Wo[:, m, hf * 512:(hf + 1) * 512],
                                                              start=(m == 0), stop=(m == 7)), reads=[mixT, Wo], writes=[pm[hf]], inc=(m == 7))
                            S.op("dve", lambda e: e.scalar_tensor_tensor(pre[:, hf * 512:(hf + 1) * 512], z[:, hf * 512:(hf + 1) * 512],
                                                                         ALPHA, pm[hf][:], ALU.mult, ALU.add),
                                 reads=[z, pm[hf]], writes=[pre])
                        lb = lnsB[i % 2]
                        layer_norm_tile(lb, pre, None, 2, None)
                        z1 = lb["z"]
                        S.op("dve", lambda e: e.tensor_tensor(z1[:], z1[:], lnbc[:, 3, :], ALU.add), reads=[z1, lnbc], writes=[z1])

                    def tile_B(i):
                        tsl = slice(i * 128, (i + 1) * 128)
                        z = lnsB[i % 2]["z"]
                        hh = hst[i % 2]
                        hb = hbs[i % 2]
                        S.op("act", lambda e: e.activation(hb[:], z[:], AF.Copy), reads=[z], writes=[hb])
                        S.op("act", lambda e: e.activation(hh[:], z[:], AF.Copy, scale=ALPHA), reads=[z], writes=[hh])
                        S.dma("sp", hs[tsl, :], hh[:], reads=[hh], writes=[hsB], sembuf=hh)
                        S.dma("sp", hbd[tsl, :], hb[:], reads=[hb], writes=[hbdB], sembuf=hb)
                        for hf in range(2):
                            for c in range(4):
                                cc = hf * 4 + c
                                S.op("pe", lambda e: e.transpose(pg[hf][:, c * 128:(c + 1) * 128], z[:, cc * 128:(cc + 1) * 128], identF[:]),
                                     reads=[z, identF], writes=[pg[hf]], inc=(c == 3))
                            S.op("act", lambda e: e.activation(hT32[:, hf * 4:(hf + 1) * 4, :], pg[hf][:].rearrange("p (c t) -> p c t", c=4), AF.Copy),
                                 reads=[pg[hf]], writes=[hT32])
                        for c in range(8):
                            S.op("pe", lambda e: e.matmul(pl[:, 0:36], hT32[:, c, :], Wr[:, c, :], start=(c == 0), stop=(c == 7)),
                                 reads=[hT32, Wr], writes=[pl], inc=(c == 7))
                        S.op("dve", lambda e: e.tensor_tensor(lgA[:, i, :], pl[:, 0:36], brb[:], ALU.add), reads=[pl, brb], writes=[lgA])

                    tile_A(0)
                    for i in range(NT):
                        if i + 1 < NT:
                            tile_A(i + 1)
                        tile_B(i)
                    NTl = NT
                    G3 = lgA[:, :, 0:4]
                    E4 = lgA[:, :, 4:36].rearrange("p t (g e) -> p t g e", g=4)
                    bc_t = lambda ap2, n: ap2.rearrange("p (t o) -> p t o", o=1).to_broadcast([128, NTl, n])
                    S.op("dve", lambda e: e.tensor_reduce(gmxA[:], G3, AX.X, ALU.max), reads=[lgA], writes=[gmxA])
                    S.op("dve", lambda e: e.tensor_tensor(gselA[:], G3, bc_t(gmxA[:], 4), ALU.is_ge), reads=[lgA, gmxA], writes=[gselA])
                    S.op("dve", lambda e: e.tensor_tensor(r4A[:], G3, bc_t(gmxA[:], 4), ALU.subtract), reads=[lgA, gmxA], writes=[r4A])
                    S.op("act", lambda e: e.activation(r4A[:], r4A[:], AF.Exp), reads=[r4A], writes=[r4A])
                    S.op("dve", lambda e: e.tensor_reduce(gwA[:], r4A[:], AX.X, ALU.add), reads=[r4A], writes=[gwA])
                    S.op("dve", lambda e: e.reciprocal(gwA[:], gwA[:]), reads=[gwA], writes=[gwA])
                    gsel4 = gselA[:].rearrange("p t (g o) -> p t g o", o=1).to_broadcast([128, NTl, 4, 8])
                    S.op("dve", lambda e: e.tensor_tensor(le4[:], E4, gsel4, ALU.mult), reads=[lgA, gselA], writes=[le4])
                    S.op("dve", lambda e: e.tensor_reduce(seA[:], le4[:].rearrange("p t g e -> p t e g"), AX.X, ALU.add), reads=[le4], writes=[seA])
                    S.op("dve", lambda e: e.tensor_reduce(mx1A[:], seA[:], AX.X, ALU.max), reads=[seA], writes=[mx1A])
                    S.op("dve", lambda e: e.tensor_tensor(oh1A[:], seA[:], bc_t(mx1A[:], 8), ALU.is_ge), reads=[seA, mx1A], writes=[oh1A])
                    S.op("dve", lambda e: e.scalar_tensor_tensor(se2A[:], oh1A[:], NEGBIG, seA[:], ALU.mult, ALU.add), reads=[oh1A, seA], writes=[se2A])
                    S.op("dve", lambda e: e.tensor_reduce(mx2A[:], se2A[:], AX.X, ALU.max), reads=[se2A], writes=[mx2A])
                    S.op("dve", lambda e: e.tensor_tensor(oh2A[:], se2A[:], bc_t(mx2A[:], 8), ALU.is_ge), reads=[se2A, mx2A], writes=[oh2A])
                    S.op("dve", lambda e: e.tensor_tensor(w2A[:], mx2A[:], mx1A[:], ALU.subtract), reads=[mx1A, mx2A], writes=[w2A])
                    S.op("act", lambda e: e.activation(w2A[:], w2A[:], AF.Exp), reads=[w2A], writes=[w2A])
                    S.op("dve", lambda e: e.tensor_scalar(w1A[:], w2A[:], 1.0, None, ALU.add), reads=[w2A], writes=[w1A])
                    S.op("dve", lambda e: e.reciprocal(w1A[:], w1A[:]), reads=[w1A], writes=[w1A])
                    S.op("dve", lambda e: e.tensor_tensor(w2A[:], w2A[:], w1A[:], ALU.mult), reads=[w1A, w2A], writes=[w2A])
                    S.op("dve", lambda e: e.tensor_tensor(ca[:], w1A[:], gwA[:], ALU.mult), reads=[w1A, gwA], writes=[ca])
                    S.op("dve", lambda e: e.tensor_tensor(cb[:], w2A[:], gwA[:], ALU.mult), reads=[w2A, gwA], writes=[cb])
                    for Mx, ohx in ((M1, oh1A), (M2, oh2A)):
                        S.op("dve", lambda e: e.tensor_tensor(Mx[:].rearrange("p t (g e) -> p t g e", g=4),
                                                              ohx[:].rearrange("p t (o e) -> p t o e", o=1).to_broadcast([128, NTl, 4, 8]),
                                                              gsel4, ALU.mult), reads=[ohx, gselA], writes=[Mx])
                    S.op("dve", lambda e: e.tensor_tensor(Mb[:], M1[:], M2[:], ALU.add), reads=[M1, M2], writes=[Mb])
                    S.barrier()

            conv_some(1000)
            with ExitStack() as st5:
                NSL = 64
                pr = [S.psum(st5, "pr%d" % i, [128, 512], F32) for i in range(2)]
                Rall = S.sbuf(st5, "Rall", [128, NT, 32], F32)
                cntf = S.sbuf(st5, "cntf", [128, 32], F32)
                cntI = S.sbuf(st5, "cntI", [128, 32], I32)
                pcf = S.sbuf(st5, "pcf", [128, 32], F32)
                scA = S.sbuf(st5, "scA", [128, 32], F32)
                scB = S.sbuf(st5, "scB", [128, 32], F32)
                off = S.sbuf(st5, "off", [128, 32], F32)
                Pm = S.sbuf(st5, "Pm", [128, NT, 32], F32)
                prod = S.sbuf(st5, "prod", [128, NT, 32], F32)
                posaF = S.sbuf(st5, "posaF", [128, NT], F32)
                posbF = S.sbuf(st5, "posbF", [128, NT], F32)
                posaI = S.sbuf(st5, "posaI", [128, NT], I32)
                posbI = S.sbuf(st5, "posbI", [128, NT], I32)
                cmpb = S.sbuf(st5, "cmpb", [128, NSL, 32], F32)
                eidf = S.sbuf(st5, "eidf", [128, NSL], F32)
                actf = S.sbuf(st5, "actf", [128, NSL], F32)
                widF = S.sbuf(st5, "widF", [128, NSL], F32)
                widI = S.sbuf(st5, "widI", [128, NSL], I32)
                for i in range(NT):
                    S.op("pe", lambda e: e.matmul(pr[0][:, 0:32], onesb[:], Mb[:, i, :], start=(i == 0), stop=(i == NT - 1)),
                         reads=[onesb, Mb], writes=[pr[0]], inc=(i == NT - 1))
                S.op("dve", lambda e: e.tensor_copy(cntf[:], pr[0][:, 0:32]), reads=[pr[0]], writes=[cntf])
                for i in range(NT):
                    pb = pr[1]
                    for i2 in range(i):
                        S.op("pe", lambda e: e.matmul(pb[:, 0:32], onesb[:], Mb[:, i2, :], start=(i2 == 0), stop=False),
                             reads=[onesb, Mb], writes=[pb], inc=False)
                    S.op("pe", lambda e: e.matmul(pb[:, 0:32], ustrb[:], Mb[:, i, :], start=(i == 0), stop=True),
                         reads=[ustrb, Mb], writes=[pb])
                    S.op("act", lambda e: e.activation(Rall[:, i, :], pb[:, 0:32], AF.Copy), reads=[pb], writes=[Rall])
                S.op("dve", lambda e: e.tensor_scalar(pcf[:], cntf[:], 127.0, None, ALU.add), reads=[cntf], writes=[pcf])
                S.op("dve", lambda e: e.tensor_copy(cntI[:], pcf[:]), reads=[pcf], writes=[cntI])
                S.op("dve", lambda e: e.tensor_scalar(cntI[:], cntI[:], 7, None, ALU.arith_shift_right), reads=[cntI], writes=[cntI])
                S.op("dve", lambda e: e.tensor_scalar(cntI[:], cntI[:], 7, None, ALU.logical_shift_left), reads=[cntI], writes=[cntI])
                S.op("dve", lambda e: e.tensor_copy(pcf[:], cntI[:]), reads=[cntI], writes=[pcf])
                S.op("dve", lambda e: e.tensor_copy(scA[:], pcf[:]), reads=[pcf], writes=[scA])
                cur, nxt = scA, scB
                for sh in (1, 2, 4, 8, 16):
                    S.op("dve", lambda e: e.tensor_copy(nxt[:, 0:sh], cur[:, 0:sh]), reads=[cur], writes=[nxt])
                    S.op("dve", lambda e: e.tensor_tensor(nxt[:, sh:32], cur[:, sh:32], cur[:, 0:32 - sh], ALU.add), reads=[cur], writes=[nxt])
                    cur, nxt = nxt, cur
                incl = cur
                S.op("dve", lambda e: e.tensor_tensor(off[:], incl[:], pcf[:], ALU.subtract), reads=[incl, pcf], writes=[off])
                S.op("dve", lambda e: e.tensor_tensor(Pm[:], Rall[:], off[:].rearrange("p (o e) -> p o e", o=1).to_broadcast([128, NT, 32]), ALU.add),
                     reads=[Rall, off], writes=[Pm])
                for Mx, pF, pI in ((M1, posaF, posaI), (M2, posbF, posbI)):
                    S.op("dve", lambda e: e.tensor_tensor(prod[:], Mx[:], Pm[:], ALU.mult), reads=[Mx, Pm], writes=[prod])
                    S.op("dve", lambda e: e.tensor_reduce(pF[:], prod[:], AX.X, ALU.add), reads=[prod], writes=[pF])
                    S.op("dve", lambda e: e.tensor_copy(pI[:], pF[:]), reads=[pF], writes=[pI])
                S.op("dve", lambda e: e.tensor_tensor(cmpb[:], off[:].rearrange("p (o e) -> p o e", o=1).to_broadcast([128, NSL, 32]),
                                                      jvS[:].rearrange("p (j o) -> p j o", o=1).to_broadcast([128, NSL, 32]), ALU.is_le),
                     reads=[off, jvS], writes=[cmpb])
                S.op("dve", lambda e: e.tensor_reduce(eidf[:], cmpb[:], AX.X, ALU.add), reads=[cmpb], writes=[eidf])
                S.op("dve", lambda e: e.tensor_scalar(actf[:], jvS[:], incl[:, 31:32], 1.0e6, ALU.is_ge, ALU.mult), reads=[jvS, incl], writes=[actf])
                S.op("dve", lambda e: e.tensor_scalar(widF[:], eidf[:], -1.0, 128.0, ALU.add, ALU.mult), reads=[eidf], writes=[widF])
                S.op("dve", lambda e: e.tensor_scalar(widF[:], widF[:], pcolS[:, 0:1], None, ALU.add), reads=[widF, pcolS], writes=[widF])
                S.op("dve", lambda e: e.tensor_tensor(widF[:], widF[:], actf[:], ALU.add), reads=[widF, actf], writes=[widF])
                S.op("dve", lambda e: e.tensor_copy(widI[:], widF[:]), reads=[widF], writes=[widI])

                hld = [S.sbuf(st5, "hld%d" % i, [128, DM], BF16) for i in range(2)]
                for i in range(NT):
                    hl = hld[i % 2]
                    S.dma("sp", hl[:], hbd[i * 128:(i + 1) * 128, :], reads=[hbdB], writes=[hl])
                    for pI in (posaI, posbI):
                        S.dmaf("pool", lambda e: e.indirect_dma_start(out=Hs, out_offset=bass.IndirectOffsetOnAxis(ap=pI[:, i:i + 1], axis=0),
                                                                      in_=hl[:, :], in_offset=None),
                               reads=[hl, pI], writes=[HsB], sembuf=hl)

                Wg_s = [S.sbuf(st5, "Wgs%d" % i, [128, 8, 256], BF16) for i in range(2)]
                Wu_s = [S.sbuf(st5, "Wus%d" % i, [128, 8, 256], BF16) for i in range(2)]
                Wd_s = [S.sbuf(st5, "Wds%d" % i, [128, 2, DM], BF16) for i in range(2)]
                hsl = [S.sbuf(st5, "hsl%d" % i, [128, DM], BF16) for i in range(2)]
                hslT = [S.sbuf(st5, "hslT%d" % i, [128, 8, 128], BF16) for i in range(2)]
                sa = [S.sbuf(st5, "sa%d" % i, [128, 256], F32) for i in range(2)]
                hid = [S.sbuf(st5, "hid%d" % i, [128, 256], BF16) for i in range(2)]
                hidT = [S.sbuf(st5, "hidT%d" % i, [128, 2, 128], BF16) for i in range(2)]
                ysb = [S.sbuf(st5, "ysb%d" % i, [128, DM], F32) for i in range(2)]
                pht = S.psum(st5, "pht", [128, 1024], BF16)
                ptx = S.psum(st5, "ptx", [128, 1024], BF16)
                pau = [S.psum(st5, "pau%d" % i, [128, 512], F32) for i in range(2)]
                py = [S.psum(st5, "py%d" % i, [128, 512], F32) for i in range(2)]

                def st_load_a(j):
                    k = j % 2
                    S.dma("sp", hsl[k][:], Hs[j * 128:(j + 1) * 128, :], reads=[HsB], writes=[hsl[k]])
                    for wt, src in ((Wg_s[k], weg_b), (Wu_s[k], weu_b)):
                        S.dmaf("pool", lambda e: e.indirect_dma_start(out=wt[:].rearrange("p a b -> p (a b)"), out_offset=None, in_=src,
                                                                      in_offset=bass.IndirectOffsetOnAxis(ap=widI[:, j:j + 1], axis=0),
                                                                      bounds_check=bcreg, oob_is_err=False),
                               reads=[widI, WcB], writes=[wt])

                def st_load_d(j):
                    k = j % 2
                    wt = Wd_s[k]
                    S.dmaf("pool", lambda e: e.indirect_dma_start(out=wt[:].rearrange("p a b -> p (a b)"), out_offset=None, in_=wed_b,
                                                                  in_offset=bass.IndirectOffsetOnAxis(ap=widI[:, j:j + 1], axis=0),
                                                                  bounds_check=bcreg, oob_is_err=False),
                           reads=[widI, WcB], writes=[wt])

                def st_au(j):
                    k = j % 2
                    for c in range(8):
                        S.op("pe", lambda e: e.transpose(ptx[:, c * 128:(c + 1) * 128], hsl[k][:, c * 128:(c + 1) * 128], identb[:]),
                             reads=[hsl[k], identb], writes=[ptx], inc=(c == 7))
                    S.op("act", lambda e: e.activation(hslT[k][:], ptx[:].rearrange("p (c t) -> p c t", c=8), AF.Copy),
                         reads=[ptx], writes=[hslT[k]])
                    pa = pau[k]
                    for c in range(8):
                        S.op("pe", lambda e: e.matmul(pa[:, 0:256], hslT[k][:, c, :], Wg_s[k][:, c, :], start=(c == 0), stop=(c == 7)),
                             reads=[hslT[k], Wg_s[k]], writes=[pa], inc=False)
                    for c in range(8):
                        S.op("pe", lambda e: e.matmul(pa[:, 256:512], hslT[k][:, c, :], Wu_s[k][:, c, :], start=(c == 0), stop=(c == 7)),
                             reads=[hslT[k], Wu_s[k]], writes=[pa], inc=(c == 7))
                    S.op("act", lambda e: e.activation(sa[k][:], pa[:, 0:256], AF.Silu), reads=[pa], writes=[sa[k]])
                    S.op("dve", lambda e: e.tensor_tensor(hid[k][:], pa[:, 256:512], sa[k][:], ALU.mult), reads=[pa, sa[k]], writes=[hid[k]])

                def st_tr(j):
                    k = j % 2
                    o = k * 512
                    for f in range(2):
                        S.op("pe", lambda e: e.transpose(pht[:, o + f * 128:o + (f + 1) * 128], hid[k][:, f * 128:(f + 1) * 128], identb[:]),
                             reads=[hid[k], identb], writes=[pht], inc=(f == 1))
                    S.op("act", lambda e: e.activation(hidT[k][:], pht[:, o:o + 256].rearrange("p (f t) -> p f t", f=2), AF.Copy),
                         reads=[pht], writes=[hidT[k]])

                def st_y(j):
                    k = j % 2
                    for hf in range(2):
                        for f in range(2):
                            S.op("pe", lambda e: e.matmul(py[hf][:], hidT[k][:, f, :], Wd_s[k][:, f, hf * 512:(hf + 1) * 512],
                                                          start=(f == 0), stop=(f == 1)),
                                 reads=[hidT[k], Wd_s[k]], writes=[py[hf]], inc=(f == 1))
                        if hf == 0:
                            S.op("act", lambda e: e.activation(ysb[k][:, 0:512], py[0][:], AF.Copy), reads=[py[0]], writes=[ysb[k]])
                        else:
                            S.op("dve", lambda e: e.tensor_copy(ysb[k][:, 512:1024], py[1][:]), reads=[py[1]], writes=[ysb[k]])
                    S.dma("sp", Ys[j * 128:(j + 1) * 128, :], ysb[k][:], reads=[ysb[k]], writes=[YsB], sembuf=ysb[k])

                st_load_a(0)
                st_load_d(0)
                for j in range(NSL + 2):
                    if j < NSL:
                        if j + 1 < NSL:
                            st_load_a(j + 1)
                        st_au(j)
                    if 1 <= j <= NSL:
                        st_tr(j - 1)
                    if j >= 2:
                        st_y(j - 2)
                    if 1 <= j < NSL:
                        st_load_d(j)

                lns5 = dict(stats=S.sbuf(st5, "stats5", [128, 2, 6], F32), mv=S.sbuf(st5, "mv5", [128, 2], F32),
                            rstd=S.sbuf(st5, "rstd5", [128, 1], F32), nmr=S.sbuf(st5, "nmr5", [128, 1], F32),
                            z=S.sbuf(st5, "z5", [128, DM], F32))
                accs = [S.sbuf(st5, "accs%d" % i, [128, DM], F32) for i in range(2)]
                yas = [S.sbuf(st5, "yas%d" % i, [128, DM], F32) for i in range(2)]
                ybs = [S.sbuf(st5, "ybs%d" % i, [128, DM], F32) for i in range(2)]
                ost = [S.sbuf(st5, "ost%d" % i, [128, DM], F32) for i in range(2)]
                for i in range(NT):
                    k = i % 2
                    S.dma("sp", accs[k][:], hs[i * 128:(i + 1) * 128, :], reads=[hsB], writes=[accs[k]])
                    for yt, pI in ((yas[k], posaI), (ybs[k], posbI)):
                        S.dmaf("pool", lambda e: e.indirect_dma_start(out=yt[:, :], out_offset=None, in_=Ys,
                                                                      in_offset=bass.IndirectOffsetOnAxis(ap=pI[:, i:i + 1], axis=0)),
                               reads=[YsB, pI], writes=[yt])
                    S.op("dve", lambda e: e.scalar_tensor_tensor(accs[k][:], yas[k][:], ca[:, i:i + 1], accs[k][:], ALU.mult, ALU.add),
                         reads=[yas[k], ca], writes=[accs[k]])
                    S.op("dve", lambda e: e.scalar_tensor_tensor(accs[k][:], ybs[k][:], cb[:, i:i + 1], accs[k][:], ALU.mult, ALU.add),
                         reads=[ybs[k], cb], writes=[accs[k]])
                    layer_norm_tile(lns5, accs[k], None, 4, None)
                    z = lns5["z"]
                    o = ost[k]
                    S.op("dve", lambda e: e.tensor_tensor(o[:], z[:], lnbc[:, 5, :], ALU.add), reads=[z, lnbc], writes=[o])
                    S.dma("sp", out[b, i * 128:(i + 1) * 128, :], o[:], reads=[o], writes=[outB], sembuf=o)
                S.barrier()
            stB.close()
        S.nobar.clear()
        S.barrier()
    return nc


_NC = None


def _bucket_tables():
    import jax
    import jax.numpy as jnp
    with jax.default_device(jax.devices("cpu")[0]):
        kk = jnp.arange(128, dtype=jnp.int32)[:, None]
        qq = jnp.arange(128, dtype=jnp.int32)[None, :]
        tabs = []
        for d in range(2):
            rel = kk - qq - 128 * d
            nb = 16
            max_exact = 8
            ret = jnp.where(rel > 0, nb, 0)
            n = jnp.abs(rel)
            large = max_exact + (jnp.log(jnp.maximum(n, 1).astype(jnp.float32) / max_exact)
                                 / math.log(128 / max_exact) * (nb - max_exact)).astype(jnp.int32)
            large = jnp.minimum(large, nb - 1)
            tabs.append(np.asarray(ret + jnp.where(n < max_exact, n, large)))
    return np.stack(tabs, 0)


def kernel(**inputs):
    global _NC
    f32 = np.float32
    g = lambda k: np.ascontiguousarray(np.asarray(inputs[k]))
    x = g("x").astype(f32, copy=False)
    pos = g("positions").astype(np.int32, copy=False)
    rel_bias = g("rel_bias")
    bk = _bucket_tables()
    tzr = rel_bias[bk]
    tzr = np.ascontiguousarray(np.transpose(tzr, (1, 3, 0, 2))).astype(f32)
    shared = {
        "w_in": g("w_in")[0], "w_uq": g("w_uq")[0], "w_uk": g("w_uk")[0], "w_uv": g("w_uv")[0],
        "w_up_a": g("w_up_a")[0], "w_up_b": g("w_up_b")[0], "w_gate": g("w_gate")[0], "w_o": g("w_o")[0],
        "w_r": np.ascontiguousarray(np.concatenate([g("w_grp")[0], g("w_rt")[0]], axis=1)),
        "b_r": np.ascontiguousarray(np.concatenate([g("b_grp")[0], g("b_rt")[0]], axis=0)),
        "w_eg": g("w_exp_gate")[0].reshape(32, 8, 128, 256).transpose(0, 2, 1, 3).reshape(32 * 128, 2048),
        "w_eu": g("w_exp_up")[0].reshape(32, 8, 128, 256).transpose(0, 2, 1, 3).reshape(32 * 128, 2048),
        "w_ed": g("w_exp_down")[0].reshape(32, 2, 128, 1024).transpose(0, 2, 1, 3).reshape(32 * 128, 2048),
        "ustr": np.triu(np.ones((128, 128), dtype=f32), 1),
        "jv": np.tile((np.arange(64, dtype=f32) * 128.0)[None, :], (128, 1)),
        "pcol": np.arange(128, dtype=f32).reshape(128, 1),
        "lnv": np.ascontiguousarray(np.stack([g("ln0_g"), g("ln0_b"), g("ln1_g")[0], g("ln1_b")[0], g("ln2_g")[0], g("ln2_b")[0]], 0)),
        "qng": np.ascontiguousarray(g("q_norm_g")[0].reshape(2, 128).T),
        "kvg": np.ascontiguousarray(g("kv_norm_g")[0].reshape(128, 1)),
        "bgate": np.ascontiguousarray(g("b_gate")[0].reshape(16, 128).T),
        "tzr": tzr,
        "cfar": np.ascontiguousarray(rel_bias[15, :]),
        "identf": np.eye(128, dtype=f32),
        "invf": np.tile((10000.0 ** (-np.arange(16, dtype=np.float64) / 16.0) / (2.0 * math.pi)).astype(f32), 2).reshape(32, 1),
    }
    shared = {k: np.ascontiguousarray(v.astype(f32, copy=False)) for k, v in shared.items()}
    if _NC is None:
        _NC = build()
    in_maps = []
    for c in range(8):
        m = dict(shared)
        m["x"] = np.ascontiguousarray(x[NB * c:NB * (c + 1)])
        m["pos"] = np.ascontiguousarray(pos[NB * c:NB * (c + 1)])
        in_maps.append(m)
    res = run_bass_kernel_spmd(_NC, in_maps, core_ids=list(range(8)))
    return np.concatenate([np.asarray(r["out"]) for r in res.results], axis=0).astype(f32, copy=False)
```

```python
import math
from contextlib import ExitStack
import numpy as np
import concourse.bass as bass
import concourse.mybir as mybir
from concourse.bass_utils import run_bass_kernel_spmd

F32 = mybir.dt.float32
BF16 = mybir.dt.bfloat16
I32 = mybir.dt.int32
ALU = mybir.AluOpType
AF = mybir.ActivationFunctionType
AX = mybir.AxisListType

T = 2048
DM = 1024
NT = 16
NB = 2
ALPHA = 2.0 ** 0.25
LN_EPS = 1e-5
RMS_EPS = 1e-6
NEGBIG = -1.0e30
BIS_ITERS = 14
TOPK = 256


class Buf:
    __slots__ = ("ap", "name", "ws", "reads", "dsem", "dcount")

    def __init__(self, ap, name=""):
        self.ap = ap
        self.name = name
        self.ws = {}
        self.reads = {}
        self.dsem = None
        self.dcount = 0

    def __getitem__(self, idx):
        return self.ap[idx]


class Sched:
    def __init__(self, nc, stack):
        self.nc = nc
        self.stack = stack
        self.engs = {}
        for name, eng in (("pe", nc.tensor), ("act", nc.scalar), ("dve", nc.vector),
                          ("pool", nc.gpsimd), ("sp", nc.sync)):
            sem = stack.enter_context(nc.semaphore("s_" + name))
            self.engs[name] = dict(eng=eng, sem=sem, count=0, waited={})
        self.ndsem = 0
        self.dpool = {}
        self.nobar = set()
        self.n_ins = 0
        self.uid = 0

    def sbuf(self, st, name, shape, dtype):
        self.uid += 1
        name = "%s_%d" % (name, self.uid)
        return Buf(st.enter_context(self.nc.sbuf_tensor(name, shape, dtype)), name)

    def psum(self, st, name, shape, dtype):
        self.uid += 1
        name = "%s_%d" % (name, self.uid)
        return Buf(st.enter_context(self.nc.psum_tensor(name, shape, dtype)), name)

    def _wait(self, engname, ev):
        sem, val, src = ev
        if src == "pe" and engname == "pe":
            return
        E = self.engs[engname]
        key = id(sem)
        if E["waited"].get(key, 0) < val:
            E["eng"].wait_ge(sem, val)
            E["waited"][key] = val
            self.n_ins += 1

    def _deps(self, engname, reads, writes):
        for b in reads:
            for ev in b.ws.values():
                self._wait(engname, ev)
        for b in writes:
            for ev in b.ws.values():
                self._wait(engname, ev)
            for ev in b.reads.values():
                self._wait(engname, ev)

    def _commit(self, ev, reads, writes):
        k = id(ev[0])
        for b in writes:
            b.ws[k] = ev
            b.reads = {}
        for b in reads:
            if b in writes:
                continue
            b.reads[k] = ev

    def op(self, engname, fn, reads=(), writes=(), inc=True):
        E = self.engs[engname]
        self._deps(engname, reads, writes)
        ins = fn(E["eng"])
        self.n_ins += 1
        if inc:
            E["count"] += 1
            ins.then_inc(E["sem"], 1)
            self._commit((E["sem"], E["count"], engname), reads, writes)
        else:
            self._commit((E["sem"], E["count"] + 1, engname), reads, writes)

    def dma(self, qname, out_ap, in_ap, reads=(), writes=(), sembuf=None):
        E = self.engs[qname]
        self._deps(qname, reads, writes)
        sb = sembuf or (writes[0] if writes else reads[0])
        key = sb.name.rsplit("_", 1)[0] if "_" in sb.name else sb.name
        ent = self.dpool.get(key)
        if ent is None:
            ent = [self.stack.enter_context(self.nc.semaphore("d%d" % self.ndsem)), 0]
            self.ndsem += 1
            self.dpool[key] = ent
        ins = E["eng"].dma_start(out=out_ap, in_=in_ap)
        ent[1] += 16
        ins.then_inc(ent[0], 16)
        self.n_ins += 1
        ev = (ent[0], ent[1], "dma")
        self._commit(ev, reads, writes)
        return ev

    def dmaf(self, qname, fn, reads=(), writes=(), sembuf=None):
        E = self.engs[qname]
        self._deps(qname, reads, writes)
        sb = sembuf or (writes[0] if writes else reads[0])
        key = sb.name.rsplit("_", 1)[0] if "_" in sb.name else sb.name
        ent = self.dpool.get(key)
        if ent is None:
            ent = [self.stack.enter_context(self.nc.semaphore("d%d" % self.ndsem)), 0]
            self.ndsem += 1
            self.dpool[key] = ent
        ins = fn(E["eng"])
        ent[1] += 16
        ins.then_inc(ent[0], 16)
        self.n_ins += 1
        ev = (ent[0], ent[1], "dma")
        self._commit(ev, reads, writes)
        return ev

    def barrier(self):
        evs = []
        for n, E in self.engs.items():
            if E["count"] > 0:
                evs.append((E["sem"], E["count"], "bar_" + n))
        for key, ent in self.dpool.items():
            if ent[1] > 0 and key not in self.nobar:
                evs.append((ent[0], ent[1], "dma"))
        for n in self.engs:
            for ev in evs:
                if ev[2] == "bar_" + n:
                    continue
                self._wait(n, ev)


def build():
    nc = bass.Bass("TRN2", target_bir_lowering=False)

    def din(name, shape, dt=F32):
        return nc.dram_tensor(name, shape, dt, kind="ExternalInput").ap()

    x = din("x", [NB, T, DM])
    pos = din("pos", [NB, T], I32)
    w_in = din("w_in", [DM, 1352])
    w_uq = din("w_uq", [256, 768])
    w_uk = din("w_uk", [128, 512])
    w_uv = din("w_uv", [128, 512])
    w_up_a = din("w_up_a", [512, DM])
    w_up_b = din("w_up_b", [512, DM])
    w_gate = din("w_gate", [DM, 2048])
    w_o = din("w_o", [DM, DM])
    w_r = din("w_r", [DM, 36])
    b_r = din("b_r", [36])
    w_eg = din("w_eg", [32 * 128, 2048])
    w_eu = din("w_eu", [32 * 128, 2048])
    w_ed = din("w_ed", [32 * 128, 2048])
    ustr = din("ustr", [128, 128])
    jv = din("jv", [128, 64])
    pcol = din("pcol", [128, 1])
    lnv = din("lnv", [6, DM])
    qng = din("qng", [128, 2])
    kvg = din("kvg", [128, 1])
    bgate = din("bgate", [128, 16])
    tzr = din("tzr", [128, 8, 2, 128])
    cfar = din("cfar", [8])
    identf = din("identf", [128, 128])
    invf = din("invf", [32, 1])
    out = nc.dram_tensor("out", [NB, T, DM], F32, kind="ExternalOutput").ap()
    hs = nc.dram_tensor("hs", [T, DM], F32, kind="Internal").ap()
    hbd = nc.dram_tensor("hbd", [T, DM], BF16, kind="Internal").ap()
    Hs = nc.dram_tensor("Hs", [64 * 128, DM], BF16, kind="Internal").ap()
    Ys = nc.dram_tensor("Ys", [64 * 128, DM], F32, kind="Internal").ap()
    weg_b = nc.dram_tensor("weg_b", [32 * 128, 2048], BF16, kind="Internal").ap()
    weu_b = nc.dram_tensor("weu_b", [32 * 128, 2048], BF16, kind="Internal").ap()
    wed_b = nc.dram_tensor("wed_b", [32 * 128, 2048], BF16, kind="Internal").ap()

    with ExitStack() as st0:
        S = Sched(nc, st0)
        hsB = Buf(hs, "hs")
        outB = Buf(out, "out")
        bcreg = st0.enter_context(nc.gpsimd.register("bcreg"))
        nc.gpsimd.reg_mov(bcreg, 32 * 128 - 1)
        hbdB = Buf(hbd, "hbd")
        HsB = Buf(Hs, "Hs")
        YsB = Buf(Ys, "Ys")

        identb = S.sbuf(st0, "identb", [128, 128], BF16)
        identF = S.sbuf(st0, "identF", [128, 128], F32)
        onesb = S.sbuf(st0, "onesb", [128, 128], BF16)
        lnbc = S.sbuf(st0, "lnbc", [128, 6, DM], F32)
        S.dma("pool", identb[:], identf, writes=[identb])
        ustrb = S.sbuf(st0, "ustrb", [128, 128], BF16)
        jvS = S.sbuf(st0, "jvS", [128, 64], F32)
        pcolS = S.sbuf(st0, "pcolS", [128, 1], F32)
        S.dma("pool", ustrb[:], ustr, writes=[ustrb])
        S.dma("sp", jvS[:], jv, writes=[jvS])
        S.dma("sp", pcolS[:], pcol, writes=[pcolS])
        WcB = Buf(None, "wcast")
        S.nobar.add("wcast")
        conv_list = [(srcw, dstw, e_) for e_ in range(32) for srcw, dstw in ((w_eg, weg_b), (w_eu, weu_b), (w_ed, wed_b))]

        def conv_some(n):
            for _ in range(n):
                if not conv_list:
                    return
                srcw, dstw, e_ = conv_list.pop(0)
                S.dma("pool", dstw[e_ * 128:(e_ + 1) * 128, :], srcw[e_ * 128:(e_ + 1) * 128, :], writes=[WcB])
        with ExitStack() as stz:
            zt = S.sbuf(stz, "zt", [128, 8, DM], BF16)
            S.op("pool", lambda e: e.memset(zt[:], 0.0), writes=[zt])
            for q in range(8):
                S.dma("sp", Hs[q * 1024:(q + 1) * 1024, :].rearrange("(p r) n -> p r n", p=128), zt[:], reads=[zt], writes=[HsB], sembuf=zt)
            S.barrier()
        S.dma("sp", identF[:], identf, writes=[identF])
        S.op("dve", lambda e: e.memset(onesb[:], 1.0), writes=[onesb])
        for k in range(6):
            S.dma("sp", lnbc[:, k, :], lnv[k].partition_broadcast(128), writes=[lnbc])

        def layer_norm_tile(st_bufs, src, dst_ap_fn, gi, out_bufs, scale_after=None):
            stats, mv, rstd, nmr, z = (st_bufs[k] for k in ("stats", "mv", "rstd", "nmr", "z"))
            S.op("dve", lambda e: e.bn_stats(stats[:, 0, :], src[:, 0:512]), reads=[src], writes=[stats])
            S.op("dve", lambda e: e.bn_stats(stats[:, 1, :], src[:, 512:1024]), reads=[src], writes=[stats])
            S.op("dve", lambda e: e.bn_aggr(mv[:], stats[:].rearrange("p a b -> p (a b)")), reads=[stats], writes=[mv])
            S.op("dve", lambda e: e.tensor_scalar(rstd[:], mv[:, 1:2], LN_EPS, None, ALU.add), reads=[mv], writes=[rstd])
            S.op("act", lambda e: e.activation(rstd[:], rstd[:], AF.Sqrt), reads=[rstd], writes=[rstd])
            S.op("dve", lambda e: e.reciprocal(rstd[:], rstd[:]), reads=[rstd], writes=[rstd])
            S.op("dve", lambda e: e.tensor_scalar(nmr[:], mv[:, 0:1], rstd[:, 0:1], -1.0, ALU.mult, ALU.mult),
                 reads=[mv, rstd], writes=[nmr])
            S.op("act", lambda e: e.activation(z[:], src[:], AF.Identity, bias=nmr[:, 0:1], scale=rstd[:, 0:1]),
                 reads=[src, nmr, rstd], writes=[z])
            S.op("dve", lambda e: e.tensor_tensor(z[:], z[:], lnbc[:, gi, :], ALU.mult), reads=[z, lnbc], writes=[z])

        for b in range(NB):
            stB = ExitStack()
            stB.__enter__()
            xnT = S.sbuf(stB, "xnT", [128, 8, T], BF16)
            ca = S.sbuf(stB, "ca", [128, NT], F32)
            cb = S.sbuf(stB, "cb", [128, NT], F32)
            M1 = S.sbuf(stB, "M1", [128, NT, 32], F32)
            M2 = S.sbuf(stB, "M2", [128, NT, 32], F32)
            Mb = S.sbuf(stB, "Mb", [128, NT, 32], BF16)
            with ExitStack() as stA:
                oaT = S.sbuf(stA, "oaT", [128, 4, T], BF16)
                obT = S.sbuf(stA, "obT", [128, 4, T], BF16)
                lns = dict(stats=S.sbuf(stA, "stats", [128, 2, 6], F32), mv=S.sbuf(stA, "mv", [128, 2], F32),
                           rstd=S.sbuf(stA, "rstd", [128, 1], F32), nmr=S.sbuf(stA, "nmr", [128, 1], F32),
                           z=S.sbuf(stA, "z", [128, DM], F32))
                xt = [S.sbuf(stA, "xt%d" % i, [128, DM], F32) for i in range(2)]

                with ExitStack() as st1:
                    xnb = [S.sbuf(st1, "xnb%d" % i, [128, DM], BF16) for i in range(2)]
                    ptr = [S.psum(st1, "ptr%d" % i, [128, 1024], BF16) for i in range(2)]
                    for i in range(NT):
                        xs = xt[i % 2]
                        S.dma("sp", xs[:], x[b, i * 128:(i + 1) * 128, :], writes=[xs])
                        layer_norm_tile(lns, xs, None, 0, None)
                        z = lns["z"]
                        xb = xnb[i % 2]
                        S.op("dve", lambda e: e.tensor_tensor(xb[:], z[:], lnbc[:, 1, :], ALU.add),
                             reads=[z, lnbc], writes=[xb])
                        pt = ptr[i % 2]
                        for c in range(8):
                            S.op("pe", lambda e: e.transpose(pt[:, c * 128:(c + 1) * 128], xb[:, c * 128:(c + 1) * 128], identb[:]),
                                 reads=[xb, identb], writes=[pt], inc=(c == 7))
                        S.op("act", lambda e: e.activation(xnT[:, :, i * 128:(i + 1) * 128],
                                                           pt[:].rearrange("p (c t) -> p c t", c=8), AF.Copy),
                             reads=[pt], writes=[xnT])
                    S.barrier()

                with ExitStack() as st2:
                    pp = [S.psum(st2, "pp%d" % i, [128, 512], F32) for i in range(3)]
                    ps = [S.psum(st2, "ps%d" % i, [128, 512], F32) for i in range(3)]
                    po = [S.psum(st2, "po%d" % i, [128, 512], F32) for i in range(2)]
                    Wa = S.sbuf(st2, "Wa", [128, 8, 416], BF16)
                    Wkrr = S.sbuf(st2, "Wkrr", [128, 8, 32], BF16)
                    Wq = S.sbuf(st2, "Wq", [128, 2, 8, 96], BF16)
                    Wqr = S.sbuf(st2, "Wqr", [128, 2, 8, 32], BF16)
                    Wk = S.sbuf(st2, "Wk", [128, 8, 64], BF16)
                    Wv = S.sbuf(st2, "Wv", [128, 512], BF16)
                    gq = S.sbuf(st2, "gq", [128, 2], F32)
                    gkv = S.sbuf(st2, "gkv", [128, 1], F32)
                    invfS = S.sbuf(st2, "invfS", [96, 1], F32)
                    cos32 = S.sbuf(st2, "cos32", [96, T], F32)
                    sin32 = S.sbuf(st2, "sin32", [96, T], F32)
                    cqn = S.sbuf(st2, "cqn", [128, 2, T], BF16)
                    ckvn = S.sbuf(st2, "ckvn", [128, T], BF16)
                    krT = S.sbuf(st2, "krT", [96, T], BF16)
                    st2a = ExitStack()
                    st2a.__enter__()
                    wq32 = S.sbuf(st2a, "wq32", [128, 2, 768], F32)
                    wk32 = S.sbuf(st2a, "wk32", [128, 512], F32)
                    wv32 = S.sbuf(st2a, "wv32", [128, 512], F32)
                    posi = S.sbuf(st2a, "posi", [96, T], I32)
                    rr = S.sbuf(st2a, "rr", [96, T], F32)
                    rf = S.sbuf(st2a, "rf", [96, T], F32)
                    tq = S.sbuf(st2a, "tq", [96, T], F32)

                    wsrc = w_in.rearrange("(c p) n -> p c n", p=128)
                    S.dma("pool", Wa[:], wsrc[:, :, 0:416], writes=[Wa])
                    S.dma("sp", wq32[:], w_uq.rearrange("(c p) n -> p c n", p=128), writes=[wq32])
                    S.dma("sp", wk32[:], w_uk, writes=[wk32])
                    S.dma("sp", wv32[:], w_uv, writes=[wv32])
                    S.dma("sp", gq[:], qng, writes=[gq])
                    S.dma("sp", gkv[:], kvg, writes=[gkv])
                    S.dma("sp", invfS[64:96, :], invf, writes=[invfS])
                    S.dma("sp", posi[64:96, :], pos[b].partition_broadcast(32), writes=[posi])
                    S.op("dve", lambda e: e.tensor_scalar(Wkrr[:, :, 0:16], Wa[:, :, 400:416], -1.0, None, ALU.mult),
                         reads=[Wa], writes=[Wkrr])
                    S.op("dve", lambda e: e.tensor_copy(Wkrr[:, :, 16:32], Wa[:, :, 384:400]), reads=[Wa], writes=[Wkrr])
                    for c in range(2):
                        src = wq32[:, c, :].rearrange("p (h d) -> p h d", h=8)
                        g = gq[:, c:c + 1]
                        S.op("dve", lambda e: e.tensor_scalar(Wq[:, c, :, :], src[:, :, :], g, None, ALU.mult),
                             reads=[wq32, gq], writes=[Wq])
                        S.op("dve", lambda e: e.tensor_scalar(Wqr[:, c, :, 0:16], src[:, :, 80:96], g, -1.0, ALU.mult, ALU.mult),
                             reads=[wq32, gq], writes=[Wqr])
                        S.op("dve", lambda e: e.tensor_scalar(Wqr[:, c, :, 16:32], src[:, :, 64:80], g, None, ALU.mult),
                             reads=[wq32, gq], writes=[Wqr])
                    S.op("dve", lambda e: e.tensor_scalar(Wk[:, :, :], wk32[:].rearrange("p (h d) -> p h d", h=8),
                                                          gkv[:, 0:1], None, ALU.mult), reads=[wk32, gkv], writes=[Wk])
                    S.op("dve", lambda e: e.tensor_scalar(Wv[:], wv32[:], gkv[:, 0:1], None, ALU.mult),
                         reads=[wv32, gkv], writes=[Wv])

                    S.op("dve", lambda e: e.tensor_copy(rr[64:96, :], posi[64:96, :]), reads=[posi], writes=[rr])
                    S.op("dve", lambda e: e.tensor_scalar(rr[64:96, :], rr[64:96, :], invfS[64:96, 0:1], None, ALU.mult), reads=[rr, invfS], writes=[rr])
                    S.op("dve", lambda e: e.tensor_copy(posi[64:96, :], rr[64:96, :]), reads=[rr], writes=[posi])
                    S.op("dve", lambda e: e.tensor_copy(rf[64:96, :], posi[64:96, :]), reads=[posi], writes=[rf])
                    S.op("dve", lambda e: e.tensor_tensor(rr[64:96, :], rr[64:96, :], rf[64:96, :], ALU.subtract), reads=[rr, rf], writes=[rr])

                    def wrap_sin(dst, shift):
                        S.op("dve", lambda e: e.tensor_scalar(rf[64:96, :], rr[64:96, :], shift, None, ALU.add), reads=[rr], writes=[rf])
                        for _ in range(2):
                            S.op("dve", lambda e: e.tensor_scalar(tq[64:96, :], rf[64:96, :], 0.5, None, ALU.is_gt), reads=[rf], writes=[tq])
                            S.op("dve", lambda e: e.tensor_tensor(rf[64:96, :], rf[64:96, :], tq[64:96, :], ALU.subtract), reads=[rf, tq], writes=[rf])
                        S.op("dve", lambda e: e.tensor_scalar(tq[64:96, :], rf[64:96, :], -0.5, None, ALU.is_lt), reads=[rf], writes=[tq])
                        S.op("dve", lambda e: e.tensor_tensor(rf[64:96, :], rf[64:96, :], tq[64:96, :], ALU.add), reads=[rf, tq], writes=[rf])
                        S.op("act", lambda e: e.activation(dst[64:96, :], rf[64:96, :], AF.Sin, scale=2.0 * math.pi * (1.0 - 2e-6)),
                             reads=[rf], writes=[dst])

                    wrap_sin(sin32, 0.0)
                    wrap_sin(cos32, 0.25)
                    S.barrier()
                    st2a.close()
                    Vh = [S.sbuf(st2, "Vh%d" % i, [128, NT, 128], BF16) for i in range(2)]
                    qTs = [S.sbuf(st2, "qT%d" % i, [96, T], BF16) for i in range(2)]
                    kTs = [S.sbuf(st2, "kT%d" % i, [96, T], BF16) for i in range(2)]
                    c32 = S.sbuf(st2, "c32", [128, 2, 512], F32)
                    sqb = S.sbuf(st2, "sqb", [128, 2, 512], BF16)
                    rq = S.sbuf(st2, "rq", [128, 512], F32)
                    t1 = S.sbuf(st2, "t1", [96, 512], F32)
                    t2 = S.sbuf(st2, "t2", [96, 512], F32)
                    PTs = [S.sbuf(st2, "PT%d" % i, [128, 512], BF16) for i in range(3)]
                    rd = S.sbuf(st2, "rd", [128, 512], F32)
                    for i in range(2):
                        S.op("pool", lambda e: e.memset(Vh[i][:], 1.0), writes=[Vh[i]])

                    def rms_block(psrc_list, dstT_fn, nfeat, tb):
                        n = len(psrc_list)
                        for m, pb in enumerate(psrc_list):
                            S.op("act", lambda e: e.activation(c32[:, m, :], pb[:], AF.Copy), reads=[pb], writes=[c32])
                            S.op("act", lambda e: e.activation(sqb[:, m, :], pb[:], AF.Square), reads=[pb], writes=[sqb])
                        pss = pp[2]
                        for m in range(n):
                            S.op("pe", lambda e: e.matmul(pss[:], onesb[:], sqb[:, m, :], start=(m == 0), stop=(m == n - 1)),
                                 reads=[onesb, sqb], writes=[pss], inc=(m == n - 1))
                        S.op("dve", lambda e: e.tensor_scalar(rq[:], pss[:], 1.0 / nfeat, RMS_EPS, ALU.mult, ALU.add),
                             reads=[pss], writes=[rq])
                        S.op("act", lambda e: e.activation(rq[:], rq[:], AF.Sqrt), reads=[rq], writes=[rq])
                        S.op("dve", lambda e: e.reciprocal(rq[:], rq[:]), reads=[rq], writes=[rq])
                        for m in range(n):
                            S.op("dve", lambda e: e.tensor_tensor(dstT_fn(m), c32[:, m, :], rq[:], ALU.mult),
                                 reads=[c32, rq], writes=[dstT_fn.buf])

                    def proj_fm(pb, lhs_fn, tb, M=128, p0=0):
                        for c in range(8):
                            S.op("pe", lambda e: e.matmul(pb[p0:p0 + M, :], lhs_fn(c), xnT[:, c, tb * 512:(tb + 1) * 512],
                                                          start=(c == 0), stop=(c == 7)),
                                 reads=[xnT, Wa, Wkrr], writes=[pb], inc=(c == 7))

                    for tb in range(4):
                        cols = slice(tb * 512, (tb + 1) * 512)
                        proj_fm(pp[0], lambda c: Wa[:, c, 0:128], tb)
                        proj_fm(pp[1], lambda c: Wa[:, c, 128:256], tb)
                        f = lambda m: cqn[:, m, cols]
                        f.buf = cqn
                        rms_block([pp[0], pp[1]], f, 256.0, tb)
                        proj_fm(pp[0], lambda c: Wa[:, c, 256:384], tb)
                        f2 = lambda m: ckvn[:, cols]
                        f2.buf = ckvn
                        rms_block([pp[0]], f2, 128.0, tb)
                        proj_fm(pp[0], lambda c: Wa[:, c, 384:416], tb, M=32, p0=64)
                        proj_fm(pp[1], lambda c: Wkrr[:, c, :], tb, M=32, p0=64)
                        S.op("dve", lambda e: e.tensor_tensor(t1[64:96, :], pp[0][64:96, :], cos32[64:96, cols], ALU.mult),
                             reads=[pp[0], cos32], writes=[t1])
                        S.op("dve", lambda e: e.tensor_tensor(t2[64:96, :], pp[1][64:96, :], sin32[64:96, cols], ALU.mult),
                             reads=[pp[1], sin32], writes=[t2])
                        S.op("dve", lambda e: e.tensor_tensor(krT[64:96, cols], t1[64:96, :], t2[64:96, :], ALU.add), reads=[t1, t2], writes=[krT])
                    sc_mla = 96.0 ** -0.5
                    LA = 2

                    def mla_proj(h):
                        qT = qTs[h % 2]
                        kT = kTs[h % 2]
                        for tb in range(4):
                            cols = slice(tb * 512, (tb + 1) * 512)
                            pa, pbb = pp[0], pp[1]
                            for m in range(2):
                                S.op("pe", lambda e: e.matmul(pa[0:96, :], Wq[:, m, h, :], cqn[:, m, cols], start=(m == 0), stop=(m == 1)),
                                     reads=[Wq, cqn], writes=[pa], inc=(m == 1))
                            for m in range(2):
                                S.op("pe", lambda e: e.matmul(pbb[64:96, :], Wqr[:, m, h, :], cqn[:, m, cols], start=(m == 0), stop=(m == 1)),
                                     reads=[Wqr, cqn], writes=[pbb], inc=(m == 1))
                            pk = pp[2]
                            S.op("pe", lambda e: e.matmul(pk[0:64, :], Wk[:, h, :], ckvn[:, cols], start=True, stop=True),
                                 reads=[Wk, ckvn], writes=[pk])
                            S.op("dve", lambda e: e.tensor_tensor(t1[64:96, :], pa[64:96, :], cos32[64:96, cols], ALU.mult), reads=[pa, cos32], writes=[t1])
                            S.op("dve", lambda e: e.tensor_tensor(t2[64:96, :], pbb[64:96, :], sin32[64:96, cols], ALU.mult), reads=[pbb, sin32], writes=[t2])
                            S.op("dve", lambda e: e.tensor_tensor(qT[64:96, cols], t1[64:96, :], t2[64:96, :], ALU.add), reads=[t1, t2], writes=[qT])
                            S.op("act", lambda e: e.activation(qT[0:64, cols], pa[0:64, :], AF.Copy), reads=[pa], writes=[qT])
                            S.op("act", lambda e: e.activation(kT[0:64, cols], pk[0:64, :], AF.Copy), reads=[pk], writes=[kT])
                        S.op("pool", lambda e: e.tensor_copy(kT[64:96, :], krT[64:96, :]), reads=[krT], writes=[kT])
                        Vc = Vh[h % 2]
                        voff = 64 * (h % 2)
                        for k4 in range(4):
                            pv = pp[k4 % 2]
                            for j in range(4):
                                kt = k4 * 4 + j
                                S.op("pe", lambda e: e.matmul(pv[:, j * 64:(j + 1) * 64], ckvn[:, kt * 128:(kt + 1) * 128],
                                                              Wv[:, h * 64:(h + 1) * 64], start=True, stop=True),
                                     reads=[ckvn, Wv], writes=[pv], inc=(j == 3))
                            S.op("act", lambda e: e.activation(Vc[:, k4 * 4:(k4 + 1) * 4, voff:voff + 64],
                                                               pv[:, 0:256].rearrange("p (j d) -> p j d", j=4), AF.Copy),
                                 reads=[pv], writes=[Vc])

                    st_ = dict(si=0, oi=0)

                    def mla_attn(h):
                        conv_some(6)
                        qT = qTs[h % 2]
                        kT = kTs[h % 2]
                        Vc = Vh[h % 2]
                        for qb in range(4):
                            pO = po[st_["oi"] % 2]
                            st_["oi"] += 1
                            nk = 4 * qb + 4
                            slots = {}

                            def emit_S(kt):
                                c0 = max(0, kt - 4 * qb) * 128
                                sl = st_["si"] % 3
                                st_["si"] += 1
                                slots[kt] = sl
                                pS, PT = ps[sl], PTs[sl]
                                S.op("pe", lambda e: e.matmul(pS[:, c0:512], kT[:, kt * 128:(kt + 1) * 128],
                                                              qT[:, qb * 512 + c0:(qb + 1) * 512], start=True, stop=True),
                                     reads=[kT, qT], writes=[pS])
                                S.op("act", lambda e: e.activation(PT[:, c0:512], pS[:, c0:512], AF.Exp, scale=sc_mla),
                                     reads=[pS], writes=[PT])
                                if kt >= 4 * qb:
                                    S.op("pool", lambda e: e.memset(PT[64:128, c0:c0 + 64], 0.0), writes=[PT])

                            def emit_PV(kt):
                                c0 = max(0, kt - 4 * qb) * 128
                                PT = PTs[slots[kt]]
                                S.op("pe", lambda e: e.matmul(pO[:, c0:512], Vc[:, kt, :], PT[:, c0:512],
                                                              start=(kt == 0), stop=(kt == nk - 1), skip_group_check=True),
                                     reads=[Vc, PT], writes=[pO], inc=(kt == nk - 1))

                            for s_ in range(nk + LA):
                                if s_ < nk:
                                    emit_S(s_)
                                if s_ >= LA:
                                    emit_PV(s_ - LA)
                            ocols = slice(qb * 512, (qb + 1) * 512)
                            if h % 2 == 0:
                                S.op("dve", lambda e: e.reciprocal(rd[0:64, :], pO[64:128, :]), reads=[pO], writes=[rd])
                                S.op("dve", lambda e: e.tensor_tensor(oaT[0:64, h // 2, ocols], pO[0:64, :], rd[0:64, :], ALU.mult),
                                     reads=[pO, rd], writes=[oaT])
                            else:
                                S.op("dve", lambda e: e.reciprocal(rd[64:128, :], pO[0:64, :]), reads=[pO], writes=[rd])
                                S.op("dve", lambda e: e.tensor_tensor(oaT[64:128, h // 2, ocols], pO[64:128, :], rd[64:128, :], ALU.mult),
                                     reads=[pO, rd], writes=[oaT])

                    mla_proj(0)
                    for h in range(8):
                        if h + 1 < 8:
                            mla_proj(h + 1)
                        mla_attn(h)
                    S.barrier()

                with ExitStack() as st3:
                    pp = [S.psum(st3, "qp%d" % i, [128, 512], F32) for i in range(2)]
                    ps = [S.psum(st3, "qs%d" % i, [128, 512], F32) for i in range(3)]
                    po = [S.psum(st3, "qo%d" % i, [128, 512], F32) for i in range(2)]
                    pmt = S.psum(st3, "pmt", [128, 1024], BF16)
                    qbT = S.sbuf(st3, "qbT", [128, 4, T], BF16)
                    kbT2 = S.sbuf(st3, "kbT2", [128, T], BF16)
                    qiT = S.sbuf(st3, "qiT", [128, 2, T], BF16)
                    kiT4 = S.sbuf(st3, "kiT4", [128, T], BF16)
                    Vb = S.sbuf(st3, "Vb", [128, NT, 2, 128], BF16)
                    widx = S.sbuf(st3, "widx", [128, NT, 8], F32)
                    tz = S.sbuf(st3, "tz", [128, 8, 2, 128], F32)
                    cfb = S.sbuf(st3, "cfb", [128, 8], F32)
                    st3a = ExitStack()
                    st3a.__enter__()
                    Wb = S.sbuf(st3a, "Wb", [128, 8, 936], BF16)
                    Wkb2 = S.sbuf(st3a, "Wkb2", [128, 8, 128], BF16)
                    Wki4 = S.sbuf(st3a, "Wki4", [128, 8, 128], BF16)

                    wsrc = w_in.rearrange("(c p) n -> p c n", p=128)
                    S.dma("pool", Wb[:], wsrc[:, :, 416:1352], writes=[Wb])
                    for r in range(2):
                        S.dma("pool", Wkb2[:, :, r * 64:(r + 1) * 64], wsrc[:, :, 928:992], writes=[Wkb2])
                    for r in range(4):
                        S.dma("pool", Wki4[:, :, r * 32:(r + 1) * 32], wsrc[:, :, 1312:1344], writes=[Wki4])
                    S.dma("sp", tz[:], tzr, writes=[tz])
                    S.dma("sp", cfb[:], cfar.partition_broadcast(128), writes=[cfb])
                    for h in range(8):
                        S.op("dve", lambda e: e.tensor_scalar(tz[:, h], tz[:, h], cfb[:, h:h + 1], 8.0, ALU.subtract, ALU.mult),
                             reads=[tz, cfb], writes=[tz])
                    S.op("pool", lambda e: e.memset(Vb[:], 1.0), writes=[Vb])

                    def proj3(dst_ap, dstbuf, lhs_fn, wbuf, tb, k):
                        pb = pp[k % 2]
                        for c in range(8):
                            S.op("pe", lambda e: e.matmul(pb[:], lhs_fn(c), xnT[:, c, tb * 512:(tb + 1) * 512],
                                                          start=(c == 0), stop=(c == 7)), reads=[xnT, wbuf], writes=[pb], inc=(c == 7))
                        S.op("act", lambda e: e.activation(dst_ap, pb[:], AF.Copy), reads=[pb], writes=[dstbuf])

                    k = 0
                    for tb in range(4):
                        cols = slice(tb * 512, (tb + 1) * 512)
                        for p in range(4):
                            proj3(qbT[:, p, cols], qbT, lambda c: Wb[:, c, p * 128:(p + 1) * 128], Wb, tb, k); k += 1
                        proj3(kbT2[:, cols], kbT2, lambda c: Wkb2[:, c, :], Wkb2, tb, k); k += 1
                        for g in range(2):
                            proj3(qiT[:, g, cols], qiT, lambda c: Wb[:, c, 640 + g * 128:640 + (g + 1) * 128], Wb, tb, k); k += 1
                        proj3(kiT4[:, cols], kiT4, lambda c: Wki4[:, c, :], Wki4, tb, k); k += 1
                    for kt in range(NT):
                        pb = pp[kt % 2]
                        tsl = slice(kt * 128, (kt + 1) * 128)
                        for c in range(8):
                            S.op("pe", lambda e: e.matmul(pb[:, 0:64], xnT[:, c, tsl], Wb[:, c, 576:640], start=(c == 0), stop=(c == 7)),
                                 reads=[xnT, Wb], writes=[pb], inc=(c == 7))
                        for c in range(8):
                            S.op("pe", lambda e: e.matmul(pb[:, 64:72], xnT[:, c, tsl], Wb[:, c, 928:936], start=(c == 0), stop=(c == 7)),
                                 reads=[xnT, Wb], writes=[pb], inc=(c == 7))
                        S.op("act", lambda e: e.activation(Vb[:, kt, 0, 0:64], pb[:, 0:64], AF.Copy), reads=[pb], writes=[Vb])
                        S.op("act", lambda e: e.activation(Vb[:, kt, 1, 64:128], pb[:, 0:64], AF.Copy), reads=[pb], writes=[Vb])
                        S.op("act", lambda e: e.activation(widx[:, kt, :], pb[:, 64:72], AF.Copy, scale=0.0625), reads=[pb], writes=[widx])

                    S.barrier()
                    st3a.close()
                    Sc = [S.sbuf(st3, "Sc%d" % i, [128, T], F32) for i in range(2)]
                    msk = [S.sbuf(st3, "msk%d" % i, [128, T], BF16) for i in range(2)]
                    mskT = S.sbuf(st3, "mskT", [128, NT, 512], BF16)
                    rl = [S.sbuf(st3, "rl%d" % i, [128, 512], F32) for i in range(2)]
                    bmx = S.sbuf(st3, "bmx", [128, 1], F32)
                    bmn = S.sbuf(st3, "bmn", [128, 1], F32)
                    brg = S.sbuf(st3, "brg", [128, 1], F32)
                    bmid = S.sbuf(st3, "bmid", [128, 1], F32)
                    bcnt = S.sbuf(st3, "bcnt", [128, 1], F32)
                    bt = S.sbuf(st3, "bt", [128, 1], F32)
                    Es = [S.sbuf(st3, "E%d" % i, [128, 512], BF16) for i in range(3)]
                    PTs = [S.sbuf(st3, "PTb%d" % i, [128, 512], BF16) for i in range(3)]
                    rd = S.sbuf(st3, "rdb", [128, 512], F32)
                    sidx = [0]
                    oi = 0

                    def idx_steps(qt):
                        n = (qt + 1) * 128
                        sc = Sc[qt % 2]
                        tsl = slice(qt * 128, (qt + 1) * 128)
                        steps = []
                        for hh in range(8):
                            for sb in range((n + 511) // 512):
                                def step(hh=hh, sb=sb):
                                    g, jj = hh // 4, hh % 4
                                    w = min(512, n - sb * 512)
                                    k = ridx[0]
                                    ridx[0] += 1
                                    pr = pp[k % 2]
                                    rlb = rl[k % 2]
                                    S.op("pe", lambda e: e.matmul(pr[:, 0:w], qiT[32 * jj:32 * jj + 32, g, tsl],
                                                                  kiT4[32 * jj:32 * jj + 32, sb * 512:sb * 512 + w],
                                                                  start=True, stop=True, tile_position=(32 * jj, 0)),
                                         reads=[qiT, kiT4], writes=[pr])
                                    S.op("act", lambda e: e.activation(rlb[:, 0:w], pr[:, 0:w], AF.Relu), reads=[pr], writes=[rlb])
                                    dst = sc[:, sb * 512:sb * 512 + w]
                                    if hh == 0:
                                        S.op("dve", lambda e: e.tensor_scalar(dst, rlb[:, 0:w], widx[:, qt, 0:1], None, ALU.mult),
                                             reads=[rlb, widx], writes=[sc])
                                    else:
                                        S.op("dve", lambda e: e.scalar_tensor_tensor(dst, rlb[:, 0:w], widx[:, qt, hh:hh + 1], dst,
                                                                                     ALU.mult, ALU.add),
                                             reads=[rlb, widx, sc], writes=[sc])
                                steps.append(step)
                        return steps

                    def bisect(qt, filler):
                        n = (qt + 1) * 128
                        sc = Sc[qt % 2]
                        mk = msk[qt % 2]
                        S.op("pool", lambda e: e.memset(sc[0:64, n - 64:n], NEGBIG), writes=[sc])
                        per = (len(filler) + BIS_ITERS - 1) // BIS_ITERS if qt >= 2 else len(filler)
                        if qt >= 2:
                            S.op("dve", lambda e: e.reduce_max(bmx[:], sc[:, 0:n], AX.X), reads=[sc], writes=[bmx])
                            S.op("dve", lambda e: e.tensor_reduce(bmn[:], sc[:, 0:n - 64], AX.X, ALU.min), reads=[sc], writes=[bmn])
                            S.op("dve", lambda e: e.tensor_tensor(brg[:], bmx[:], bmn[:], ALU.subtract), reads=[bmx, bmn], writes=[brg])
                            S.op("dve", lambda e: e.tensor_scalar(brg[:], brg[:], 1e-20, None, ALU.add), reads=[brg], writes=[brg])
                            S.op("dve", lambda e: e.reciprocal(brg[:], brg[:]), reads=[brg], writes=[brg])
                            S.op("dve", lambda e: e.tensor_scalar(sc[:, 0:n], sc[:, 0:n], bmn[:, 0:1], brg[:, 0:1], ALU.subtract, ALU.mult),
                                 reads=[sc, bmn, brg], writes=[sc])
                            S.op("dve", lambda e: e.memset(bmid[:], 0.5), writes=[bmid])
                            for it in range(BIS_ITERS):
                                S.op("dve", lambda e: e.tensor_scalar(mk[:, 0:n], sc[:, 0:n], bmid[:, 0:1], None, ALU.is_ge, ALU.add,
                                                                      accum_out=bcnt[:]),
                                     reads=[sc, bmid], writes=[mk, bcnt])
                                for _ in range(per):
                                    if filler:
                                        filler.pop(0)()
                                s_next = 2.0 ** -(it + 2)
                                S.op("dve", lambda e: e.tensor_scalar(bt[:], bcnt[:], float(TOPK), 2.0 * s_next, ALU.is_ge, ALU.mult),
                                     reads=[bcnt], writes=[bt])
                                S.op("dve", lambda e: e.scalar_tensor_tensor(bmid[:], bt[:], -s_next, bmid[:], ALU.add, ALU.add),
                                     reads=[bt, bmid], writes=[bmid])
                            S.op("dve", lambda e: e.tensor_scalar(bmid[:], bmid[:], -(2.0 ** -(BIS_ITERS + 1)), None, ALU.add),
                                 reads=[bmid], writes=[bmid])
                            S.op("dve", lambda e: e.tensor_scalar(mk[:, 0:n], sc[:, 0:n], bmid[:, 0:1], None, ALU.is_ge),
                                 reads=[sc, bmid], writes=[mk])
                        else:
                            S.op("dve", lambda e: e.tensor_scalar(mk[:, 0:n], sc[:, 0:n], -1.0e29, None, ALU.is_ge),
                                 reads=[sc], writes=[mk])
                        while filler:
                            filler.pop(0)()

                    def mask_transpose(qt):
                        mk = msk[qt % 2]
                        j = qt % 4
                        for k0 in range(0, qt + 1, 8):
                            nk_ = min(8, qt + 1 - k0)
                            for q in range(nk_):
                                kt = k0 + q
                                S.op("pe", lambda e: e.transpose(pmt[:, q * 128:(q + 1) * 128], mk[:, kt * 128:(kt + 1) * 128], identb[:]),
                                     reads=[mk, identb], writes=[pmt], inc=(q == nk_ - 1))
                            S.op("act", lambda e: e.activation(mskT[:, k0:k0 + nk_, j * 128:(j + 1) * 128],
                                                               pmt[:, 0:nk_ * 128].rearrange("p (q t) -> p q t", q=nk_), AF.Copy),
                                 reads=[pmt], writes=[mskT])

                    ridx = [0]
                    for st_ in idx_steps(0):
                        st_()
                    for qt_ in range(NT):
                        filler = idx_steps(qt_ + 1) if qt_ + 1 < NT else []
                        bisect(qt_, filler)
                        mask_transpose(qt_)
                        if qt_ % 4 != 3:
                            continue
                        qb = qt_ // 4
                        for h in range(8):
                            conv_some(2)
                            p, hf = h // 2, h % 2
                            base = 64 * hf
                            pO = po[oi % 2]
                            oi += 1
                            nk = 4 * qb + 4
                            slots = {}

                            def emit_S(kt):
                                global_si = sidx[0]
                                sidx[0] += 1
                                sl = global_si % 3
                                slots[kt] = sl
                                j0 = max(0, kt - 4 * qb)
                                c0 = j0 * 128
                                pS, E, PT = ps[sl], Es[sl], PTs[sl]
                                S.op("pe", lambda e: e.matmul(pS[:, c0:512], kbT2[base:base + 64, kt * 128:(kt + 1) * 128],
                                                              qbT[base:base + 64, p, qb * 512 + c0:(qb + 1) * 512], start=True, stop=True),
                                     reads=[kbT2, qbT], writes=[pS])
                                for j in range(j0, 4):
                                    d = 4 * qb + j - kt
                                    if d in (0, 1):
                                        S.op("dve", lambda e: e.tensor_tensor(pS[:, j * 128:(j + 1) * 128], pS[:, j * 128:(j + 1) * 128],
                                                                              tz[:, h, d, :], ALU.add), reads=[pS, tz], writes=[pS])
                                S.op("act", lambda e: e.activation(E[:, c0:512], pS[:, c0:512], AF.Exp, bias=cfb[:, h:h + 1], scale=0.125),
                                     reads=[pS, cfb], writes=[E])
                                S.op("dve", lambda e: e.tensor_tensor(PT[:, c0:512], E[:, c0:512], mskT[:, kt, c0:512], ALU.mult),
                                     reads=[E, mskT], writes=[PT])

                            def emit_PV(kt):
                                c0 = max(0, kt - 4 * qb) * 128
                                PT = PTs[slots[kt]]
                                S.op("pe", lambda e: e.matmul(pO[:, c0:512], Vb[:, kt, hf, :], PT[:, c0:512],
                                                              start=(kt == 0), stop=(kt == nk - 1), skip_group_check=True),
                                     reads=[Vb, PT], writes=[pO], inc=(kt == nk - 1))

                            for s_ in range(nk + 2):
                                if s_ < nk:
                                    emit_S(s_)
                                if s_ >= 2:
                                    emit_PV(s_ - 2)
                            ocols = slice(qb * 512, (qb + 1) * 512)
                            if hf == 0:
                                S.op("act", lambda e: e.activation(rd[0:64, :], pO[64:128, :], AF.Ln), reads=[pO], writes=[rd])
                                S.op("act", lambda e: e.activation(rd[0:64, :], rd[0:64, :], AF.Exp, scale=-1.0), reads=[rd], writes=[rd])
                                S.op("dve", lambda e: e.tensor_tensor(obT[0:64, p, ocols], pO[0:64, :], rd[0:64, :], ALU.mult),
                                     reads=[pO, rd], writes=[obT])
                            else:
                                S.op("act", lambda e: e.activation(rd[64:128, :], pO[0:64, :], AF.Ln), reads=[pO], writes=[rd])
                                S.op("act", lambda e: e.activation(rd[64:128, :], rd[64:128, :], AF.Exp, scale=-1.0), reads=[rd], writes=[rd])
                                S.op("dve", lambda e: e.tensor_tensor(obT[64:128, p, ocols], pO[64:128, :], rd[64:128, :], ALU.mult),
                                     reads=[pO, rd], writes=[obT])
                    S.barrier()

                with ExitStack() as st4:
                    Wo = S.sbuf(st4, "Wo", [128, 8, DM], BF16)
                    mixT = S.sbuf(st4, "mixT", [128, 8, T], BF16)
                    bg = S.sbuf(st4, "bg", [128, 16], F32)
                    Wr = S.sbuf(st4, "Wr", [128, 8, 36], F32)
                    brb = S.sbuf(st4, "brb", [128, 36], F32)
                    st4a = ExitStack()
                    st4a.__enter__()
                    Wua = S.sbuf(st4a, "Wua", [128, 4, DM], BF16)
                    Wub = S.sbuf(st4a, "Wub", [128, 4, DM], BF16)
                    Wg = [S.sbuf(st4a, "Wg%d" % i, [128, 8, 2, 128], BF16) for i in range(2)]
                    sga = S.sbuf(st4a, "sga", [128, 512], F32)
                    sgb = S.sbuf(st4a, "sgb", [128, 512], F32)
                    m1 = S.sbuf(st4a, "m1", [128, 512], F32)
                    m2 = S.sbuf(st4a, "m2", [128, 512], F32)
                    pgs = [[S.psum(st4a, "pg%d_%d" % (q, i), [128, 512], F32) for i in range(4)] for q in range(2)]
                    sgas = [sga, S.sbuf(st4a, "sga2", [128, 512], F32)]
                    sgbs = [sgb, S.sbuf(st4a, "sgb2", [128, 512], F32)]
                    m1s = [m1, S.sbuf(st4a, "m1b", [128, 512], F32)]
                    m2s = [m2, S.sbuf(st4a, "m2b", [128, 512], F32)]
                    git = 0

                    S.dma("pool", Wua[:], w_up_a.rearrange("(c p) n -> p c n", p=128), writes=[Wua])
                    S.dma("pool", Wub[:], w_up_b.rearrange("(c p) n -> p c n", p=128), writes=[Wub])
                    S.dma("pool", Wo[:], w_o.rearrange("(c p) n -> p c n", p=128), writes=[Wo])
                    S.dma("sp", bg[:], bgate, writes=[bg])
                    S.dma("sp", Wr[:], w_r.rearrange("(c p) n -> p c n", p=128), writes=[Wr])
                    S.dma("sp", brb[:], b_r.partition_broadcast(128), writes=[brb])
                    gsrc = w_gate.rearrange("(c p) n -> p c n", p=128)
                    for m in range(8):
                        wg = Wg[m % 2]
                        S.dma("pool", wg[:, :, 0, :], gsrc[:, :, m * 128:(m + 1) * 128], writes=[wg])
                        S.dma("pool", wg[:, :, 1, :], gsrc[:, :, 1024 + m * 128:1024 + (m + 1) * 128], writes=[wg])
                        for tb in range(4):
                            cols = slice(tb * 512, (tb + 1) * 512)
                            pg = pgs[git % 2]
                            sga, sgb, m1, m2 = sgas[git % 2], sgbs[git % 2], m1s[git % 2], m2s[git % 2]
                            git += 1
                            for c in range(8):
                                S.op("pe", lambda e: e.matmul(pg[0][:], wg[:, c, 0, :], xnT[:, c, cols], start=(c == 0), stop=(c == 7)),
                                     reads=[wg, xnT], writes=[pg[0]], inc=(c == 7))
                            for c in range(8):
                                S.op("pe", lambda e: e.matmul(pg[1][:], wg[:, c, 1, :], xnT[:, c, cols], start=(c == 0), stop=(c == 7)),
                                     reads=[wg, xnT], writes=[pg[1]], inc=(c == 7))
                            for c in range(4):
                                S.op("pe", lambda e: e.matmul(pg[2][:], Wua[:, c, m * 128:(m + 1) * 128], oaT[:, c, cols], start=(c == 0), stop=(c == 3)),
                                     reads=[Wua, oaT], writes=[pg[2]], inc=(c == 3))
                            for c in range(4):
                                S.op("pe", lambda e: e.matmul(pg[3][:], Wub[:, c, m * 128:(m + 1) * 128], obT[:, c, cols], start=(c == 0), stop=(c == 3)),
                                     reads=[Wub, obT], writes=[pg[3]], inc=(c == 3))
                            S.op("act", lambda e: e.activation(sga[:], pg[0][:], AF.Sigmoid, bias=bg[:, m:m + 1]), reads=[pg[0], bg], writes=[sga])
                            S.op("act", lambda e: e.activation(sgb[:], pg[1][:], AF.Sigmoid, bias=bg[:, 8 + m:9 + m]), reads=[pg[1], bg], writes=[sgb])
                            S.op("dve", lambda e: e.tensor_tensor(m1[:], pg[2][:], sga[:], ALU.mult), reads=[pg[2], sga], writes=[m1])
                            S.op("dve", lambda e: e.tensor_tensor(m2[:], pg[3][:], sgb[:], ALU.mult), reads=[pg[3], sgb], writes=[m2])
                            S.op("dve", lambda e: e.tensor_tensor(mixT[:, m, cols], m1[:], m2[:], ALU.add), reads=[m1, m2], writes=[mixT])
                    S.barrier()
                    st4a.close()
                    pg = [S.psum(st4, "pgt%d" % i, [128, 512], F32) for i in range(2)]
                    pm = [S.psum(st4, "pm%d" % i, [128, 512], F32) for i in range(2)]
                    pl = S.psum(st4, "pl", [128, 512], F32)
                    pre = S.sbuf(st4, "pre", [128, DM], F32)
                    hbs = [S.sbuf(st4, "hb%d" % i, [128, DM], BF16) for i in range(2)]
                    hst = [S.sbuf(st4, "hst%d" % i, [128, DM], F32) for i in range(2)]
                    hT32 = S.sbuf(st4, "hT32", [128, 8, 128], F32)
                    lgA = S.sbuf(st4, "lgA", [128, NT, 36], F32)
                    gmxA = S.sbuf(st4, "gmxA", [128, NT], F32)
                    gselA = S.sbuf(st4, "gselA", [128, NT, 4], F32)
                    r4A = S.sbuf(st4, "r4A", [128, NT, 4], F32)
                    gwA = S.sbuf(st4, "gwA", [128, NT], F32)
                    le4 = S.sbuf(st4, "le4", [128, NT, 4, 8], F32)
                    seA = S.sbuf(st4, "seA", [128, NT, 8], F32)
                    se2A = S.sbuf(st4, "se2A", [128, NT, 8], F32)
                    oh1A = S.sbuf(st4, "oh1A", [128, NT, 8], F32)
                    oh2A = S.sbuf(st4, "oh2A", [128, NT, 8], F32)
                    mx1A = S.sbuf(st4, "mx1A", [128, NT], F32)
                    mx2A = S.sbuf(st4, "mx2A", [128, NT], F32)
                    w1A = S.sbuf(st4, "w1A", [128, NT], F32)
                    w2A = S.sbuf(st4, "w2A", [128, NT], F32)
                    lnsB = [dict(stats=S.sbuf(st4, "statsB%d" % k, [128, 2, 6], F32), mv=S.sbuf(st4, "mvB%d" % k, [128, 2], F32),
                                 rstd=S.sbuf(st4, "rstdB%d" % k, [128, 1], F32), nmr=S.sbuf(st4, "nmrB%d" % k, [128, 1], F32),
                                 z=S.sbuf(st4, "zB%d" % k, [128, DM], F32)) for k in range(2)]

                    def tile_A(i):
                        tsl = slice(i * 128, (i + 1) * 128)
                        xs = xt[i % 2]
                        S.dma("sp", xs[:], x[b, tsl, :], writes=[xs])
                        layer_norm_tile(lns, xs, None, 0, None)
                        z = lns["z"]
                        S.op("dve", lambda e: e.tensor_tensor(z[:], z[:], lnbc[:, 1, :], ALU.add), reads=[z, lnbc], writes=[z])
                        for hf in range(2):
                            for m in range(8):
                                S.op("pe", lambda e: e.matmul(pm[hf][:], mixT[:, m, tsl], Wo[:, m, hf * 512:(hf + 1) * 512],
                                                              start=(m == 0), stop=(m == 7)), reads=[mixT, Wo], writes=[pm[hf]], inc=(m == 7))
                            S.op("dve", lambda e: e.scalar_tensor_tensor(pre[:, hf * 512:(hf + 1) * 512], z[:, hf * 512:(hf + 1) * 512],
                                                                         ALPHA, pm[hf][:], ALU.mult, ALU.add),
                                 reads=[z, pm[hf]], writes=[pre])
                        lb = lnsB[i % 2]
                        layer_norm_tile(lb, pre, None, 2, None)
                        z1 = lb["z"]
                        S.op("dve", lambda e: e.tensor_tensor(z1[:], z1[:], lnbc[:, 3, :], ALU.add), reads=[z1, lnbc], writes=[z1])

                    def tile_B(i):
                        tsl = slice(i * 128, (i + 1) * 128)
                        z = lnsB[i % 2]["z"]
                        hh = hst[i % 2]
                        hb = hbs[i % 2]
                        S.op("act", lambda e: e.activation(hb[:], z[:], AF.Copy), reads=[z], writes=[hb])
                        S.op("act", lambda e: e.activation(hh[:], z[:], AF.Copy, scale=ALPHA), reads=[z], writes=[hh])
                        S.dma("sp", hs[tsl, :], hh[:], reads=[hh], writes=[hsB], sembuf=hh)
                        S.dma("sp", hbd[tsl, :], hb[:], reads=[hb], writes=[hbdB], sembuf=hb)
                        for hf in range(2):
                            for c in range(4):
                                cc = hf * 4 + c
                                S.op("pe", lambda e: e.transpose(pg[hf][:, c * 128:(c + 1) * 128], z[:, cc * 128:(cc + 1) * 128], identF[:]),
                                     reads=[z, identF], writes=[pg[hf]], inc=(c == 3))
                            S.op("act", lambda e: e.activation(hT32[:, hf * 4:(hf + 1) * 4, :], pg[hf][:].rearrange("p (c t) -> p c t", c=4), AF.Copy),
                                 reads=[pg[hf]], writes=[hT32])
                        for c in range(8):
                            S.op("pe", lambda e: e.matmul(pl[:, 0:36], hT32[:, c, :], Wr[:, c, :], start=(c == 0), stop=(c == 7)),
                                 reads=[hT32, Wr], writes=[pl], inc=(c == 7))
                        S.op("dve", lambda e: e.tensor_tensor(lgA[:, i, :], pl[:, 0:36], brb[:], ALU.add), reads=[pl, brb], writes=[lgA])

                    tile_A(0)
                    for i in range(NT):
                        if i + 1 < NT:
                            tile_A(i + 1)
                        tile_B(i)
                    NTl = NT
                    G3 = lgA[:, :, 0:4]
                    E4 = lgA[:, :, 4:36].rearrange("p t (g e) -> p t g e", g=4)
                    bc_t = lambda ap2, n: ap2.rearrange("p (t o) -> p t o", o=1).to_broadcast([128, NTl, n])
                    S.op("dve", lambda e: e.tensor_reduce(gmxA[:], G3, AX.X, ALU.max), reads=[lgA], writes=[gmxA])
                    S.op("dve", lambda e: e.tensor_tensor(gselA[:], G3, bc_t(gmxA[:], 4), ALU.is_ge), reads=[lgA, gmxA], writes=[gselA])
                    S.op("dve", lambda e: e.tensor_tensor(r4A[:], G3, bc_t(gmxA[:], 4), ALU.subtract), reads=[lgA, gmxA], writes=[r4A])
                    S.op("act", lambda e: e.activation(r4A[:], r4A[:], AF.Exp), reads=[r4A], writes=[r4A])
                    S.op("dve", lambda e: e.tensor_reduce(gwA[:], r4A[:], AX.X, ALU.add), reads=[r4A], writes=[gwA])
                    S.op("dve", lambda e: e.reciprocal(gwA[:], gwA[:]), reads=[gwA], writes=[gwA])
                    gsel4 = gselA[:].rearrange("p t (g o) -> p t g o", o=1).to_broadcast([128, NTl, 4, 8])
                    S.op("dve", lambda e: e.tensor_tensor(le4[:], E4, gsel4, ALU.mult), reads=[lgA, gselA], writes=[le4])
                    S.op("dve", lambda e: e.tensor_reduce(seA[:], le4[:].rearrange("p t g e -> p t e g"), AX.X, ALU.add), reads=[le4], writes=[seA])
                    S.op("dve", lambda e: e.tensor_reduce(mx1A[:], seA[:], AX.X, ALU.max), reads=[seA], writes=[mx1A])
                    S.op("dve", lambda e: e.tensor_tensor(oh1A[:], seA[:], bc_t(mx1A[:], 8), ALU.is_ge), reads=[seA, mx1A], writes=[oh1A])
                    S.op("dve", lambda e: e.scalar_tensor_tensor(se2A[:], oh1A[:], NEGBIG, seA[:], ALU.mult, ALU.add), reads=[oh1A, seA], writes=[se2A])
                    S.op("dve", lambda e: e.tensor_reduce(mx2A[:], se2A[:], AX.X, ALU.max), reads=[se2A], writes=[mx2A])
                    S.op("dve", lambda e: e.tensor_tensor(oh2A[:], se2A[:], bc_t(mx2A[:], 8), ALU.is_ge), reads=[se2A, mx2A], writes=[oh2A])
                    S.op("dve", lambda e: e.tensor_tensor(w2A[:], mx2A[:], mx1A[:], ALU.subtract), reads=[mx1A, mx2A], writes=[w2A])
                    S.op("act", lambda e: e.activation(w2A[:], w2A[:], AF.Exp), reads=[w2A], writes=[w2A])
                    S.op("dve", lambda e: e.tensor_scalar(w1A[:], w2A[:], 1.0, None, ALU.add), reads=[w2A], writes=[w1A])
                    S.op("dve", lambda e: e.reciprocal(w1A[:], w1A[:]), reads=[w1A], writes=[w1A])
                    S.op("dve", lambda e: e.tensor_tensor(w2A[:], w2A[:], w1A[:], ALU.mult), reads=[w1A, w2A], writes=[w2A])
                    S.op("dve", lambda e: e.tensor_tensor(ca[:], w1A[:], gwA[:], ALU.mult), reads=[w1A, gwA], writes=[ca])
                    S.op("dve", lambda e: e.tensor_tensor(cb[:], w2A[:], gwA[:], ALU.mult), reads=[w2A, gwA], writes=[cb])
                    for Mx, ohx in ((M1, oh1A), (M2, oh2A)):
                        S.op("dve", lambda e: e.tensor_tensor(Mx[:].rearrange("p t (g e) -> p t g e", g=4),
                                                              ohx[:].rearrange("p t (o e) -> p t o e", o=1).to_broadcast([128, NTl, 4, 8]),
                                                              gsel4, ALU.mult), reads=[ohx, gselA], writes=[Mx])
                    S.op("dve", lambda e: e.tensor_tensor(Mb[:], M1[:], M2[:], ALU.add), reads=[M1, M2], writes=[Mb])
                    S.barrier()

            conv_some(1000)
            with ExitStack() as st5:
                NSL = 64
                pr = [S.psum(st5, "pr%d" % i, [128, 512], F32) for i in range(2)]
                Rall = S.sbuf(st5, "Rall", [128, NT, 32], F32)
                cntf = S.sbuf(st5, "cntf", [128, 32], F32)
                cntI = S.sbuf(st5, "cntI", [128, 32], I32)
                pcf = S.sbuf(st5, "pcf", [128, 32], F32)
                scA = S.sbuf(st5, "scA", [128, 32], F32)
                scB = S.sbuf(st5, "scB", [128, 32], F32)
                off = S.sbuf(st5, "off", [128, 32], F32)
                Pm = S.sbuf(st5, "Pm", [128, NT, 32], F32)
                prod = S.sbuf(st5, "prod", [128, NT, 32], F32)
                posaF = S.sbuf(st5, "posaF", [128, NT], F32)
                posbF = S.sbuf(st5, "posbF", [128, NT], F32)
                posaI = S.sbuf(st5, "posaI", [128, NT], I32)
                posbI = S.sbuf(st5, "posbI", [128, NT], I32)
                cmpb = S.sbuf(st5, "cmpb", [128, NSL, 32], F32)
                eidf = S.sbuf(st5, "eidf", [128, NSL], F32)
                actf = S.sbuf(st5, "actf", [128, NSL], F32)
                widF = S.sbuf(st5, "widF", [128, NSL], F32)
                widI = S.sbuf(st5, "widI", [128, NSL], I32)
                for i in range(NT):
                    S.op("pe", lambda e: e.matmul(pr[0][:, 0:32], onesb[:], Mb[:, i, :], start=(i == 0), stop=(i == NT - 1)),
                         reads=[onesb, Mb], writes=[pr[0]], inc=(i == NT - 1))
                S.op("dve", lambda e: e.tensor_copy(cntf[:], pr[0][:, 0:32]), reads=[pr[0]], writes=[cntf])
                for i in range(NT):
                    pb = pr[1]
                    for i2 in range(i):
                        S.op("pe", lambda e: e.matmul(pb[:, 0:32], onesb[:], Mb[:, i2, :], start=(i2 == 0), stop=False),
                             reads=[onesb, Mb], writes=[pb], inc=False)
                    S.op("pe", lambda e: e.matmul(pb[:, 0:32], ustrb[:], Mb[:, i, :], start=(i == 0), stop=True),
                         reads=[ustrb, Mb], writes=[pb])
                    S.op("act", lambda e: e.activation(Rall[:, i, :], pb[:, 0:32], AF.Copy), reads=[pb], writes=[Rall])
                S.op("dve", lambda e: e.tensor_scalar(pcf[:], cntf[:], 127.0, None, ALU.add), reads=[cntf], writes=[pcf])
                S.op("dve", lambda e: e.tensor_copy(cntI[:], pcf[:]), reads=[pcf], writes=[cntI])
                S.op("dve", lambda e: e.tensor_scalar(cntI[:], cntI[:], 7, None, ALU.arith_shift_right), reads=[cntI], writes=[cntI])
                S.op("dve", lambda e: e.tensor_scalar(cntI[:], cntI[:], 7, None, ALU.logical_shift_left), reads=[cntI], writes=[cntI])
                S.op("dve", lambda e: e.tensor_copy(pcf[:], cntI[:]), reads=[cntI], writes=[pcf])
                S.op("dve", lambda e: e.tensor_copy(scA[:], pcf[:]), reads=[pcf], writes=[scA])
                cur, nxt = scA, scB
                for sh in (1, 2, 4, 8, 16):
                    S.op("dve", lambda e: e.tensor_copy(nxt[:, 0:sh], cur[:, 0:sh]), reads=[cur], writes=[nxt])
                    S.op("dve", lambda e: e.tensor_tensor(nxt[:, sh:32], cur[:, sh:32], cur[:, 0:32 - sh], ALU.add), reads=[cur], writes=[nxt])
                    cur, nxt = nxt, cur
                incl = cur
                S.op("dve", lambda e: e.tensor_tensor(off[:], incl[:], pcf[:], ALU.subtract), reads=[incl, pcf], writes=[off])
                S.op("dve", lambda e: e.tensor_tensor(Pm[:], Rall[:], off[:].rearrange("p (o e) -> p o e", o=1).to_broadcast([128, NT, 32]), ALU.add),
                     reads=[Rall, off], writes=[Pm])
                for Mx, pF, pI in ((M1, posaF, posaI), (M2, posbF, posbI)):
                    S.op("dve", lambda e: e.tensor_tensor(prod[:], Mx[:], Pm[:], ALU.mult), reads=[Mx, Pm], writes=[prod])
                    S.op("dve", lambda e: e.tensor_reduce(pF[:], prod[:], AX.X, ALU.add), reads=[prod], writes=[pF])
                    S.op("dve", lambda e: e.tensor_copy(pI[:], pF[:]), reads=[pF], writes=[pI])
                S.op("dve", lambda e: e.tensor_tensor(cmpb[:], off[:].rearrange("p (o e) -> p o e", o=1).to_broadcast([128, NSL, 32]),
                                                      jvS[:].rearrange("p (j o) -> p j o", o=1).to_broadcast([128, NSL, 32]), ALU.is_le),
                     reads=[off, jvS], writes=[cmpb])
                S.op("dve", lambda e: e.tensor_reduce(eidf[:], cmpb[:], AX.X, ALU.add), reads=[cmpb], writes=[eidf])
                S.op("dve", lambda e: e.tensor_scalar(actf[:], jvS[:], incl[:, 31:32], 1.0e6, ALU.is_ge, ALU.mult), reads=[jvS, incl], writes=[actf])
                S.op("dve", lambda e: e.tensor_scalar(widF[:], eidf[:], -1.0, 128.0, ALU.add, ALU.mult), reads=[eidf], writes=[widF])
                S.op("dve", lambda e: e.tensor_scalar(widF[:], widF[:], pcolS[:, 0:1], None, ALU.add), reads=[widF, pcolS], writes=[widF])
                S.op("dve", lambda e: e.tensor_tensor(widF[:], widF[:], actf[:], ALU.add), reads=[widF, actf], writes=[widF])
                S.op("dve", lambda e: e.tensor_copy(widI[:], widF[:]), reads=[widF], writes=[widI])

                hld = [S.sbuf(st5, "hld%d" % i, [128, DM], BF16) for i in range(2)]
                for i in range(NT):
                    hl = hld[i % 2]
                    S.dma("sp", hl[:], hbd[i * 128:(i + 1) * 128, :], reads=[hbdB], writes=[hl])
                    for pI in (posaI, posbI):
                        S.dmaf("pool", lambda e: e.indirect_dma_start(out=Hs, out_offset=bass.IndirectOffsetOnAxis(ap=pI[:, i:i + 1], axis=0),
                                                                      in_=hl[:, :], in_offset=None),
                               reads=[hl, pI], writes=[HsB], sembuf=hl)

                Wg_s = [S.sbuf(st5, "Wgs%d" % i, [128, 8, 256], BF16) for i in range(2)]
                Wu_s = [S.sbuf(st5, "Wus%d" % i, [128, 8, 256], BF16) for i in range(2)]
                Wd_s = [S.sbuf(st5, "Wds%d" % i, [128, 2, DM], BF16) for i in range(2)]
                hsl = [S.sbuf(st5, "hsl%d" % i, [128, DM], BF16) for i in range(2)]
                hslT = [S.sbuf(st5, "hslT%d" % i, [128, 8, 128], BF16) for i in range(2)]
                sa = [S.sbuf(st5, "sa%d" % i, [128, 256], F32) for i in range(2)]
                hid = [S.sbuf(st5, "hid%d" % i, [128, 256], BF16) for i in range(2)]
                hidT = [S.sbuf(st5, "hidT%d" % i, [128, 2, 128], BF16) for i in range(2)]
                ysb = [S.sbuf(st5, "ysb%d" % i, [128, DM], F32) for i in range(2)]
                pht = S.psum(st5, "pht", [128, 1024], BF16)
                ptx = S.psum(st5, "ptx", [128, 1024], BF16)
                pau = [S.psum(st5, "pau%d" % i, [128, 512], F32) for i in range(2)]
                py = [S.psum(st5, "py%d" % i, [128, 512], F32) for i in range(2)]

                def st_load_a(j):
                    k = j % 2
                    S.dma("sp", hsl[k][:], Hs[j * 128:(j + 1) * 128, :], reads=[HsB], writes=[hsl[k]])
                    for wt, src in ((Wg_s[k], weg_b), (Wu_s[k], weu_b)):
                        S.dmaf("pool", lambda e: e.indirect_dma_start(out=wt[:].rearrange("p a b -> p (a b)"), out_offset=None, in_=src,
                                                                      in_offset=bass.IndirectOffsetOnAxis(ap=widI[:, j:j + 1], axis=0),
                                                                      bounds_check=bcreg, oob_is_err=False),
                               reads=[widI, WcB], writes=[wt])

                def st_load_d(j):
                    k = j % 2
                    wt = Wd_s[k]
                    S.dmaf("pool", lambda e: e.indirect_dma_start(out=wt[:].rearrange("p a b -> p (a b)"), out_offset=None, in_=wed_b,
                                                                  in_offset=bass.IndirectOffsetOnAxis(ap=widI[:, j:j + 1], axis=0),
                                                                  bounds_check=bcreg, oob_is_err=False),
                           reads=[widI, WcB], writes=[wt])

                def st_au(j):
                    k = j % 2
                    for c in range(8):
                        S.op("pe", lambda e: e.transpose(ptx[:, c * 128:(c + 1) * 128], hsl[k][:, c * 128:(c + 1) * 128], identb[:]),
                             reads=[hsl[k], identb], writes=[ptx], inc=(c == 7))
                    S.op("act", lambda e: e.activation(hslT[k][:], ptx[:].rearrange("p (c t) -> p c t", c=8), AF.Copy),
                         reads=[ptx], writes=[hslT[k]])
                    pa = pau[k]
                    for c in range(8):
                        S.op("pe", lambda e: e.matmul(pa[:, 0:256], hslT[k][:, c, :], Wg_s[k][:, c, :], start=(c == 0), stop=(c == 7)),
                             reads=[hslT[k], Wg_s[k]], writes=[pa], inc=False)
                    for c in range(8):
                        S.op("pe", lambda e: e.matmul(pa[:, 256:512], hslT[k][:, c, :], Wu_s[k][:, c, :], start=(c == 0), stop=(c == 7)),
                             reads=[hslT[k], Wu_s[k]], writes=[pa], inc=(c == 7))
                    S.op("act", lambda e: e.activation(sa[k][:], pa[:, 0:256], AF.Silu), reads=[pa], writes=[sa[k]])
                    S.op("dve", lambda e: e.tensor_tensor(hid[k][:], pa[:, 256:512], sa[k][:], ALU.mult), reads=[pa, sa[k]], writes=[hid[k]])

                def st_tr(j):
                    k = j % 2
                    o = k * 512
                    for f in range(2):
                        S.op("pe", lambda e: e.transpose(pht[:, o + f * 128:o + (f + 1) * 128], hid[k][:, f * 128:(f + 1) * 128], identb[:]),
                             reads=[hid[k], identb], writes=[pht], inc=(f == 1))
                    S.op("act", lambda e: e.activation(hidT[k][:], pht[:, o:o + 256].rearrange("p (f t) -> p f t", f=2), AF.Copy),
                         reads=[pht], writes=[hidT[k]])

                def st_y(j):
                    k = j % 2
                    for hf in range(2):
                        for f in range(2):
                            S.op("pe", lambda e: e.matmul(py[hf][:], hidT[k][:, f, :], Wd_s[k][:, f, hf * 512:(hf + 1) * 512],
                                                          start=(f == 0), stop=(f == 1)),
                                 reads=[hidT[k], Wd_s[k]], writes=[py[hf]], inc=(f == 1))
                        if hf == 0:
                            S.op("act", lambda e: e.activation(ysb[k][:, 0:512], py[0][:], AF.Copy), reads=[py[0]], writes=[ysb[k]])
                        else:
                            S.op("dve", lambda e: e.tensor_copy(ysb[k][:, 512:1024], py[1][:]), reads=[py[1]], writes=[ysb[k]])
                    S.dma("sp", Ys[j * 128:(j + 1) * 128, :], ysb[k][:], reads=[ysb[k]], writes=[YsB], sembuf=ysb[k])

                st_load_a(0)
                st_load_d(0)
                for j in range(NSL + 2):
                    if j < NSL:
                        if j + 1 < NSL:
                            st_load_a(j + 1)
                        st_au(j)
                    if 1 <= j <= NSL:
                        st_tr(j - 1)
                    if j >= 2:
                        st_y(j - 2)
                    if 1 <= j < NSL:
                        st_load_d(j)

                lns5 = dict(stats=S.sbuf(st5, "stats5", [128, 2, 6], F32), mv=S.sbuf(st5, "mv5", [128, 2], F32),
                            rstd=S.sbuf(st5, "rstd5", [128, 1], F32), nmr=S.sbuf(st5, "nmr5", [128, 1], F32),
                            z=S.sbuf(st5, "z5", [128, DM], F32))
                accs = [S.sbuf(st5, "accs%d" % i, [128, DM], F32) for i in range(2)]
                yas = [S.sbuf(st5, "yas%d" % i, [128, DM], F32) for i in range(2)]
                ybs = [S.sbuf(st5, "ybs%d" % i, [128, DM], F32) for i in range(2)]
                ost = [S.sbuf(st5, "ost%d" % i, [128, DM], F32) for i in range(2)]
                for i in range(NT):
                    k = i % 2
                    S.dma("sp", accs[k][:], hs[i * 128:(i + 1) * 128, :], reads=[hsB], writes=[accs[k]])
                    for yt, pI in ((yas[k], posaI), (ybs[k], posbI)):
                        S.dmaf("pool", lambda e: e.indirect_dma_start(out=yt[:, :], out_offset=None, in_=Ys,
                                                                      in_offset=bass.IndirectOffsetOnAxis(ap=pI[:, i:i + 1], axis=0)),
                               reads=[YsB, pI], writes=[yt])
                    S.op("dve", lambda e: e.scalar_tensor_tensor(accs[k][:], yas[k][:], ca[:, i:i + 1], accs[k][:], ALU.mult, ALU.add),
                         reads=[yas[k], ca], writes=[accs[k]])
                    S.op("dve", lambda e: e.scalar_tensor_tensor(accs[k][:], ybs[k][:], cb[:, i:i + 1], accs[k][:], ALU.mult, ALU.add),
                         reads=[ybs[k], cb], writes=[accs[k]])
                    layer_norm_tile(lns5, accs[k], None, 4, None)
                    z = lns5["z"]
                    o = ost[k]
                    S.op("dve", lambda e: e.tensor_tensor(o[:], z[:], lnbc[:, 5, :], ALU.add), reads=[z, lnbc], writes=[o])
                    S.dma("sp", out[b, i * 128:(i + 1) * 128, :], o[:], reads=[o], writes=[outB], sembuf=o)
                S.barrier()
            stB.close()
        S.nobar.clear()
        S.barrier()
    return nc


_NC = None


def _bucket_tables():
    import jax
    import jax.numpy as jnp
    with jax.default_device(jax.devices("cpu")[0]):
        kk = jnp.arange(128, dtype=jnp.int32)[:, None]
        qq = jnp.arange(128, dtype=jnp.int32)[None, :]
        tabs = []
        for d in range(2):
            rel = kk - qq - 128 * d
            nb = 16
            max_exact = 8
            ret = jnp.where(rel > 0, nb, 0)
            n = jnp.abs(rel)
            large = max_exact + (jnp.log(jnp.maximum(n, 1).astype(jnp.float32) / max_exact)
                                 / math.log(128 / max_exact) * (nb - max_exact)).astype(jnp.int32)
            large = jnp.minimum(large, nb - 1)
            tabs.append(np.asarray(ret + jnp.where(n < max_exact, n, large)))
    return np.stack(tabs, 0)


def kernel(**inputs):
    global _NC
    f32 = np.float32
    g = lambda k: np.ascontiguousarray(np.asarray(inputs[k]))
    x = g("x").astype(f32, copy=False)
    pos = g("positions").astype(np.int32, copy=False)
    rel_bias = g("rel_bias")
    bk = _bucket_tables()
    tzr = rel_bias[bk]
    tzr = np.ascontiguousarray(np.transpose(tzr, (1, 3, 0, 2))).astype(f32)
    shared = {
        "w_in": g("w_in")[0], "w_uq": g("w_uq")[0], "w_uk": g("w_uk")[0], "w_uv": g("w_uv")[0],
        "w_up_a": g("w_up_a")[0], "w_up_b": g("w_up_b")[0], "w_gate": g("w_gate")[0], "w_o": g("w_o")[0],
        "w_r": np.ascontiguousarray(np.concatenate([g("w_grp")[0], g("w_rt")[0]], axis=1)),
        "b_r": np.ascontiguousarray(np.concatenate([g("b_grp")[0], g("b_rt")[0]], axis=0)),
        "w_eg": g("w_exp_gate")[0].reshape(32, 8, 128, 256).transpose(0, 2, 1, 3).reshape(32 * 128, 2048),
        "w_eu": g("w_exp_up")[0].reshape(32, 8, 128, 256).transpose(0, 2, 1, 3).reshape(32 * 128, 2048),
        "w_ed": g("w_exp_down")[0].reshape(32, 2, 128, 1024).transpose(0, 2, 1, 3).reshape(32 * 128, 2048),
        "ustr": np.triu(np.ones((128, 128), dtype=f32), 1),
        "jv": np.tile((np.arange(64, dtype=f32) * 128.0)[None, :], (128, 1)),
        "pcol": np.arange(128, dtype=f32).reshape(128, 1),
        "lnv": np.ascontiguousarray(np.stack([g("ln0_g"), g("ln0_b"), g("ln1_g")[0], g("ln1_b")[0], g("ln2_g")[0], g("ln2_b")[0]], 0)),
        "qng": np.ascontiguousarray(g("q_norm_g")[0].reshape(2, 128).T),
        "kvg": np.ascontiguousarray(g("kv_norm_g")[0].reshape(128, 1)),
        "bgate": np.ascontiguousarray(g("b_gate")[0].reshape(16, 128).T),
        "tzr": tzr,
        "cfar": np.ascontiguousarray(rel_bias[15, :]),
        "identf": np.eye(128, dtype=f32),
        "invf": np.tile((10000.0 ** (-np.arange(16, dtype=np.float64) / 16.0) / (2.0 * math.pi)).astype(f32), 2).reshape(32, 1),
    }
    shared = {k: np.ascontiguousarray(v.astype(f32, copy=False)) for k, v in shared.items()}
    if _NC is None:
        _NC = build()
    in_maps = []
    for c in range(8):
        m = dict(shared)
        m["x"] = np.ascontiguousarray(x[NB * c:NB * (c + 1)])
        m["pos"] = np.ascontiguousarray(pos[NB * c:NB * (c + 1)])
        in_maps.append(m)
    res = run_bass_kernel_spmd(_NC, in_maps, core_ids=list(range(8)))
    return np.concatenate([np.asarray(r["out"]) for r in res.results], axis=0).astype(f32, copy=False)
```

```python
import math
from contextlib import ExitStack
import numpy as np
import concourse.bass as bass
import concourse.mybir as mybir
from concourse.bass_utils import run_bass_kernel_spmd

F32 = mybir.dt.float32
BF16 = mybir.dt.bfloat16
I32 = mybir.dt.int32
ALU = mybir.AluOpType
AF = mybir.ActivationFunctionType
AX = mybir.AxisListType

T = 2048
DM = 1024
NT = 16
NB = 2
ALPHA = 2.0 ** 0.25
LN_EPS = 1e-5
RMS_EPS = 1e-6
NEGBIG = -1.0e30
BIS_ITERS = 14
TOPK = 256


class Buf:
    __slots__ = ("ap", "name", "ws", "reads", "dsem", "dcount")

    def __init__(self, ap, name=""):
        self.ap = ap
        self.name = name
        self.ws = {}
        self.reads = {}
        self.dsem = None
        self.dcount = 0

    def __getitem__(self, idx):
        return self.ap[idx]


class Sched:
    def __init__(self, nc, stack):
        self.nc = nc
        self.stack = stack
        self.engs = {}
        for name, eng in (("pe", nc.tensor), ("act", nc.scalar), ("dve", nc.vector),
                          ("pool", nc.gpsimd), ("sp", nc.sync)):
            sem = stack.enter_context(nc.semaphore("s_" + name))
            self.engs[name] = dict(eng=eng, sem=sem, count=0, waited={})
        self.ndsem = 0
        self.dpool = {}
        self.nobar = set()
        self.n_ins = 0
        self.uid = 0

    def sbuf(self, st, name, shape, dtype):
        self.uid += 1
        name = "%s_%d" % (name, self.uid)
        return Buf(st.enter_context(self.nc.sbuf_tensor(name, shape, dtype)), name)

    def psum(self, st, name, shape, dtype):
        self.uid += 1
        name = "%s_%d" % (name, self.uid)
        return Buf(st.enter_context(self.nc.psum_tensor(name, shape, dtype)), name)

    def _wait(self, engname, ev):
        sem, val, src = ev
        if src == "pe" and engname == "pe":
            return
        E = self.engs[engname]
        key = id(sem)
        if E["waited"].get(key, 0) < val:
            E["eng"].wait_ge(sem, val)
            E["waited"][key] = val
            self.n_ins += 1

    def _deps(self, engname, reads, writes):
        for b in reads:
            for ev in b.ws.values():
                self._wait(engname, ev)
        for b in writes:
            for ev in b.ws.values():
                self._wait(engname, ev)
            for ev in b.reads.values():
                self._wait(engname, ev)

    def _commit(self, ev, reads, writes):
        k = id(ev[0])
        for b in writes:
            b.ws[k] = ev
            b.reads = {}
        for b in reads:
            if b in writes:
                continue
            b.reads[k] = ev

    def op(self, engname, fn, reads=(), writes=(), inc=True):
        E = self.engs[engname]
        self._deps(engname, reads, writes)
        ins = fn(E["eng"])
        self.n_ins += 1
        if inc:
            E["count"] += 1
            ins.then_inc(E["sem"], 1)
            self._commit((E["sem"], E["count"], engname), reads, writes)
        else:
            self._commit((E["sem"], E["count"] + 1, engname), reads, writes)

    def dma(self, qname, out_ap, in_ap, reads=(), writes=(), sembuf=None):
        E = self.engs[qname]
        self._deps(qname, reads, writes)
        sb = sembuf or (writes[0] if writes else reads[0])
        key = sb.name.rsplit("_", 1)[0] if "_" in sb.name else sb.name
        ent = self.dpool.get(key)
        if ent is None:
            ent = [self.stack.enter_context(self.nc.semaphore("d%d" % self.ndsem)), 0]
            self.ndsem += 1
            self.dpool[key] = ent
        ins = E["eng"].dma_start(out=out_ap, in_=in_ap)
        ent[1] += 16
        ins.then_inc(ent[0], 16)
        self.n_ins += 1
        ev = (ent[0], ent[1], "dma")
        self._commit(ev, reads, writes)
        return ev

    def dmaf(self, qname, fn, reads=(), writes=(), sembuf=None):
        E = self.engs[qname]
        self._deps(qname, reads, writes)
        sb = sembuf or (writes[0] if writes else reads[0])
        key = sb.name.rsplit("_", 1)[0] if "_" in sb.name else sb.name
        ent = self.dpool.get(key)
        if ent is None:
            ent = [self.stack.enter_context(self.nc.semaphore("d%d" % self.ndsem)), 0]
            self.ndsem += 1
            self.dpool[key] = ent
        ins = fn(E["eng"])
        ent[1] += 16
        ins.then_inc(ent[0], 16)
        self.n_ins += 1
        ev = (ent[0], ent[1], "dma")
        self._commit(ev, reads, writes)
        return ev

    def barrier(self):
        evs = []
        for n, E in self.engs.items():
            if E["count"] > 0:
                evs.append((E["sem"], E["count"], "bar_" + n))
        for key, ent in self.dpool.items():
            if ent[1] > 0 and key not in self.nobar:
                evs.append((ent[0], ent[1], "dma"))
        for n in self.engs:
            for ev in evs:
                if ev[2] == "bar_" + n:
                    continue
                self._wait(n, ev)


def build():
    nc = bass.Bass("TRN2", target_bir_lowering=False)

    def din(name, shape, dt=F32):
        return nc.dram_tensor(name, shape, dt, kind="ExternalInput").ap()

    x = din("x", [NB, T, DM])
    pos = din("pos", [NB, T], I32)
    w_in = din("w_in", [DM, 1352])
    w_uq = din("w_uq", [256, 768])
    w_uk = din("w_uk", [128, 512])
    w_uv = din("w_uv", [128, 512])
    w_up_a = din("w_up_a", [512, DM])
    w_up_b = din("w_up_b", [512, DM])
    w_gate = din("w_gate", [DM, 2048])
    w_o = din("w_o", [DM, DM])
    w_r = din("w_r", [DM, 36])
    b_r = din("b_r", [36])
    w_eg = din("w_eg", [32 * 128, 2048])
    w_eu = din("w_eu", [32 * 128, 2048])
    w_ed = din("w_ed", [32 * 128, 2048])
    ustr = din("ustr", [128, 128])
    jv = din("jv", [128, 64])
    pcol = din("pcol", [128, 1])
    lnv = din("lnv", [6, DM])
    qng = din("qng", [128, 2])
    kvg = din("kvg", [128, 1])
    bgate = din("bgate", [128, 16])
    tzr = din("tzr", [128, 8, 2, 128])
    cfar = din("cfar", [8])
    identf = din("identf", [128, 128])
    invf = din("invf", [32, 1])
    out = nc.dram_tensor("out", [NB, T, DM], F32, kind="ExternalOutput").ap()
    hs = nc.dram_tensor("hs", [T, DM], F32, kind="Internal").ap()
    xns = nc.dram_tensor("xns", [T, DM], F32, kind="Internal").ap()
    hbd = nc.dram_tensor("hbd", [T, DM], BF16, kind="Internal").ap()
    Hs = nc.dram_tensor("Hs", [64 * 128, DM], BF16, kind="Internal").ap()
    Ys = nc.dram_tensor("Ys", [64 * 128, DM], F32, kind="Internal").ap()
    weg_b = nc.dram_tensor("weg_b", [32 * 128, 2048], BF16, kind="Internal").ap()
    weu_b = nc.dram_tensor("weu_b", [32 * 128, 2048], BF16, kind="Internal").ap()
    wed_b = nc.dram_tensor("wed_b", [32 * 128, 2048], BF16, kind="Internal").ap()

    with ExitStack() as st0:
        S = Sched(nc, st0)
        hsB = Buf(hs, "hs")
        xnsB = Buf(xns, "xns")
        outB = Buf(out, "out")
        bcreg = st0.enter_context(nc.gpsimd.register("bcreg"))
        nc.gpsimd.reg_mov(bcreg, 32 * 128 - 1)
        hbdB = Buf(hbd, "hbd")
        HsB = Buf(Hs, "Hs")
        YsB = Buf(Ys, "Ys")

        identb = S.sbuf(st0, "identb", [128, 128], BF16)
        identF = S.sbuf(st0, "identF", [128, 128], F32)
        onesb = S.sbuf(st0, "onesb", [128, 128], BF16)
        lnbc = S.sbuf(st0, "lnbc", [128, 6, DM], F32)
        S.dma("pool", identb[:], identf, writes=[identb])
        ustrb = S.sbuf(st0, "ustrb", [128, 128], BF16)
        jvS = S.sbuf(st0, "jvS", [128, 64], F32)
        pcolS = S.sbuf(st0, "pcolS", [128, 1], F32)
        S.dma("pool", ustrb[:], ustr, writes=[ustrb])
        S.dma("sp", jvS[:], jv, writes=[jvS])
        S.dma("sp", pcolS[:], pcol, writes=[pcolS])
        WcB = Buf(None, "wcast")
        S.nobar.add("wcast")
        conv_list = [(srcw, dstw, e_) for e_ in range(32) for srcw, dstw in ((w_eg, weg_b), (w_eu, weu_b), (w_ed, wed_b))]

        def conv_some(n):
            for _ in range(n):
                if not conv_list:
                    return
                srcw, dstw, e_ = conv_list.pop(0)
                S.dma("pool", dstw[e_ * 128:(e_ + 1) * 128, :], srcw[e_ * 128:(e_ + 1) * 128, :], writes=[WcB])
        with ExitStack() as stz:
            zt = S.sbuf(stz, "zt", [128, 8, DM], BF16)
            S.op("pool", lambda e: e.memset(zt[:], 0.0), writes=[zt])
            for q in range(8):
                S.dma("sp", Hs[q * 1024:(q + 1) * 1024, :].rearrange("(p r) n -> p r n", p=128), zt[:], reads=[zt], writes=[HsB], sembuf=zt)
            S.barrier()
        S.dma("sp", identF[:], identf, writes=[identF])
        S.op("dve", lambda e: e.memset(onesb[:], 1.0), writes=[onesb])
        for k in range(6):
            S.dma("sp", lnbc[:, k, :], lnv[k].partition_broadcast(128), writes=[lnbc])

        def layer_norm_tile(st_bufs, src, dst_ap_fn, gi, out_bufs, scale_after=None):
            stats, mv, rstd, nmr, z = (st_bufs[k] for k in ("stats", "mv", "rstd", "nmr", "z"))
            S.op("dve", lambda e: e.bn_stats(stats[:, 0, :], src[:, 0:512]), reads=[src], writes=[stats])
            S.op("dve", lambda e: e.bn_stats(stats[:, 1, :], src[:, 512:1024]), reads=[src], writes=[stats])
            S.op("dve", lambda e: e.bn_aggr(mv[:], stats[:].rearrange("p a b -> p (a b)")), reads=[stats], writes=[mv])
            S.op("dve", lambda e: e.tensor_scalar(rstd[:], mv[:, 1:2], LN_EPS, None, ALU.add), reads=[mv], writes=[rstd])
            S.op("act", lambda e: e.activation(rstd[:], rstd[:], AF.Sqrt), reads=[rstd], writes=[rstd])
            S.op("dve", lambda e: e.reciprocal(rstd[:], rstd[:]), reads=[rstd], writes=[rstd])
            S.op("dve", lambda e: e.tensor_scalar(nmr[:], mv[:, 0:1], rstd[:, 0:1], -1.0, ALU.mult, ALU.mult),
                 reads=[mv, rstd], writes=[nmr])
            S.op("act", lambda e: e.activation(z[:], src[:], AF.Identity, bias=nmr[:, 0:1], scale=rstd[:, 0:1]),
                 reads=[src, nmr, rstd], writes=[z])
            S.op("dve", lambda e: e.tensor_tensor(z[:], z[:], lnbc[:, gi, :], ALU.mult), reads=[z, lnbc], writes=[z])

        for b in range(NB):
            stB = ExitStack()
            stB.__enter__()
            xnT = S.sbuf(stB, "xnT", [128, 8, T], BF16)
            ca = S.sbuf(stB, "ca", [128, NT], F32)
            cb = S.sbuf(stB, "cb", [128, NT], F32)
            M1 = S.sbuf(stB, "M1", [128, NT, 32], F32)
            M2 = S.sbuf(stB, "M2", [128, NT, 32], F32)
            Mb = S.sbuf(stB, "Mb", [128, NT, 32], BF16)
            with ExitStack() as stA:
                oaT = S.sbuf(stA, "oaT", [128, 4, T], BF16)
                obT = S.sbuf(stA, "obT", [128, 4, T], BF16)
                lns = dict(stats=S.sbuf(stA, "stats", [128, 2, 6], F32), mv=S.sbuf(stA, "mv", [128, 2], F32),
                           rstd=S.sbuf(stA, "rstd", [128, 1], F32), nmr=S.sbuf(stA, "nmr", [128, 1], F32),
                           z=S.sbuf(stA, "z", [128, DM], F32))
                xt = [S.sbuf(stA, "xt%d" % i, [128, DM], F32) for i in range(2)]

                with ExitStack() as st1:
                    xnb = [S.sbuf(st1, "xnb%d" % i, [128, DM], BF16) for i in range(2)]
                    xnf = [S.sbuf(st1, "xnf%d" % i, [128, DM], F32) for i in range(2)]
                    ptr = [S.psum(st1, "ptr%d" % i, [128, 1024], BF16) for i in range(2)]
                    for i in range(NT):
                        xs = xt[i % 2]
                        S.dma("sp", xs[:], x[b, i * 128:(i + 1) * 128, :], writes=[xs])
                        layer_norm_tile(lns, xs, None, 0, None)
                        z = lns["z"]
                        xb = xnb[i % 2]
                        xf = xnf[i % 2]
                        S.op("dve", lambda e: e.tensor_tensor(xf[:], z[:], lnbc[:, 1, :], ALU.add),
                             reads=[z, lnbc], writes=[xf])
                        S.op("act", lambda e: e.activation(xb[:], xf[:], AF.Copy), reads=[xf], writes=[xb])
                        S.dma("sp", xns[i * 128:(i + 1) * 128, :], xf[:], reads=[xf], writes=[xnsB], sembuf=xf)
                        pt = ptr[i % 2]
                        for c in range(8):
                            S.op("pe", lambda e: e.transpose(pt[:, c * 128:(c + 1) * 128], xb[:, c * 128:(c + 1) * 128], identb[:]),
                                 reads=[xb, identb], writes=[pt], inc=(c == 7))
                        S.op("act", lambda e: e.activation(xnT[:, :, i * 128:(i + 1) * 128],
                                                           pt[:].rearrange("p (c t) -> p c t", c=8), AF.Copy),
                             reads=[pt], writes=[xnT])
                    S.barrier()

                with ExitStack() as st2:
                    pp = [S.psum(st2, "pp%d" % i, [128, 512], F32) for i in range(3)]
                    ps = [S.psum(st2, "ps%d" % i, [128, 512], F32) for i in range(3)]
                    po = [S.psum(st2, "po%d" % i, [128, 512], F32) for i in range(2)]
                    Wa = S.sbuf(st2, "Wa", [128, 8, 416], BF16)
                    Wkrr = S.sbuf(st2, "Wkrr", [128, 8, 32], BF16)
                    Wq = S.sbuf(st2, "Wq", [128, 2, 8, 96], BF16)
                    Wqr = S.sbuf(st2, "Wqr", [128, 2, 8, 32], BF16)
                    Wk = S.sbuf(st2, "Wk", [128, 8, 64], BF16)
                    Wv = S.sbuf(st2, "Wv", [128, 512], BF16)
                    gq = S.sbuf(st2, "gq", [128, 2], F32)
                    gkv = S.sbuf(st2, "gkv", [128, 1], F32)
                    invfS = S.sbuf(st2, "invfS", [96, 1], F32)
                    cos32 = S.sbuf(st2, "cos32", [96, T], F32)
                    sin32 = S.sbuf(st2, "sin32", [96, T], F32)
                    cqn = S.sbuf(st2, "cqn", [128, 2, T], BF16)
                    ckvn = S.sbuf(st2, "ckvn", [128, T], BF16)
                    krT = S.sbuf(st2, "krT", [96, T], BF16)
                    st2a = ExitStack()
                    st2a.__enter__()
                    wq32 = S.sbuf(st2a, "wq32", [128, 2, 768], F32)
                    wk32 = S.sbuf(st2a, "wk32", [128, 512], F32)
                    wv32 = S.sbuf(st2a, "wv32", [128, 512], F32)
                    posi = S.sbuf(st2a, "posi", [96, T], I32)
                    rr = S.sbuf(st2a, "rr", [96, T], F32)
                    rf = S.sbuf(st2a, "rf", [96, T], F32)
                    tq = S.sbuf(st2a, "tq", [96, T], F32)

                    wsrc = w_in.rearrange("(c p) n -> p c n", p=128)
                    S.dma("pool", Wa[:], wsrc[:, :, 0:416], writes=[Wa])
                    S.dma("sp", wq32[:], w_uq.rearrange("(c p) n -> p c n", p=128), writes=[wq32])
                    S.dma("sp", wk32[:], w_uk, writes=[wk32])
                    S.dma("sp", wv32[:], w_uv, writes=[wv32])
                    S.dma("sp", gq[:], qng, writes=[gq])
                    S.dma("sp", gkv[:], kvg, writes=[gkv])
                    S.dma("sp", invfS[64:96, :], invf, writes=[invfS])
                    S.dma("sp", posi[64:96, :], pos[b].partition_broadcast(32), writes=[posi])
                    S.op("dve", lambda e: e.tensor_scalar(Wkrr[:, :, 0:16], Wa[:, :, 400:416], -1.0, None, ALU.mult),
                         reads=[Wa], writes=[Wkrr])
                    S.op("dve", lambda e: e.tensor_copy(Wkrr[:, :, 16:32], Wa[:, :, 384:400]), reads=[Wa], writes=[Wkrr])
                    for c in range(2):
                        src = wq32[:, c, :].rearrange("p (h d) -> p h d", h=8)
                        g = gq[:, c:c + 1]
                        S.op("dve", lambda e: e.tensor_scalar(Wq[:, c, :, :], src[:, :, :], g, None, ALU.mult),
                             reads=[wq32, gq], writes=[Wq])
                        S.op("dve", lambda e: e.tensor_scalar(Wqr[:, c, :, 0:16], src[:, :, 80:96], g, -1.0, ALU.mult, ALU.mult),
                             reads=[wq32, gq], writes=[Wqr])
                        S.op("dve", lambda e: e.tensor_scalar(Wqr[:, c, :, 16:32], src[:, :, 64:80], g, None, ALU.mult),
                             reads=[wq32, gq], writes=[Wqr])
                    S.op("dve", lambda e: e.tensor_scalar(Wk[:, :, :], wk32[:].rearrange("p (h d) -> p h d", h=8),
                                                          gkv[:, 0:1], None, ALU.mult), reads=[wk32, gkv], writes=[Wk])
                    S.op("dve", lambda e: e.tensor_scalar(Wv[:], wv32[:], gkv[:, 0:1], None, ALU.mult),
                         reads=[wv32, gkv], writes=[Wv])

                    S.op("dve", lambda e: e.tensor_copy(rr[64:96, :], posi[64:96, :]), reads=[posi], writes=[rr])
                    S.op("dve", lambda e: e.tensor_scalar(rr[64:96, :], rr[64:96, :], invfS[64:96, 0:1], None, ALU.mult), reads=[rr, invfS], writes=[rr])
                    S.op("dve", lambda e: e.tensor_copy(posi[64:96, :], rr[64:96, :]), reads=[rr], writes=[posi])
                    S.op("dve", lambda e: e.tensor_copy(rf[64:96, :], posi[64:96, :]), reads=[posi], writes=[rf])
                    S.op("dve", lambda e: e.tensor_tensor(rr[64:96, :], rr[64:96, :], rf[64:96, :], ALU.subtract), reads=[rr, rf], writes=[rr])

                    def wrap_sin(dst, shift):
                        S.op("dve", lambda e: e.tensor_scalar(rf[64:96, :], rr[64:96, :], shift, None, ALU.add), reads=[rr], writes=[rf])
                        for _ in range(2):
                            S.op("dve", lambda e: e.tensor_scalar(tq[64:96, :], rf[64:96, :], 0.5, None, ALU.is_gt), reads=[rf], writes=[tq])
                            S.op("dve", lambda e: e.tensor_tensor(rf[64:96, :], rf[64:96, :], tq[64:96, :], ALU.subtract), reads=[rf, tq], writes=[rf])
                        S.op("dve", lambda e: e.tensor_scalar(tq[64:96, :], rf[64:96, :], -0.5, None, ALU.is_lt), reads=[rf], writes=[tq])
                        S.op("dve", lambda e: e.tensor_tensor(rf[64:96, :], rf[64:96, :], tq[64:96, :], ALU.add), reads=[rf, tq], writes=[rf])
                        S.op("act", lambda e: e.activation(dst[64:96, :], rf[64:96, :], AF.Sin, scale=2.0 * math.pi * (1.0 - 2e-6)),
                             reads=[rf], writes=[dst])

                    wrap_sin(sin32, 0.0)
                    wrap_sin(cos32, 0.25)
                    S.barrier()
                    st2a.close()
                    Vh = [S.sbuf(st2, "Vh%d" % i, [128, NT, 128], BF16) for i in range(2)]
                    qTs = [S.sbuf(st2, "qT%d" % i, [96, T], BF16) for i in range(2)]
                    kTs = [S.sbuf(st2, "kT%d" % i, [96, T], BF16) for i in range(2)]
                    c32 = S.sbuf(st2, "c32", [128, 2, 512], F32)
                    sqb = S.sbuf(st2, "sqb", [128, 2, 512], BF16)
                    rq = S.sbuf(st2, "rq", [128, 512], F32)
                    t1 = S.sbuf(st2, "t1", [96, 512], F32)
                    t2 = S.sbuf(st2, "t2", [96, 512], F32)
                    PTs = [S.sbuf(st2, "PT%d" % i, [128, 512], BF16) for i in range(3)]
                    rd = S.sbuf(st2, "rd", [128, 512], F32)
                    for i in range(2):
                        S.op("pool", lambda e: e.memset(Vh[i][:], 1.0), writes=[Vh[i]])

                    def rms_block(psrc_list, dstT_fn, nfeat, tb):
                        n = len(psrc_list)
                        for m, pb in enumerate(psrc_list):
                            S.op("act", lambda e: e.activation(c32[:, m, :], pb[:], AF.Copy), reads=[pb], writes=[c32])
                            S.op("act", lambda e: e.activation(sqb[:, m, :], pb[:], AF.Square), reads=[pb], writes=[sqb])
                        pss = pp[2]
                        for m in range(n):
                            S.op("pe", lambda e: e.matmul(pss[:], onesb[:], sqb[:, m, :], start=(m == 0), stop=(m == n - 1)),
                                 reads=[onesb, sqb], writes=[pss], inc=(m == n - 1))
                        S.op("dve", lambda e: e.tensor_scalar(rq[:], pss[:], 1.0 / nfeat, RMS_EPS, ALU.mult, ALU.add),
                             reads=[pss], writes=[rq])
                        S.op("act", lambda e: e.activation(rq[:], rq[:], AF.Sqrt), reads=[rq], writes=[rq])
                        S.op("dve", lambda e: e.reciprocal(rq[:], rq[:]), reads=[rq], writes=[rq])
                        for m in range(n):
                            S.op("dve", lambda e: e.tensor_tensor(dstT_fn(m), c32[:, m, :], rq[:], ALU.mult),
                                 reads=[c32, rq], writes=[dstT_fn.buf])

                    def proj_fm(pb, lhs_fn, tb, M=128, p0=0):
                        for c in range(8):
                            S.op("pe", lambda e: e.matmul(pb[p0:p0 + M, :], lhs_fn(c), xnT[:, c, tb * 512:(tb + 1) * 512],
                                                          start=(c == 0), stop=(c == 7)),
                                 reads=[xnT, Wa, Wkrr], writes=[pb], inc=(c == 7))

                    for tb in range(4):
                        cols = slice(tb * 512, (tb + 1) * 512)
                        proj_fm(pp[0], lambda c: Wa[:, c, 0:128], tb)
                        proj_fm(pp[1], lambda c: Wa[:, c, 128:256], tb)
                        f = lambda m: cqn[:, m, cols]
                        f.buf = cqn
                        rms_block([pp[0], pp[1]], f, 256.0, tb)
                        proj_fm(pp[0], lambda c: Wa[:, c, 256:384], tb)
                        f2 = lambda m: ckvn[:, cols]
                        f2.buf = ckvn
                        rms_block([pp[0]], f2, 128.0, tb)
                        proj_fm(pp[0], lambda c: Wa[:, c, 384:416], tb, M=32, p0=64)
                        proj_fm(pp[1], lambda c: Wkrr[:, c, :], tb, M=32, p0=64)
                        S.op("dve", lambda e: e.tensor_tensor(t1[64:96, :], pp[0][64:96, :], cos32[64:96, cols], ALU.mult),
                             reads=[pp[0], cos32], writes=[t1])
                        S.op("dve", lambda e: e.tensor_tensor(t2[64:96, :], pp[1][64:96, :], sin32[64:96, cols], ALU.mult),
                             reads=[pp[1], sin32], writes=[t2])
                        S.op("dve", lambda e: e.tensor_tensor(krT[64:96, cols], t1[64:96, :], t2[64:96, :], ALU.add), reads=[t1, t2], writes=[krT])
                    sc_mla = 96.0 ** -0.5
                    LA = 2

                    def mla_proj(h):
                        qT = qTs[h % 2]
                        kT = kTs[h % 2]
                        for tb in range(4):
                            cols = slice(tb * 512, (tb + 1) * 512)
                            pa, pbb = pp[0], pp[1]
                            for m in range(2):
                                S.op("pe", lambda e: e.matmul(pa[0:96, :], Wq[:, m, h, :], cqn[:, m, cols], start=(m == 0), stop=(m == 1)),
                                     reads=[Wq, cqn], writes=[pa], inc=(m == 1))
                            for m in range(2):
                                S.op("pe", lambda e: e.matmul(pbb[64:96, :], Wqr[:, m, h, :], cqn[:, m, cols], start=(m == 0), stop=(m == 1)),
                                     reads=[Wqr, cqn], writes=[pbb], inc=(m == 1))
                            pk = pp[2]
                            S.op("pe", lambda e: e.matmul(pk[0:64, :], Wk[:, h, :], ckvn[:, cols], start=True, stop=True),
                                 reads=[Wk, ckvn], writes=[pk])
                            S.op("dve", lambda e: e.tensor_tensor(t1[64:96, :], pa[64:96, :], cos32[64:96, cols], ALU.mult), reads=[pa, cos32], writes=[t1])
                            S.op("dve", lambda e: e.tensor_tensor(t2[64:96, :], pbb[64:96, :], sin32[64:96, cols], ALU.mult), reads=[pbb, sin32], writes=[t2])
                            S.op("dve", lambda e: e.tensor_tensor(qT[64:96, cols], t1[64:96, :], t2[64:96, :], ALU.add), reads=[t1, t2], writes=[qT])
                            S.op("act", lambda e: e.activation(qT[0:64, cols], pa[0:64, :], AF.Copy), reads=[pa], writes=[qT])
                            S.op("act", lambda e: e.activation(kT[0:64, cols], pk[0:64, :], AF.Copy), reads=[pk], writes=[kT])
                        S.op("pool", lambda e: e.tensor_copy(kT[64:96, :], krT[64:96, :]), reads=[krT], writes=[kT])
                        Vc = Vh[h % 2]
                        voff = 64 * (h % 2)
                        for k4 in range(4):
                            pv = pp[k4 % 2]
                            for j in range(4):
                                kt = k4 * 4 + j
                                S.op("pe", lambda e: e.matmul(pv[:, j * 64:(j + 1) * 64], ckvn[:, kt * 128:(kt + 1) * 128],
                                                              Wv[:, h * 64:(h + 1) * 64], start=True, stop=True),
                                     reads=[ckvn, Wv], writes=[pv], inc=(j == 3))
                            S.op("act", lambda e: e.activation(Vc[:, k4 * 4:(k4 + 1) * 4, voff:voff + 64],
                                                               pv[:, 0:256].rearrange("p (j d) -> p j d", j=4), AF.Copy),
                                 reads=[pv], writes=[Vc])

                    st_ = dict(si=0, oi=0)

                    def mla_attn(h):
                        conv_some(6)
                        qT = qTs[h % 2]
                        kT = kTs[h % 2]
                        Vc = Vh[h % 2]
                        for qb in range(4):
                            pO = po[st_["oi"] % 2]
                            st_["oi"] += 1
                            nk = 4 * qb + 4
                            slots = {}

                            def emit_S(kt):
                                c0 = max(0, kt - 4 * qb) * 128
                                sl = st_["si"] % 3
                                st_["si"] += 1
                                slots[kt] = sl
                                pS, PT = ps[sl], PTs[sl]
                                S.op("pe", lambda e: e.matmul(pS[:, c0:512], kT[:, kt * 128:(kt + 1) * 128],
                                                              qT[:, qb * 512 + c0:(qb + 1) * 512], start=True, stop=True),
                                     reads=[kT, qT], writes=[pS])
                                S.op("act", lambda e: e.activation(PT[:, c0:512], pS[:, c0:512], AF.Exp, scale=sc_mla),
                                     reads=[pS], writes=[PT])
                                if kt >= 4 * qb:
                                    S.op("pool", lambda e: e.memset(PT[64:128, c0:c0 + 64], 0.0), writes=[PT])

                            def emit_PV(kt):
                                c0 = max(0, kt - 4 * qb) * 128
                                PT = PTs[slots[kt]]
                                S.op("pe", lambda e: e.matmul(pO[:, c0:512], Vc[:, kt, :], PT[:, c0:512],
                                                              start=(kt == 0), stop=(kt == nk - 1), skip_group_check=True),
                                     reads=[Vc, PT], writes=[pO], inc=(kt == nk - 1))

                            for s_ in range(nk + LA):
                                if s_ < nk:
                                    emit_S(s_)
                                if s_ >= LA:
                                    emit_PV(s_ - LA)
                            ocols = slice(qb * 512, (qb + 1) * 512)
                            if h % 2 == 0:
                                S.op("dve", lambda e: e.reciprocal(rd[0:64, :], pO[64:128, :]), reads=[pO], writes=[rd])
                                S.op("dve", lambda e: e.tensor_tensor(oaT[0:64, h // 2, ocols], pO[0:64, :], rd[0:64, :], ALU.mult),
                                     reads=[pO, rd], writes=[oaT])
                            else:
                                S.op("dve", lambda e: e.reciprocal(rd[64:128, :], pO[0:64, :]), reads=[pO], writes=[rd])
                                S.op("dve", lambda e: e.tensor_tensor(oaT[64:128, h // 2, ocols], pO[64:128, :], rd[64:128, :], ALU.mult),
                                     reads=[pO, rd], writes=[oaT])

                    mla_proj(0)
                    for h in range(8):
                        if h + 1 < 8:
                            mla_proj(h + 1)
                        mla_attn(h)
                    S.barrier()

                with ExitStack() as st3:
                    pp = [S.psum(st3, "qp%d" % i, [128, 512], F32) for i in range(2)]
                    ps = [S.psum(st3, "qs%d" % i, [128, 512], F32) for i in range(3)]
                    po = [S.psum(st3, "qo%d" % i, [128, 512], F32) for i in range(2)]
                    pmt = S.psum(st3, "pmt", [128, 1024], BF16)
                    qbT = S.sbuf(st3, "qbT", [128, 4, T], BF16)
                    kbT2 = S.sbuf(st3, "kbT2", [128, T], BF16)
                    qiT = S.sbuf(st3, "qiT", [128, 2, T], BF16)
                    kiT4 = S.sbuf(st3, "kiT4", [128, T], BF16)
                    Vb = S.sbuf(st3, "Vb", [128, NT, 2, 128], BF16)
                    widx = S.sbuf(st3, "widx", [128, NT, 8], F32)
                    tz = S.sbuf(st3, "tz", [128, 8, 2, 128], F32)
                    cfb = S.sbuf(st3, "cfb", [128, 8], F32)
                    st3a = ExitStack()
                    st3a.__enter__()
                    Wb = S.sbuf(st3a, "Wb", [128, 8, 936], BF16)
                    Wkb2 = S.sbuf(st3a, "Wkb2", [128, 8, 128], BF16)
                    Wki4 = S.sbuf(st3a, "Wki4", [128, 8, 128], BF16)

                    wsrc = w_in.rearrange("(c p) n -> p c n", p=128)
                    S.dma("pool", Wb[:], wsrc[:, :, 416:1352], writes=[Wb])
                    for r in range(2):
                        S.dma("pool", Wkb2[:, :, r * 64:(r + 1) * 64], wsrc[:, :, 928:992], writes=[Wkb2])
                    for r in range(4):
                        S.dma("pool", Wki4[:, :, r * 32:(r + 1) * 32], wsrc[:, :, 1312:1344], writes=[Wki4])
                    S.dma("sp", tz[:], tzr, writes=[tz])
                    S.dma("sp", cfb[:], cfar.partition_broadcast(128), writes=[cfb])
                    for h in range(8):
                        S.op("dve", lambda e: e.tensor_scalar(tz[:, h], tz[:, h], cfb[:, h:h + 1], 8.0, ALU.subtract, ALU.mult),
                             reads=[tz, cfb], writes=[tz])
                    S.op("pool", lambda e: e.memset(Vb[:], 1.0), writes=[Vb])

                    def proj3(dst_ap, dstbuf, lhs_fn, wbuf, tb, k):
                        pb = pp[k % 2]
                        for c in range(8):
                            S.op("pe", lambda e: e.matmul(pb[:], lhs_fn(c), xnT[:, c, tb * 512:(tb + 1) * 512],
                                                          start=(c == 0), stop=(c == 7)), reads=[xnT, wbuf], writes=[pb], inc=(c == 7))
                        S.op("act", lambda e: e.activation(dst_ap, pb[:], AF.Copy), reads=[pb], writes=[dstbuf])

                    k = 0
                    for tb in range(4):
                        cols = slice(tb * 512, (tb + 1) * 512)
                        for p in range(4):
                            proj3(qbT[:, p, cols], qbT, lambda c: Wb[:, c, p * 128:(p + 1) * 128], Wb, tb, k); k += 1
                        proj3(kbT2[:, cols], kbT2, lambda c: Wkb2[:, c, :], Wkb2, tb, k); k += 1
                        for g in range(2):
                            proj3(qiT[:, g, cols], qiT, lambda c: Wb[:, c, 640 + g * 128:640 + (g + 1) * 128], Wb, tb, k); k += 1
                        proj3(kiT4[:, cols], kiT4, lambda c: Wki4[:, c, :], Wki4, tb, k); k += 1
                    for kt in range(NT):
                        pb = pp[kt % 2]
                        tsl = slice(kt * 128, (kt + 1) * 128)
                        for c in range(8):
                            S.op("pe", lambda e: e.matmul(pb[:, 0:64], xnT[:, c, tsl], Wb[:, c, 576:640], start=(c == 0), stop=(c == 7)),
                                 reads=[xnT, Wb], writes=[pb], inc=(c == 7))
                        for c in range(8):
                            S.op("pe", lambda e: e.matmul(pb[:, 64:72], xnT[:, c, tsl], Wb[:, c, 928:936], start=(c == 0), stop=(c == 7)),
                                 reads=[xnT, Wb], writes=[pb], inc=(c == 7))
                        S.op("act", lambda e: e.activation(Vb[:, kt, 0, 0:64], pb[:, 0:64], AF.Copy), reads=[pb], writes=[Vb])
                        S.op("act", lambda e: e.activation(Vb[:, kt, 1, 64:128], pb[:, 0:64], AF.Copy), reads=[pb], writes=[Vb])
                        S.op("act", lambda e: e.activation(widx[:, kt, :], pb[:, 64:72], AF.Copy, scale=0.0625), reads=[pb], writes=[widx])

                    S.barrier()
                    st3a.close()
                    Sc = [S.sbuf(st3, "Sc%d" % i, [128, T], F32) for i in range(2)]
                    msk = [S.sbuf(st3, "msk%d" % i, [128, T], BF16) for i in range(2)]
                    mskT = S.sbuf(st3, "mskT", [128, NT, 512], BF16)
                    rl = [S.sbuf(st3, "rl%d" % i, [128, 512], F32) for i in range(2)]
                    bmx = S.sbuf(st3, "bmx", [128, 1], F32)
                    bmn = S.sbuf(st3, "bmn", [128, 1], F32)
                    brg = S.sbuf(st3, "brg", [128, 1], F32)
                    bmid = S.sbuf(st3, "bmid", [128, 1], F32)
                    bcnt = S.sbuf(st3, "bcnt", [128, 1], F32)
                    bt = S.sbuf(st3, "bt", [128, 1], F32)
                    Es = [S.sbuf(st3, "E%d" % i, [128, 512], BF16) for i in range(3)]
                    PTs = [S.sbuf(st3, "PTb%d" % i, [128, 512], BF16) for i in range(3)]
                    rd = S.sbuf(st3, "rdb", [128, 512], F32)
                    sidx = [0]
                    oi = 0

                    def idx_steps(qt):
                        n = (qt + 1) * 128
                        sc = Sc[qt % 2]
                        tsl = slice(qt * 128, (qt + 1) * 128)
                        steps = []
                        for hh in range(8):
                            for sb in range((n + 511) // 512):
                                def step(hh=hh, sb=sb):
                                    g, jj = hh // 4, hh % 4
                                    w = min(512, n - sb * 512)
                                    k = ridx[0]
                                    ridx[0] += 1
                                    pr = pp[k % 2]
                                    rlb = rl[k % 2]
                                    S.op("pe", lambda e: e.matmul(pr[:, 0:w], qiT[32 * jj:32 * jj + 32, g, tsl],
                                                                  kiT4[32 * jj:32 * jj + 32, sb * 512:sb * 512 + w],
                                                                  start=True, stop=True, tile_position=(32 * jj, 0)),
                                         reads=[qiT, kiT4], writes=[pr])
                                    S.op("act", lambda e: e.activation(rlb[:, 0:w], pr[:, 0:w], AF.Relu), reads=[pr], writes=[rlb])
                                    dst = sc[:, sb * 512:sb * 512 + w]
                                    if hh == 0:
                                        S.op("dve", lambda e: e.tensor_scalar(dst, rlb[:, 0:w], widx[:, qt, 0:1], None, ALU.mult),
                                             reads=[rlb, widx], writes=[sc])
                                    else:
                                        S.op("dve", lambda e: e.scalar_tensor_tensor(dst, rlb[:, 0:w], widx[:, qt, hh:hh + 1], dst,
                                                                                     ALU.mult, ALU.add),
                                             reads=[rlb, widx, sc], writes=[sc])
                                steps.append(step)
                        return steps

                    def bisect(qt, filler):
                        n = (qt + 1) * 128
                        sc = Sc[qt % 2]
                        mk = msk[qt % 2]
                        S.op("pool", lambda e: e.memset(sc[0:64, n - 64:n], NEGBIG), writes=[sc])
                        per = (len(filler) + BIS_ITERS - 1) // BIS_ITERS if qt >= 2 else len(filler)
                        if qt >= 2:
                            S.op("dve", lambda e: e.reduce_max(bmx[:], sc[:, 0:n], AX.X), reads=[sc], writes=[bmx])
                            S.op("dve", lambda e: e.tensor_reduce(bmn[:], sc[:, 0:n - 64], AX.X, ALU.min), reads=[sc], writes=[bmn])
                            S.op("dve", lambda e: e.tensor_tensor(brg[:], bmx[:], bmn[:], ALU.subtract), reads=[bmx, bmn], writes=[brg])
                            S.op("dve", lambda e: e.tensor_scalar(brg[:], brg[:], 1e-20, None, ALU.add), reads=[brg], writes=[brg])
                            S.op("dve", lambda e: e.reciprocal(brg[:], brg[:]), reads=[brg], writes=[brg])
                            S.op("dve", lambda e: e.tensor_scalar(sc[:, 0:n], sc[:, 0:n], bmn[:, 0:1], brg[:, 0:1], ALU.subtract, ALU.mult),
                                 reads=[sc, bmn, brg], writes=[sc])
                            S.op("dve", lambda e: e.memset(bmid[:], 0.5), writes=[bmid])
                            for it in range(BIS_ITERS):
                                S.op("dve", lambda e: e.tensor_scalar(mk[:, 0:n], sc[:, 0:n], bmid[:, 0:1], None, ALU.is_ge, ALU.add,
                                                                      accum_out=bcnt[:]),
                                     reads=[sc, bmid], writes=[mk, bcnt])
                                for _ in range(per):
                                    if filler:
                                        filler.pop(0)()
                                s_next = 2.0 ** -(it + 2)
                                S.op("dve", lambda e: e.tensor_scalar(bt[:], bcnt[:], float(TOPK), 2.0 * s_next, ALU.is_ge, ALU.mult),
                                     reads=[bcnt], writes=[bt])
                                S.op("dve", lambda e: e.scalar_tensor_tensor(bmid[:], bt[:], -s_next, bmid[:], ALU.add, ALU.add),
                                     reads=[bt, bmid], writes=[bmid])
                            S.op("dve", lambda e: e.tensor_scalar(bmid[:], bmid[:], -(2.0 ** -(BIS_ITERS + 1)), None, ALU.add),
                                 reads=[bmid], writes=[bmid])
                            S.op("dve", lambda e: e.tensor_scalar(mk[:, 0:n], sc[:, 0:n], bmid[:, 0:1], None, ALU.is_ge),
                                 reads=[sc, bmid], writes=[mk])
                        else:
                            S.op("dve", lambda e: e.tensor_scalar(mk[:, 0:n], sc[:, 0:n], -1.0e29, None, ALU.is_ge),
                                 reads=[sc], writes=[mk])
                        while filler:
                            filler.pop(0)()

                    def mask_transpose(qt):
                        mk = msk[qt % 2]
                        j = qt % 4
                        for k0 in range(0, qt + 1, 8):
                            nk_ = min(8, qt + 1 - k0)
                            for q in range(nk_):
                                kt = k0 + q
                                S.op("pe", lambda e: e.transpose(pmt[:, q * 128:(q + 1) * 128], mk[:, kt * 128:(kt + 1) * 128], identb[:]),
                                     reads=[mk, identb], writes=[pmt], inc=(q == nk_ - 1))
                            S.op("act", lambda e: e.activation(mskT[:, k0:k0 + nk_, j * 128:(j + 1) * 128],
                                                               pmt[:, 0:nk_ * 128].rearrange("p (q t) -> p q t", q=nk_), AF.Copy),
                                 reads=[pmt], writes=[mskT])

                    ridx = [0]
                    for st_ in idx_steps(0):
                        st_()
                    for qt_ in range(NT):
                        filler = idx_steps(qt_ + 1) if qt_ + 1 < NT else []
                        bisect(qt_, filler)
                        mask_transpose(qt_)
                        if qt_ % 4 != 3:
                            continue
                        qb = qt_ // 4
                        for h in range(8):
                            conv_some(2)
                            p, hf = h // 2, h % 2
                            base = 64 * hf
                            pO = po[oi % 2]
                            oi += 1
                            nk = 4 * qb + 4
                            slots = {}

                            def emit_S(kt):
                                global_si = sidx[0]
                                sidx[0] += 1
                                sl = global_si % 3
                                slots[kt] = sl
                                j0 = max(0, kt - 4 * qb)
                                c0 = j0 * 128
                                pS, E, PT = ps[sl], Es[sl], PTs[sl]
                                S.op("pe", lambda e: e.matmul(pS[:, c0:512], kbT2[base:base + 64, kt * 128:(kt + 1) * 128],
                                                              qbT[base:base + 64, p, qb * 512 + c0:(qb + 1) * 512], start=True, stop=True),
                                     reads=[kbT2, qbT], writes=[pS])
                                for j in range(j0, 4):
                                    d = 4 * qb + j - kt
                                    if d in (0, 1):
                                        S.op("dve", lambda e: e.tensor_tensor(pS[:, j * 128:(j + 1) * 128], pS[:, j * 128:(j + 1) * 128],
                                                                              tz[:, h, d, :], ALU.add), reads=[pS, tz], writes=[pS])
                                S.op("act", lambda e: e.activation(E[:, c0:512], pS[:, c0:512], AF.Exp, bias=cfb[:, h:h + 1], scale=0.125),
                                     reads=[pS, cfb], writes=[E])
                                S.op("dve", lambda e: e.tensor_tensor(PT[:, c0:512], E[:, c0:512], mskT[:, kt, c0:512], ALU.mult),
                                     reads=[E, mskT], writes=[PT])

                            def emit_PV(kt):
                                c0 = max(0, kt - 4 * qb) * 128
                                PT = PTs[slots[kt]]
                                S.op("pe", lambda e: e.matmul(pO[:, c0:512], Vb[:, kt, hf, :], PT[:, c0:512],
                                                              start=(kt == 0), stop=(kt == nk - 1), skip_group_check=True),
                                     reads=[Vb, PT], writes=[pO], inc=(kt == nk - 1))

                            for s_ in range(nk + 2):
                                if s_ < nk:
                                    emit_S(s_)
                                if s_ >= 2:
                                    emit_PV(s_ - 2)
                            ocols = slice(qb * 512, (qb + 1) * 512)
                            if hf == 0:
                                S.op("act", lambda e: e.activation(rd[0:64, :], pO[64:128, :], AF.Ln), reads=[pO], writes=[rd])
                                S.op("act", lambda e: e.activation(rd[0:64, :], rd[0:64, :], AF.Exp, scale=-1.0), reads=[rd], writes=[rd])
                                S.op("dve", lambda e: e.tensor_tensor(obT[0:64, p, ocols], pO[0:64, :], rd[0:64, :], ALU.mult),
                                     reads=[pO, rd], writes=[obT])
                            else:
                                S.op("act", lambda e: e.activation(rd[64:128, :], pO[0:64, :], AF.Ln), reads=[pO], writes=[rd])
                                S.op("act", lambda e: e.activation(rd[64:128, :], rd[64:128, :], AF.Exp, scale=-1.0), reads=[rd], writes=[rd])
                                S.op("dve", lambda e: e.tensor_tensor(obT[64:128, p, ocols], pO[64:128, :], rd[64:128, :], ALU.mult),
                                     reads=[pO, rd], writes=[obT])
                    S.barrier()

                with ExitStack() as st4:
                    Wo = S.sbuf(st4, "Wo", [128, 8, DM], BF16)
                    mixT = S.sbuf(st4, "mixT", [128, 8, T], BF16)
                    bg = S.sbuf(st4, "bg", [128, 16], F32)
                    Wr = S.sbuf(st4, "Wr", [128, 8, 36], F32)
                    brb = S.sbuf(st4, "brb", [128, 36], F32)
                    st4a = ExitStack()
                    st4a.__enter__()
                    Wua = S.sbuf(st4a, "Wua", [128, 4, DM], BF16)
                    Wub = S.sbuf(st4a, "Wub", [128, 4, DM], BF16)
                    Wg = [S.sbuf(st4a, "Wg%d" % i, [128, 8, 2, 128], BF16) for i in range(2)]
                    sga = S.sbuf(st4a, "sga", [128, 512], F32)
                    sgb = S.sbuf(st4a, "sgb", [128, 512], F32)
                    m1 = S.sbuf(st4a, "m1", [128, 512], F32)
                    m2 = S.sbuf(st4a, "m2", [128, 512], F32)
                    pgs = [[S.psum(st4a, "pg%d_%d" % (q, i), [128, 512], F32) for i in range(4)] for q in range(2)]
                    sgas = [sga, S.sbuf(st4a, "sga2", [128, 512], F32)]
                    sgbs = [sgb, S.sbuf(st4a, "sgb2", [128, 512], F32)]
                    m1s = [m1, S.sbuf(st4a, "m1b", [128, 512], F32)]
                    m2s = [m2, S.sbuf(st4a, "m2b", [128, 512], F32)]
                    git = 0

                    S.dma("pool", Wua[:], w_up_a.rearrange("(c p) n -> p c n", p=128), writes=[Wua])
                    S.dma("pool", Wub[:], w_up_b.rearrange("(c p) n -> p c n", p=128), writes=[Wub])
                    S.dma("pool", Wo[:], w_o.rearrange("(c p) n -> p c n", p=128), writes=[Wo])
                    S.dma("sp", bg[:], bgate, writes=[bg])
                    S.dma("sp", Wr[:], w_r.rearrange("(c p) n -> p c n", p=128), writes=[Wr])
                    S.dma("sp", brb[:], b_r.partition_broadcast(128), writes=[brb])
                    gsrc = w_gate.rearrange("(c p) n -> p c n", p=128)
                    for m in range(8):
                        wg = Wg[m % 2]
                        S.dma("pool", wg[:, :, 0, :], gsrc[:, :, m * 128:(m + 1) * 128], writes=[wg])
                        S.dma("pool", wg[:, :, 1, :], gsrc[:, :, 1024 + m * 128:1024 + (m + 1) * 128], writes=[wg])
                        for tb in range(4):
                            cols = slice(tb * 512, (tb + 1) * 512)
                            pg = pgs[git % 2]
                            sga, sgb, m1, m2 = sgas[git % 2], sgbs[git % 2], m1s[git % 2], m2s[git % 2]
                            git += 1
                            for c in range(8):
                                S.op("pe", lambda e: e.matmul(pg[0][:], wg[:, c, 0, :], xnT[:, c, cols], start=(c == 0), stop=(c == 7)),
                                     reads=[wg, xnT], writes=[pg[0]], inc=(c == 7))
                            for c in range(8):
                                S.op("pe", lambda e: e.matmul(pg[1][:], wg[:, c, 1, :], xnT[:, c, cols], start=(c == 0), stop=(c == 7)),
                                     reads=[wg, xnT], writes=[pg[1]], inc=(c == 7))
                            for c in range(4):
                                S.op("pe", lambda e: e.matmul(pg[2][:], Wua[:, c, m * 128:(m + 1) * 128], oaT[:, c, cols], start=(c == 0), stop=(c == 3)),
                                     reads=[Wua, oaT], writes=[pg[2]], inc=(c == 3))
                            for c in range(4):
                                S.op("pe", lambda e: e.matmul(pg[3][:], Wub[:, c, m * 128:(m + 1) * 128], obT[:, c, cols], start=(c == 0), stop=(c == 3)),
                                     reads=[Wub, obT], writes=[pg[3]], inc=(c == 3))
                            S.op("act", lambda e: e.activation(sga[:], pg[0][:], AF.Sigmoid, bias=bg[:, m:m + 1]), reads=[pg[0], bg], writes=[sga])
                            S.op("act", lambda e: e.activation(sgb[:], pg[1][:], AF.Sigmoid, bias=bg[:, 8 + m:9 + m]), reads=[pg[1], bg], writes=[sgb])
                            S.op("dve", lambda e: e.tensor_tensor(m1[:], pg[2][:], sga[:], ALU.mult), reads=[pg[2], sga], writes=[m1])
                            S.op("dve", lambda e: e.tensor_tensor(m2[:], pg[3][:], sgb[:], ALU.mult), reads=[pg[3], sgb], writes=[m2])
                            S.op("dve", lambda e: e.tensor_tensor(mixT[:, m, cols], m1[:], m2[:], ALU.add), reads=[m1, m2], writes=[mixT])
                    S.barrier()
                    st4a.close()
                    pg = [S.psum(st4, "pgt%d" % i, [128, 512], F32) for i in range(2)]
                    pm = [S.psum(st4, "pm%d" % i, [128, 512], F32) for i in range(2)]
                    pl = S.psum(st4, "pl", [128, 512], F32)
                    pre = S.sbuf(st4, "pre", [128, DM], F32)
                    hbs = [S.sbuf(st4, "hb%d" % i, [128, DM], BF16) for i in range(2)]
                    hst = [S.sbuf(st4, "hst%d" % i, [128, DM], F32) for i in range(2)]
                    hT32 = S.sbuf(st4, "hT32", [128, 8, 128], F32)
                    lgA = S.sbuf(st4, "lgA", [128, NT, 36], F32)
                    gmxA = S.sbuf(st4, "gmxA", [128, NT], F32)
                    gselA = S.sbuf(st4, "gselA", [128, NT, 4], F32)
                    r4A = S.sbuf(st4, "r4A", [128, NT, 4], F32)
                    gwA = S.sbuf(st4, "gwA", [128, NT], F32)
                    le4 = S.sbuf(st4, "le4", [128, NT, 4, 8], F32)
                    seA = S.sbuf(st4, "seA", [128, NT, 8], F32)
                    se2A = S.sbuf(st4, "se2A", [128, NT, 8], F32)
                    oh1A = S.sbuf(st4, "oh1A", [128, NT, 8], F32)
                    oh2A = S.sbuf(st4, "oh2A", [128, NT, 8], F32)
                    mx1A = S.sbuf(st4, "mx1A", [128, NT], F32)
                    mx2A = S.sbuf(st4, "mx2A", [128, NT], F32)
                    w1A = S.sbuf(st4, "w1A", [128, NT], F32)
                    w2A = S.sbuf(st4, "w2A", [128, NT], F32)
                    lnsB = [dict(stats=S.sbuf(st4, "statsB%d" % k, [128, 2, 6], F32), mv=S.sbuf(st4, "mvB%d" % k, [128, 2], F32),
                                 rstd=S.sbuf(st4, "rstdB%d" % k, [128, 1], F32), nmr=S.sbuf(st4, "nmrB%d" % k, [128, 1], F32),
                                 z=S.sbuf(st4, "zB%d" % k, [128, DM], F32)) for k in range(2)]

                    def tile_A(i):
                        tsl = slice(i * 128, (i + 1) * 128)
                        xs = xt[i % 2]
                        S.dma("sp", xs[:], xns[tsl, :], reads=[xnsB], writes=[xs])
                        z = xs
                        for hf in range(2):
                            for m in range(8):
                                S.op("pe", lambda e: e.matmul(pm[hf][:], mixT[:, m, tsl], Wo[:, m, hf * 512:(hf + 1) * 512],
                                                              start=(m == 0), stop=(m == 7)), reads=[mixT, Wo], writes=[pm[hf]], inc=(m == 7))
                            S.op("dve", lambda e: e.scalar_tensor_tensor(pre[:, hf * 512:(hf + 1) * 512], z[:, hf * 512:(hf + 1) * 512],
                                                                         ALPHA, pm[hf][:], ALU.mult, ALU.add),
                                 reads=[z, pm[hf]], writes=[pre])
                        lb = lnsB[i % 2]
                        layer_norm_tile(lb, pre, None, 2, None)
                        z1 = lb["z"]
                        S.op("dve", lambda e: e.tensor_tensor(z1[:], z1[:], lnbc[:, 3, :], ALU.add), reads=[z1, lnbc], writes=[z1])

                    def tile_B(i):
                        tsl = slice(i * 128, (i + 1) * 128)
                        z = lnsB[i % 2]["z"]
                        hh = hst[i % 2]
                        hb = hbs[i % 2]
                        S.op("act", lambda e: e.activation(hb[:], z[:], AF.Copy), reads=[z], writes=[hb])
                        S.op("act", lambda e: e.activation(hh[:], z[:], AF.Copy, scale=ALPHA), reads=[z], writes=[hh])
                        S.dma("sp", hs[tsl, :], hh[:], reads=[hh], writes=[hsB], sembuf=hh)
                        S.dma("sp", hbd[tsl, :], hb[:], reads=[hb], writes=[hbdB], sembuf=hb)
                        for hf in range(2):
                            for c in range(4):
                                cc = hf * 4 + c
                                S.op("pe", lambda e: e.transpose(pg[hf][:, c * 128:(c + 1) * 128], z[:, cc * 128:(cc + 1) * 128], identF[:]),
                                     reads=[z, identF], writes=[pg[hf]], inc=(c == 3))
                            S.op("act", lambda e: e.activation(hT32[:, hf * 4:(hf + 1) * 4, :], pg[hf][:].rearrange("p (c t) -> p c t", c=4), AF.Copy),
                                 reads=[pg[hf]], writes=[hT32])
                        for c in range(8):
                            S.op("pe", lambda e: e.matmul(pl[:, 0:36], hT32[:, c, :], Wr[:, c, :], start=(c == 0), stop=(c == 7)),
                                 reads=[hT32, Wr], writes=[pl], inc=(c == 7))
                        S.op("dve", lambda e: e.tensor_tensor(lgA[:, i, :], pl[:, 0:36], brb[:], ALU.add), reads=[pl, brb], writes=[lgA])

                    tile_A(0)
                    for i in range(NT):
                        if i + 1 < NT:
                            tile_A(i + 1)
                        tile_B(i)
                    NTl = NT
                    G3 = lgA[:, :, 0:4]
                    E4 = lgA[:, :, 4:36].rearrange("p t (g e) -> p t g e", g=4)
                    bc_t = lambda ap2, n: ap2.rearrange("p (t o) -> p t o", o=1).to_broadcast([128, NTl, n])
                    S.op("dve", lambda e: e.tensor_reduce(gmxA[:], G3, AX.X, ALU.max), reads=[lgA], writes=[gmxA])
                    S.op("dve", lambda e: e.tensor_tensor(gselA[:], G3, bc_t(gmxA[:], 4), ALU.is_ge), reads=[lgA, gmxA], writes=[gselA])
                    S.op("dve", lambda e: e.tensor_tensor(r4A[:], G3, bc_t(gmxA[:], 4), ALU.subtract), reads=[lgA, gmxA], writes=[r4A])
                    S.op("act", lambda e: e.activation(r4A[:], r4A[:], AF.Exp), reads=[r4A], writes=[r4A])
                    S.op("dve", lambda e: e.tensor_reduce(gwA[:], r4A[:], AX.X, ALU.add), reads=[r4A], writes=[gwA])
                    S.op("dve", lambda e: e.reciprocal(gwA[:], gwA[:]), reads=[gwA], writes=[gwA])
                    gsel4 = gselA[:].rearrange("p t (g o) -> p t g o", o=1).to_broadcast([128, NTl, 4, 8])
                    S.op("dve", lambda e: e.tensor_tensor(le4[:], E4, gsel4, ALU.mult), reads=[lgA, gselA], writes=[le4])
                    S.op("dve", lambda e: e.tensor_reduce(seA[:], le4[:].rearrange("p t g e -> p t e g"), AX.X, ALU.add), reads=[le4], writes=[seA])
                    S.op("dve", lambda e: e.tensor_reduce(mx1A[:], seA[:], AX.X, ALU.max), reads=[seA], writes=[mx1A])
                    S.op("dve", lambda e: e.tensor_tensor(oh1A[:], seA[:], bc_t(mx1A[:], 8), ALU.is_ge), reads=[seA, mx1A], writes=[oh1A])
                    S.op("dve", lambda e: e.scalar_tensor_tensor(se2A[:], oh1A[:], NEGBIG, seA[:], ALU.mult, ALU.add), reads=[oh1A, seA], writes=[se2A])
                    S.op("dve", lambda e: e.tensor_reduce(mx2A[:], se2A[:], AX.X, ALU.max), reads=[se2A], writes=[mx2A])
                    S.op("dve", lambda e: e.tensor_tensor(oh2A[:], se2A[:], bc_t(mx2A[:], 8), ALU.is_ge), reads=[se2A, mx2A], writes=[oh2A])
                    S.op("dve", lambda e: e.tensor_tensor(w2A[:], mx2A[:], mx1A[:], ALU.subtract), reads=[mx1A, mx2A], writes=[w2A])
                    S.op("act", lambda e: e.activation(w2A[:], w2A[:], AF.Exp), reads=[w2A], writes=[w2A])
                    S.op("dve", lambda e: e.tensor_scalar(w1A[:], w2A[:], 1.0, None, ALU.add), reads=[w2A], writes=[w1A])
                    S.op("dve", lambda e: e.reciprocal(w1A[:], w1A[:]), reads=[w1A], writes=[w1A])
                    S.op("dve", lambda e: e.tensor_tensor(w2A[:], w2A[:], w1A[:], ALU.mult), reads=[w1A, w2A], writes=[w2A])
                    S.op("dve", lambda e: e.tensor_tensor(ca[:], w1A[:], gwA[:], ALU.mult), reads=[w1A, gwA], writes=[ca])
                    S.op("dve", lambda e: e.tensor_tensor(cb[:], w2A[:], gwA[:], ALU.mult), reads=[w2A, gwA], writes=[cb])
                    for Mx, ohx in ((M1, oh1A), (M2, oh2A)):
                        S.op("dve", lambda e: e.tensor_tensor(Mx[:].rearrange("p t (g e) -> p t g e", g=4),
                                                              ohx[:].rearrange("p t (o e) -> p t o e", o=1).to_broadcast([128, NTl, 4, 8]),
                                                              gsel4, ALU.mult), reads=[ohx, gselA], writes=[Mx])
                    S.op("dve", lambda e: e.tensor_tensor(Mb[:], M1[:], M2[:], ALU.add), reads=[M1, M2], writes=[Mb])
                    S.barrier()

            conv_some(1000)
            with ExitStack() as st5:
                NSL = 64
                pr = [S.psum(st5, "pr%d" % i, [128, 512], F32) for i in range(2)]
                Rall = S.sbuf(st5, "Rall", [128, NT, 32], F32)
                cntf = S.sbuf(st5, "cntf", [128, 32], F32)
                cntI = S.sbuf(st5, "cntI", [128, 32], I32)
                pcf = S.sbuf(st5, "pcf", [128, 32], F32)
                scA = S.sbuf(st5, "scA", [128, 32], F32)
                scB = S.sbuf(st5, "scB", [128, 32], F32)
                off = S.sbuf(st5, "off", [128, 32], F32)
                Pm = S.sbuf(st5, "Pm", [128, NT, 32], F32)
                prod = S.sbuf(st5, "prod", [128, NT, 32], F32)
                posaF = S.sbuf(st5, "posaF", [128, NT], F32)
                posbF = S.sbuf(st5, "posbF", [128, NT], F32)
                posaI = S.sbuf(st5, "posaI", [128, NT], I32)
                posbI = S.sbuf(st5, "posbI", [128, NT], I32)
                cmpb = S.sbuf(st5, "cmpb", [128, NSL, 32], F32)
                eidf = S.sbuf(st5, "eidf", [128, NSL], F32)
                actf = S.sbuf(st5, "actf", [128, NSL], F32)
                widF = S.sbuf(st5, "widF", [128, NSL], F32)
                widI = S.sbuf(st5, "widI", [128, NSL], I32)
                for i in range(NT):
                    S.op("pe", lambda e: e.matmul(pr[0][:, 0:32], onesb[:], Mb[:, i, :], start=(i == 0), stop=(i == NT - 1)),
                         reads=[onesb, Mb], writes=[pr[0]], inc=(i == NT - 1))
                S.op("dve", lambda e: e.tensor_copy(cntf[:], pr[0][:, 0:32]), reads=[pr[0]], writes=[cntf])
                for i in range(NT):
                    pb = pr[1]
                    for i2 in range(i):
                        S.op("pe", lambda e: e.matmul(pb[:, 0:32], onesb[:], Mb[:, i2, :], start=(i2 == 0), stop=False),
                             reads=[onesb, Mb], writes=[pb], inc=False)
                    S.op("pe", lambda e: e.matmul(pb[:, 0:32], ustrb[:], Mb[:, i, :], start=(i == 0), stop=True),
                         reads=[ustrb, Mb], writes=[pb])
                    S.op("act", lambda e: e.activation(Rall[:, i, :], pb[:, 0:32], AF.Copy), reads=[pb], writes=[Rall])
                S.op("dve", lambda e: e.tensor_scalar(pcf[:], cntf[:], 127.0, None, ALU.add), reads=[cntf], writes=[pcf])
                S.op("dve", lambda e: e.tensor_copy(cntI[:], pcf[:]), reads=[pcf], writes=[cntI])
                S.op("dve", lambda e: e.tensor_scalar(cntI[:], cntI[:], 7, None, ALU.arith_shift_right), reads=[cntI], writes=[cntI])
                S.op("dve", lambda e: e.tensor_scalar(cntI[:], cntI[:], 7, None, ALU.logical_shift_left), reads=[cntI], writes=[cntI])
                S.op("dve", lambda e: e.tensor_copy(pcf[:], cntI[:]), reads=[cntI], writes=[pcf])
                S.op("dve", lambda e: e.tensor_copy(scA[:], pcf[:]), reads=[pcf], writes=[scA])
                cur, nxt = scA, scB
                for sh in (1, 2, 4, 8, 16):
                    S.op("dve", lambda e: e.tensor_copy(nxt[:, 0:sh], cur[:, 0:sh]), reads=[cur], writes=[nxt])
                    S.op("dve", lambda e: e.tensor_tensor(nxt[:, sh:32], cur[:, sh:32], cur[:, 0:32 - sh], ALU.add), reads=[cur], writes=[nxt])
                    cur, nxt = nxt, cur
                incl = cur
                S.op("dve", lambda e: e.tensor_tensor(off[:], incl[:], pcf[:], ALU.subtract), reads=[incl, pcf], writes=[off])
                S.op("dve", lambda e: e.tensor_tensor(Pm[:], Rall[:], off[:].rearrange("p (o e) -> p o e", o=1).to_broadcast([128, NT, 32]), ALU.add),
                     reads=[Rall, off], writes=[Pm])
                for Mx, pF, pI in ((M1, posaF, posaI), (M2, posbF, posbI)):
                    S.op("dve", lambda e: e.tensor_tensor(prod[:], Mx[:], Pm[:], ALU.mult), reads=[Mx, Pm], writes=[prod])
                    S.op("dve", lambda e: e.tensor_reduce(pF[:], prod[:], AX.X, ALU.add), reads=[prod], writes=[pF])
                    S.op("dve", lambda e: e.tensor_copy(pI[:], pF[:]), reads=[pF], writes=[pI])
                S.op("dve", lambda e: e.tensor_tensor(cmpb[:], off[:].rearrange("p (o e) -> p o e", o=1).to_broadcast([128, NSL, 32]),
                                                      jvS[:].rearrange("p (j o) -> p j o", o=1).to_broadcast([128, NSL, 32]), ALU.is_le),
                     reads=[off, jvS], writes=[cmpb])
                S.op("dve", lambda e: e.tensor_reduce(eidf[:], cmpb[:], AX.X, ALU.add), reads=[cmpb], writes=[eidf])
                S.op("dve", lambda e: e.tensor_scalar(actf[:], jvS[:], incl[:, 31:32], 1.0e6, ALU.is_ge, ALU.mult), reads=[jvS, incl], writes=[actf])
                S.op("dve", lambda e: e.tensor_scalar(widF[:], eidf[:], -1.0, 128.0, ALU.add, ALU.mult), reads=[eidf], writes=[widF])
                S.op("dve", lambda e: e.tensor_scalar(widF[:], widF[:], pcolS[:, 0:1], None, ALU.add), reads=[widF, pcolS], writes=[widF])
                S.op("dve", lambda e: e.tensor_tensor(widF[:], widF[:], actf[:], ALU.add), reads=[widF, actf], writes=[widF])
                S.op("dve", lambda e: e.tensor_copy(widI[:], widF[:]), reads=[widF], writes=[widI])

                hld = [S.sbuf(st5, "hld%d" % i, [128, DM], BF16) for i in range(2)]
                for i in range(NT):
                    hl = hld[i % 2]
                    S.dma("sp", hl[:], hbd[i * 128:(i + 1) * 128, :], reads=[hbdB], writes=[hl])
                    for pI in (posaI, posbI):
                        S.dmaf("pool", lambda e: e.indirect_dma_start(out=Hs, out_offset=bass.IndirectOffsetOnAxis(ap=pI[:, i:i + 1], axis=0),
                                                                      in_=hl[:, :], in_offset=None),
                               reads=[hl, pI], writes=[HsB], sembuf=hl)

                Wg_s = [S.sbuf(st5, "Wgs%d" % i, [128, 8, 256], BF16) for i in range(2)]
                Wu_s = [S.sbuf(st5, "Wus%d" % i, [128, 8, 256], BF16) for i in range(2)]
                Wd_s = [S.sbuf(st5, "Wds%d" % i, [128, 2, DM], BF16) for i in range(2)]
                hsl = [S.sbuf(st5, "hsl%d" % i, [128, DM], BF16) for i in range(2)]
                hslT = [S.sbuf(st5, "hslT%d" % i, [128, 8, 128], BF16) for i in range(2)]
                sa = [S.sbuf(st5, "sa%d" % i, [128, 256], F32) for i in range(2)]
                hid = [S.sbuf(st5, "hid%d" % i, [128, 256], BF16) for i in range(2)]
                hidT = [S.sbuf(st5, "hidT%d" % i, [128, 2, 128], BF16) for i in range(2)]
                ysb = [S.sbuf(st5, "ysb%d" % i, [128, DM], F32) for i in range(2)]
                pht = S.psum(st5, "pht", [128, 1024], BF16)
                ptx = S.psum(st5, "ptx", [128, 1024], BF16)
                pau = [S.psum(st5, "pau%d" % i, [128, 512], F32) for i in range(2)]
                py = [S.psum(st5, "py%d" % i, [128, 512], F32) for i in range(2)]

                def st_load_a(j):
                    k = j % 2
                    S.dma("sp", hsl[k][:], Hs[j * 128:(j + 1) * 128, :], reads=[HsB], writes=[hsl[k]])
                    for wt, src in ((Wg_s[k], weg_b), (Wu_s[k], weu_b)):
                        S.dmaf("pool", lambda e: e.indirect_dma_start(out=wt[:].rearrange("p a b -> p (a b)"), out_offset=None, in_=src,
                                                                      in_offset=bass.IndirectOffsetOnAxis(ap=widI[:, j:j + 1], axis=0),
                                                                      bounds_check=bcreg, oob_is_err=False),
                               reads=[widI, WcB], writes=[wt])

                def st_load_d(j):
                    k = j % 2
                    wt = Wd_s[k]
                    S.dmaf("pool", lambda e: e.indirect_dma_start(out=wt[:].rearrange("p a b -> p (a b)"), out_offset=None, in_=wed_b,
                                                                  in_offset=bass.IndirectOffsetOnAxis(ap=widI[:, j:j + 1], axis=0),
                                                                  bounds_check=bcreg, oob_is_err=False),
                           reads=[widI, WcB], writes=[wt])

                def st_au(j):
                    k = j % 2
                    for c in range(8):
                        S.op("pe", lambda e: e.transpose(ptx[:, c * 128:(c + 1) * 128], hsl[k][:, c * 128:(c + 1) * 128], identb[:]),
                             reads=[hsl[k], identb], writes=[ptx], inc=(c == 7))
                    S.op("act", lambda e: e.activation(hslT[k][:], ptx[:].rearrange("p (c t) -> p c t", c=8), AF.Copy),
                         reads=[ptx], writes=[hslT[k]])
                    pa = pau[k]
                    for c in range(8):
                        S.op("pe", lambda e: e.matmul(pa[:, 0:256], hslT[k][:, c, :], Wg_s[k][:, c, :], start=(c == 0), stop=(c == 7)),
                             reads=[hslT[k], Wg_s[k]], writes=[pa], inc=False)
                    for c in range(8):
                        S.op("pe", lambda e: e.matmul(pa[:, 256:512], hslT[k][:, c, :], Wu_s[k][:, c, :], start=(c == 0), stop=(c == 7)),
                             reads=[hslT[k], Wu_s[k]], writes=[pa], inc=(c == 7))
                    S.op("act", lambda e: e.activation(sa[k][:], pa[:, 0:256], AF.Silu), reads=[pa], writes=[sa[k]])
                    S.op("dve", lambda e: e.tensor_tensor(hid[k][:], pa[:, 256:512], sa[k][:], ALU.mult), reads=[pa, sa[k]], writes=[hid[k]])

                def st_tr(j):
                    k = j % 2
                    o = k * 512
                    for f in range(2):
                        S.op("pe", lambda e: e.transpose(pht[:, o + f * 128:o + (f + 1) * 128], hid[k][:, f * 128:(f + 1) * 128], identb[:]),
                             reads=[hid[k], identb], writes=[pht], inc=(f == 1))
                    S.op("act", lambda e: e.activation(hidT[k][:], pht[:, o:o + 256].rearrange("p (f t) -> p f t", f=2), AF.Copy),
                         reads=[pht], writes=[hidT[k]])

                def st_y(j):
                    k = j % 2
                    for hf in range(2):
                        for f in range(2):
                            S.op("pe", lambda e: e.matmul(py[hf][:], hidT[k][:, f, :], Wd_s[k][:, f, hf * 512:(hf + 1) * 512],
                                                          start=(f == 0), stop=(f == 1)),
                                 reads=[hidT[k], Wd_s[k]], writes=[py[hf]], inc=(f == 1))
                        if hf == 0:
                            S.op("act", lambda e: e.activation(ysb[k][:, 0:512], py[0][:], AF.Copy), reads=[py[0]], writes=[ysb[k]])
                        else:
                            S.op("dve", lambda e: e.tensor_copy(ysb[k][:, 512:1024], py[1][:]), reads=[py[1]], writes=[ysb[k]])
                    S.dma("sp", Ys[j * 128:(j + 1) * 128, :], ysb[k][:], reads=[ysb[k]], writes=[YsB], sembuf=ysb[k])

                st_load_a(0)
                st_load_d(0)
                for j in range(NSL + 2):
                    if j < NSL:
                        if j + 1 < NSL:
                            st_load_a(j + 1)
                        st_au(j)
                    if 1 <= j <= NSL:
                        st_tr(j - 1)
                    if j >= 2:
                        st_y(j - 2)
                    if 1 <= j < NSL:
                        st_load_d(j)

                lns5 = dict(stats=S.sbuf(st5, "stats5", [128, 2, 6], F32), mv=S.sbuf(st5, "mv5", [128, 2], F32),
                            rstd=S.sbuf(st5, "rstd5", [128, 1], F32), nmr=S.sbuf(st5, "nmr5", [128, 1], F32),
                            z=S.sbuf(st5, "z5", [128, DM], F32))
                accs = [S.sbuf(st5, "accs%d" % i, [128, DM], F32) for i in range(2)]
                yas = [S.sbuf(st5, "yas%d" % i, [128, DM], F32) for i in range(2)]
                ybs = [S.sbuf(st5, "ybs%d" % i, [128, DM], F32) for i in range(2)]
                ost = [S.sbuf(st5, "ost%d" % i, [128, DM], F32) for i in range(2)]
                for i in range(NT):
                    k = i % 2
                    S.dma("sp", accs[k][:], hs[i * 128:(i + 1) * 128, :], reads=[hsB], writes=[accs[k]])
                    for yt, pI in ((yas[k], posaI), (ybs[k], posbI)):
                        S.dmaf("pool", lambda e: e.indirect_dma_start(out=yt[:, :], out_offset=None, in_=Ys,
                                                                      in_offset=bass.IndirectOffsetOnAxis(ap=pI[:, i:i + 1], axis=0)),
                               reads=[YsB, pI], writes=[yt])
                    S.op("dve", lambda e: e.scalar_tensor_tensor(accs[k][:], yas[k][:], ca[:, i:i + 1], accs[k][:], ALU.mult, ALU.add),
                         reads=[yas[k], ca], writes=[accs[k]])
                    S.op("dve", lambda e: e.scalar_tensor_tensor(accs[k][:], ybs[k][:], cb[:, i:i + 1], accs[k][:], ALU.mult, ALU.add),
                         reads=[ybs[k], cb], writes=[accs[k]])
                    layer_norm_tile(lns5, accs[k], None, 4, None)
                    z = lns5["z"]
                    o = ost[k]
                    S.op("dve", lambda e: e.tensor_tensor(o[:], z[:], lnbc[:, 5, :], ALU.add), reads=[z, lnbc], writes=[o])
                    S.dma("sp", out[b, i * 128:(i + 1) * 128, :], o[:], reads=[o], writes=[outB], sembuf=o)
                S.barrier()
            stB.close()
        S.nobar.clear()
        S.barrier()
    return nc


_NC = None


def _bucket_tables():
    import jax
    import jax.numpy as jnp
    with jax.default_device(jax.devices("cpu")[0]):
        kk = jnp.arange(128, dtype=jnp.int32)[:, None]
        qq = jnp.arange(128, dtype=jnp.int32)[None, :]
        tabs = []
        for d in range(2):
            rel = kk - qq - 128 * d
            nb = 16
            max_exact = 8
            ret = jnp.where(rel > 0, nb, 0)
            n = jnp.abs(rel)
            large = max_exact + (jnp.log(jnp.maximum(n, 1).astype(jnp.float32) / max_exact)
                                 / math.log(128 / max_exact) * (nb - max_exact)).astype(jnp.int32)
            large = jnp.minimum(large, nb - 1)
            tabs.append(np.asarray(ret + jnp.where(n < max_exact, n, large)))
    return np.stack(tabs, 0)


def kernel(**inputs):
    global _NC
    f32 = np.float32
    g = lambda k: np.ascontiguousarray(np.asarray(inputs[k]))
    x = g("x").astype(f32, copy=False)
    pos = g("positions").astype(np.int32, copy=False)
    rel_bias = g("rel_bias")
    bk = _bucket_tables()
    tzr = rel_bias[bk]
    tzr = np.ascontiguousarray(np.transpose(tzr, (1, 3, 0, 2))).astype(f32)
    shared = {
        "w_in": g("w_in")[0], "w_uq": g("w_uq")[0], "w_uk": g("w_uk")[0], "w_uv": g("w_uv")[0],
        "w_up_a": g("w_up_a")[0], "w_up_b": g("w_up_b")[0], "w_gate": g("w_gate")[0], "w_o": g("w_o")[0],
        "w_r": np.ascontiguousarray(np.concatenate([g("w_grp")[0], g("w_rt")[0]], axis=1)),
        "b_r": np.ascontiguousarray(np.concatenate([g("b_grp")[0], g("b_rt")[0]], axis=0)),
        "w_eg": g("w_exp_gate")[0].reshape(32, 8, 128, 256).transpose(0, 2, 1, 3).reshape(32 * 128, 2048),
        "w_eu": g("w_exp_up")[0].reshape(32, 8, 128, 256).transpose(0, 2, 1, 3).reshape(32 * 128, 2048),
        "w_ed": g("w_exp_down")[0].reshape(32, 2, 128, 1024).transpose(0, 2, 1, 3).reshape(32 * 128, 2048),
        "ustr": np.triu(np.ones((128, 128), dtype=f32), 1),
        "jv": np.tile((np.arange(64, dtype=f32) * 128.0)[None, :], (128, 1)),
        "pcol": np.arange(128, dtype=f32).reshape(128, 1),
        "lnv": np.ascontiguousarray(np.stack([g("ln0_g"), g("ln0_b"), g("ln1_g")[0], g("ln1_b")[0], g("ln2_g")[0], g("ln2_b")[0]], 0)),
        "qng": np.ascontiguousarray(g("q_norm_g")[0].reshape(2, 128).T),
        "kvg": np.ascontiguousarray(g("kv_norm_g")[0].reshape(128, 1)),
        "bgate": np.ascontiguousarray(g("b_gate")[0].reshape(16, 128).T),
        "tzr": tzr,
        "cfar": np.ascontiguousarray(rel_bias[15, :]),
        "identf": np.eye(128, dtype=f32),
        "invf": np.tile((10000.0 ** (-np.arange(16, dtype=np.float64) / 16.0) / (2.0 * math.pi)).astype(f32), 2).reshape(32, 1),
    }
    shared = {k: np.ascontiguousarray(v.astype(f32, copy=False)) for k, v in shared.items()}
    if _NC is None:
        _NC = build()
    in_maps = []
    for c in range(8):
        m = dict(shared)
        m["x"] = np.ascontiguousarray(x[NB * c:NB * (c + 1)])
        m["pos"] = np.ascontiguousarray(pos[NB * c:NB * (c + 1)])
        in_maps.append(m)
    res = run_bass_kernel_spmd(_NC, in_maps, core_ids=list(range(8)))
    return np.concatenate([np.asarray(r["out"]) for r in res.results], axis=0).astype(f32, copy=False)
```

```python
import math
from contextlib import ExitStack
import numpy as np
import concourse.bass as bass
import concourse.mybir as mybir
from concourse.bass_utils import run_bass_kernel_spmd

F32 = mybir.dt.float32
BF16 = mybir.dt.bfloat16
I32 = mybir.dt.int32
ALU = mybir.AluOpType
AF = mybir.ActivationFunctionType
AX = mybir.AxisListType

T = 2048
DM = 1024
NT = 16
NB = 2
ALPHA = 2.0 ** 0.25
LN_EPS = 1e-5
RMS_EPS = 1e-6
NEGBIG = -1.0e30
BIS_ITERS = 14
TOPK = 256


class Buf:
    __slots__ = ("ap", "name", "ws", "reads", "dsem", "dcount")

    def __init__(self, ap, name=""):
        self.ap = ap
        self.name = name
        self.ws = {}
        self.reads = {}
        self.dsem = None
        self.dcount = 0

    def __getitem__(self, idx):
        return self.ap[idx]


class Sched:
    def __init__(self, nc, stack):
        self.nc = nc
        self.stack = stack
        self.engs = {}
        for name, eng in (("pe", nc.tensor), ("act", nc.scalar), ("dve", nc.vector),
                          ("pool", nc.gpsimd), ("sp", nc.sync)):
            sem = stack.enter_context(nc.semaphore("s_" + name))
            self.engs[name] = dict(eng=eng, sem=sem, count=0, waited={})
        self.ndsem = 0
        self.dpool = {}
        self.nobar = set()
        self.n_ins = 0
        self.uid = 0

    def sbuf(self, st, name, shape, dtype):
        self.uid += 1
        name = "%s_%d" % (name, self.uid)
        return Buf(st.enter_context(self.nc.sbuf_tensor(name, shape, dtype)), name)

    def psum(self, st, name, shape, dtype):
        self.uid += 1
        name = "%s_%d" % (name, self.uid)
        return Buf(st.enter_context(self.nc.psum_tensor(name, shape, dtype)), name)

    def _wait(self, engname, ev):
        sem, val, src = ev
        if src == "pe" and engname == "pe":
            return
        E = self.engs[engname]
        key = id(sem)
        if E["waited"].get(key, 0) < val:
            E["eng"].wait_ge(sem, val)
            E["waited"][key] = val
            self.n_ins += 1

    def _deps(self, engname, reads, writes):
        for b in reads:
            for ev in b.ws.values():
                self._wait(engname, ev)
        for b in writes:
            for ev in b.ws.values():
                self._wait(engname, ev)
            for ev in b.reads.values():
                self._wait(engname, ev)

    def _commit(self, ev, reads, writes):
        k = id(ev[0])
        for b in writes:
            b.ws[k] = ev
            b.reads = {}
        for b in reads:
            if b in writes:
                continue
            b.reads[k] = ev

    def op(self, engname, fn, reads=(), writes=(), inc=True):
        E = self.engs[engname]
        self._deps(engname, reads, writes)
        ins = fn(E["eng"])
        self.n_ins += 1
        if inc:
            E["count"] += 1
            ins.then_inc(E["sem"], 1)
            self._commit((E["sem"], E["count"], engname), reads, writes)
        else:
            self._commit((E["sem"], E["count"] + 1, engname), reads, writes)

    def dma(self, qname, out_ap, in_ap, reads=(), writes=(), sembuf=None):
        E = self.engs[qname]
        self._deps(qname, reads, writes)
        sb = sembuf or (writes[0] if writes else reads[0])
        key = sb.name.rsplit("_", 1)[0] if "_" in sb.name else sb.name
        ent = self.dpool.get(key)
        if ent is None:
            ent = [self.stack.enter_context(self.nc.semaphore("d%d" % self.ndsem)), 0]
            self.ndsem += 1
            self.dpool[key] = ent
        ins = E["eng"].dma_start(out=out_ap, in_=in_ap)
        ent[1] += 16
        ins.then_inc(ent[0], 16)
        self.n_ins += 1
        ev = (ent[0], ent[1], "dma")
        self._commit(ev, reads, writes)
        return ev

    def dmaf(self, qname, fn, reads=(), writes=(), sembuf=None):
        E = self.engs[qname]
        self._deps(qname, reads, writes)
        sb = sembuf or (writes[0] if writes else reads[0])
        key = sb.name.rsplit("_", 1)[0] if "_" in sb.name else sb.name
        ent = self.dpool.get(key)
        if ent is None:
            ent = [self.stack.enter_context(self.nc.semaphore("d%d" % self.ndsem)), 0]
            self.ndsem += 1
            self.dpool[key] = ent
        ins = fn(E["eng"])
        ent[1] += 16
        ins.then_inc(ent[0], 16)
        self.n_ins += 1
        ev = (ent[0], ent[1], "dma")
        self._commit(ev, reads, writes)
        return ev

    def barrier(self):
        evs = []
        for n, E in self.engs.items():
            if E["count"] > 0:
                evs.append((E["sem"], E["count"], "bar_" + n))
        for key, ent in self.dpool.items():
            if ent[1] > 0 and key not in self.nobar:
                evs.append((ent[0], ent[1], "dma"))
        for n in self.engs:
            for ev in evs:
                if ev[2] == "bar_" + n:
                    continue
                self._wait(n, ev)


def build():
    nc = bass.Bass("TRN2", target_bir_lowering=False)

    def din(name, shape, dt=F32):
        return nc.dram_tensor(name, shape, dt, kind="ExternalInput").ap()

    x = din("x", [NB, T, DM])
    pos = din("pos", [NB, T], I32)
    w_in = din("w_in", [DM, 1352])
    w_uq = din("w_uq", [256, 768])
    w_uk = din("w_uk", [128, 512])
    w_uv = din("w_uv", [128, 512])
    w_up_a = din("w_up_a", [512, DM])
    w_up_b = din("w_up_b", [512, DM])
    w_gate = din("w_gate", [DM, 2048])
    w_o = din("w_o", [DM, DM])
    w_r = din("w_r", [DM, 36])
    b_r = din("b_r", [36])
    w_eg = din("w_eg", [32 * 128, 2048])
    w_eu = din("w_eu", [32 * 128, 2048])
    w_ed = din("w_ed", [32 * 128, 2048])
    ustr = din("ustr", [128, 128])
    jv = din("jv", [128, 64])
    pcol = din("pcol", [128, 1])
    lnv = din("lnv", [6, DM])
    qng = din("qng", [128, 2])
    kvg = din("kvg", [128, 1])
    bgate = din("bgate", [128, 16])
    tzr = din("tzr", [128, 8, 2, 128])
    cfar = din("cfar", [8])
    identf = din("identf", [128, 128])
    invf = din("invf", [32, 1])
    out = nc.dram_tensor("out", [NB, T, DM], F32, kind="ExternalOutput").ap()
    hs = nc.dram_tensor("hs", [T, DM], F32, kind="Internal").ap()
    hbd = nc.dram_tensor("hbd", [T, DM], BF16, kind="Internal").ap()
    Hs = nc.dram_tensor("Hs", [64 * 128, DM], BF16, kind="Internal").ap()
    Ys = nc.dram_tensor("Ys", [64 * 128, DM], F32, kind="Internal").ap()
    weg_b = nc.dram_tensor("weg_b", [32 * 128, 2048], BF16, kind="Internal").ap()
    weu_b = nc.dram_tensor("weu_b", [32 * 128, 2048], BF16, kind="Internal").ap()
    wed_b = nc.dram_tensor("wed_b", [32 * 128, 2048], BF16, kind="Internal").ap()

    with ExitStack() as st0:
        S = Sched(nc, st0)
        hsB = Buf(hs, "hs")
        outB = Buf(out, "out")
        bcreg = st0.enter_context(nc.gpsimd.register("bcreg"))
        nc.gpsimd.reg_mov(bcreg, 32 * 128 - 1)
        hbdB = Buf(hbd, "hbd")
        HsB = Buf(Hs, "Hs")
        YsB = Buf(Ys, "Ys")

        identb = S.sbuf(st0, "identb", [128, 128], BF16)
        identF = S.sbuf(st0, "identF", [128, 128], F32)
        onesb = S.sbuf(st0, "onesb", [128, 128], BF16)
        lnbc = S.sbuf(st0, "lnbc", [128, 6, DM], F32)
        S.dma("pool", identb[:], identf, writes=[identb])
        ustrb = S.sbuf(st0, "ustrb", [128, 128], BF16)
        jvS = S.sbuf(st0, "jvS", [128, 64], F32)
        pcolS = S.sbuf(st0, "pcolS", [128, 1], F32)
        S.dma("pool", ustrb[:], ustr, writes=[ustrb])
        S.dma("sp", jvS[:], jv, writes=[jvS])
        S.dma("sp", pcolS[:], pcol, writes=[pcolS])
        WcB = Buf(None, "wcast")
        S.nobar.add("wcast")
        conv_list = [(srcw, dstw, e_) for e_ in range(32) for srcw, dstw in ((w_eg, weg_b), (w_eu, weu_b), (w_ed, wed_b))]

        def conv_some(n):
            for _ in range(n):
                if not conv_list:
                    return
                srcw, dstw, e_ = conv_list.pop(0)
                S.dma("pool", dstw[e_ * 128:(e_ + 1) * 128, :], srcw[e_ * 128:(e_ + 1) * 128, :], writes=[WcB])
        with ExitStack() as stz:
            zt = S.sbuf(stz, "zt", [128, 8, DM], BF16)
            S.op("pool", lambda e: e.memset(zt[:], 0.0), writes=[zt])
            for q in range(8):
                S.dma("sp", Hs[q * 1024:(q + 1) * 1024, :].rearrange("(p r) n -> p r n", p=128), zt[:], reads=[zt], writes=[HsB], sembuf=zt)
            S.barrier()
        S.dma("sp", identF[:], identf, writes=[identF])
        S.op("dve", lambda e: e.memset(onesb[:], 1.0), writes=[onesb])
        for k in range(6):
            S.dma("sp", lnbc[:, k, :], lnv[k].partition_broadcast(128), writes=[lnbc])

        def layer_norm_tile(st_bufs, src, dst_ap_fn, gi, out_bufs, scale_after=None):
            stats, mv, rstd, nmr, z = (st_bufs[k] for k in ("stats", "mv", "rstd", "nmr", "z"))
            S.op("dve", lambda e: e.bn_stats(stats[:, 0, :], src[:, 0:512]), reads=[src], writes=[stats])
            S.op("dve", lambda e: e.bn_stats(stats[:, 1, :], src[:, 512:1024]), reads=[src], writes=[stats])
            S.op("dve", lambda e: e.bn_aggr(mv[:], stats[:].rearrange("p a b -> p (a b)")), reads=[stats], writes=[mv])
            S.op("dve", lambda e: e.tensor_scalar(rstd[:], mv[:, 1:2], LN_EPS, None, ALU.add), reads=[mv], writes=[rstd])
            S.op("act", lambda e: e.activation(rstd[:], rstd[:], AF.Sqrt), reads=[rstd], writes=[rstd])
            S.op("dve", lambda e: e.reciprocal(rstd[:], rstd[:]), reads=[rstd], writes=[rstd])
            S.op("dve", lambda e: e.tensor_scalar(nmr[:], mv[:, 0:1], rstd[:, 0:1], -1.0, ALU.mult, ALU.mult),
                 reads=[mv, rstd], writes=[nmr])
            S.op("act", lambda e: e.activation(z[:], src[:], AF.Identity, bias=nmr[:, 0:1], scale=rstd[:, 0:1]),
                 reads=[src, nmr, rstd], writes=[z])
            S.op("dve", lambda e: e.tensor_tensor(z[:], z[:], lnbc[:, gi, :], ALU.mult), reads=[z, lnbc], writes=[z])

        for b in range(NB):
            stB = ExitStack()
            stB.__enter__()
            xnT = S.sbuf(stB, "xnT", [128, 8, T], BF16)
            ca = S.sbuf(stB, "ca", [128, NT], F32)
            cb = S.sbuf(stB, "cb", [128, NT], F32)
            M1 = S.sbuf(stB, "M1", [128, NT, 32], F32)
            M2 = S.sbuf(stB, "M2", [128, NT, 32], F32)
            Mb = S.sbuf(stB, "Mb", [128, NT, 32], BF16)
            with ExitStack() as stA:
                oaT = S.sbuf(stA, "oaT", [128, 4, T], BF16)
                obT = S.sbuf(stA, "obT", [128, 4, T], BF16)
                lns = dict(stats=S.sbuf(stA, "stats", [128, 2, 6], F32), mv=S.sbuf(stA, "mv", [128, 2], F32),
                           rstd=S.sbuf(stA, "rstd", [128, 1], F32), nmr=S.sbuf(stA, "nmr", [128, 1], F32),
                           z=S.sbuf(stA, "z", [128, DM], F32))
                xt = [S.sbuf(stA, "xt%d" % i, [128, DM], F32) for i in range(2)]

                with ExitStack() as st1:
                    xnb = [S.sbuf(st1, "xnb%d" % i, [128, DM], BF16) for i in range(2)]
                    ptr = [S.psum(st1, "ptr%d" % i, [128, 1024], BF16) for i in range(2)]
                    for i in range(NT):
                        xs = xt[i % 2]
                        S.dma("sp", xs[:], x[b, i * 128:(i + 1) * 128, :], writes=[xs])
                        layer_norm_tile(lns, xs, None, 0, None)
                        z = lns["z"]
                        xb = xnb[i % 2]
                        S.op("dve", lambda e: e.tensor_tensor(xb[:], z[:], lnbc[:, 1, :], ALU.add),
                             reads=[z, lnbc], writes=[xb])
                        pt = ptr[i % 2]
                        for c in range(8):
                            S.op("pe", lambda e: e.transpose(pt[:, c * 128:(c + 1) * 128], xb[:, c * 128:(c + 1) * 128], identb[:]),
                                 reads=[xb, identb], writes=[pt], inc=(c == 7))
                        S.op("act", lambda e: e.activation(xnT[:, :, i * 128:(i + 1) * 128],
                                                           pt[:].rearrange("p (c t) -> p c t", c=8), AF.Copy),
                             reads=[pt], writes=[xnT])
                    S.barrier()

                with ExitStack() as st2:
                    pp = [S.psum(st2, "pp%d" % i, [128, 512], F32) for i in range(3)]
                    ps = [S.psum(st2, "ps%d" % i, [128, 512], F32) for i in range(3)]
                    po = [S.psum(st2, "po%d" % i, [128, 512], F32) for i in range(2)]
                    Wa = S.sbuf(st2, "Wa", [128, 8, 416], BF16)
                    Wkrr = S.sbuf(st2, "Wkrr", [128, 8, 32], BF16)
                    Wq = S.sbuf(st2, "Wq", [128, 2, 8, 96], BF16)
                    Wqr = S.sbuf(st2, "Wqr", [128, 2, 8, 32], BF16)
                    Wk = S.sbuf(st2, "Wk", [128, 8, 64], BF16)
                    Wv = S.sbuf(st2, "Wv", [128, 512], BF16)
                    gq = S.sbuf(st2, "gq", [128, 2], F32)
                    gkv = S.sbuf(st2, "gkv", [128, 1], F32)
                    invfS = S.sbuf(st2, "invfS", [96, 1], F32)
                    cos32 = S.sbuf(st2, "cos32", [96, T], F32)
                    sin32 = S.sbuf(st2, "sin32", [96, T], F32)
                    cqn = S.sbuf(st2, "cqn", [128, 2, T], BF16)
                    ckvn = S.sbuf(st2, "ckvn", [128, T], BF16)
                    krT = S.sbuf(st2, "krT", [96, T], BF16)
                    st2a = ExitStack()
                    st2a.__enter__()
                    wq32 = S.sbuf(st2a, "wq32", [128, 2, 768], F32)
                    wk32 = S.sbuf(st2a, "wk32", [128, 512], F32)
                    wv32 = S.sbuf(st2a, "wv32", [128, 512], F32)
                    posi = S.sbuf(st2a, "posi", [96, T], I32)
                    rr = S.sbuf(st2a, "rr", [96, T], F32)
                    rf = S.sbuf(st2a, "rf", [96, T], F32)
                    tq = S.sbuf(st2a, "tq", [96, T], F32)

                    wsrc = w_in.rearrange("(c p) n -> p c n", p=128)
                    S.dma("pool", Wa[:], wsrc[:, :, 0:416], writes=[Wa])
                    S.dma("sp", wq32[:], w_uq.rearrange("(c p) n -> p c n", p=128), writes=[wq32])
                    S.dma("sp", wk32[:], w_uk, writes=[wk32])
                    S.dma("sp", wv32[:], w_uv, writes=[wv32])
                    S.dma("sp", gq[:], qng, writes=[gq])
                    S.dma("sp", gkv[:], kvg, writes=[gkv])
                    S.dma("sp", invfS[64:96, :], invf, writes=[invfS])
                    S.dma("sp", posi[64:96, :], pos[b].partition_broadcast(32), writes=[posi])
                    S.op("dve", lambda e: e.tensor_scalar(Wkrr[:, :, 0:16], Wa[:, :, 400:416], -1.0, None, ALU.mult),
                         reads=[Wa], writes=[Wkrr])
                    S.op("dve", lambda e: e.tensor_copy(Wkrr[:, :, 16:32], Wa[:, :, 384:400]), reads=[Wa], writes=[Wkrr])
                    for c in range(2):
                        src = wq32[:, c, :].rearrange("p (h d) -> p h d", h=8)
                        g = gq[:, c:c + 1]
                        S.op("dve", lambda e: e.tensor_scalar(Wq[:, c, :, :], src[:, :, :], g, None, ALU.mult),
                             reads=[wq32, gq], writes=[Wq])
                        S.op("dve", lambda e: e.tensor_scalar(Wqr[:, c, :, 0:16], src[:, :, 80:96], g, -1.0, ALU.mult, ALU.mult),
                             reads=[wq32, gq], writes=[Wqr])
                        S.op("dve", lambda e: e.tensor_scalar(Wqr[:, c, :, 16:32], src[:, :, 64:80], g, None, ALU.mult),
                             reads=[wq32, gq], writes=[Wqr])
                    S.op("dve", lambda e: e.tensor_scalar(Wk[:, :, :], wk32[:].rearrange("p (h d) -> p h d", h=8),
                                                          gkv[:, 0:1], None, ALU.mult), reads=[wk32, gkv], writes=[Wk])
                    S.op("dve", lambda e: e.tensor_scalar(Wv[:], wv32[:], gkv[:, 0:1], None, ALU.mult),
                         reads=[wv32, gkv], writes=[Wv])

                    S.op("dve", lambda e: e.tensor_copy(rr[64:96, :], posi[64:96, :]), reads=[posi], writes=[rr])
                    S.op("dve", lambda e: e.tensor_scalar(rr[64:96, :], rr[64:96, :], invfS[64:96, 0:1], None, ALU.mult), reads=[rr, invfS], writes=[rr])
                    S.op("dve", lambda e: e.tensor_copy(posi[64:96, :], rr[64:96, :]), reads=[rr], writes=[posi])
                    S.op("dve", lambda e: e.tensor_copy(rf[64:96, :], posi[64:96, :]), reads=[posi], writes=[rf])
                    S.op("dve", lambda e: e.tensor_tensor(rr[64:96, :], rr[64:96, :], rf[64:96, :], ALU.subtract), reads=[rr, rf], writes=[rr])

                    def wrap_sin(dst, shift):
                        S.op("dve", lambda e: e.tensor_scalar(rf[64:96, :], rr[64:96, :], shift, None, ALU.add), reads=[rr], writes=[rf])
                        for _ in range(2):
                            S.op("dve", lambda e: e.tensor_scalar(tq[64:96, :], rf[64:96, :], 0.5, None, ALU.is_gt), reads=[rf], writes=[tq])
                            S.op("dve", lambda e: e.tensor_tensor(rf[64:96, :], rf[64:96, :], tq[64:96, :], ALU.subtract), reads=[rf, tq], writes=[rf])
                        S.op("dve", lambda e: e.tensor_scalar(tq[64:96, :], rf[64:96, :], -0.5, None, ALU.is_lt), reads=[rf], writes=[tq])
                        S.op("dve", lambda e: e.tensor_tensor(rf[64:96, :], rf[64:96, :], tq[64:96, :], ALU.add), reads=[rf, tq], writes=[rf])
                        S.op("act", lambda e: e.activation(dst[64:96, :], rf[64:96, :], AF.Sin, scale=2.0 * math.pi * (1.0 - 2e-6)),
                             reads=[rf], writes=[dst])

                    wrap_sin(sin32, 0.0)
                    wrap_sin(cos32, 0.25)
                    S.barrier()
                    st2a.close()
                    Vh = [S.sbuf(st2, "Vh%d" % i, [128, NT, 128], BF16) for i in range(2)]
                    qTs = [S.sbuf(st2, "qT%d" % i, [96, T], BF16) for i in range(2)]
                    kTs = [S.sbuf(st2, "kT%d" % i, [96, T], BF16) for i in range(2)]
                    c32 = S.sbuf(st2, "c32", [128, 2, 512], F32)
                    sqb = S.sbuf(st2, "sqb", [128, 2, 512], BF16)
                    rq = S.sbuf(st2, "rq", [128, 512], F32)
                    t1 = S.sbuf(st2, "t1", [96, 512], F32)
                    t2 = S.sbuf(st2, "t2", [96, 512], F32)
                    PTs = [S.sbuf(st2, "PT%d" % i, [128, 512], BF16) for i in range(3)]
                    rd = S.sbuf(st2, "rd", [128, 512], F32)
                    for i in range(2):
                        S.op("pool", lambda e: e.memset(Vh[i][:], 1.0), writes=[Vh[i]])

                    def rms_block(psrc_list, dstT_fn, nfeat, tb):
                        n = len(psrc_list)
                        for m, pb in enumerate(psrc_list):
                            S.op("act", lambda e: e.activation(c32[:, m, :], pb[:], AF.Copy), reads=[pb], writes=[c32])
                            S.op("act", lambda e: e.activation(sqb[:, m, :], pb[:], AF.Square), reads=[pb], writes=[sqb])
                        pss = pp[2]
                        for m in range(n):
                            S.op("pe", lambda e: e.matmul(pss[:], onesb[:], sqb[:, m, :], start=(m == 0), stop=(m == n - 1)),
                                 reads=[onesb, sqb], writes=[pss], inc=(m == n - 1))
                        S.op("dve", lambda e: e.tensor_scalar(rq[:], pss[:], 1.0 / nfeat, RMS_EPS, ALU.mult, ALU.add),
                             reads=[pss], writes=[rq])
                        S.op("act", lambda e: e.activation(rq[:], rq[:], AF.Sqrt), reads=[rq], writes=[rq])
                        S.op("dve", lambda e: e.reciprocal(rq[:], rq[:]), reads=[rq], writes=[rq])
                        for m in range(n):
                            S.op("dve", lambda e: e.tensor_tensor(dstT_fn(m), c32[:, m, :], rq[:], ALU.mult),
                                 reads=[c32, rq], writes=[dstT_fn.buf])

                    def proj_fm(pb, lhs_fn, tb, M=128, p0=0):
                        for c in range(8):
                            S.op("pe", lambda e: e.matmul(pb[p0:p0 + M, :], lhs_fn(c), xnT[:, c, tb * 512:(tb + 1) * 512],
                                                          start=(c == 0), stop=(c == 7)),
                                 reads=[xnT, Wa, Wkrr], writes=[pb], inc=(c == 7))

                    for tb in range(4):
                        cols = slice(tb * 512, (tb + 1) * 512)
                        proj_fm(pp[0], lambda c: Wa[:, c, 0:128], tb)
                        proj_fm(pp[1], lambda c: Wa[:, c, 128:256], tb)
                        f = lambda m: cqn[:, m, cols]
                        f.buf = cqn
                        rms_block([pp[0], pp[1]], f, 256.0, tb)
                        proj_fm(pp[0], lambda c: Wa[:, c, 256:384], tb)
                        f2 = lambda m: ckvn[:, cols]
                        f2.buf = ckvn
                        rms_block([pp[0]], f2, 128.0, tb)
                        proj_fm(pp[0], lambda c: Wa[:, c, 384:416], tb, M=32, p0=64)
                        proj_fm(pp[1], lambda c: Wkrr[:, c, :], tb, M=32, p0=64)
                        S.op("dve", lambda e: e.tensor_tensor(t1[64:96, :], pp[0][64:96, :], cos32[64:96, cols], ALU.mult),
                             reads=[pp[0], cos32], writes=[t1])
                        S.op("dve", lambda e: e.tensor_tensor(t2[64:96, :], pp[1][64:96, :], sin32[64:96, cols], ALU.mult),
                             reads=[pp[1], sin32], writes=[t2])
                        S.op("dve", lambda e: e.tensor_tensor(krT[64:96, cols], t1[64:96, :], t2[64:96, :], ALU.add), reads=[t1, t2], writes=[krT])
                    sc_mla = 96.0 ** -0.5
                    LA = 2

                    def mla_proj(h):
                        qT = qTs[h % 2]
                        kT = kTs[h % 2]
                        for tb in range(4):
                            cols = slice(tb * 512, (tb + 1) * 512)
                            pa, pbb = pp[0], pp[1]
                            for m in range(2):
                                S.op("pe", lambda e: e.matmul(pa[0:96, :], Wq[:, m, h, :], cqn[:, m, cols], start=(m == 0), stop=(m == 1)),
                                     reads=[Wq, cqn], writes=[pa], inc=(m == 1))
                            for m in range(2):
                                S.op("pe", lambda e: e.matmul(pbb[64:96, :], Wqr[:, m, h, :], cqn[:, m, cols], start=(m == 0), stop=(m == 1)),
                                     reads=[Wqr, cqn], writes=[pbb], inc=(m == 1))
                            pk = pp[2]
                            S.op("pe", lambda e: e.matmul(pk[0:64, :], Wk[:, h, :], ckvn[:, cols], start=True, stop=True),
                                 reads=[Wk, ckvn], writes=[pk])
                            S.op("dve", lambda e: e.tensor_tensor(t1[64:96, :], pa[64:96, :], cos32[64:96, cols], ALU.mult), reads=[pa, cos32], writes=[t1])
                            S.op("dve", lambda e: e.tensor_tensor(t2[64:96, :], pbb[64:96, :], sin32[64:96, cols], ALU.mult), reads=[pbb, sin32], writes=[t2])
                            S.op("dve", lambda e: e.tensor_tensor(qT[64:96, cols], t1[64:96, :], t2[64:96, :], ALU.add), reads=[t1, t2], writes=[qT])
                            S.op("act", lambda e: e.activation(qT[0:64, cols], pa[0:64, :], AF.Copy), reads=[pa], writes=[qT])
                            S.op("act", lambda e: e.activation(kT[0:64, cols], pk[0:64, :], AF.Copy), reads=[pk], writes=[kT])
                        S.op("pool", lambda e: e.tensor_copy(kT[64:96, :], krT[64:96, :]), reads=[krT], writes=[kT])
                        Vc = Vh[h % 2]
                        voff = 64 * (h % 2)
                        for k4 in range(4):
                            pv = pp[k4 % 2]
                            for j in range(4):
                                kt = k4 * 4 + j
                                S.op("pe", lambda e: e.matmul(pv[:, j * 64:(j + 1) * 64], ckvn[:, kt * 128:(kt + 1) * 128],
                                                              Wv[:, h * 64:(h + 1) * 64], start=True, stop=True),
                                     reads=[ckvn, Wv], writes=[pv], inc=(j == 3))
                            S.op("act", lambda e: e.activation(Vc[:, k4 * 4:(k4 + 1) * 4, voff:voff + 64],
                                                               pv[:, 0:256].rearrange("p (j d) -> p j d", j=4), AF.Copy),
                                 reads=[pv], writes=[Vc])

                    st_ = dict(si=0, oi=0)

                    def mla_attn(h):
                        conv_some(6)
                        qT = qTs[h % 2]
                        kT = kTs[h % 2]
                        Vc = Vh[h % 2]
                        pOs = {}
                        for qb in range(4):
                            pOs[qb] = po[st_["oi"] % 2]
                            st_["oi"] += 1
                        slots = {}
                        steps = [(qb, kt) for qb in range(4) for kt in range(4 * qb + 4)]

                        def emit_S(qb, kt):
                            c0 = max(0, kt - 4 * qb) * 128
                            sl = st_["si"] % 3
                            st_["si"] += 1
                            slots[(qb, kt)] = sl
                            pS, PT = ps[sl], PTs[sl]
                            S.op("pe", lambda e: e.matmul(pS[:, c0:512], kT[:, kt * 128:(kt + 1) * 128],
                                                          qT[:, qb * 512 + c0:(qb + 1) * 512], start=True, stop=True),
                                 reads=[kT, qT], writes=[pS])
                            S.op("act", lambda e: e.activation(PT[:, c0:512], pS[:, c0:512], AF.Exp, scale=sc_mla),
                                 reads=[pS], writes=[PT])
                            if kt >= 4 * qb:
                                S.op("pool", lambda e: e.memset(PT[64:128, c0:c0 + 64], 0.0), writes=[PT])

                        def emit_PV(qb, kt):
                            nk = 4 * qb + 4
                            pO = pOs[qb]
                            c0 = max(0, kt - 4 * qb) * 128
                            PT = PTs[slots[(qb, kt)]]
                            S.op("pe", lambda e: e.matmul(pO[:, c0:512], Vc[:, kt, :], PT[:, c0:512],
                                                          start=(kt == 0), stop=(kt == nk - 1), skip_group_check=True),
                                 reads=[Vc, PT], writes=[pO], inc=(kt == nk - 1))
                            if kt == nk - 1:
                                ocols = slice(qb * 512, (qb + 1) * 512)
                                if h % 2 == 0:
                                    S.op("dve", lambda e: e.reciprocal(rd[0:64, :], pO[64:128, :]), reads=[pO], writes=[rd])
                                    S.op("dve", lambda e: e.tensor_tensor(oaT[0:64, h // 2, ocols], pO[0:64, :], rd[0:64, :], ALU.mult),
                                         reads=[pO, rd], writes=[oaT])
                                else:
                                    S.op("dve", lambda e: e.reciprocal(rd[64:128, :], pO[0:64, :]), reads=[pO], writes=[rd])
                                    S.op("dve", lambda e: e.tensor_tensor(oaT[64:128, h // 2, ocols], pO[64:128, :], rd[64:128, :], ALU.mult),
                                         reads=[pO, rd], writes=[oaT])

                        for s_ in range(len(steps) + LA):
                            if s_ < len(steps):
                                emit_S(*steps[s_])
                            if s_ >= LA:
                                emit_PV(*steps[s_ - LA])

                    mla_proj(0)
                    for h in range(8):
                        if h + 1 < 8:
                            mla_proj(h + 1)
                        mla_attn(h)
                    S.barrier()

                with ExitStack() as st3:
                    pp = [S.psum(st3, "qp%d" % i, [128, 512], F32) for i in range(2)]
                    ps = [S.psum(st3, "qs%d" % i, [128, 512], F32) for i in range(3)]
                    po = [S.psum(st3, "qo%d" % i, [128, 512], F32) for i in range(2)]
                    pmt = S.psum(st3, "pmt", [128, 1024], BF16)
                    qbT = S.sbuf(st3, "qbT", [128, 4, T], BF16)
                    kbT2 = S.sbuf(st3, "kbT2", [128, T], BF16)
                    qiT = S.sbuf(st3, "qiT", [128, 2, T], BF16)
                    kiT4 = S.sbuf(st3, "kiT4", [128, T], BF16)
                    Vb = S.sbuf(st3, "Vb", [128, NT, 2, 128], BF16)
                    widx = S.sbuf(st3, "widx", [128, NT, 8], F32)
                    tz = S.sbuf(st3, "tz", [128, 8, 2, 128], F32)
                    cfb = S.sbuf(st3, "cfb", [128, 8], F32)
                    st3a = ExitStack()
                    st3a.__enter__()
                    Wb = S.sbuf(st3a, "Wb", [128, 8, 936], BF16)
                    Wkb2 = S.sbuf(st3a, "Wkb2", [128, 8, 128], BF16)
                    Wki4 = S.sbuf(st3a, "Wki4", [128, 8, 128], BF16)

                    wsrc = w_in.rearrange("(c p) n -> p c n", p=128)
                    S.dma("pool", Wb[:], wsrc[:, :, 416:1352], writes=[Wb])
                    for r in range(2):
                        S.dma("pool", Wkb2[:, :, r * 64:(r + 1) * 64], wsrc[:, :, 928:992], writes=[Wkb2])
                    for r in range(4):
                        S.dma("pool", Wki4[:, :, r * 32:(r + 1) * 32], wsrc[:, :, 1312:1344], writes=[Wki4])
                    S.dma("sp", tz[:], tzr, writes=[tz])
                    S.dma("sp", cfb[:], cfar.partition_broadcast(128), writes=[cfb])
                    for h in range(8):
                        S.op("dve", lambda e: e.tensor_scalar(tz[:, h], tz[:, h], cfb[:, h:h + 1], 8.0, ALU.subtract, ALU.mult),
                             reads=[tz, cfb], writes=[tz])
                    S.op("pool", lambda e: e.memset(Vb[:], 1.0), writes=[Vb])

                    def proj3(dst_ap, dstbuf, lhs_fn, wbuf, tb, k):
                        pb = pp[k % 2]
                        for c in range(8):
                            S.op("pe", lambda e: e.matmul(pb[:], lhs_fn(c), xnT[:, c, tb * 512:(tb + 1) * 512],
                                                          start=(c == 0), stop=(c == 7)), reads=[xnT, wbuf], writes=[pb], inc=(c == 7))
                        S.op("act", lambda e: e.activation(dst_ap, pb[:], AF.Copy), reads=[pb], writes=[dstbuf])

                    k = 0
                    for tb in range(4):
                        cols = slice(tb * 512, (tb + 1) * 512)
                        for p in range(4):
                            proj3(qbT[:, p, cols], qbT, lambda c: Wb[:, c, p * 128:(p + 1) * 128], Wb, tb, k); k += 1
                        proj3(kbT2[:, cols], kbT2, lambda c: Wkb2[:, c, :], Wkb2, tb, k); k += 1
                        for g in range(2):
                            proj3(qiT[:, g, cols], qiT, lambda c: Wb[:, c, 640 + g * 128:640 + (g + 1) * 128], Wb, tb, k); k += 1
                        proj3(kiT4[:, cols], kiT4, lambda c: Wki4[:, c, :], Wki4, tb, k); k += 1
                    for kt in range(NT):
                        pb = pp[kt % 2]
                        tsl = slice(kt * 128, (kt + 1) * 128)
                        for c in range(8):
                            S.op("pe", lambda e: e.matmul(pb[:, 0:64], xnT[:, c, tsl], Wb[:, c, 576:640], start=(c == 0), stop=(c == 7)),
                                 reads=[xnT, Wb], writes=[pb], inc=(c == 7))
                        for c in range(8):
                            S.op("pe", lambda e: e.matmul(pb[:, 64:72], xnT[:, c, tsl], Wb[:, c, 928:936], start=(c == 0), stop=(c == 7)),
                                 reads=[xnT, Wb], writes=[pb], inc=(c == 7))
                        S.op("act", lambda e: e.activation(Vb[:, kt, 0, 0:64], pb[:, 0:64], AF.Copy), reads=[pb], writes=[Vb])
                        S.op("act", lambda e: e.activation(Vb[:, kt, 1, 64:128], pb[:, 0:64], AF.Copy), reads=[pb], writes=[Vb])
                        S.op("act", lambda e: e.activation(widx[:, kt, :], pb[:, 64:72], AF.Copy, scale=0.0625), reads=[pb], writes=[widx])

                    S.barrier()
                    st3a.close()
                    Sc = [S.sbuf(st3, "Sc%d" % i, [128, T], F32) for i in range(2)]
                    msk = [S.sbuf(st3, "msk%d" % i, [128, T], BF16) for i in range(2)]
                    mskT = S.sbuf(st3, "mskT", [128, NT, 512], BF16)
                    rl = [S.sbuf(st3, "rl%d" % i, [128, 512], F32) for i in range(2)]
                    bmx = S.sbuf(st3, "bmx", [128, 1], F32)
                    bmn = S.sbuf(st3, "bmn", [128, 1], F32)
                    brg = S.sbuf(st3, "brg", [128, 1], F32)
                    bmid = S.sbuf(st3, "bmid", [128, 1], F32)
                    bcnt = S.sbuf(st3, "bcnt", [128, 1], F32)
                    bt = S.sbuf(st3, "bt", [128, 1], F32)
                    Es = [S.sbuf(st3, "E%d" % i, [128, 512], BF16) for i in range(3)]
                    PTs = [S.sbuf(st3, "PTb%d" % i, [128, 512], BF16) for i in range(3)]
                    rd = S.sbuf(st3, "rdb", [128, 512], F32)
                    sidx = [0]
                    oi = 0

                    def idx_steps(qt):
                        n = (qt + 1) * 128
                        sc = Sc[qt % 2]
                        tsl = slice(qt * 128, (qt + 1) * 128)
                        steps = []
                        for hh in range(8):
                            for sb in range((n + 511) // 512):
                                def step(hh=hh, sb=sb):
                                    g, jj = hh // 4, hh % 4
                                    w = min(512, n - sb * 512)
                                    k = ridx[0]
                                    ridx[0] += 1
                                    pr = pp[k % 2]
                                    rlb = rl[k % 2]
                                    S.op("pe", lambda e: e.matmul(pr[:, 0:w], qiT[32 * jj:32 * jj + 32, g, tsl],
                                                                  kiT4[32 * jj:32 * jj + 32, sb * 512:sb * 512 + w],
                                                                  start=True, stop=True, tile_position=(32 * jj, 0)),
                                         reads=[qiT, kiT4], writes=[pr])
                                    S.op("act", lambda e: e.activation(rlb[:, 0:w], pr[:, 0:w], AF.Relu), reads=[pr], writes=[rlb])
                                    dst = sc[:, sb * 512:sb * 512 + w]
                                    if hh == 0:
                                        S.op("dve", lambda e: e.tensor_scalar(dst, rlb[:, 0:w], widx[:, qt, 0:1], None, ALU.mult),
                                             reads=[rlb, widx], writes=[sc])
                                    else:
                                        S.op("dve", lambda e: e.scalar_tensor_tensor(dst, rlb[:, 0:w], widx[:, qt, hh:hh + 1], dst,
                                                                                     ALU.mult, ALU.add),
                                             reads=[rlb, widx, sc], writes=[sc])
                                steps.append(step)
                        return steps

                    def bisect(qt, filler):
                        n = (qt + 1) * 128
                        sc = Sc[qt % 2]
                        mk = msk[qt % 2]
                        S.op("pool", lambda e: e.memset(sc[0:64, n - 64:n], NEGBIG), writes=[sc])
                        per = (len(filler) + BIS_ITERS - 1) // BIS_ITERS if qt >= 2 else len(filler)
                        if qt >= 2:
                            S.op("dve", lambda e: e.reduce_max(bmx[:], sc[:, 0:n], AX.X), reads=[sc], writes=[bmx])
                            S.op("dve", lambda e: e.tensor_reduce(bmn[:], sc[:, 0:n - 64], AX.X, ALU.min), reads=[sc], writes=[bmn])
                            S.op("dve", lambda e: e.tensor_tensor(brg[:], bmx[:], bmn[:], ALU.subtract), reads=[bmx, bmn], writes=[brg])
                            S.op("dve", lambda e: e.tensor_scalar(brg[:], brg[:], 1e-20, None, ALU.add), reads=[brg], writes=[brg])
                            S.op("dve", lambda e: e.reciprocal(brg[:], brg[:]), reads=[brg], writes=[brg])
                            S.op("dve", lambda e: e.tensor_scalar(sc[:, 0:n], sc[:, 0:n], bmn[:, 0:1], brg[:, 0:1], ALU.subtract, ALU.mult),
                                 reads=[sc, bmn, brg], writes=[sc])
                            S.op("dve", lambda e: e.memset(bmid[:], 0.5), writes=[bmid])
                            for it in range(BIS_ITERS):
                                S.op("dve", lambda e: e.tensor_scalar(mk[:, 0:n], sc[:, 0:n], bmid[:, 0:1], None, ALU.is_ge, ALU.add,
                                                                      accum_out=bcnt[:]),
                                     reads=[sc, bmid], writes=[mk, bcnt])
                                for _ in range(per):
                                    if filler:
                                        filler.pop(0)()
                                s_next = 2.0 ** -(it + 2)
                                S.op("dve", lambda e: e.tensor_scalar(bt[:], bcnt[:], float(TOPK), 2.0 * s_next, ALU.is_ge, ALU.mult),
                                     reads=[bcnt], writes=[bt])
                                S.op("dve", lambda e: e.scalar_tensor_tensor(bmid[:], bt[:], -s_next, bmid[:], ALU.add, ALU.add),
                                     reads=[bt, bmid], writes=[bmid])
                            S.op("dve", lambda e: e.tensor_scalar(bmid[:], bmid[:], -(2.0 ** -(BIS_ITERS + 1)), None, ALU.add),
                                 reads=[bmid], writes=[bmid])
                            S.op("dve", lambda e: e.tensor_scalar(mk[:, 0:n], sc[:, 0:n], bmid[:, 0:1], None, ALU.is_ge),
                                 reads=[sc, bmid], writes=[mk])
                        else:
                            S.op("dve", lambda e: e.tensor_scalar(mk[:, 0:n], sc[:, 0:n], -1.0e29, None, ALU.is_ge),
                                 reads=[sc], writes=[mk])
                        while filler:
                            filler.pop(0)()

                    def mask_transpose(qt):
                        mk = msk[qt % 2]
                        j = qt % 4
                        for k0 in range(0, qt + 1, 8):
                            nk_ = min(8, qt + 1 - k0)
                            for q in range(nk_):
                                kt = k0 + q
                                S.op("pe", lambda e: e.transpose(pmt[:, q * 128:(q + 1) * 128], mk[:, kt * 128:(kt + 1) * 128], identb[:]),
                                     reads=[mk, identb], writes=[pmt], inc=(q == nk_ - 1))
                            S.op("act", lambda e: e.activation(mskT[:, k0:k0 + nk_, j * 128:(j + 1) * 128],
                                                               pmt[:, 0:nk_ * 128].rearrange("p (q t) -> p q t", q=nk_), AF.Copy),
                                 reads=[pmt], writes=[mskT])

                    ridx = [0]
                    for st_ in idx_steps(0):
                        st_()
                    for qt_ in range(NT):
                        filler = idx_steps(qt_ + 1) if qt_ + 1 < NT else []
                        bisect(qt_, filler)
                        mask_transpose(qt_)
                        if qt_ % 4 != 3:
                            continue
                        qb = qt_ // 4
                        for h in range(8):
                            conv_some(2)
                            p, hf = h // 2, h % 2
                            base = 64 * hf
                            pO = po[oi % 2]
                            oi += 1
                            nk = 4 * qb + 4
                            slots = {}

                            def emit_S(kt):
                                global_si = sidx[0]
                                sidx[0] += 1
                                sl = global_si % 3
                                slots[kt] = sl
                                j0 = max(0, kt - 4 * qb)
                                c0 = j0 * 128
                                pS, E, PT = ps[sl], Es[sl], PTs[sl]
                                S.op("pe", lambda e: e.matmul(pS[:, c0:512], kbT2[base:base + 64, kt * 128:(kt + 1) * 128],
                                                              qbT[base:base + 64, p, qb * 512 + c0:(qb + 1) * 512], start=True, stop=True),
                                     reads=[kbT2, qbT], writes=[pS])
                                for j in range(j0, 4):
                                    d = 4 * qb + j - kt
                                    if d in (0, 1):
                                        S.op("dve", lambda e: e.tensor_tensor(pS[:, j * 128:(j + 1) * 128], pS[:, j * 128:(j + 1) * 128],
                                                                              tz[:, h, d, :], ALU.add), reads=[pS, tz], writes=[pS])
                                S.op("act", lambda e: e.activation(E[:, c0:512], pS[:, c0:512], AF.Exp, bias=cfb[:, h:h + 1], scale=0.125),
                                     reads=[pS, cfb], writes=[E])
                                S.op("dve", lambda e: e.tensor_tensor(PT[:, c0:512], E[:, c0:512], mskT[:, kt, c0:512], ALU.mult),
                                     reads=[E, mskT], writes=[PT])

                            def emit_PV(kt):
                                c0 = max(0, kt - 4 * qb) * 128
                                PT = PTs[slots[kt]]
                                S.op("pe", lambda e: e.matmul(pO[:, c0:512], Vb[:, kt, hf, :], PT[:, c0:512],
                                                              start=(kt == 0), stop=(kt == nk - 1), skip_group_check=True),
                                     reads=[Vb, PT], writes=[pO], inc=(kt == nk - 1))

                            for s_ in range(nk + 2):
                                if s_ < nk:
                                    emit_S(s_)
                                if s_ >= 2:
                                    emit_PV(s_ - 2)
                            ocols = slice(qb * 512, (qb + 1) * 512)
                            if hf == 0:
                                S.op("act", lambda e: e.activation(rd[0:64, :], pO[64:128, :], AF.Ln), reads=[pO], writes=[rd])
                                S.op("act", lambda e: e.activation(rd[0:64, :], rd[0:64, :], AF.Exp, scale=-1.0), reads=[rd], writes=[rd])
                                S.op("dve", lambda e: e.tensor_tensor(obT[0:64, p, ocols], pO[0:64, :], rd[0:64, :], ALU.mult),
                                     reads=[pO, rd], writes=[obT])
                            else:
                                S.op("act", lambda e: e.activation(rd[64:128, :], pO[0:64, :], AF.Ln), reads=[pO], writes=[rd])
                                S.op("act", lambda e: e.activation(rd[64:128, :], rd[64:128, :], AF.Exp, scale=-1.0), reads=[rd], writes=[rd])
                                S.op("dve", lambda e: e.tensor_tensor(obT[64:128, p, ocols], pO[64:128, :], rd[64:128, :], ALU.mult),
                                     reads=[pO, rd], writes=[obT])
                    S.barrier()

                with ExitStack() as st4:
                    Wo = S.sbuf(st4, "Wo", [128, 8, DM], BF16)
                    mixT = S.sbuf(st4, "mixT", [128, 8, T], BF16)
                    bg = S.sbuf(st4, "bg", [128, 16], F32)
                    Wr = S.sbuf(st4, "Wr", [128, 8, 36], F32)
                    brb = S.sbuf(st4, "brb", [128, 36], F32)
                    st4a = ExitStack()
                    st4a.__enter__()
                    Wua = S.sbuf(st4a, "Wua", [128, 4, DM], BF16)
                    Wub = S.sbuf(st4a, "Wub", [128, 4, DM], BF16)
                    Wg = [S.sbuf(st4a, "Wg%d" % i, [128, 8, 2, 128], BF16) for i in range(2)]
                    sga = S.sbuf(st4a, "sga", [128, 512], F32)
                    sgb = S.sbuf(st4a, "sgb", [128, 512], F32)
                    m1 = S.sbuf(st4a, "m1", [128, 512], F32)
                    m2 = S.sbuf(st4a, "m2", [128, 512], F32)
                    pgs = [[S.psum(st4a, "pg%d_%d" % (q, i), [128, 512], F32) for i in range(4)] for q in range(2)]
                    sgas = [sga, S.sbuf(st4a, "sga2", [128, 512], F32)]
                    sgbs = [sgb, S.sbuf(st4a, "sgb2", [128, 512], F32)]
                    m1s = [m1, S.sbuf(st4a, "m1b", [128, 512], F32)]
                    m2s = [m2, S.sbuf(st4a, "m2b", [128, 512], F32)]
                    git = 0

                    S.dma("pool", Wua[:], w_up_a.rearrange("(c p) n -> p c n", p=128), writes=[Wua])
                    S.dma("pool", Wub[:], w_up_b.rearrange("(c p) n -> p c n", p=128), writes=[Wub])
                    S.dma("pool", Wo[:], w_o.rearrange("(c p) n -> p c n", p=128), writes=[Wo])
                    S.dma("sp", bg[:], bgate, writes=[bg])
                    S.dma("sp", Wr[:], w_r.rearrange("(c p) n -> p c n", p=128), writes=[Wr])
                    S.dma("sp", brb[:], b_r.partition_broadcast(128), writes=[brb])
                    gsrc = w_gate.rearrange("(c p) n -> p c n", p=128)
                    for m in range(8):
                        wg = Wg[m % 2]
                        S.dma("pool", wg[:, :, 0, :], gsrc[:, :, m * 128:(m + 1) * 128], writes=[wg])
                        S.dma("pool", wg[:, :, 1, :], gsrc[:, :, 1024 + m * 128:1024 + (m + 1) * 128], writes=[wg])
                        for tb in range(4):
                            cols = slice(tb * 512, (tb + 1) * 512)
                            pg = pgs[git % 2]
                            sga, sgb, m1, m2 = sgas[git % 2], sgbs[git % 2], m1s[git % 2], m2s[git % 2]
                            git += 1
                            for c in range(8):
                                S.op("pe", lambda e: e.matmul(pg[0][:], wg[:, c, 0, :], xnT[:, c, cols], start=(c == 0), stop=(c == 7)),
                                     reads=[wg, xnT], writes=[pg[0]], inc=(c == 7))
                            for c in range(8):
                                S.op("pe", lambda e: e.matmul(pg[1][:], wg[:, c, 1, :], xnT[:, c, cols], start=(c == 0), stop=(c == 7)),
                                     reads=[wg, xnT], writes=[pg[1]], inc=(c == 7))
                            for c in range(4):
                                S.op("pe", lambda e: e.matmul(pg[2][:], Wua[:, c, m * 128:(m + 1) * 128], oaT[:, c, cols], start=(c == 0), stop=(c == 3)),
                                     reads=[Wua, oaT], writes=[pg[2]], inc=(c == 3))
                            for c in range(4):
                                S.op("pe", lambda e: e.matmul(pg[3][:], Wub[:, c, m * 128:(m + 1) * 128], obT[:, c, cols], start=(c == 0), stop=(c == 3)),
                                     reads=[Wub, obT], writes=[pg[3]], inc=(c == 3))
                            S.op("act", lambda e: e.activation(sga[:], pg[0][:], AF.Sigmoid, bias=bg[:, m:m + 1]), reads=[pg[0], bg], writes=[sga])
                            S.op("act", lambda e: e.activation(sgb[:], pg[1][:], AF.Sigmoid, bias=bg[:, 8 + m:9 + m]), reads=[pg[1], bg], writes=[sgb])
                            S.op("dve", lambda e: e.tensor_tensor(m1[:], pg[2][:], sga[:], ALU.mult), reads=[pg[2], sga], writes=[m1])
                            S.op("dve", lambda e: e.tensor_tensor(m2[:], pg[3][:], sgb[:], ALU.mult), reads=[pg[3], sgb], writes=[m2])
                            S.op("dve", lambda e: e.tensor_tensor(mixT[:, m, cols], m1[:], m2[:], ALU.add), reads=[m1, m2], writes=[mixT])
                    S.barrier()
                    st4a.close()
                    pg = [S.psum(st4, "pgt%d" % i, [128, 512], F32) for i in range(2)]
                    pm = [S.psum(st4, "pm%d" % i, [128, 512], F32) for i in range(2)]
                    pl = S.psum(st4, "pl", [128, 512], F32)
                    pre = S.sbuf(st4, "pre", [128, DM], F32)
                    hbs = [S.sbuf(st4, "hb%d" % i, [128, DM], BF16) for i in range(2)]
                    hst = [S.sbuf(st4, "hst%d" % i, [128, DM], F32) for i in range(2)]
                    hT32 = S.sbuf(st4, "hT32", [128, 8, 128], F32)
                    lgA = S.sbuf(st4, "lgA", [128, NT, 36], F32)
                    gmxA = S.sbuf(st4, "gmxA", [128, NT], F32)
                    gselA = S.sbuf(st4, "gselA", [128, NT, 4], F32)
                    r4A = S.sbuf(st4, "r4A", [128, NT, 4], F32)
                    gwA = S.sbuf(st4, "gwA", [128, NT], F32)
                    le4 = S.sbuf(st4, "le4", [128, NT, 4, 8], F32)
                    seA = S.sbuf(st4, "seA", [128, NT, 8], F32)
                    se2A = S.sbuf(st4, "se2A", [128, NT, 8], F32)
                    oh1A = S.sbuf(st4, "oh1A", [128, NT, 8], F32)
                    oh2A = S.sbuf(st4, "oh2A", [128, NT, 8], F32)
                    mx1A = S.sbuf(st4, "mx1A", [128, NT], F32)
                    mx2A = S.sbuf(st4, "mx2A", [128, NT], F32)
                    w1A = S.sbuf(st4, "w1A", [128, NT], F32)
                    w2A = S.sbuf(st4, "w2A", [128, NT], F32)
                    lnsB = [dict(stats=S.sbuf(st4, "statsB%d" % k, [128, 2, 6], F32), mv=S.sbuf(st4, "mvB%d" % k, [128, 2], F32),
                                 rstd=S.sbuf(st4, "rstdB%d" % k, [128, 1], F32), nmr=S.sbuf(st4, "nmrB%d" % k, [128, 1], F32),
                                 z=S.sbuf(st4, "zB%d" % k, [128, DM], F32)) for k in range(2)]

                    def tile_A(i):
                        tsl = slice(i * 128, (i + 1) * 128)
                        xs = xt[i % 2]
                        S.dma("sp", xs[:], x[b, tsl, :], writes=[xs])
                        layer_norm_tile(lns, xs, None, 0, None)
                        z = lns["z"]
                        S.op("dve", lambda e: e.tensor_tensor(z[:], z[:], lnbc[:, 1, :], ALU.add), reads=[z, lnbc], writes=[z])
                        for hf in range(2):
                            for m in range(8):
                                S.op("pe", lambda e: e.matmul(pm[hf][:], mixT[:, m, tsl], Wo[:, m, hf * 512:(hf + 1) * 512],
                                                              start=(m == 0), stop=(m == 7)), reads=[mixT, Wo], writes=[pm[hf]], inc=(m == 7))
                            S.op("dve", lambda e: e.scalar_tensor_tensor(pre[:, hf * 512:(hf + 1) * 512], z[:, hf * 512:(hf + 1) * 512],
                                                                         ALPHA, pm[hf][:], ALU.mult, ALU.add),
                                 reads=[z, pm[hf]], writes=[pre])
                        lb = lnsB[i % 2]
                        layer_norm_tile(lb, pre, None, 2, None)
                        z1 = lb["z"]
                        S.op("dve", lambda e: e.tensor_tensor(z1[:], z1[:], lnbc[:, 3, :], ALU.add), reads=[z1, lnbc], writes=[z1])

                    def tile_B(i):
                        tsl = slice(i * 128, (i + 1) * 128)
                        z = lnsB[i % 2]["z"]
                        hh = hst[i % 2]
                        hb = hbs[i % 2]
                        S.op("act", lambda e: e.activation(hb[:], z[:], AF.Copy), reads=[z], writes=[hb])
                        S.op("act", lambda e: e.activation(hh[:], z[:], AF.Copy, scale=ALPHA), reads=[z], writes=[hh])
                        S.dma("sp", hs[tsl, :], hh[:], reads=[hh], writes=[hsB], sembuf=hh)
                        S.dma("sp", hbd[tsl, :], hb[:], reads=[hb], writes=[hbdB], sembuf=hb)
                        for hf in range(2):
                            for c in range(4):
                                cc = hf * 4 + c
                                S.op("pe", lambda e: e.transpose(pg[hf][:, c * 128:(c + 1) * 128], z[:, cc * 128:(cc + 1) * 128], identF[:]),
                                     reads=[z, identF], writes=[pg[hf]], inc=(c == 3))
                            S.op("act", lambda e: e.activation(hT32[:, hf * 4:(hf + 1) * 4, :], pg[hf][:].rearrange("p (c t) -> p c t", c=4), AF.Copy),
                                 reads=[pg[hf]], writes=[hT32])
                        for c in range(8):
                            S.op("pe", lambda e: e.matmul(pl[:, 0:36], hT32[:, c, :], Wr[:, c, :], start=(c == 0), stop=(c == 7)),
                                 reads=[hT32, Wr], writes=[pl], inc=(c == 7))
                        S.op("dve", lambda e: e.tensor_tensor(lgA[:, i, :], pl[:, 0:36], brb[:], ALU.add), reads=[pl, brb], writes=[lgA])

                    tile_A(0)
                    for i in range(NT):
                        if i + 1 < NT:
                            tile_A(i + 1)
                        tile_B(i)
                    NTl = NT
                    G3 = lgA[:, :, 0:4]
                    E4 = lgA[:, :, 4:36].rearrange("p t (g e) -> p t g e", g=4)
                    bc_t = lambda ap2, n: ap2.rearrange("p (t o) -> p t o", o=1).to_broadcast([128, NTl, n])
                    S.op("dve", lambda e: e.tensor_reduce(gmxA[:], G3, AX.X, ALU.max), reads=[lgA], writes=[gmxA])
                    S.op("dve", lambda e: e.tensor_tensor(gselA[:], G3, bc_t(gmxA[:], 4), ALU.is_ge), reads=[lgA, gmxA], writes=[gselA])
                    S.op("dve", lambda e: e.tensor_tensor(r4A[:], G3, bc_t(gmxA[:], 4), ALU.subtract), reads=[lgA, gmxA], writes=[r4A])
                    S.op("act", lambda e: e.activation(r4A[:], r4A[:], AF.Exp), reads=[r4A], writes=[r4A])
                    S.op("dve", lambda e: e.tensor_reduce(gwA[:], r4A[:], AX.X, ALU.add), reads=[r4A], writes=[gwA])
                    S.op("dve", lambda e: e.reciprocal(gwA[:], gwA[:]), reads=[gwA], writes=[gwA])
                    gsel4 = gselA[:].rearrange("p t (g o) -> p t g o", o=1).to_broadcast([128, NTl, 4, 8])
                    S.op("dve", lambda e: e.tensor_tensor(le4[:], E4, gsel4, ALU.mult), reads=[lgA, gselA], writes=[le4])
                    S.op("dve", lambda e: e.tensor_reduce(seA[:], le4[:].rearrange("p t g e -> p t e g"), AX.X, ALU.add), reads=[le4], writes=[seA])
                    S.op("dve", lambda e: e.tensor_reduce(mx1A[:], seA[:], AX.X, ALU.max), reads=[seA], writes=[mx1A])
                    S.op("dve", lambda e: e.tensor_tensor(oh1A[:], seA[:], bc_t(mx1A[:], 8), ALU.is_ge), reads=[seA, mx1A], writes=[oh1A])
                    S.op("dve", lambda e: e.scalar_tensor_tensor(se2A[:], oh1A[:], NEGBIG, seA[:], ALU.mult, ALU.add), reads=[oh1A, seA], writes=[se2A])
                    S.op("dve", lambda e: e.tensor_reduce(mx2A[:], se2A[:], AX.X, ALU.max), reads=[se2A], writes=[mx2A])
                    S.op("dve", lambda e: e.tensor_tensor(oh2A[:], se2A[:], bc_t(mx2A[:], 8), ALU.is_ge), reads=[se2A, mx2A], writes=[oh2A])
                    S.op("dve", lambda e: e.tensor_tensor(w2A[:], mx2A[:], mx1A[:], ALU.subtract), reads=[mx1A, mx2A], writes=[w2A])
                    S.op("act", lambda e: e.activation(w2A[:], w2A[:], AF.Exp), reads=[w2A], writes=[w2A])
                    S.op("dve", lambda e: e.tensor_scalar(w1A[:], w2A[:], 1.0, None, ALU.add), reads=[w2A], writes=[w1A])
                    S.op("dve", lambda e: e.reciprocal(w1A[:], w1A[:]), reads=[w1A], writes=[w1A])
                    S.op("dve", lambda e: e.tensor_tensor(w2A[:], w2A[:], w1A[:], ALU.mult), reads=[w1A, w2A], writes=[w2A])
                    S.op("dve", lambda e: e.tensor_tensor(ca[:], w1A[:], gwA[:], ALU.mult), reads=[w1A, gwA], writes=[ca])
                    S.op("dve", lambda e: e.tensor_tensor(cb[:], w2A[:], gwA[:], ALU.mult), reads=[w2A, gwA], writes=[cb])
                    for Mx, ohx in ((M1, oh1A), (M2, oh2A)):
                        S.op("dve", lambda e: e.tensor_tensor(Mx[:].rearrange("p t (g e) -> p t g e", g=4),
                                                              ohx[:].rearrange("p t (o e) -> p t o e", o=1).to_broadcast([128, NTl, 4, 8]),
                                                              gsel4, ALU.mult), reads=[ohx, gselA], writes=[Mx])
                    S.op("dve", lambda e: e.tensor_tensor(Mb[:], M1[:], M2[:], ALU.add), reads=[M1, M2], writes=[Mb])
                    S.barrier()

            conv_some(1000)
            with ExitStack() as st5:
                NSL = 64
                NWB = 4
                pr = [S.psum(st5, "pr%d" % i, [128, 512], F32) for i in range(2)]
                Rall = S.sbuf(st5, "Rall", [128, NT, 32], F32)
                cntf = S.sbuf(st5, "cntf", [128, 32], F32)
                cntI = S.sbuf(st5, "cntI", [128, 32], I32)
                pcf = S.sbuf(st5, "pcf", [128, 32], F32)
                scA = S.sbuf(st5, "scA", [128, 32], F32)
                scB = S.sbuf(st5, "scB", [128, 32], F32)
                off = S.sbuf(st5, "off", [128, 32], F32)
                Pm = S.sbuf(st5, "Pm", [128, NT, 32], F32)
                prod = S.sbuf(st5, "prod", [128, NT, 32], F32)
                posaF = S.sbuf(st5, "posaF", [128, NT], F32)
                posbF = S.sbuf(st5, "posbF", [128, NT], F32)
                posaI = S.sbuf(st5, "posaI", [128, NT], I32)
                posbI = S.sbuf(st5, "posbI", [128, NT], I32)
                cmpb = S.sbuf(st5, "cmpb", [128, NSL, 32], F32)
                eidf = S.sbuf(st5, "eidf", [128, NSL], F32)
                actf = S.sbuf(st5, "actf", [128, NSL], F32)
                widF = S.sbuf(st5, "widF", [128, NSL], F32)
                widI = S.sbuf(st5, "widI", [128, NSL], I32)
                for i in range(NT):
                    S.op("pe", lambda e: e.matmul(pr[0][:, 0:32], onesb[:], Mb[:, i, :], start=(i == 0), stop=(i == NT - 1)),
                         reads=[onesb, Mb], writes=[pr[0]], inc=(i == NT - 1))
                S.op("dve", lambda e: e.tensor_copy(cntf[:], pr[0][:, 0:32]), reads=[pr[0]], writes=[cntf])
                for i in range(NT):
                    pb = pr[1]
                    for i2 in range(i):
                        S.op("pe", lambda e: e.matmul(pb[:, 0:32], onesb[:], Mb[:, i2, :], start=(i2 == 0), stop=False),
                             reads=[onesb, Mb], writes=[pb], inc=False)
                    S.op("pe", lambda e: e.matmul(pb[:, 0:32], ustrb[:], Mb[:, i, :], start=(i == 0), stop=True),
                         reads=[ustrb, Mb], writes=[pb])
                    S.op("act", lambda e: e.activation(Rall[:, i, :], pb[:, 0:32], AF.Copy), reads=[pb], writes=[Rall])
                S.op("dve", lambda e: e.tensor_scalar(pcf[:], cntf[:], 127.0, None, ALU.add), reads=[cntf], writes=[pcf])
                S.op("dve", lambda e: e.tensor_copy(cntI[:], pcf[:]), reads=[pcf], writes=[cntI])
                S.op("dve", lambda e: e.tensor_scalar(cntI[:], cntI[:], 7, None, ALU.arith_shift_right), reads=[cntI], writes=[cntI])
                S.op("dve", lambda e: e.tensor_scalar(cntI[:], cntI[:], 7, None, ALU.logical_shift_left), reads=[cntI], writes=[cntI])
                S.op("dve", lambda e: e.tensor_copy(pcf[:], cntI[:]), reads=[cntI], writes=[pcf])
                S.op("dve", lambda e: e.tensor_copy(scA[:], pcf[:]), reads=[pcf], writes=[scA])
                cur, nxt = scA, scB
                for sh in (1, 2, 4, 8, 16):
                    S.op("dve", lambda e: e.tensor_copy(nxt[:, 0:sh], cur[:, 0:sh]), reads=[cur], writes=[nxt])
                    S.op("dve", lambda e: e.tensor_tensor(nxt[:, sh:32], cur[:, sh:32], cur[:, 0:32 - sh], ALU.add), reads=[cur], writes=[nxt])
                    cur, nxt = nxt, cur
                incl = cur
                S.op("dve", lambda e: e.tensor_tensor(off[:], incl[:], pcf[:], ALU.subtract), reads=[incl, pcf], writes=[off])
                S.op("dve", lambda e: e.tensor_tensor(Pm[:], Rall[:], off[:].rearrange("p (o e) -> p o e", o=1).to_broadcast([128, NT, 32]), ALU.add),
                     reads=[Rall, off], writes=[Pm])
                for Mx, pF, pI in ((M1, posaF, posaI), (M2, posbF, posbI)):
                    S.op("dve", lambda e: e.tensor_tensor(prod[:], Mx[:], Pm[:], ALU.mult), reads=[Mx, Pm], writes=[prod])
                    S.op("dve", lambda e: e.tensor_reduce(pF[:], prod[:], AX.X, ALU.add), reads=[prod], writes=[pF])
                    S.op("dve", lambda e: e.tensor_copy(pI[:], pF[:]), reads=[pF], writes=[pI])
                S.op("dve", lambda e: e.tensor_tensor(cmpb[:], off[:].rearrange("p (o e) -> p o e", o=1).to_broadcast([128, NSL, 32]),
                                                      jvS[:].rearrange("p (j o) -> p j o", o=1).to_broadcast([128, NSL, 32]), ALU.is_le),
                     reads=[off, jvS], writes=[cmpb])
                S.op("dve", lambda e: e.tensor_reduce(eidf[:], cmpb[:], AX.X, ALU.add), reads=[cmpb], writes=[eidf])
                S.op("dve", lambda e: e.tensor_scalar(actf[:], jvS[:], incl[:, 31:32], 1.0e6, ALU.is_ge, ALU.mult), reads=[jvS, incl], writes=[actf])
                S.op("dve", lambda e: e.tensor_scalar(widF[:], eidf[:], -1.0, 128.0, ALU.add, ALU.mult), reads=[eidf], writes=[widF])
                S.op("dve", lambda e: e.tensor_scalar(widF[:], widF[:], pcolS[:, 0:1], None, ALU.add), reads=[widF, pcolS], writes=[widF])
                S.op("dve", lambda e: e.tensor_tensor(widF[:], widF[:], actf[:], ALU.add), reads=[widF, actf], writes=[widF])
                S.op("dve", lambda e: e.tensor_copy(widI[:], widF[:]), reads=[widF], writes=[widI])

                hld = [S.sbuf(st5, "hld%d" % i, [128, DM], BF16) for i in range(2)]
                for i in range(NT):
                    hl = hld[i % 2]
                    S.dma("sp", hl[:], hbd[i * 128:(i + 1) * 128, :], reads=[hbdB], writes=[hl])
                    for pI in (posaI, posbI):
                        S.dmaf("pool", lambda e: e.indirect_dma_start(out=Hs, out_offset=bass.IndirectOffsetOnAxis(ap=pI[:, i:i + 1], axis=0),
                                                                      in_=hl[:, :], in_offset=None),
                               reads=[hl, pI], writes=[HsB], sembuf=hl)

                Wg_s = [S.sbuf(st5, "Wgs%d" % i, [128, 8, 256], BF16) for i in range(NWB)]
                Wu_s = [S.sbuf(st5, "Wus%d" % i, [128, 8, 256], BF16) for i in range(NWB)]
                Wd_s = [S.sbuf(st5, "Wds%d" % i, [128, 2, DM], BF16) for i in range(NWB)]
                hsl = [S.sbuf(st5, "hsl%d" % i, [128, DM], BF16) for i in range(NWB)]
                hslT = [S.sbuf(st5, "hslT%d" % i, [128, 8, 128], BF16) for i in range(2)]
                sa = [S.sbuf(st5, "sa%d" % i, [128, 256], F32) for i in range(2)]
                hid = [S.sbuf(st5, "hid%d" % i, [128, 256], BF16) for i in range(2)]
                hidT = [S.sbuf(st5, "hidT%d" % i, [128, 2, 128], BF16) for i in range(2)]
                ysb = [S.sbuf(st5, "ysb%d" % i, [128, DM], F32) for i in range(2)]
                pht = S.psum(st5, "pht", [128, 1024], BF16)
                ptx = S.psum(st5, "ptx", [128, 1024], BF16)
                pau = [S.psum(st5, "pau%d" % i, [128, 512], F32) for i in range(2)]
                py = [S.psum(st5, "py%d" % i, [128, 512], F32) for i in range(2)]

                def st_load_a(j):
                    k = j % NWB
                    S.dma("sp", hsl[k][:], Hs[j * 128:(j + 1) * 128, :], reads=[HsB], writes=[hsl[k]])
                    for wt, src in ((Wg_s[k], weg_b), (Wu_s[k], weu_b)):
                        S.dmaf("pool", lambda e: e.indirect_dma_start(out=wt[:].rearrange("p a b -> p (a b)"), out_offset=None, in_=src,
                                                                      in_offset=bass.IndirectOffsetOnAxis(ap=widI[:, j:j + 1], axis=0),
                                                                      bounds_check=bcreg, oob_is_err=False),
                               reads=[widI, WcB], writes=[wt])

                def st_load_d(j):
                    k = j % NWB
                    wt = Wd_s[k]
                    S.dmaf("pool", lambda e: e.indirect_dma_start(out=wt[:].rearrange("p a b -> p (a b)"), out_offset=None, in_=wed_b,
                                                                  in_offset=bass.IndirectOffsetOnAxis(ap=widI[:, j:j + 1], axis=0),
                                                                  bounds_check=bcreg, oob_is_err=False),
                           reads=[widI, WcB], writes=[wt])

                def st_au(j):
                    k = j % 2
                    for c in range(8):
                        S.op("pe", lambda e: e.transpose(ptx[:, c * 128:(c + 1) * 128], hsl[j % NWB][:, c * 128:(c + 1) * 128], identb[:]),
                             reads=[hsl[j % NWB], identb], writes=[ptx], inc=(c == 7))
                    S.op("act", lambda e: e.activation(hslT[k][:], ptx[:].rearrange("p (c t) -> p c t", c=8), AF.Copy),
                         reads=[ptx], writes=[hslT[k]])
                    pa = pau[k]
                    for c in range(8):
                        S.op("pe", lambda e: e.matmul(pa[:, 0:256], hslT[k][:, c, :], Wg_s[j % NWB][:, c, :], start=(c == 0), stop=(c == 7)),
                             reads=[hslT[k], Wg_s[j % NWB]], writes=[pa], inc=False)
                    for c in range(8):
                        S.op("pe", lambda e: e.matmul(pa[:, 256:512], hslT[k][:, c, :], Wu_s[j % NWB][:, c, :], start=(c == 0), stop=(c == 7)),
                             reads=[hslT[k], Wu_s[j % NWB]], writes=[pa], inc=(c == 7))
                    S.op("act", lambda e: e.activation(sa[k][:], pa[:, 0:256], AF.Silu), reads=[pa], writes=[sa[k]])
                    S.op("dve", lambda e: e.tensor_tensor(hid[k][:], pa[:, 256:512], sa[k][:], ALU.mult), reads=[pa, sa[k]], writes=[hid[k]])

                def st_tr(j):
                    k = j % 2
                    o = k * 512
                    for f in range(2):
                        S.op("pe", lambda e: e.transpose(pht[:, o + f * 128:o + (f + 1) * 128], hid[k][:, f * 128:(f + 1) * 128], identb[:]),
                             reads=[hid[k], identb], writes=[pht], inc=(f == 1))
                    S.op("act", lambda e: e.activation(hidT[k][:], pht[:, o:o + 256].rearrange("p (f t) -> p f t", f=2), AF.Copy),
                         reads=[pht], writes=[hidT[k]])

                def st_y(j):
                    k = j % 2
                    for hf in range(2):
                        for f in range(2):
                            S.op("pe", lambda e: e.matmul(py[hf][:], hidT[k][:, f, :], Wd_s[j % NWB][:, f, hf * 512:(hf + 1) * 512],
                                                          start=(f == 0), stop=(f == 1)),
                                 reads=[hidT[k], Wd_s[j % NWB]], writes=[py[hf]], inc=(f == 1))
                        if hf == 0:
                            S.op("act", lambda e: e.activation(ysb[k][:, 0:512], py[0][:], AF.Copy), reads=[py[0]], writes=[ysb[k]])
                        else:
                            S.op("dve", lambda e: e.tensor_copy(ysb[k][:, 512:1024], py[1][:]), reads=[py[1]], writes=[ysb[k]])
                    S.dma("sp", Ys[j * 128:(j + 1) * 128, :], ysb[k][:], reads=[ysb[k]], writes=[YsB], sembuf=ysb[k])

                for j0 in range(NWB - 1):
                    st_load_a(j0)
                    st_load_d(j0)
                for j in range(NSL + 2):
                    if j < NSL:
                        if j + NWB - 1 < NSL:
                            st_load_a(j + NWB - 1)
                        st_au(j)
                    if 1 <= j <= NSL:
                        st_tr(j - 1)
                    if j >= 2:
                        st_y(j - 2)
                    if j >= 1 and j + NWB - 2 < NSL:
                        st_load_d(j + NWB - 2)

                lns5 = dict(stats=S.sbuf(st5, "stats5", [128, 2, 6], F32), mv=S.sbuf(st5, "mv5", [128, 2], F32),
                            rstd=S.sbuf(st5, "rstd5", [128, 1], F32), nmr=S.sbuf(st5, "nmr5", [128, 1], F32),
                            z=S.sbuf(st5, "z5", [128, DM], F32))
                accs = [S.sbuf(st5, "accs%d" % i, [128, DM], F32) for i in range(2)]
                yas = [S.sbuf(st5, "yas%d" % i, [128, DM], F32) for i in range(2)]
                ybs = [S.sbuf(st5, "ybs%d" % i, [128, DM], F32) for i in range(2)]
                ost = [S.sbuf(st5, "ost%d" % i, [128, DM], F32) for i in range(2)]
                for i in range(NT):
                    k = i % 2
                    S.dma("sp", accs[k][:], hs[i * 128:(i + 1) * 128, :], reads=[hsB], writes=[accs[k]])
                    for yt, pI in ((yas[k], posaI), (ybs[k], posbI)):
                        S.dmaf("pool", lambda e: e.indirect_dma_start(out=yt[:, :], out_offset=None, in_=Ys,
                                                                      in_offset=bass.IndirectOffsetOnAxis(ap=pI[:, i:i + 1], axis=0)),
                               reads=[YsB, pI], writes=[yt])
                    S.op("dve", lambda e: e.scalar_tensor_tensor(accs[k][:], yas[k][:], ca[:, i:i + 1], accs[k][:], ALU.mult, ALU.add),
                         reads=[yas[k], ca], writes=[accs[k]])
                    S.op("dve", lambda e: e.scalar_tensor_tensor(accs[k][:], ybs[k][:], cb[:, i:i + 1], accs[k][:], ALU.mult, ALU.add),
                         reads=[ybs[k], cb], writes=[accs[k]])
                    layer_norm_tile(lns5, accs[k], None, 4, None)
                    z = lns5["z"]
                    o = ost[k]
                    S.op("dve", lambda e: e.tensor_tensor(o[:], z[:], lnbc[:, 5, :], ALU.add), reads=[z, lnbc], writes=[o])
                    S.dma("sp", out[b, i * 128:(i + 1) * 128, :], o[:], reads=[o], writes=[outB], sembuf=o)
                S.barrier()
            stB.close()
        S.nobar.clear()
        S.barrier()
    return nc


_NC = None


def _bucket_tables():
    import jax
    import jax.numpy as jnp
    with jax.default_device(jax.devices("cpu")[0]):
        kk = jnp.arange(128, dtype=jnp.int32)[:, None]
        qq = jnp.arange(128, dtype=jnp.int32)[None, :]
        tabs = []
        for d in range(2):
            rel = kk - qq - 128 * d
            nb = 16
            max_exact = 8
            ret = jnp.where(rel > 0, nb, 0)
            n = jnp.abs(rel)
            large = max_exact + (jnp.log(jnp.maximum(n, 1).astype(jnp.float32) / max_exact)
                                 / math.log(128 / max_exact) * (nb - max_exact)).astype(jnp.int32)
            large = jnp.minimum(large, nb - 1)
            tabs.append(np.asarray(ret + jnp.where(n < max_exact, n, large)))
    return np.stack(tabs, 0)


def kernel(**inputs):
    global _NC
    f32 = np.float32
    g = lambda k: np.ascontiguousarray(np.asarray(inputs[k]))
    x = g("x").astype(f32, copy=False)
    pos = g("positions").astype(np.int32, copy=False)
    rel_bias = g("rel_bias")
    bk = _bucket_tables()
    tzr = rel_bias[bk]
    tzr = np.ascontiguousarray(np.transpose(tzr, (1, 3, 0, 2))).astype(f32)
    shared = {
        "w_in": g("w_in")[0], "w_uq": g("w_uq")[0], "w_uk": g("w_uk")[0], "w_uv": g("w_uv")[0],
        "w_up_a": g("w_up_a")[0], "w_up_b": g("w_up_b")[0], "w_gate": g("w_gate")[0], "w_o": g("w_o")[0],
        "w_r": np.ascontiguousarray(np.concatenate([g("w_grp")[0], g("w_rt")[0]], axis=1)),
        "b_r": np.ascontiguousarray(np.concatenate([g("b_grp")[0], g("b_rt")[0]], axis=0)),
        "w_eg": g("w_exp_gate")[0].reshape(32, 8, 128, 256).transpose(0, 2, 1, 3).reshape(32 * 128, 2048),
        "w_eu": g("w_exp_up")[0].reshape(32, 8, 128, 256).transpose(0, 2, 1, 3).reshape(32 * 128, 2048),
        "w_ed": g("w_exp_down")[0].reshape(32, 2, 128, 1024).transpose(0, 2, 1, 3).reshape(32 * 128, 2048),
        "ustr": np.triu(np.ones((128, 128), dtype=f32), 1),
        "jv": np.tile((np.arange(64, dtype=f32) * 128.0)[None, :], (128, 1)),
        "pcol": np.arange(128, dtype=f32).reshape(128, 1),
        "lnv": np.ascontiguousarray(np.stack([g("ln0_g"), g("ln0_b"), g("ln1_g")[0], g("ln1_b")[0], g("ln2_g")[0], g("ln2_b")[0]], 0)),
        "qng": np.ascontiguousarray(g("q_norm_g")[0].reshape(2, 128).T),
        "kvg": np.ascontiguousarray(g("kv_norm_g")[0].reshape(128, 1)),
        "bgate": np.ascontiguousarray(g("b_gate")[0].reshape(16, 128).T),
        "tzr": tzr,
        "cfar": np.ascontiguousarray(rel_bias[15, :]),
        "identf": np.eye(128, dtype=f32),
        "invf": np.tile((10000.0 ** (-np.arange(16, dtype=np.float64) / 16.0) / (2.0 * math.pi)).astype(f32), 2).reshape(32, 1),
    }
    shared = {k: np.ascontiguousarray(v.astype(f32, copy=False)) for k, v in shared.items()}
    if _NC is None:
        _NC = build()
    in_maps = []
    for c in range(8):
        m = dict(shared)
        m["x"] = np.ascontiguousarray(x[NB * c:NB * (c + 1)])
        m["pos"] = np.ascontiguousarray(pos[NB * c:NB * (c + 1)])
        in_maps.append(m)
    res = run_bass_kernel_spmd(_NC, in_maps, core_ids=list(range(8)))
    return np.concatenate([np.asarray(r["out"]) for r in res.results], axis=0).astype(f32, copy=False)
```
